# Optimizing a Trainium2 kernel written in Bass

```python
import math
import jax
import jax.numpy as jnp
from jax import lax
import numpy as np

D_MODEL = 1024
BATCH = 8
SEQ = 2048
DEPTH = 1

HG_DK = 128
HG_DV = 128
HG_HEADS = (D_MODEL // 2) // HG_DK
HG_KW = HG_HEADS * HG_DK
HG_OUT = HG_HEADS * HG_DV
HG_CHUNK = 32
DA_DH = 64
DA_HEADS = (D_MODEL // 2) // (2 * DA_DH)
DA_WIDTH = DA_HEADS * 2 * DA_DH
Q_BLOCK = 128
MIX_WIDTH = HG_OUT + DA_WIDTH
N_EXPERTS = 32
TOP_K = 4
D_FF = D_MODEL
SWIGLU_ALPHA = 1.702
SWIGLU_LIMIT = 7.0
MOE_BLOCK = 128
NORM_EPS = 1e-6
N_MOD = 6

IN_SIZES = (HG_KW, HG_KW, HG_OUT, HG_OUT, DA_WIDTH, DA_WIDTH, DA_WIDTH, D_MODEL, D_MODEL)
IN_WIDTH = sum(IN_SIZES)
IN_SPLITS = tuple(sum(IN_SIZES[:i + 1]) for i in range(len(IN_SIZES) - 1))

kernel_name = "hybrid_hgrn2_diffattn_moe_block"


def _rmsnorm(x, g):
    xf = x.astype(jnp.float32)
    y = xf * lax.rsqrt(jnp.mean(xf * xf, axis=-1, keepdims=True) + NORM_EPS)
    return y * g.astype(jnp.float32)


def _modulate(xn, shift, scale):
    return xn * (1.0 + scale[:, None, :]) + shift[:, None, :]


def _hgrn2(q_raw, f_raw, i_raw, og_raw, lb, norm_g):
    B, S, _ = q_raw.shape
    N = S // HG_CHUNK

    def heads(t, d):
        t = t.astype(jnp.float32).reshape(B, N, HG_CHUNK, HG_HEADS, d)
        return t.transpose(0, 3, 1, 2, 4)

    q = jax.nn.silu(heads(q_raw, HG_DK))
    lbh = lb.astype(jnp.float32).reshape(1, HG_HEADS, 1, 1, HG_DK)
    f = lbh + (1.0 - lbh) * jax.nn.sigmoid(heads(f_raw, HG_DK))
    k = 1.0 - f
    v = heads(i_raw, HG_DV)
    bcum = jnp.cumsum(jnp.log(f), axis=3)
    q_t = q * jnp.exp(bcum)
    k_t = k * jnp.exp(-bcum)
    causal = jnp.tril(jnp.ones((HG_CHUNK, HG_CHUNK), dtype=bool))
    scores = jnp.where(causal, jnp.einsum('bhncd,bhnsd->bhncs', q_t, k_t), 0.0)
    o_intra = jnp.einsum('bhncs,bhnsv->bhncv', scores, v)
    b_last = bcum[:, :, :, -1:, :]
    kv = jnp.einsum('bhnsd,bhnsv->bhndv', k * jnp.exp(b_last - bcum), v)
    decay = jnp.exp(b_last[:, :, :, 0, :])

    def step(state, inp):
        dec, upd = inp
        return dec[..., None] * state + upd, state

    s0 = jnp.zeros((B, HG_HEADS, HG_DK, HG_DV), jnp.float32)
    _, s_starts = lax.scan(step, s0, (jnp.moveaxis(decay, 2, 0), jnp.moveaxis(kv, 2, 0)))
    s_starts = jnp.moveaxis(s_starts, 0, 2)
    o_inter = jnp.einsum('bhncd,bhndv->bhncv', q_t, s_starts)
    o = (o_intra + o_inter).transpose(0, 2, 3, 1, 4).reshape(B, S, HG_HEADS, HG_DV)
    og = og_raw.astype(jnp.float32).reshape(B, S, HG_HEADS, HG_DV)
    o = _rmsnorm(o, norm_g) * jax.nn.silu(og)
    return o.reshape(B, S, HG_OUT)


def _diff_attention(q_raw, k_raw, v_raw, qn_g, kn_g, lq1, lk1, lq2, lk2, subln_g, lambda_init):
    B, S, _ = q_raw.shape
    q = _rmsnorm(q_raw.reshape(B, S, DA_HEADS, 2, DA_DH), qn_g) * (DA_DH ** -0.5)
    k = _rmsnorm(k_raw.reshape(B, S, DA_HEADS, 2, DA_DH), kn_g)
    v = v_raw.astype(jnp.float32).reshape(B, S, DA_HEADS, 2 * DA_DH)
    lam = (jnp.exp(jnp.sum(lq1.astype(jnp.float32) * lk1.astype(jnp.float32)))
           - jnp.exp(jnp.sum(lq2.astype(jnp.float32) * lk2.astype(jnp.float32))) + lambda_init)
    outs = []
    for blk in range(S // Q_BLOCK):
        q0 = blk * Q_BLOCK
        kv_len = q0 + Q_BLOCK
        s = jnp.einsum('bqhcd,bkhcd->bhcqk', q[:, q0:kv_len], k[:, :kv_len])
        mask = (q0 + jnp.arange(Q_BLOCK))[:, None] >= jnp.arange(kv_len)[None, :]
        p = jax.nn.softmax(jnp.where(mask, s, -jnp.inf), axis=-1)
        a = p[:, :, 0] - lam * p[:, :, 1]
        outs.append(jnp.einsum('bhqk,bkhv->bqhv', a, v[:, :kv_len]))
    o = jnp.concatenate(outs, axis=1)
    o = _rmsnorm(o, subln_g) * (1.0 - lambda_init)
    return o.reshape(B, S, DA_WIDTH)


def _clamped_swiglu(u):
    x_glu, x_lin = u[..., ::2], u[..., 1::2]
    x_glu = jnp.minimum(x_glu, SWIGLU_LIMIT)
    x_lin = jnp.clip(x_lin, -SWIGLU_LIMIT, SWIGLU_LIMIT)
    return x_glu * jax.nn.sigmoid(SWIGLU_ALPHA * x_glu) * (x_lin + 1.0)


def _moe(h, w_router, b_router, w1, b1, w2, b2):
    B, S, D = h.shape
    T = B * S
    A = T * TOP_K
    hf = h.reshape(T, D)
    logits = (hf @ w_router + b_router).astype(jnp.float32)
    top_vals, top_idx = lax.top_k(logits, TOP_K)
    gates = jax.nn.softmax(top_vals, axis=-1)
    e_flat = top_idx.reshape(-1).astype(jnp.int32)
    w_flat = gates.reshape(-1)
    tok_flat = jnp.arange(A, dtype=jnp.int32) // TOP_K
    order = jnp.argsort(e_flat)
    e_sorted, tok_sorted, w_sorted = e_flat[order], tok_flat[order], w_flat[order]
    counts = jax.ops.segment_sum(jnp.ones_like(e_flat), e_flat, num_segments=N_EXPERTS)
    starts = jnp.cumsum(counts) - counts
    padded = (counts + MOE_BLOCK - 1) // MOE_BLOCK * MOE_BLOCK
    pad_ends = jnp.cumsum(padded)
    pad_starts = pad_ends - padded
    dest = pad_starts[e_sorted] + (jnp.arange(A, dtype=jnp.int32) - starts[e_sorted])
    n_blocks = -(-(A + N_EXPERTS * (MOE_BLOCK - 1)) // MOE_BLOCK)
    P = n_blocks * MOE_BLOCK
    row_tok = jnp.full((P,), T, jnp.int32).at[dest].set(tok_sorted)
    row_w = jnp.zeros((P,), jnp.float32).at[dest].set(w_sorted)
    block_expert = jnp.minimum(
        jnp.searchsorted(pad_ends, jnp.arange(n_blocks, dtype=jnp.int32) * MOE_BLOCK, side='right'),
        N_EXPERTS - 1).astype(jnp.int32)
    h_pad = jnp.concatenate([hf, jnp.zeros((1, D), hf.dtype)], axis=0)
    xb = h_pad[row_tok].reshape(n_blocks, MOE_BLOCK, D)

    def expert_block(args):
        xblk, e = args
        u = (xblk @ w1[e] + b1[e]).astype(jnp.float32)
        return _clamped_swiglu(u) @ w2[e] + b2[e]

    yb = lax.map(expert_block, (xb, block_expert))
    y = yb.reshape(P, D).astype(jnp.float32) * row_w[:, None]
    out = jax.ops.segment_sum(y, row_tok, num_segments=T + 1)[:T]
    return out.reshape(B, S, D)


def setup_inputs(seed: int = 0) -> dict:
    key = jax.random.key(seed)
    ks = jax.random.split(key, 24)
    f32 = jnp.float32

    def nrm(k, shape, scale):
        return jax.random.normal(k, shape, f32) * scale

    def gain(k, shape):
        return 1.0 + 0.05 * jax.random.normal(k, shape, f32)

    L = DEPTH
    return {
        "x": nrm(ks[0], (BATCH, SEQ, D_MODEL), 1.0),
        "c": nrm(ks[1], (BATCH, D_MODEL), 1.0),
        "w_ada": nrm(ks[2], (L, D_MODEL, N_MOD * D_MODEL), 0.5 * D_MODEL ** -0.5),
        "b_ada": nrm(ks[3], (L, N_MOD * D_MODEL), 0.02),
        "mix_norm_g": gain(ks[4], (L, D_MODEL)),
        "ffn_norm_g": gain(ks[5], (L, D_MODEL)),
        "w_in": nrm(ks[6], (L, D_MODEL, IN_WIDTH), D_MODEL ** -0.5),
        "hg_lower_bound_logits": nrm(ks[7], (L + 1, HG_KW), 0.1),
        "hg_out_norm_g": gain(ks[8], (L, HG_DV)),
        "da_q_norm_g": gain(ks[9], (L, DA_DH)),
        "da_k_norm_g": gain(ks[10], (L, DA_DH)),
        "da_lambda_q1": nrm(ks[11], (L, DA_DH), 0.1),
        "da_lambda_k1": nrm(ks[12], (L, DA_DH), 0.1),
        "da_lambda_q2": nrm(ks[13], (L, DA_DH), 0.1),
        "da_lambda_k2": nrm(ks[14], (L, DA_DH), 0.1),
        "da_subln_g": gain(ks[15], (L, 2 * DA_DH)),
        "w_out": nrm(ks[16], (L, MIX_WIDTH, D_MODEL), MIX_WIDTH ** -0.5),
        "w_router": nrm(ks[17], (L, D_MODEL, N_EXPERTS), D_MODEL ** -0.5),
        "b_router": nrm(ks[18], (L, N_EXPERTS), 0.01),
        "w1": nrm(ks[19], (L, N_EXPERTS, D_MODEL, 2 * D_FF), D_MODEL ** -0.5),
        "b1": nrm(ks[20], (L, N_EXPERTS, 2 * D_FF), 0.01),
        "w2": nrm(ks[21], (L, N_EXPERTS, D_FF, D_MODEL), D_FF ** -0.5),
        "b2": nrm(ks[22], (L, N_EXPERTS, D_MODEL), 0.01),
    }


def reference(x, c, w_ada, b_ada, mix_norm_g, ffn_norm_g, w_in, hg_lower_bound_logits, hg_out_norm_g,
              da_q_norm_g, da_k_norm_g, da_lambda_q1, da_lambda_k1, da_lambda_q2, da_lambda_k2, da_subln_g,
              w_out, w_router, b_router, w1, b1, w2, b2):
    out_dtype = x.dtype
    lb_table = jnp.cumsum(jax.nn.softmax(hg_lower_bound_logits.astype(jnp.float32), axis=0), axis=0)
    c_act = jax.nn.silu(c.astype(jnp.float32))
    for l in range(DEPTH):
        ada = c_act @ w_ada[l] + b_ada[l]
        sh1, sc1, g1, sh2, sc2, g2 = jnp.split(ada, N_MOD, axis=-1)
        h = _modulate(_rmsnorm(x, mix_norm_g[l]), sh1, sc1)
        proj = h @ w_in[l]
        q_a, f_a, i_a, og_a, q_d, k_d, v_d, gate_a, gate_d = jnp.split(proj, IN_SPLITS, axis=-1)
        o_a = _hgrn2(q_a, f_a, i_a, og_a, lb_table[l], hg_out_norm_g[l])
        lambda_init = 0.8 - 0.6 * math.exp(-0.3 * l)
        o_d = _diff_attention(q_d, k_d, v_d, da_q_norm_g[l], da_k_norm_g[l], da_lambda_q1[l], da_lambda_k1[l],
                              da_lambda_q2[l], da_lambda_k2[l], da_subln_g[l], lambda_init)
        wo = w_out[l]
        y = (jax.nn.sigmoid(gate_a.astype(jnp.float32)) * (o_a @ wo[:HG_OUT])
             + jax.nn.sigmoid(gate_d.astype(jnp.float32)) * (o_d @ wo[HG_OUT:]))
        x = (x + g1[:, None, :] * y).astype(out_dtype)
        h2 = _modulate(_rmsnorm(x, ffn_norm_g[l]), sh2, sc2)
        m = _moe(h2, w_router[l], b_router[l], w1[l], b1[l], w2[l], b2[l])
        x = (x + g2[:, None, :] * m).astype(out_dtype)
    return x
```

```python
import numpy as np
import ml_dtypes
from contextlib import ExitStack
import concourse.bass as bass
import concourse.mybir as mybir
from concourse.bass_utils import run_bass_kernel_spmd

F32 = mybir.dt.float32
BF16 = mybir.dt.bfloat16
AF = mybir.ActivationFunctionType
ALU = mybir.AluOpType
AX = mybir.AxisListType
NPBF = ml_dtypes.bfloat16

S_TOK = 2048
D = 1024
NT = 16
NE = 32
EPS = 1e-6
LAMBDA_INIT = 0.2


class Tok:
    __slots__ = ("name", "w", "r")

    def __init__(self, name=""):
        self.name = name
        self.w = None
        self.r = []


class Sched:
    def __init__(self, nc, stack, same_engine_sync=True):
        self.nc = nc
        self.stack = stack
        self.eng = {"pe": nc.tensor, "act": nc.scalar, "dve": nc.vector, "pool": nc.gpsimd, "sp": nc.sync}
        self.sems = {}
        self.cnt = {}
        self.waited = {e: {} for e in self.eng}
        self.same = same_engine_sync
        self.n_inst = 0
        self.n_wait = 0

    def sem(self, key):
        if key not in self.sems:
            self.sems[key] = self.stack.enter_context(self.nc.semaphore("s_%s" % key))
            self.cnt[key] = 0
        return self.sems[key]

    def _deps(self, reads, writes):
        deps = {}

        def add(t):
            if t is None:
                return
            k, v = t
            if deps.get(k, 0) < v:
                deps[k] = v
        for t in reads:
            add(t.w)
        for t in writes:
            add(t.w)
            for r in t.r:
                add(r)
        return deps

    def _emit_waits(self, e, deps):
        eng = self.eng[e]
        for k, v in deps.items():
            if k == e and (e == "pe" or not self.same):
                continue
            if self.waited[e].get(k, 0) >= v:
                continue
            eng.wait_ge(self.sems[k], v)
            self.waited[e][k] = v
            self.n_wait += 1

    def op(self, e, fn, reads=(), writes=(), signal=True):
        self.sem(e)
        deps = self._deps(reads, writes)
        self._emit_waits(e, deps)
        ins = fn(self.eng[e])
        self.n_inst += 1
        if signal:
            self.cnt[e] += 1
            ins.then_inc(self.sems[e], 1)
            tk = (e, self.cnt[e])
        else:
            tk = (e, self.cnt[e] + 1)
        for t in writes:
            t.w = tk
            t.r = []
        for t in reads:
            if len(t.r) > 64:
                best = {}
                for k, v in t.r:
                    if best.get(k, 0) < v:
                        best[k] = v
                t.r = list(best.items())
            t.r.append(tk)
        return tk

    def dma(self, q, semkey, fn, reads=(), writes=()):
        self.sem(semkey)
        deps = self._deps(reads, writes)
        self._emit_waits(q, deps) if False else None
        eng = self.eng[q]
        for k, v in deps.items():
            if self.waited[q].get(k, 0) >= v:
                continue
            eng.wait_ge(self.sems[k], v)
            self.waited[q][k] = v
            self.n_wait += 1
        ins = fn(eng)
        self.n_inst += 1
        self.cnt[semkey] += 16
        ins.then_inc(self.sems[semkey], 16)
        tk = (semkey, self.cnt[semkey])
        for t in writes:
            t.w = tk
            t.r = []
        for t in reads:
            t.r.append(tk)
        return tk

    def barrier(self):
        for e in self.eng:
            for k, v in self.cnt.items():
                if v == 0 or k == e and e == "pe":
                    continue
                if self.waited[e].get(k, 0) >= v:
                    continue
                self.eng[e].wait_ge(self.sems[k], v)
                self.waited[e][k] = v
                self.n_wait += 1

    def final_wait(self, q, toks):
        deps = {}
        for t in toks:
            if t.w is not None:
                k, v = t.w
                deps[k] = max(deps.get(k, 0), v)
        for k, v in deps.items():
            self.eng[q].wait_ge(self.sems[k], v)


class Ring:
    def __init__(self, bufs, name):
        self.bufs = bufs
        self.toks = [Tok("%s%d" % (name, i)) for i in range(len(bufs))]
        self.i = 0

    def next(self):
        b, t = self.bufs[self.i], self.toks[self.i]
        self.i = (self.i + 1) % len(self.bufs)
        return b, t


def interleave(*gens):
    gens = [g for g in gens if g is not None]
    while gens:
        for g in list(gens):
            try:
                next(g)
            except StopIteration:
                gens.remove(g)


def build_nc(stop_after="all", dbg=()):
    nc = bass.Bass("TRN2", target_bir_lowering=False)

    def din(name, shape, dt=F32):
        return nc.dram_tensor(name, list(shape), dt, kind="ExternalInput").ap()

    x = din("x", [S_TOK, D])
    cT = din("cT", [128, 8])
    w_ada = din("w_ada", [D, 6 * D])
    badaF = din("badaF", [128, 48])
    bada_row = din("bada_row", [1, 6 * D])
    gmixF = din("gmixF", [128, 8])
    gffnF = din("gffnF", [128, 8])
    w_in = din("w_in", [D, 5632])
    lbl = din("lbl", [1, 1024])
    hgn = din("hgn", [1, 128])
    gq = din("gq", [128, 1])
    gk = din("gk", [128, 1])
    lam4 = din("lam4", [1, 256])
    subg = din("subg", [1, 128])
    w_out = din("w_out", [D, D])
    w_r = din("w_r", [D, NE])
    b_r = din("b_r", [1, NE])
    w1p = din("w1p", [NE, D, 2 * D])
    b1F = din("b1F", [128, NE * 16])
    w2 = din("w2", [NE, D, D])
    b2 = din("b2", [NE, D])
    identb_d = din("identb", [128, 128], BF16)
    identf_d = din("identf", [128, 128])
    amat_d = din("amat", [128, 130])
    tri_d = din("tri", [128, 128])
    bones_d = din("bones", [128, 128], BF16)
    out = nc.dram_tensor("out", [S_TOK, D], F32, kind="ExternalOutput").ap()
    dbg_outs = {}

    with ExitStack() as st:
        S = Sched(nc, st)
        out_toks = []

        def sbuf(stack, name, shape, dt):
            return stack.enter_context(nc.sbuf_tensor("sb_" + name, list(shape), dt))

        def psum(stack, name, shape, dt):
            return stack.enter_context(nc.psum_tensor("ps_" + name, list(shape), dt))

        ld_i = [0]

        def load(dst, src, tok, q="sp"):
            ld_i[0] += 1
            return S.dma(q, "ld_%s" % tok.name, lambda e: e.dma_start(out=dst, in_=src), writes=[tok])

        def dump(name, ap, tok, shape, dt):
            d = nc.dram_tensor("dbg_" + name, list(shape), dt, kind="ExternalOutput").ap()
            t = Tok("dbgo_" + name)
            S.dma("sp", "dbg_" + name, lambda e: e.dma_start(out=d, in_=ap), reads=[tok], writes=[t])
            out_toks.append(t)
            dbg_outs[name] = d

        identb = sbuf(st, "identb", [128, 128], BF16)
        identf = sbuf(st, "identf", [128, 128], F32)
        amat = sbuf(st, "amat", [128, 130], F32)
        tri = sbuf(st, "tri", [128, 128], F32)
        bones = sbuf(st, "bones", [128, 128], BF16)
        T_c = Tok("consts")
        for (d_, s_, nm) in [(identb, identb_d, "c0"), (identf, identf_d, "c1"), (amat, amat_d, "c2"), (tri, tri_d, "c3"),
                             (bones, bones_d, "c4")]:
            S.dma("sp", "ld_c", lambda e, d_=d_, s_=s_: e.dma_start(out=d_[:], in_=s_[:, :]), writes=[])
        T_c.w = ("ld_c", S.cnt["ld_c"])

        adaF = sbuf(st, "adaF", [128, 6, 8], F32)
        a1 = sbuf(st, "a1", [128, 8], F32)
        a2 = sbuf(st, "a2", [128, 8], F32)
        g1bc = sbuf(st, "g1bc", [128, D], F32)
        g2bc = sbuf(st, "g2bc", [128, D], F32)
        gnbc = sbuf(st, "gnbc", [128, 128], F32)
        sg8 = sbuf(st, "sg8", [128, 128], F32)
        gqs = sbuf(st, "gqs", [128, 1], F32)
        gks = sbuf(st, "gks", [128, 1], F32)
        nlam = sbuf(st, "nlam", [128, 1], F32)
        brbc = sbuf(st, "brbc", [128, NE], F32)
        b1s = sbuf(st, "b1s", [128, NE * 16], F32)
        bufA = sbuf(st, "bufA", [128, 8, S_TOK], BF16)
        bufB = sbuf(st, "bufB", [128, 8, S_TOK], BF16)
        T_ada = Tok("ada")
        T_a1 = Tok("a1")
        T_a2 = Tok("a2")
        T_g1 = Tok("g1")
        T_g2 = Tok("g2")
        T_misc = Tok("misc")
        T_in0 = Tok("in0")
        T_hT = [Tok("hT%d" % g) for g in range(4)]
        T_mixA = [Tok("mixA%d" % i) for i in range(NT)]
        T_mixD = [Tok("mixD%d" % i) for i in range(NT)]
        hT = bufA
        mixT = bufB

        def norm_transpose(stack, tag, tile_srcs, dstT, a_sc, sh_idx, T_a, T_dst_groups, src_is_dram, nxn=5, split=False, q="sp"):
            xt_r = Ring([sbuf(stack, "%s_xt%d" % (tag, i), [128, D], F32) for i in range(2)], tag + "xt") if src_is_dram else None
            xn_r = Ring([sbuf(stack, "%s_xn%d" % (tag, i), [128, D], BF16) for i in range(nxn)], tag + "xn")
            ssq = sbuf(stack, tag + "_ssq", [128, 64], F32)
            pT = [psum(stack, "%s_pT%d" % (tag, i), [128, 512], BF16) for i in range(2)]
            T_pT = [Tok(tag + "pT0"), Tok(tag + "pT1")]
            T_ssq = Tok(tag + "ssq")
            npt = [0]
            ntile = len(tile_srcs)

            def pre(i):
                src, tsrc = tile_srcs[i]
                if src_is_dram:
                    xt, txt = xt_r.next()
                    S.dma(q, "ld_" + txt.name, lambda e: e.dma_start(out=xt[:], in_=src), writes=[txt])
                    xin, tin = xt[:], [txt]
                else:
                    xin, tin = src, list(tsrc)
                xn, txn = xn_r.next()
                S.op("act", lambda e: e.activation(out=xn[:], in_=xin, func=AF.Square, accum_out=ssq[:, i:i + 1]), reads=tin, writes=[txn, T_ssq])
                S.op("act", lambda e: e.activation(out=ssq[:, 32 + i:33 + i], in_=ssq[:, i:i + 1], func=AF.Ln, scale=1.0 / D, bias=epsb[:, 0:1]),
                     reads=[T_ssq, T_eps], writes=[T_ssq])
                S.op("act", lambda e: e.activation(out=ssq[:, 32 + i:33 + i], in_=ssq[:, 32 + i:33 + i], func=AF.Exp, scale=-0.5), reads=[T_ssq], writes=[T_ssq])
                S.op("dve", lambda e: e.tensor_scalar(out=xn[:], in0=xin, scalar1=ssq[:, 32 + i:33 + i], scalar2=None, op0=ALU.mult),
                     reads=tin + [T_ssq], writes=[txn])
                return (xn, txn)

            def post(g, xns):
                for k in range(8):
                    p, tp = pT[npt[0] % 2], T_pT[npt[0] % 2]
                    npt[0] += 1
                    for ii in range(4):
                        xn, txn = xns[ii]
                        S.op("pe", lambda e: e.transpose(p[:, ii * 128:(ii + 1) * 128], xn[:, k * 128:(k + 1) * 128], identb[:]),
                             reads=[txn, T_c], writes=[tp], signal=(ii == 3))
                    S.op("act", lambda e: e.activation(out=dstT[:, k, g * 512:(g + 1) * 512], in_=p[:], func=AF.Identity,
                                                       scale=a_sc[:, k:k + 1], bias=adaF[:, sh_idx, k:k + 1]),
                         reads=[tp, T_a, T_ada], writes=[T_dst_groups[g]])
            if split:
                allx = [pre(i) for i in range(ntile)]
                yield
                for g in range(ntile // 4):
                    post(g, allx[g * 4:g * 4 + 4])
            else:
                for g in range(ntile // 4):
                    xns = [pre(g * 4 + ii) for ii in range(4)]
                    post(g, xns)
                    yield

        epsb = sbuf(st, "epsb", [128, 4], F32)
        T_eps = Tok("eps")
        S.op("dve", lambda e: e.memset(epsb[:, 0:1], EPS), writes=[T_eps])
        S.op("dve", lambda e: e.memset(epsb[:, 1:2], 64 * EPS), writes=[T_eps])
        S.op("dve", lambda e: e.memset(epsb[:, 2:3], 1.0), writes=[T_eps])

        with ExitStack() as sa:
            cTs = sbuf(sa, "cTs", [128, 8], F32)
            cact = sbuf(sa, "cact", [128, 8], F32)
            cbc = sbuf(sa, "cbc", [128, 8, 128], F32)
            wa = [sbuf(sa, "wa%d" % i, [128, 8, 1024], F32) for i in range(2)]
            T_wa = [Tok("wa0"), Tok("wa1")]
            badaFs = sbuf(sa, "badaFs", [128, 48], F32)
            bgrow = sbuf(sa, "bgrow", [128, 2, 1024], F32)
            gmixs = sbuf(sa, "gmixs", [128, 8], F32)
            gffns = sbuf(sa, "gffns", [128, 8], F32)
            lam4bc = sbuf(sa, "lam4bc", [128, 256], F32)
            lamtmp = sbuf(sa, "lamtmp", [128, 128], F32)
            lams = sbuf(sa, "lams", [128, 2], F32)
            subgbc = sbuf(sa, "subgbc", [128, 128], F32)
            pA = psum(sa, "pA", [128, 512], F32)
            pG = psum(sa, "pG", [128, 512], F32)
            T_pA, T_pG = Tok("pA"), Tok("pG")
            T_in = Tok("smallin")
            smalls = [(cTs[:], cT[:, :]), (badaFs[:], badaF[:, :]), (gmixs[:], gmixF[:, :]), (gffns[:], gffnF[:, :]),
                      (gqs[:], gq[:, :]), (gks[:], gk[:, :]), (b1s[:], b1F[:, :]),
                      (lam4bc[:], lam4[0:1, :].partition_broadcast(128)),
                      (subgbc[:], subg[0:1, :].partition_broadcast(128)),
                      (gnbc[:], hgn[0:1, :].partition_broadcast(128)),
                      (brbc[:], b_r[0:1, :].partition_broadcast(128)),
                      (bgrow[:, 0, :], bada_row[0:1, 2 * D:3 * D].partition_broadcast(128)),
                      (bgrow[:, 1, :], bada_row[0:1, 5 * D:6 * D].partition_broadcast(128))]
            for d_, s_ in smalls:
                S.dma("sp", "ld_s", lambda e, d_=d_, s_=s_: e.dma_start(out=d_, in_=s_), writes=[])
            T_in.w = ("ld_s", S.cnt["ld_s"])
            T_in0.w = T_in.w

            T_cact = Tok("cact")
            S.op("act", lambda e: e.activation(out=cact[:], in_=cTs[:], func=AF.Silu), reads=[T_in], writes=[T_cact])
            T_cbc = Tok("cbc")
            S.op("dve", lambda e: e.tensor_copy(out=cbc[:], in_=cact[:].unsqueeze(2).to_broadcast([128, 8, 128])),
                 reads=[T_cact], writes=[T_cbc])
            S.op("dve", lambda e: e.tensor_scalar(out=sg8[:], in0=subgbc[:], scalar1=1.0 - LAMBDA_INIT, scalar2=None, op0=ALU.mult),
                 reads=[T_in], writes=[T_misc])
            b1v = b1s[:].rearrange("p (e j) -> p e j", j=16)
            S.op("dve", lambda e: e.tensor_scalar(out=b1v[:, :, 8:16], in0=b1v[:, :, 8:16], scalar1=1.0, scalar2=None, op0=ALU.add),
                 reads=[T_in], writes=[T_misc])
            S.op("dve", lambda e: e.tensor_tensor(out=lamtmp[:, 0:64], in0=lam4bc[:, 0:64], in1=lam4bc[:, 64:128], op=ALU.mult),
                 reads=[T_in], writes=[T_misc])
            S.op("dve", lambda e: e.tensor_tensor(out=lamtmp[:, 64:128], in0=lam4bc[:, 128:192], in1=lam4bc[:, 192:256], op=ALU.mult),
                 reads=[T_in], writes=[T_misc])
            S.op("dve", lambda e: e.reduce_sum(out=lams[:], in_=lamtmp[:].rearrange("p (a b) -> p a b", a=2), axis=AX.X),
                 reads=[T_misc], writes=[T_misc])
            S.op("act", lambda e: e.activation(out=lams[:], in_=lams[:], func=AF.Exp), reads=[T_misc], writes=[T_misc])
            S.op("dve", lambda e: e.tensor_tensor(out=nlam[:], in0=lams[:, 1:2], in1=lams[:, 0:1], op=ALU.subtract),
                 reads=[T_misc], writes=[T_misc])
            S.op("dve", lambda e: e.tensor_scalar(out=nlam[:], in0=nlam[:], scalar1=-LAMBDA_INIT, scalar2=None, op0=ALU.add),
                 reads=[T_misc], writes=[T_misc])

            srcsB = [(x[i * 128:(i + 1) * 128, :], None) for i in range(NT)]
            genB = norm_transpose(sa, "B", srcsB, hT, a1, 0, T_a1, T_hT, True, nxn=NT, split=True, q="act")
            next(genB)
            wav = w_ada.rearrange("(k p) n -> p k n", p=128)
            for j in range(6):
                wb, tw = wa[j % 2], T_wa[j % 2]
                S.dma("sp", "ld_wa%d" % (j % 2), lambda e, wb=wb, j=j: e.dma_start(out=wb[:, 0:4, :], in_=wav[:, 0:4, j * D:(j + 1) * D]),
                      writes=[tw])
                S.dma("sp", "ld_wa%d" % (j % 2), lambda e, wb=wb, j=j: e.dma_start(out=wb[:, 4:8, :], in_=wav[:, 4:8, j * D:(j + 1) * D]),
                      writes=[])
                tw.w = ("ld_wa%d" % (j % 2), S.cnt["ld_wa%d" % (j % 2)])
                if j in (2, 5):
                    gdst = g1bc if j == 2 else g2bc
                    tg = T_g1 if j == 2 else T_g2
                    for half in range(2):
                        for k in range(8):
                            S.op("pe", lambda e, wb=wb, k=k, half=half: e.matmul(
                                pG[:], lhsT=cbc[:, k, :], rhs=wb[:, k, half * 512:(half + 1) * 512], start=(k == 0), stop=(k == 7)),
                                reads=[T_cbc, tw], writes=[T_pG], signal=(k == 7))
                        S.op("dve", lambda e, gdst=gdst, half=half, j=j: e.tensor_tensor(
                            out=gdst[:, half * 512:(half + 1) * 512], in0=pG[:], in1=bgrow[:, 0 if j == 2 else 1, half * 512:(half + 1) * 512],
                            op=ALU.add), reads=[T_pG, T_in], writes=[tg])
                else:
                    for m in range(8):
                        for k in range(8):
                            S.op("pe", lambda e, wb=wb, k=k, m=m: e.matmul(
                                pA[:, m:m + 1], lhsT=wb[:, k, m * 128:(m + 1) * 128], rhs=cact[:, k:k + 1], start=(k == 0), stop=(k == 7)),
                                reads=[T_cact, tw], writes=[T_pA], signal=(k == 7))
                    S.op("dve", lambda e, j=j: e.tensor_tensor(out=adaF[:, j, :], in0=pA[:, 0:8], in1=badaFs[:, j * 8:(j + 1) * 8], op=ALU.add),
                         reads=[T_pA, T_in], writes=[T_ada])
            S.op("dve", lambda e: e.scalar_tensor_tensor(out=a1[:], in0=adaF[:, 1, :], scalar=1.0, in1=gmixs[:], op0=ALU.add, op1=ALU.mult),
                 reads=[T_ada, T_in], writes=[T_a1])
            S.op("dve", lambda e: e.scalar_tensor_tensor(out=a2[:], in0=adaF[:, 4, :], scalar=1.0, in1=gffns[:], op0=ALU.add, op1=ALU.mult),
                 reads=[T_ada, T_in], writes=[T_a2])
            for _ in genB:
                pass
            if "ada" in dbg:
                dump("adaF", adaF[:].rearrange("p a b -> p (a b)"), T_ada, [128, 48], F32)
                dump("g1bc", g1bc[:], T_g1, [128, D], F32)
                dump("g2bc", g2bc[:], T_g2, [128, D], F32)
                dump("nlam", nlam[:], T_misc, [128, 1], F32)

        S.barrier()
        if "hT" in dbg:
            for g in range(4):
                dump("hT%d" % g, hT[:, :, g * 512:(g + 1) * 512], T_hT[g], [128, 8, 512], BF16)

        if stop_after == "B":
            S.final_wait("sp", out_toks)
            return nc, dbg_outs

        with ExitStack() as s1:
            wblk2 = sbuf(s1, "wblk2", [128, 8, 1536], BF16)
            T_wblk2 = Tok("wblk2")
            wst_r = Ring([sbuf(s1, "wst%d" % i, [128, 2048], F32) for i in range(2)], "wst")

            def load_wblk(src2d, col0, W, dst, T_dst, dcol0=0, eng="pool"):
                srcv = src2d.rearrange("(k p) n -> p k n", p=128)
                for k in range(8):
                    stg, tst = wst_r.next()
                    S.dma("sp", "ld_" + tst.name, lambda e, stg=stg, k=k: e.dma_start(out=stg[:, 0:W], in_=srcv[:, k, col0:col0 + W]), writes=[tst])
                    en = eng if eng != "alt" else ("act" if k % 2 == 0 else "dve")
                    if en == "act":
                        S.op("act", lambda e, stg=stg, k=k: e.copy(out=dst[:, k, dcol0:dcol0 + W], in_=stg[:, 0:W]), reads=[tst], writes=[T_dst])
                    else:
                        S.op(en, lambda e, stg=stg, k=k: e.tensor_copy(out=dst[:, k, dcol0:dcol0 + W], in_=stg[:, 0:W]), reads=[tst], writes=[T_dst])

            with ExitStack() as sh:
                wblk = sbuf(sh, "wblk", [128, 8, 2048], BF16)
                T_wblk = Tok("wblk")
                load_wblk(w_in, 0, 2048, wblk, T_wblk, eng="alt")
                lblbc = sbuf(sh, "lblbc", [128, 1024], F32)
                lbbc = sbuf(sh, "lbbc", [128, 512], F32)
                omlbc = sbuf(sh, "omlbc", [128, 512], F32)
                T_lb = Tok("lb")
                S.dma("sp", "ld_lb", lambda e: e.dma_start(out=lblbc[:], in_=lbl[0:1, :].partition_broadcast(128)), writes=[T_lb])
                S.op("dve", lambda e: e.tensor_tensor(out=lbbc[:], in0=lblbc[:, 0:512], in1=lblbc[:, 512:1024], op=ALU.subtract), reads=[T_lb], writes=[T_lb])
                S.op("act", lambda e: e.activation(out=lbbc[:], in_=lbbc[:], func=AF.Sigmoid), reads=[T_lb], writes=[T_lb])
                S.op("dve", lambda e: e.tensor_scalar(out=omlbc[:], in0=lbbc[:], scalar1=-1.0, scalar2=1.0, op0=ALU.mult, op1=ALU.add), reads=[T_lb], writes=[T_lb])
                load_wblk(w_in, 2048, 1536, wblk2, T_wblk2, eng="pool")
                pq = psum(sh, "pq", [128, 512], F32); pf = psum(sh, "pf", [128, 512], F32)
                pi_ = psum(sh, "pi", [128, 512], F32); pog = psum(sh, "pog", [128, 512], F32)
                pT = psum(sh, "pT", [128, 1024], BF16); pT2 = psum(sh, "pT2", [128, 1024], BF16)
                po = psum(sh, "po", [128, 512], F32); pkv = psum(sh, "pkv", [128, 512], F32)
                psc = pf
                T_pq, T_pf, T_pi, T_pog, T_pT, T_pT2, T_po, T_pkv = [Tok(n) for n in "pq pf pi pog pT pT2 po pkv".split()]
                T_psc = T_pf
                def f32t(n): return sbuf(sh, n, [128, 512], F32), Tok(n)
                def bft(n, w=512): return sbuf(sh, n, [128, w], BF16), Tok(n)
                fs, T_fs = f32t("fs"); lf, T_lf = f32t("lf"); kk, T_kk = f32t("kk"); qs, T_qs = f32t("qs")
                eb, T_eb = f32t("eb"); enb, T_enb = f32t("enb")
                Sst, T_Sst = f32t("Sst"); tmpkv, T_tmpkv = f32t("tmpkv"); onf, T_onf = f32t("onf")
                qt, T_qt = bft("qt"); Stb, T_Stb = bft("Stb"); oa, T_oa = bft("oa")
                ogs_r = Ring([sbuf(sh, "ogs%d" % a, [128, 512], F32) for a in range(2)], "ogs")
                kt_r = Ring([sbuf(sh, "kt%d" % a, [128, 512], BF16) for a in range(2)], "kt")
                vb_r = Ring([sbuf(sh, "vb%d" % a, [128, 512], BF16) for a in range(2)], "vb")
                qkT_r = Ring([sbuf(sh, "qkT%d" % a, [128, 1024], BF16) for a in range(2)], "qkT")
                scm_r = Ring([sbuf(sh, "scm%d" % a, [128, 512], BF16) for a in range(2)], "scm")
                ebm_r = Ring([sbuf(sh, "ebm%d" % a, [128, 3, 4], F32) for a in range(2)], "ebm")
                bm3 = sbuf(sh, "bm3", [128, 3, 4], F32); T_bm3 = Tok("bm3")
                ssq4 = sbuf(sh, "ssq4", [128, 4], F32); T_ssq4 = Tok("ssq4")
                v4 = lambda ap: ap.rearrange("p (h v) -> p h v", h=4)
                hctx = {}

                def front(i):
                    tc0, tc1 = i * 128, (i + 1) * 128
                    g = i // 4
                    ogs, T_ogs = ogs_r.next(); kt, T_kt = kt_r.next(); vb, T_vb = vb_r.next()
                    qkT, T_qkT = qkT_r.next(); scm, T_scm = scm_r.next(); ebm, T_ebm = ebm_r.next()
                    hctx[i] = (ogs, T_ogs, kt, T_kt, vb, T_vb, qkT, T_qkT, scm, T_scm, ebm, T_ebm)
                    for (p, tp, c0) in [(pf, T_pf, 512), (pq, T_pq, 0), (pi_, T_pi, 1024), (pog, T_pog, 1536)]:
                        for k in range(8):
                            S.op("pe", lambda e, p=p, k=k, c0=c0: e.matmul(p[:], lhsT=hT[:, k, tc0:tc1], rhs=wblk[:, k, c0:c0 + 512], start=(k == 0), stop=(k == 7)),
                                 reads=[T_hT[g], T_wblk], writes=[tp], signal=(k == 7))
                        yield
                    S.op("act", lambda e: e.activation(out=fs[:], in_=pf[:], func=AF.Exp, scale=-1.0), reads=[T_pf], writes=[T_fs]); yield
                    S.op("act", lambda e: e.activation(out=fs[:], in_=fs[:], func=AF.Identity, bias=epsb[:, 2:3]), reads=[T_fs, T_eps], writes=[T_fs]); yield
                    S.op("dve", lambda e: e.reciprocal(out=fs[:], in_=fs[:]), reads=[T_fs], writes=[T_fs]); yield
                    S.op("dve", lambda e: e.tensor_tensor(out=fs[:], in0=fs[:], in1=omlbc[:], op=ALU.mult), reads=[T_fs, T_lb], writes=[T_fs]); yield
                    S.op("dve", lambda e: e.tensor_tensor(out=fs[:], in0=fs[:], in1=lbbc[:], op=ALU.add), reads=[T_fs, T_lb], writes=[T_fs]); yield
                    S.op("act", lambda e: e.activation(out=lf[:], in_=fs[:], func=AF.Ln), reads=[T_fs], writes=[T_lf]); yield
                    S.op("act", lambda e: e.activation(out=kk[:], in_=fs[:], func=AF.Identity, scale=-1.0, bias=epsb[:, 2:3]), reads=[T_fs, T_eps], writes=[T_kk]); yield
                    S.op("pe", lambda e: e.matmul(pf[:], lhsT=amat[:, 0:128], rhs=lf[:], start=True, stop=True), reads=[T_lf, T_c], writes=[T_pf]); yield
                    S.op("act", lambda e: e.activation(out=qs[:], in_=pq[:], func=AF.Exp, scale=-1.0), reads=[T_pq], writes=[T_qs]); yield
                    S.op("act", lambda e: e.activation(out=qs[:], in_=qs[:], func=AF.Identity, bias=epsb[:, 2:3]), reads=[T_qs, T_eps], writes=[T_qs]); yield
                    S.op("dve", lambda e: e.reciprocal(out=qs[:], in_=qs[:]), reads=[T_qs], writes=[T_qs]); yield
                    S.op("dve", lambda e: e.tensor_tensor(out=qs[:], in0=qs[:], in1=pq[:], op=ALU.mult), reads=[T_qs, T_pq], writes=[T_qs]); yield
                    for h in range(4):
                        S.op("pe", lambda e, h=h: e.matmul(pq[:, 2 * h:2 * h + 2], lhsT=lf[:, h * 128:(h + 1) * 128], rhs=amat[:, 128:130], start=True, stop=True),
                             reads=[T_lf, T_c], writes=[T_pq], signal=(h == 3))
                    yield
                    S.op("act", lambda e: e.copy(out=vb[:], in_=pi_[:]), reads=[T_pi], writes=[T_vb]); yield
                    S.op("act", lambda e: e.activation(out=ogs[:], in_=pog[:], func=AF.Exp, scale=-1.0), reads=[T_pog], writes=[T_ogs]); yield
                    S.op("act", lambda e: e.activation(out=ogs[:], in_=ogs[:], func=AF.Identity, bias=epsb[:, 2:3]), reads=[T_ogs, T_eps], writes=[T_ogs]); yield
                    S.op("dve", lambda e: e.reciprocal(out=ogs[:], in_=ogs[:]), reads=[T_ogs], writes=[T_ogs]); yield
                    S.op("dve", lambda e: e.tensor_tensor(out=ogs[:], in0=ogs[:], in1=pog[:], op=ALU.mult), reads=[T_ogs, T_pog], writes=[T_ogs]); yield
                    S.op("act", lambda e: e.activation(out=eb[:], in_=pf[:], func=AF.Exp), reads=[T_pf], writes=[T_eb]); yield
                    S.op("act", lambda e: e.activation(out=enb[:], in_=pf[:], func=AF.Exp, scale=-1.0), reads=[T_pf], writes=[T_enb]); yield
                    S.op("dve", lambda e: e.tensor_tensor(out=qt[:], in0=qs[:], in1=eb[:], op=ALU.mult), reads=[T_qs, T_eb], writes=[T_qt]); yield
                    S.op("dve", lambda e: e.tensor_tensor(out=kt[:], in0=kk[:], in1=enb[:], op=ALU.mult), reads=[T_kk, T_enb], writes=[T_kt]); yield
                    S.op("dve", lambda e: e.tensor_copy(out=bm3[:, 0:2, :], in_=pq[:, 0:8].rearrange("p (h t) -> p t h", t=2)), reads=[T_pq], writes=[T_bm3]); yield
                    S.op("dve", lambda e: e.tensor_tensor(out=bm3[:, 2, :], in0=bm3[:, 1, :], in1=bm3[:, 0, :], op=ALU.subtract), reads=[T_bm3], writes=[T_bm3]); yield
                    S.op("act", lambda e: e.activation(out=ebm[:].rearrange("p a b -> p (a b)"), in_=bm3[:].rearrange("p a b -> p (a b)"), func=AF.Exp),
                         reads=[T_bm3], writes=[T_ebm]); yield
                    for h in range(4):
                        S.op("pe", lambda e, h=h: e.transpose(pT[:, h * 128:(h + 1) * 128], qt[:, h * 128:(h + 1) * 128], identb[:]),
                             reads=[T_qt, T_c], writes=[T_pT], signal=False)
                    for h in range(4):
                        S.op("pe", lambda e, h=h: e.transpose(pT[:, 512 + h * 128:512 + (h + 1) * 128], kt[:, h * 128:(h + 1) * 128], identb[:]),
                             reads=[T_kt, T_c], writes=[T_pT], signal=(h == 3))
                    yield
                    S.op("act", lambda e: e.copy(out=qkT[:], in_=pT[:]), reads=[T_pT], writes=[T_qkT]); yield
                    for h in range(4):
                        S.op("pe", lambda e, h=h: e.matmul(psc[:, h * 128:(h + 1) * 128], lhsT=qkT[:, 512 + h * 128:512 + (h + 1) * 128],
                                                           rhs=qkT[:, h * 128:(h + 1) * 128], start=True, stop=True),
                             reads=[T_qkT], writes=[T_psc], signal=(h == 3))
                    yield
                    S.op("dve", lambda e: e.tensor_tensor(out=v4(scm[:]), in0=v4(psc[:]), in1=tri[:].unsqueeze(1).to_broadcast([128, 4, 128]), op=ALU.mult),
                         reads=[T_psc, T_c], writes=[T_scm]); yield

                def back(i):
                    tc0, tc1 = i * 128, (i + 1) * 128
                    (ogs, T_ogs, kt, T_kt, vb, T_vb, qkT, T_qkT, scm, T_scm, ebm, T_ebm) = hctx.pop(i)
                    if i > 0:
                        S.op("dve", lambda e: e.tensor_tensor(out=v4(Stb[:]), in0=v4(Sst[:]), in1=ebm[:, 0, :].unsqueeze(2).to_broadcast([128, 4, 128]), op=ALU.mult),
                             reads=[T_Sst, T_ebm], writes=[T_Stb]); yield
                    for h in range(4):
                        hs0, hs1 = h * 128, (h + 1) * 128
                        S.op("pe", lambda e, hs0=hs0, hs1=hs1: e.matmul(pkv[:, hs0:hs1], lhsT=kt[:, hs0:hs1], rhs=vb[:, hs0:hs1], start=True, stop=True),
                             reads=[T_kt, T_vb], writes=[T_pkv], signal=(h == 3))
                    yield
                    S.op("dve", lambda e: e.tensor_tensor(out=v4(tmpkv[:]), in0=v4(pkv[:]), in1=ebm[:, 2, :].unsqueeze(2).to_broadcast([128, 4, 128]), op=ALU.mult),
                         reads=[T_pkv, T_ebm], writes=[T_tmpkv]); yield
                    for h in range(4):
                        hs0, hs1 = h * 128, (h + 1) * 128
                        S.op("pe", lambda e, hs0=hs0, hs1=hs1: e.matmul(po[:, hs0:hs1], lhsT=scm[:, hs0:hs1], rhs=vb[:, hs0:hs1], start=True, stop=(i == 0)),
                             reads=[T_scm, T_vb], writes=[T_po], signal=(i == 0 and h == 3))
                        if i > 0:
                            S.op("pe", lambda e, hs0=hs0, hs1=hs1: e.matmul(po[:, hs0:hs1], lhsT=qkT[:, hs0:hs1], rhs=Stb[:, hs0:hs1], start=False, stop=True),
                                 reads=[T_qkT, T_Stb], writes=[T_po], signal=(h == 3))
                    yield
                    if i == 0:
                        S.op("dve", lambda e: e.tensor_copy(out=Sst[:], in_=tmpkv[:]), reads=[T_tmpkv], writes=[T_Sst]); yield
                    else:
                        S.op("dve", lambda e: e.tensor_tensor(out=v4(Sst[:]), in0=v4(Sst[:]), in1=ebm[:, 1, :].unsqueeze(2).to_broadcast([128, 4, 128]), op=ALU.mult),
                             reads=[T_Sst, T_ebm, T_Stb], writes=[T_Sst]); yield
                        S.op("dve", lambda e: e.tensor_tensor(out=Sst[:], in0=Sst[:], in1=tmpkv[:], op=ALU.add), reads=[T_Sst, T_tmpkv], writes=[T_Sst]); yield
                    S.op("act", lambda e: e.activation(out=onf[:], in_=po[:], func=AF.Square), reads=[T_po], writes=[T_onf]); yield
                    S.op("dve", lambda e: e.reduce_sum(out=ssq4[:], in_=v4(onf[:]), axis=AX.X), reads=[T_onf], writes=[T_ssq4]); yield
                    S.op("act", lambda e: e.activation(out=ssq4[:], in_=ssq4[:], func=AF.Ln, scale=1.0 / 128, bias=epsb[:, 0:1]), reads=[T_ssq4, T_eps], writes=[T_ssq4]); yield
                    S.op("act", lambda e: e.activation(out=ssq4[:], in_=ssq4[:], func=AF.Exp, scale=-0.5), reads=[T_ssq4], writes=[T_ssq4]); yield
                    S.op("dve", lambda e: e.tensor_tensor(out=v4(onf[:]), in0=v4(po[:]), in1=ssq4[:].unsqueeze(2).to_broadcast([128, 4, 128]), op=ALU.mult),
                         reads=[T_po, T_ssq4], writes=[T_onf]); yield
                    S.op("pool", lambda e: e.tensor_tensor(out=v4(onf[:]), in0=v4(onf[:]), in1=gnbc[:].unsqueeze(1).to_broadcast([128, 4, 128]), op=ALU.mult),
                         reads=[T_onf, T_in0], writes=[T_onf]); yield
                    S.op("pool", lambda e: e.tensor_tensor(out=oa[:], in0=onf[:], in1=ogs[:], op=ALU.mult), reads=[T_onf, T_ogs], writes=[T_oa]); yield
                    for h in range(4):
                        S.op("pe", lambda e, h=h: e.transpose(pT2[:, h * 128:(h + 1) * 128], oa[:, h * 128:(h + 1) * 128], identb[:]),
                             reads=[T_oa, T_c], writes=[T_pT2], signal=(h == 3))
                    yield
                    S.op("act", lambda e: e.copy(out=mixT[:, 0:4, tc0:tc1], in_=pT2[:, 0:512].rearrange("p (h v) -> p h v", h=4)),
                         reads=[T_pT2], writes=[T_mixA[i]]); yield

                interleave(front(0))
                for i in range(NT):
                    interleave(front(i + 1) if i + 1 < NT else None, back(i))
                if "oa" in dbg:
                    T_all = Tok("mixAall")
                    T_all.w = T_mixA[NT - 1].w
                    dump("oaT", mixT[:, 0:4, :], T_all, [128, 4, S_TOK], BF16)
            S.barrier()
            if stop_after == "C1":
                S.final_wait("sp", out_toks)
                return nc, dbg_outs

            with ExitStack() as sd:
                pp = [psum(sd, "pp%d" % i, [128, 512], F32) for i in range(2)]
                T_pp = [Tok("pp0"), Tok("pp1")]
                pss = psum(sd, "pss", [128, 512], F32); T_pss = Tok("pss")
                pOb = [psum(sd, "pO%d" % i, [128, 512], F32) for i in range(4)]
                T_pOb = [[Tok("pO%d_%d" % (a, i)) for i in range(2)] for a in range(2)]
                T_pO = [[T_pOb[a][i // 2] for i in range(4)] for a in range(2)]

                def pOap(a, ql, w):
                    c0 = (ql % 2) * 256
                    return pOb[a * 2 + ql // 2][:, c0:c0 + w]
                pTo = psum(sd, "pTo", [128, 512], BF16); T_pTo = Tok("pTo")
                qn = [sbuf(sd, "qnT%d" % i, [128, S_TOK], BF16) for i in range(2)]
                kn = [sbuf(sd, "knT%d" % i, [128, S_TOK], BF16) for i in range(2)]
                T_qn = [[Tok("qn%d_%d" % (i, t)) for t in range(8)] for i in range(2)]
                T_kn = [[Tok("kn%d_%d" % (i, t)) for t in range(8)] for i in range(2)]
                Vext = sbuf(sd, "Vext", [128, NT, 4, 130], BF16); T_V = Tok("Vext")
                sq_r = Ring([sbuf(sd, "sq%d" % i, [128, 512], BF16) for i in range(2)], "sq")
                rs_r = Ring([sbuf(sd, "rs%d" % i, [128, 512], F32) for i in range(2)], "rs")
                PTall = [sbuf(sd, "PTall%d" % i, [128, NT, 512], BF16) for i in range(2)]
                T_PT = [[Tok("PT%d_%d" % (i, j)) for j in range(NT)] for i in range(2)]
                o1 = sbuf(sd, "o1", [128, 4, 128], F32); T_o1 = [Tok("o1_%d" % i) for i in range(4)]
                od_r = Ring([sbuf(sd, "od%d" % i, [128, 128], F32) for i in range(2)], "od")
                odn_r = Ring([sbuf(sd, "odn%d" % i, [128, 128], BF16) for i in range(2)], "odn")
                jk = sbuf(sd, "jk", [128, 128], BF16); T_jk = Tok("jk")
                rr_r = Ring([sbuf(sd, "rr%d" % i, [128, 4], F32) for i in range(8)], "rr")
                npp = [0]

                def nextpp():
                    a = npp[0] % 2
                    npp[0] += 1
                    return pp[a], T_pp[a]
                S.op("dve", lambda e: e.memset(Vext[:, :, :, 128:130], 1.0), writes=[T_V])
                for i in range(NT):
                    p, tp = nextpp()
                    for k in range(8):
                        S.op("pe", lambda e, p=p, k=k, i=i: e.matmul(p[:], lhsT=hT[:, k, i * 128:(i + 1) * 128], rhs=wblk2[:, k, 1024:1536], start=(k == 0), stop=(k == 7)),
                             reads=[T_hT[i // 4], T_wblk2], writes=[tp], signal=(k == 7))
                    S.op("act", lambda e, p=p, i=i: e.copy(out=Vext[:, i, :, 0:128], in_=p[:].rearrange("p (h v) -> p h v", h=4)), reads=[tp], writes=[T_V])

                T_pssA, T_pssB = Tok("pssA"), Tok("pssB")

                def proj_chunk(h, n):
                    t8, which = divmod(n, 2)
                    hb = h % 2
                    if which == 0:
                        dst, T_dst, c0, gs = qn[hb], T_qn[hb][t8], h * 128, gqs
                    else:
                        dst, T_dst, c0, gs = kn[hb], T_kn[hb][t8], 512 + h * 128, gks
                    p = pss[:, 0:256]
                    pb = pss[:, 256:512]
                    for k in range(8):
                        S.op("pe", lambda e, k=k: e.matmul(p, lhsT=wblk2[:, k, c0:c0 + 128], rhs=hT[:, k, t8 * 256:(t8 + 1) * 256], start=(k == 0), stop=(k == 7)),
                             reads=[T_hT[t8 // 2], T_wblk2], writes=[T_pssA], signal=(k == 7))
                    yield
                    sq, tsq = sq_r.next()
                    S.op("act", lambda e: e.activation(out=sq[:, 0:256], in_=p, func=AF.Square), reads=[T_pssA], writes=[tsq]); yield
                    S.op("pe", lambda e: e.matmul(pb, lhsT=bones[:], rhs=sq[:, 0:256], start=True, stop=True), reads=[tsq, T_c], writes=[T_pssB]); yield
                    rs, trs = rs_r.next()
                    S.op("act", lambda e: e.activation(out=rs[:, 0:256], in_=pb, func=AF.Ln, bias=epsb[:, 1:2]), reads=[T_pssB, T_eps], writes=[trs]); yield
                    S.op("act", lambda e: e.activation(out=rs[:, 0:256], in_=rs[:, 0:256], func=AF.Exp, scale=-0.5), reads=[trs], writes=[trs]); yield
                    S.op("dve", lambda e: e.scalar_tensor_tensor(out=dst[:, t8 * 256:(t8 + 1) * 256], in0=p, scalar=gs[:, 0:1], in1=rs[:, 0:256], op0=ALU.mult, op1=ALU.mult),
                         reads=[T_pssA, trs, T_in0], writes=[T_dst]); yield

                def proj_two(h, n):
                    yield from proj_chunk(h, 2 * n)
                    yield from proj_chunk(h, 2 * n + 1)

                def att_geom(g, j):
                    if j < 4 * g:
                        return 4 * g * 128, 512, False
                    return j * 128, (4 * g + 4 - j) * 128, True

                def att_qk(h, g, c, b):
                    hb = h % 2
                    cs0, cs1 = c * 64, (c + 1) * 64
                    nj = 4 * g + 4

                    def qk(j):
                        q0, nq, diag = att_geom(g, j)
                        p, tp = nextpp()
                        S.op("pe", lambda e: e.matmul(p[:, 0:nq], lhsT=kn[hb][cs0:cs1, j * 128:(j + 1) * 128], rhs=qn[hb][cs0:cs1, q0:q0 + nq], start=True, stop=True),
                             reads=[T_kn[hb][j // 2], T_qn[hb][2 * g], T_qn[hb][2 * g + 1]], writes=[tp])
                        return p, tp
                    cur = qk(0)
                    yield
                    for j in range(nj):
                        q0, nq, diag = att_geom(g, j)
                        p, tp = cur
                        PT, tPT = PTall[b][:, j, :], T_PT[b][j]
                        S.op("act", lambda e: e.activation(out=PT[:, 0:nq], in_=p[:, 0:nq], func=AF.Exp, scale=8.0), reads=[tp], writes=[tPT])
                        yield
                        if j + 1 < nj:
                            cur = qk(j + 1)
                            yield
                        if diag:
                            S.op("dve", lambda e: e.tensor_tensor(out=PT[:, 0:128], in0=PT[:, 0:128], in1=tri[:], op=ALU.mult), reads=[tPT, T_c], writes=[tPT])
                            yield

                def att_pv(h, g, c, a, b):
                    for ql in range(4):
                        qi = 4 * g + ql
                        for j in range(qi + 1):
                            q0, nq, diag = att_geom(g, j)
                            off = qi * 128 - q0
                            S.op("pe", lambda e: e.matmul(pOap(a, ql, 129), lhsT=PTall[b][:, j, off:off + 128], rhs=Vext[:, j, h, 0:129], start=(j == 0), stop=(j == qi)),
                                 reads=[T_PT[b][j], T_V], writes=[T_pO[a][ql]], signal=(j == qi))
                            if j % 2 == 1:
                                yield
                        yield

                def att_epi(h, g, c, a):
                    for ql in range(4):
                        qi = 4 * g + ql
                        rr, trr = rr_r.next()
                        S.op("dve", lambda e: e.reciprocal(out=rr[:, 0:1], in_=pOap(a, ql, 129)[:, 128:129]), reads=[T_pO[a][ql]], writes=[trr]); yield
                        if c == 0:
                            S.op("dve", lambda e: e.tensor_scalar(out=o1[:, ql, :], in0=pOap(a, ql, 128), scalar1=rr[:, 0:1], scalar2=None, op0=ALU.mult),
                                 reads=[T_pO[a][ql], trr], writes=[T_o1[ql]]); yield
                        else:
                            S.op("dve", lambda e: e.tensor_tensor(out=rr[:, 1:2], in0=rr[:, 0:1], in1=nlam[:], op=ALU.mult), reads=[trr, T_misc], writes=[trr]); yield
                            od, tod = od_r.next()
                            S.op("dve", lambda e: e.scalar_tensor_tensor(out=od[:], in0=pOap(a, ql, 128), scalar=rr[:, 1:2], in1=o1[:, ql, :], op0=ALU.mult, op1=ALU.add),
                                 reads=[T_pO[a][ql], trr, T_o1[ql]], writes=[tod]); yield
                            S.op("act", lambda e: e.activation(out=jk[:], in_=od[:], func=AF.Square, accum_out=rr[:, 2:3]), reads=[tod], writes=[T_jk, trr]); yield
                            S.op("act", lambda e: e.activation(out=rr[:, 3:4], in_=rr[:, 2:3], func=AF.Ln, scale=1.0 / 128, bias=epsb[:, 0:1]),
                                 reads=[trr, T_eps], writes=[trr]); yield
                            S.op("act", lambda e: e.activation(out=rr[:, 3:4], in_=rr[:, 3:4], func=AF.Exp, scale=-0.5), reads=[trr], writes=[trr]); yield
                            odn, todn = odn_r.next()
                            S.op("dve", lambda e: e.scalar_tensor_tensor(out=odn[:], in0=od[:], scalar=rr[:, 3:4], in1=sg8[:], op0=ALU.mult, op1=ALU.mult),
                                 reads=[tod, trr, T_misc], writes=[todn]); yield
                            S.op("pe", lambda e: e.transpose(pTo[:, 0:128], odn[:], identb[:]), reads=[todn, T_c], writes=[T_pTo]); yield
                            S.op("act", lambda e: e.copy(out=mixT[:, 4 + h, qi * 128:(qi + 1) * 128], in_=pTo[:, 0:128]), reads=[T_pTo], writes=[T_mixD[qi]]); yield

                for n in range(8):
                    interleave(proj_two(0, n))
                its = [(h, g, c) for h in range(4) for g in range(4) for c in range(2)]
                NI = len(its)
                for n in range(NI + 2):
                    gens = []
                    if n < NI:
                        h, g, c = its[n]
                        gens.append(att_qk(h, g, c, n % 2))
                    if 1 <= n <= NI:
                        h1, g1, c1 = its[n - 1]
                        gens.append(att_pv(h1, g1, c1, (n - 1) % 2, (n - 1) % 2))
                    if 2 <= n <= NI + 1:
                        h2, g2, c2 = its[n - 2]
                        gens.append(att_epi(h2, g2, c2, (n - 2) % 2))
                    if n < NI and its[n][0] < 3:
                        gens.append(proj_two(its[n][0] + 1, n % 8))
                    interleave(*gens)
                if "od" in dbg:
                    T_all = Tok("mixDall")
                    T_all.w = ("act", S.cnt["act"])
                    dump("odT", mixT[:, 4:8, :], T_all, [128, 4, S_TOK], BF16)
            S.barrier()
            if stop_after == "C2":
                S.final_wait("sp", out_toks)
                return nc, dbg_outs

        with ExitStack() as s2:
            acc = sbuf(s2, "acc", [128, NT, D], F32)
            T_acc = [[Tok("acc%d_%d" % (i, hf)) for hf in range(2)] for i in range(NT)]
            with ExitStack() as so:
                wst2_r = Ring([sbuf(so, "wst2_%d" % i, [128, 1024], F32) for i in range(3)], "wst2")
                wg = sbuf(so, "wg", [128, 8, 1024], BF16); T_wg = Tok("wg")
                wo = sbuf(so, "wo", [128, 8, 512], BF16); T_wo = Tok("wo")
                pz = [[psum(so, "pz%d_%d" % (a, b), [128, 512], F32) for b in range(4)] for a in range(2)]
                T_pz = [[Tok("pz%d_%d" % (a, b)) for b in range(4)] for a in range(2)]
                sa_r = Ring([sbuf(so, "sa%d" % i, [128, 512], F32) for i in range(2)], "sa")
                sd_r = Ring([sbuf(so, "sdg%d" % i, [128, 512], F32) for i in range(2)], "sdg")
                t1_r = Ring([sbuf(so, "t1_%d" % i, [128, 512], F32) for i in range(2)], "t1")
                t2_r = Ring([sbuf(so, "t2_%d" % i, [128, 512], F32) for i in range(2)], "t2")
                xh_r = Ring([sbuf(so, "xh%d" % i, [128, 512], F32) for i in range(2)], "xh")

                def load_w2(src2d, col0, W, dst, T_dst, dcol0):
                    srcv = src2d.rearrange("(k p) n -> p k n", p=128)
                    for k in range(8):
                        stg, tst = wst2_r.next()
                        S.dma("sp", "ld_" + tst.name, lambda e, stg=stg, k=k: e.dma_start(out=stg[:, 0:W], in_=srcv[:, k, col0:col0 + W]), writes=[tst])
                        if k % 2 == 0:
                            S.op("act", lambda e, stg=stg, k=k: e.copy(out=dst[:, k, dcol0:dcol0 + W], in_=stg[:, 0:W]), reads=[tst], writes=[T_dst])
                        else:
                            S.op("dve", lambda e, stg=stg, k=k: e.tensor_copy(out=dst[:, k, dcol0:dcol0 + W], in_=stg[:, 0:W]), reads=[tst], writes=[T_dst])
                for hf in range(2):
                    load_w2(w_in, 3584 + hf * 512, 512, wg, T_wg, 0)
                    load_w2(w_in, 4608 + hf * 512, 512, wg, T_wg, 512)
                    load_w2(w_out, hf * 512, 512, wo, T_wo, 0)
                    for i in range(NT):
                        tc0, tc1 = i * 128, (i + 1) * 128
                        pzz, tzz = pz[i % 2], T_pz[i % 2]
                        for k in range(8):
                            S.op("pe", lambda e, k=k, p=pzz[0]: e.matmul(p[:], lhsT=hT[:, k, tc0:tc1], rhs=wg[:, k, 0:512], start=(k == 0), stop=(k == 7)),
                                 reads=[T_hT[i // 4], T_wg], writes=[tzz[0]], signal=(k == 7))
                        for k in range(8):
                            S.op("pe", lambda e, k=k, p=pzz[1]: e.matmul(p[:], lhsT=hT[:, k, tc0:tc1], rhs=wg[:, k, 512:1024], start=(k == 0), stop=(k == 7)),
                                 reads=[T_hT[i // 4], T_wg], writes=[tzz[1]], signal=(k == 7))
                        for k in range(4):
                            S.op("pe", lambda e, k=k, p=pzz[2]: e.matmul(p[:], lhsT=mixT[:, k, tc0:tc1], rhs=wo[:, k, :], start=(k == 0), stop=(k == 3)),
                                 reads=[T_mixA[i], T_wo], writes=[tzz[2]], signal=(k == 3))
                        for k in range(4, 8):
                            S.op("pe", lambda e, k=k, p=pzz[3]: e.matmul(p[:], lhsT=mixT[:, k, tc0:tc1], rhs=wo[:, k, :], start=(k == 4), stop=(k == 7)),
                                 reads=[T_mixD[i], T_wo], writes=[tzz[3]], signal=(k == 7))
                        sa, tsa = sa_r.next(); sdg, tsd = sd_r.next(); t1, tt1 = t1_r.next(); t2, tt2 = t2_r.next(); xh, txh = xh_r.next()
                        S.dma("sp", "ld_" + txh.name, lambda e, xh=xh, i=i, hf=hf: e.dma_start(out=xh[:], in_=x[i * 128:(i + 1) * 128, hf * 512:(hf + 1) * 512]), writes=[txh])
                        S.op("act", lambda e, sa=sa, p=pzz[0]: e.activation(out=sa[:], in_=p[:], func=AF.Sigmoid), reads=[tzz[0]], writes=[tsa])
                        S.op("act", lambda e, sdg=sdg, p=pzz[1]: e.activation(out=sdg[:], in_=p[:], func=AF.Sigmoid), reads=[tzz[1]], writes=[tsd])
                        S.op("dve", lambda e, t1=t1, sa=sa, p=pzz[2]: e.tensor_tensor(out=t1[:], in0=sa[:], in1=p[:], op=ALU.mult), reads=[tsa, tzz[2]], writes=[tt1])
                        S.op("dve", lambda e, t2=t2, sdg=sdg, p=pzz[3]: e.tensor_tensor(out=t2[:], in0=sdg[:], in1=p[:], op=ALU.mult), reads=[tsd, tzz[3]], writes=[tt2])
                        S.op("pool", lambda e, t1=t1, t2=t2: e.tensor_tensor(out=t1[:], in0=t1[:], in1=t2[:], op=ALU.add), reads=[tt1, tt2], writes=[tt1])
                        S.op("dve", lambda e, t1=t1, hf=hf: e.tensor_tensor(out=t1[:], in0=t1[:], in1=g1bc[:, hf * 512:(hf + 1) * 512], op=ALU.mult), reads=[tt1, T_g1], writes=[tt1])
                        S.op("dve", lambda e, t1=t1, xh=xh, i=i, hf=hf: e.tensor_tensor(out=acc[:, i, hf * 512:(hf + 1) * 512], in0=t1[:], in1=xh[:], op=ALU.add),
                             reads=[tt1, txh], writes=[T_acc[i][hf]])
            S.barrier()
            if "x1" in dbg:
                for i in range(NT):
                    tt = Tok("x1d%d" % i)
                    tt.w = T_acc[i][1].w
                    dump("x1_%d" % i, acc[:, i, :], tt, [128, D], F32)
            if stop_after == "C3":
                S.final_wait("sp", out_toks)
                return nc, dbg_outs

            with ExitStack() as sf:
                NSLOT = 8
                ring2 = None

                def slot_ap(sl):
                    base = bufB[:, sl, :] if sl < 8 else ring2[:, sl - 8, :]
                    return base.rearrange("p (k c) -> p k c", k=8)
                T_slot = [Tok("slot%d" % i) for i in range(NSLOT)]
                w2b = sbuf(sf, "w2b", [128, 8, D], BF16); T_w2b = Tok("w2b")
                stg_r = Ring([sbuf(sf, "stg%d" % i, [128, 1024], F32) for i in range(4)], "stg")
                wrb = sbuf(sf, "wrb", [128, 8, NE], BF16); T_wrb = Tok("wrb")
                b2g = sbuf(sf, "b2g", [NE, D], BF16); T_b2g = Tok("b2g")
                gates = sbuf(sf, "gates", [128, 8, NE], F32); T_gates = [Tok("gates%d" % i) for i in range(8)]
                h2T = bufA
                T_h2T = [Tok("h2T0"), Tok("h2T1")]
                T_actT = [[Tok("actT%d_%d" % (j, tg)) for tg in range(2)] for j in range(8)]
                with ExitStack() as sw:
                    wrs = sbuf(sw, "wrs", [128, 8, NE], F32)
                    S.dma("sp", "ld_wrs", lambda e: e.dma_start(out=wrs[:], in_=w_r.rearrange("(k p) n -> p k n", p=128)), writes=[T_wrb])
                    S.op("dve", lambda e: e.tensor_copy(out=wrb[:], in_=wrs[:]), reads=[T_wrb], writes=[T_wrb])
                    b2f = sbuf(sw, "b2f", [NE, D], F32)
                    S.dma("sp", "ld_b2g", lambda e: e.dma_start(out=b2f[:], in_=b2[:, :]), writes=[T_b2g])
                    S.op("dve", lambda e: e.tensor_tensor(out=b2g[:], in0=b2f[:], in1=g2bc[0:NE, :], op=ALU.mult), reads=[T_b2g, T_g2], writes=[T_b2g])
                S.barrier()
                slot_ctr = [0]
                for th in range(2):
                    tiles = list(range(th * 8, th * 8 + 8))
                    with ExitStack() as sn:
                        srcs = [(acc[:, i, :], T_acc[i]) for i in tiles]
                        for _ in norm_transpose(sn, "D%d" % th, srcs, h2T, a2, 3, T_a2, T_h2T, False, nxn=4):
                            pass
                    S.barrier()
                    with ExitStack() as se:
                        pgb = [psum(se, "pgb%d_%d" % (th, a), [128, 512], F32) for a in range(2)]; T_pgb = [Tok("pgb0"), Tok("pgb1")]
                        plb = [psum(se, "plb%d_%d" % (th, a), [128, 512], F32) for a in range(2)]; T_plb = [Tok("plb0"), Tok("plb1")]
                        pyb = [psum(se, "pyb%d_%d" % (th, a), [128, 512], F32) for a in range(2)]; T_pyb = [Tok("pyb0"), Tok("pyb1")]
                        gm_r = Ring([sbuf(se, "gm%d_%d" % (th, a), [128, 512], F32) for a in range(2)], "gm")
                        sg_r = Ring([sbuf(se, "sg%d_%d" % (th, a), [128, 512], F32) for a in range(1)], "sg")
                        lm_r = Ring([sbuf(se, "lm%d_%d" % (th, a), [128, 512], F32) for a in range(1)], "lm")
                        tt_r = Ring([sbuf(se, "tt%d_%d" % (th, a), [128, 512], F32) for a in range(1)], "tt")
                        h2m = sbuf(se, "h2m%d" % th, [128, 8, 1024], BF16)
                        T_h2m = [Tok("h2m0"), Tok("h2m1")]
                        pmk = [psum(se, "pmk%d_%d" % (th, a), [128, 512], F32) for a in range(2)]
                        T_pmk = [Tok("pmk0"), Tok("pmk1")]
                        gTall = g1bc[0:NE, :]
                        T_gTall = Tok("gTall")

                        maskbf = sbuf(se, "maskbf%d" % th, [128, 1024], BF16)
                        T_maskbf = [Tok("maskbf0"), Tok("maskbf1")]

                        def mask_h2(e_, tg):
                            S.op("pe", lambda e: e.matmul(pmk[tg][:], lhsT=identf[0:NE, e_:e_ + 1].to_broadcast([NE, 128]), rhs=gTall[:, tg * 512:(tg + 1) * 512],
                                                          start=True, stop=True), reads=[T_gTall, T_c], writes=[T_pmk[tg]])
                            S.op("act", lambda e: e.copy(out=maskbf[:, tg * 512:(tg + 1) * 512], in_=pmk[tg][:]), reads=[T_pmk[tg]], writes=[T_maskbf[tg]])
                            for k in range(8):
                                S.op("pool", lambda e: e.tensor_tensor(out=h2m[:, k, tg * 512:(tg + 1) * 512], in0=h2T[:, k, tg * 512:(tg + 1) * 512],
                                                                       in1=maskbf[:, tg * 512:(tg + 1) * 512], op=ALU.mult),
                                     reads=[T_h2T[tg], T_maskbf[tg]], writes=[T_h2m[tg]])
                        n1 = 0
                        n2 = 0
                        if th == 0:
                            print("sbuf bytes remaining in FFN expert scope:", nc.sbuf_bytes_remaining)
                        ne_run = NE if stop_after == "all" else int(stop_after[1:]) if stop_after.startswith("E") else NE
                        pend = []

                        def flush_casts():
                            while pend:
                                pend.pop(0)()

                        def load_piece(qidx):
                            e_, j_ = divmod(qidx, 8)
                            if e_ >= ne_run:
                                return
                            w1v = w1p[e_].rearrange("(k p) n -> p k n", p=128)
                            sl = qidx % NSLOT
                            sap = slot_ap(sl)
                            for part in range(2):
                                stg, tst = stg_r.next()
                                cc0 = part * D + j_ * 128
                                S.dma("sp", "ld_" + tst.name, lambda e, stg=stg, cc0=cc0: e.dma_start(
                                    out=stg[:].rearrange("p (k c) -> p k c", k=8), in_=w1v[:, :, cc0:cc0 + 128]), writes=[tst])
                                pend.append(lambda stg=stg, tst=tst, sap=sap, part=part, sl=sl: S.op("act", lambda e: e.copy(
                                    out=sap[:, :, part * 128:(part + 1) * 128], in_=stg[:].rearrange("p (k c) -> p k c", k=8)),
                                    reads=[tst], writes=[T_slot[sl]]))

                        def load_w2chunk(e_, k):
                            if e_ >= ne_run:
                                return
                            stg, tst = stg_r.next()
                            S.dma("sp", "ld_" + tst.name, lambda e, stg=stg, k=k: e.dma_start(out=stg[:], in_=w2[e_, k * 128:(k + 1) * 128, :]), writes=[tst])
                            pend.append(lambda stg=stg, tst=tst, k=k: S.op("dve" if k % 4 != 3 else "pool", lambda e: e.tensor_tensor(
                                out=w2b[:, k, :], in0=stg[:], in1=g2bc[:], op=ALU.mult), reads=[tst, T_g2], writes=[T_w2b]))
                        for qidx in range(NSLOT):
                            load_piece(qidx)
                            flush_casts()
                        rtmp = []
                        for ch in range(2):
                            rtmp.append(dict(
                                lg=sbuf(se, "lg%d_%d" % (th, ch), [128, NE], F32), T_lg=Tok("lg"),
                                top8=sbuf(se, "top8_%d_%d" % (th, ch), [128, 8], F32), T_top8=Tok("top8"),
                                msk=sbuf(se, "msk%d_%d" % (th, ch), [128, NE], F32), T_msk=Tok("msk"),
                                ex=sbuf(se, "ex%d_%d" % (th, ch), [128, NE], F32), T_ex=Tok("ex"),
                                sm=sbuf(se, "sm%d_%d" % (th, ch), [128, 4], F32), T_sm=Tok("sm"),
                                gTb=sbuf(se, "gTb%d_%d" % (th, ch), [NE, 128], BF16), T_gT=Tok("gT")))

                        def router_tile(il, ch):
                            i = tiles[il]
                            c0, c1 = il * 128, (il + 1) * 128
                            R = rtmp[ch]
                            lg, top8, msk, ex, sm = R["lg"], R["top8"], R["msk"], R["ex"], R["sm"]
                            gT = gTall[:, c0:c1]
                            T_lg, T_top8, T_msk, T_ex, T_sm, T_gT = R["T_lg"], R["T_top8"], R["T_msk"], R["T_ex"], R["T_sm"], R["T_gT"]
                            plog, T_plog = pgb[ch], T_pgb[ch]
                            pgT, T_pgT = plb[ch], T_plb[ch]
                            pbk, T_pbk = pyb[ch], T_pyb[ch]
                            for k in range(8):
                                S.op("pe", lambda e, k=k: e.matmul(plog[:, 0:NE], lhsT=h2T[:, k, c0:c1], rhs=wrb[:, k, :], start=(k == 0), stop=(k == 7)),
                                     reads=[T_h2T[il // 4], T_wrb], writes=[T_plog], signal=(k == 7))
                            yield
                            S.op("dve", lambda e: e.tensor_tensor(out=lg[:], in0=plog[:, 0:NE], in1=brbc[:], op=ALU.add), reads=[T_plog, T_in0], writes=[T_lg]); yield
                            S.op("dve", lambda e: e.max(out=top8[:], in_=lg[:]), reads=[T_lg], writes=[T_top8]); yield
                            S.op("dve", lambda e: e.tensor_scalar(out=msk[:], in0=lg[:], scalar1=top8[:, 3:4], scalar2=None, op0=ALU.is_ge), reads=[T_lg, T_top8], writes=[T_msk]); yield
                            S.op("dve", lambda e: e.tensor_scalar(out=sm[:, 0:1], in0=top8[:, 0:1], scalar1=-1.0, scalar2=None, op0=ALU.mult), reads=[T_top8], writes=[T_sm]); yield
                            S.op("act", lambda e: e.activation(out=ex[:], in_=lg[:], func=AF.Exp, bias=sm[:, 0:1]), reads=[T_lg, T_sm], writes=[T_ex]); yield
                            S.op("dve", lambda e: e.tensor_tensor(out=ex[:], in0=ex[:], in1=msk[:], op=ALU.mult), reads=[T_ex, T_msk], writes=[T_ex]); yield
                            S.op("dve", lambda e: e.reduce_sum(out=sm[:, 1:2], in_=ex[:], axis=AX.X), reads=[T_ex], writes=[T_sm]); yield
                            S.op("dve", lambda e: e.reciprocal(out=sm[:, 2:3], in_=sm[:, 1:2]), reads=[T_sm], writes=[T_sm]); yield
                            S.op("dve", lambda e: e.tensor_scalar(out=gates[:, il, :], in0=ex[:], scalar1=sm[:, 2:3], scalar2=None, op0=ALU.mult),
                                 reads=[T_ex, T_sm], writes=[T_gates[il]]); yield
                            S.op("pe", lambda e: e.matmul(pgT[0:NE, 0:128], lhsT=gates[:, il, :], rhs=identf[:], start=True, stop=True),
                                 reads=[T_gates[il], T_c], writes=[T_pgT]); yield
                            gTb = R["gTb"]
                            S.op("act", lambda e: e.copy(out=gT, in_=pgT[0:NE, 0:128]), reads=[T_pgT], writes=[T_gT, T_gTall]); yield
                            S.op("act", lambda e: e.copy(out=gTb[:], in_=pgT[0:NE, 0:128]), reads=[T_pgT], writes=[T_gT]); yield
                            for nh in range(2):
                                S.op("pe", lambda e: e.matmul(pbk[:], lhsT=gTb[:], rhs=b2g[:, nh * 512:(nh + 1) * 512], start=True, stop=True),
                                     reads=[T_gT, T_b2g], writes=[T_pbk]); yield
                                S.op("dve", lambda e: e.tensor_tensor(out=acc[:, i, nh * 512:(nh + 1) * 512], in0=acc[:, i, nh * 512:(nh + 1) * 512], in1=pbk[:], op=ALU.add),
                                     reads=[T_pbk, T_acc[i][nh]], writes=[T_acc[i][nh]]); yield
                        for il2 in range(4):
                            interleave(router_tile(2 * il2, 0), router_tile(2 * il2 + 1, 1))
                        S.op("dve", lambda e: e.tensor_scalar(out=gTall, in0=gTall, scalar1=0.0, scalar2=None, op0=ALU.is_gt),
                             reads=[rtmp[0]["T_gT"], rtmp[1]["T_gT"], T_gTall], writes=[T_gTall])
                        mask_h2(0, 0)
                        mask_h2(0, 1)
                        if "gates" in dbg and th == 0:
                            tt_ = Tok("gd"); tt_.w = T_gates[7].w
                            dump("gates", gates[:].rearrange("p a b -> p (a b)"), tt_, [128, 8 * NE], F32)
                        for ex_i in range(ne_run):
                            slots = [(ex_i * 8 + j) % NSLOT for j in range(8)]
                            order = [(j, tg) for j in range(6) for tg in range(2)] + [(6, 0), (7, 0), (6, 1), (7, 1)]
                            for oi, (j, tg) in enumerate(order):
                                sl = slots[j]
                                sap = slot_ap(sl)
                                if True:
                                    a = n1 % 2
                                    n1 += 1
                                    for k in range(8):
                                        S.op("pe", lambda e, k=k, a=a, sap=sap, tg=tg: e.matmul(pgb[a][:], lhsT=sap[:, k, 0:128], rhs=h2m[:, k, tg * 512:(tg + 1) * 512],
                                                                                          start=(k == 0), stop=(k == 7)),
                                             reads=[T_slot[sl], T_h2m[tg]], writes=[T_pgb[a]], signal=(k == 7))
                                    for k in range(8):
                                        S.op("pe", lambda e, k=k, a=a, sap=sap, tg=tg: e.matmul(plb[a][:], lhsT=sap[:, k, 128:256], rhs=h2m[:, k, tg * 512:(tg + 1) * 512],
                                                                                          start=(k == 0), stop=(k == 7)),
                                             reads=[T_slot[sl], T_h2m[tg]], writes=[T_plb[a]], signal=(k == 7))
                                    gm, tgm = gm_r.next(); sg, tsg = sg_r.next(); lm, tlm = lm_r.next(); tt, ttt = tt_r.next()
                                    bg = b1s[:, ex_i * 16 + j:ex_i * 16 + j + 1]
                                    bl = b1s[:, ex_i * 16 + 8 + j:ex_i * 16 + 8 + j + 1]
                                    S.op("dve", lambda e, gm=gm, a=a, bg=bg: e.tensor_scalar(out=gm[:], in0=pgb[a][:], scalar1=bg, scalar2=7.0, op0=ALU.add, op1=ALU.min),
                                         reads=[T_pgb[a], T_misc], writes=[tgm])
                                    S.op("act", lambda e, gm=gm, sg=sg: e.activation(out=sg[:], in_=gm[:], func=AF.Sigmoid, scale=1.702), reads=[tgm], writes=[tsg])
                                    S.op("dve", lambda e, lm=lm, a=a, bl=bl: e.tensor_scalar(out=lm[:], in0=plb[a][:], scalar1=bl, scalar2=8.0, op0=ALU.add, op1=ALU.min),
                                         reads=[T_plb[a], T_misc], writes=[tlm])
                                    S.op("pool", lambda e, gm=gm, sg=sg, tt=tt: e.tensor_tensor(out=tt[:], in0=gm[:], in1=sg[:], op=ALU.mult), reads=[tgm, tsg], writes=[ttt])
                                    S.op("dve", lambda e, lm=lm, tt=tt, j=j, tg=tg: e.scalar_tensor_tensor(
                                        out=bufA[:, j, 1024 + tg * 512:1024 + (tg + 1) * 512], in0=lm[:], scalar=-6.0, in1=tt[:], op0=ALU.max, op1=ALU.mult),
                                        reads=[tlm, ttt], writes=[T_actT[j][tg]])
                                if tg == 1:
                                    flush_casts()
                                    if j < 4:
                                        load_w2chunk(ex_i, 2 * j)
                                        load_w2chunk(ex_i, 2 * j + 1)
                                    load_piece(ex_i * 8 + j + NSLOT)
                                if (j, tg) == (7, 0) and ex_i + 1 < ne_run:
                                    mask_h2(ex_i + 1, 0)
                            flush_casts()
                            if ex_i + 1 < ne_run:
                                mask_h2(ex_i + 1, 1)
                            for il, i in enumerate(tiles):
                                for nh in range(2):
                                    a = n2 % 2
                                    n2 += 1
                                    for k in range(8):
                                        S.op("pe", lambda e, k=k, a=a, il=il, nh=nh: e.matmul(
                                            pyb[a][:], lhsT=bufA[:, k, 1024 + il * 128:1024 + (il + 1) * 128], rhs=w2b[:, k, nh * 512:(nh + 1) * 512],
                                            start=(k == 0), stop=(k == 7)),
                                            reads=[T_actT[k][il // 4], T_w2b], writes=[T_pyb[a]], signal=(k == 7))
                                    S.op("dve", lambda e, a=a, il=il, i=i, nh=nh: e.scalar_tensor_tensor(
                                        out=acc[:, i, nh * 512:(nh + 1) * 512], in0=pyb[a][:], scalar=gates[:, il, ex_i:ex_i + 1], in1=acc[:, i, nh * 512:(nh + 1) * 512],
                                        op0=ALU.mult, op1=ALU.add),
                                        reads=[T_pyb[a], T_gates[il], T_acc[i][nh]], writes=[T_acc[i][nh]])
                        for i in tiles:
                            to = Tok("out%d" % i)
                            S.dma("sp", "st_out", lambda e, i=i: e.dma_start(out=out[i * 128:(i + 1) * 128, :], in_=acc[:, i, :]), reads=[T_acc[i][0], T_acc[i][1]], writes=[to])
                            out_toks.append(to)
                    S.barrier()

        S.final_wait("sp", out_toks)
    return nc, dbg_outs


def _consts():
    s = np.arange(128)
    mid = 63
    amat = np.zeros((128, 130), np.float32)
    amat[:, :128] = (s[:, None] <= s[None, :]).astype(np.float32) - (s[:, None] <= mid).astype(np.float32)
    amat[:, 128] = (s <= mid).astype(np.float32)
    amat[:, 129] = 1.0
    tri = (s[None, :] >= s[:, None]).astype(np.float32)
    bones = np.zeros((128, 128), np.float32)
    bones[:64, :64] = 1.0
    bones[64:, 64:] = 1.0
    return {
        "identb": np.eye(128, dtype=np.float32).astype(NPBF),
        "identf": np.eye(128, dtype=np.float32),
        "amat": amat,
        "tri": tri,
        "bones": bones.astype(NPBF),
    }


def _prep_shared(inp):
    f = lambda a: np.ascontiguousarray(np.asarray(a, dtype=np.float32))
    w1 = f(inp["w1"])[0]
    w1p = np.ascontiguousarray(np.concatenate([w1[:, :, 0::2], w1[:, :, 1::2]], axis=2))
    b1 = f(inp["b1"])[0]
    b1p = np.concatenate([b1[:, 0::2], b1[:, 1::2]], axis=1)
    b1F = np.ascontiguousarray(b1p.reshape(NE, 16, 128).transpose(2, 0, 1).reshape(128, NE * 16))
    sh = {
        "w_ada": f(inp["w_ada"])[0],
        "badaF": np.ascontiguousarray(f(inp["b_ada"])[0].reshape(48, 128).T),
        "bada_row": f(inp["b_ada"])[0].reshape(1, -1),
        "gmixF": np.ascontiguousarray(f(inp["mix_norm_g"])[0].reshape(8, 128).T),
        "gffnF": np.ascontiguousarray(f(inp["ffn_norm_g"])[0].reshape(8, 128).T),
        "w_in": f(inp["w_in"])[0],
        "lbl": f(inp["hg_lower_bound_logits"]).reshape(1, 1024),
        "hgn": f(inp["hg_out_norm_g"]).reshape(1, 128),
        "gq": np.tile(f(inp["da_q_norm_g"]).reshape(64), 2).reshape(128, 1),
        "gk": np.tile(f(inp["da_k_norm_g"]).reshape(64), 2).reshape(128, 1),
        "lam4": np.concatenate([f(inp["da_lambda_q1"]).reshape(64), f(inp["da_lambda_k1"]).reshape(64),
                                f(inp["da_lambda_q2"]).reshape(64), f(inp["da_lambda_k2"]).reshape(64)]).reshape(1, 256),
        "subg": f(inp["da_subln_g"]).reshape(1, 128),
        "w_out": f(inp["w_out"])[0],
        "w_r": f(inp["w_router"])[0],
        "b_r": f(inp["b_router"]).reshape(1, NE),
        "w1p": w1p,
        "b1F": b1F,
        "w2": f(inp["w2"])[0],
        "b2": f(inp["b2"])[0],
    }
    sh.update(_consts())
    return sh


def _prep_core(inp, sh, b):
    m = dict(sh)
    m["x"] = np.ascontiguousarray(np.asarray(inp["x"], dtype=np.float32)[b])
    m["cT"] = np.ascontiguousarray(np.asarray(inp["c"], dtype=np.float32)[b].reshape(8, 128).T)
    return m


def kernel(**inputs):
    nc, _ = build_nc()
    sh = _prep_shared(inputs)
    in_maps = [_prep_core(inputs, sh, b) for b in range(8)]
    res = run_bass_kernel_spmd(nc, in_maps, core_ids=list(range(8)))
    return np.stack([np.asarray(r["out"], dtype=np.float32) for r in res.results], axis=0)
```

```python
import numpy as np
import ml_dtypes
from contextlib import ExitStack
import concourse.bass as bass
import concourse.mybir as mybir
from concourse.bass_utils import run_bass_kernel_spmd

F32 = mybir.dt.float32
BF16 = mybir.dt.bfloat16
AF = mybir.ActivationFunctionType
ALU = mybir.AluOpType
AX = mybir.AxisListType
NPBF = ml_dtypes.bfloat16

S_TOK = 2048
D = 1024
NT = 16
NE = 32
EPS = 1e-6
LAMBDA_INIT = 0.2


class Tok:
    __slots__ = ("name", "w", "r")

    def __init__(self, name=""):
        self.name = name
        self.w = None
        self.r = []


class Sched:
    def __init__(self, nc, stack, same_engine_sync=True):
        self.nc = nc
        self.stack = stack
        self.eng = {"pe": nc.tensor, "act": nc.scalar, "dve": nc.vector, "pool": nc.gpsimd, "sp": nc.sync}
        self.sems = {}
        self.cnt = {}
        self.waited = {e: {} for e in self.eng}
        self.same = same_engine_sync
        self.n_inst = 0
        self.n_wait = 0

    def sem(self, key):
        if key not in self.sems:
            self.sems[key] = self.stack.enter_context(self.nc.semaphore("s_%s" % key))
            self.cnt[key] = 0
        return self.sems[key]

    def _deps(self, reads, writes):
        deps = {}

        def add(t):
            if t is None:
                return
            k, v = t
            if deps.get(k, 0) < v:
                deps[k] = v
        for t in reads:
            add(t.w)
        for t in writes:
            add(t.w)
            for r in t.r:
                add(r)
        return deps

    def _emit_waits(self, e, deps):
        eng = self.eng[e]
        for k, v in deps.items():
            if k == e and (e == "pe" or not self.same):
                continue
            if self.waited[e].get(k, 0) >= v:
                continue
            eng.wait_ge(self.sems[k], v)
            self.waited[e][k] = v
            self.n_wait += 1

    def op(self, e, fn, reads=(), writes=(), signal=True):
        self.sem(e)
        deps = self._deps(reads, writes)
        self._emit_waits(e, deps)
        ins = fn(self.eng[e])
        self.n_inst += 1
        if signal:
            self.cnt[e] += 1
            ins.then_inc(self.sems[e], 1)
            tk = (e, self.cnt[e])
        else:
            tk = (e, self.cnt[e] + 1)
        for t in writes:
            t.w = tk
            t.r = []
        for t in reads:
            if len(t.r) > 64:
                best = {}
                for k, v in t.r:
                    if best.get(k, 0) < v:
                        best[k] = v
                t.r = list(best.items())
            t.r.append(tk)
        return tk

    def dma(self, q, semkey, fn, reads=(), writes=()):
        self.sem(semkey)
        deps = self._deps(reads, writes)
        self._emit_waits(q, deps) if False else None
        eng = self.eng[q]
        for k, v in deps.items():
            if self.waited[q].get(k, 0) >= v:
                continue
            eng.wait_ge(self.sems[k], v)
            self.waited[q][k] = v
            self.n_wait += 1
        ins = fn(eng)
        self.n_inst += 1
        self.cnt[semkey] += 16
        ins.then_inc(self.sems[semkey], 16)
        tk = (semkey, self.cnt[semkey])
        for t in writes:
            t.w = tk
            t.r = []
        for t in reads:
            t.r.append(tk)
        return tk

    def barrier(self):
        for e in self.eng:
            for k, v in self.cnt.items():
                if v == 0 or k == e and e == "pe":
                    continue
                if self.waited[e].get(k, 0) >= v:
                    continue
                self.eng[e].wait_ge(self.sems[k], v)
                self.waited[e][k] = v
                self.n_wait += 1

    def final_wait(self, q, toks):
        deps = {}
        for t in toks:
            if t.w is not None:
                k, v = t.w
                deps[k] = max(deps.get(k, 0), v)
        for k, v in deps.items():
            self.eng[q].wait_ge(self.sems[k], v)


class Ring:
    def __init__(self, bufs, name):
        self.bufs = bufs
        self.toks = [Tok("%s%d" % (name, i)) for i in range(len(bufs))]
        self.i = 0

    def next(self):
        b, t = self.bufs[self.i], self.toks[self.i]
        self.i = (self.i + 1) % len(self.bufs)
        return b, t


def interleave(*gens):
    gens = [g for g in gens if g is not None]
    while gens:
        for g in list(gens):
            try:
                next(g)
            except StopIteration:
                gens.remove(g)


def build_nc(stop_after="all", dbg=()):
    nc = bass.Bass("TRN2", target_bir_lowering=False)

    def din(name, shape, dt=F32):
        return nc.dram_tensor(name, list(shape), dt, kind="ExternalInput").ap()

    x = din("x", [S_TOK, D])
    cT = din("cT", [128, 8])
    w_ada = din("w_ada", [D, 6 * D])
    badaF = din("badaF", [128, 48])
    bada_row = din("bada_row", [1, 6 * D])
    gmixF = din("gmixF", [128, 8])
    gffnF = din("gffnF", [128, 8])
    w_in = din("w_in", [D, 5632])
    lbl = din("lbl", [1, 1024])
    hgn = din("hgn", [1, 128])
    gq = din("gq", [128, 1])
    gk = din("gk", [128, 1])
    lam4 = din("lam4", [1, 256])
    subg = din("subg", [1, 128])
    w_out = din("w_out", [D, D])
    w_r = din("w_r", [D, NE])
    b_r = din("b_r", [1, NE])
    w1p = din("w1p", [NE, D, 2 * D])
    b1F = din("b1F", [128, NE * 16])
    w2 = din("w2", [NE, D, D])
    b2 = din("b2", [NE, D])
    identb_d = din("identb", [128, 128], BF16)
    identf_d = din("identf", [128, 128])
    amat_d = din("amat", [128, 130])
    tri_d = din("tri", [128, 128])
    bones_d = din("bones", [128, 128], BF16)
    out = nc.dram_tensor("out", [S_TOK, D], F32, kind="ExternalOutput").ap()
    dbg_outs = {}

    with ExitStack() as st:
        S = Sched(nc, st)
        out_toks = []

        def sbuf(stack, name, shape, dt):
            return stack.enter_context(nc.sbuf_tensor("sb_" + name, list(shape), dt))

        def psum(stack, name, shape, dt):
            return stack.enter_context(nc.psum_tensor("ps_" + name, list(shape), dt))

        ld_i = [0]

        def load(dst, src, tok, q="sp"):
            ld_i[0] += 1
            return S.dma(q, "ld_%s" % tok.name, lambda e: e.dma_start(out=dst, in_=src), writes=[tok])

        def dump(name, ap, tok, shape, dt):
            d = nc.dram_tensor("dbg_" + name, list(shape), dt, kind="ExternalOutput").ap()
            t = Tok("dbgo_" + name)
            S.dma("sp", "dbg_" + name, lambda e: e.dma_start(out=d, in_=ap), reads=[tok], writes=[t])
            out_toks.append(t)
            dbg_outs[name] = d

        identb = sbuf(st, "identb", [128, 128], BF16)
        identf = sbuf(st, "identf", [128, 128], F32)
        amat = sbuf(st, "amat", [128, 130], F32)
        tri = sbuf(st, "tri", [128, 128], F32)
        bones = sbuf(st, "bones", [128, 128], BF16)
        T_c = Tok("consts")
        for (d_, s_, nm) in [(identb, identb_d, "c0"), (identf, identf_d, "c1"), (amat, amat_d, "c2"), (tri, tri_d, "c3"),
                             (bones, bones_d, "c4")]:
            S.dma("sp", "ld_c", lambda e, d_=d_, s_=s_: e.dma_start(out=d_[:], in_=s_[:, :]), writes=[])
        T_c.w = ("ld_c", S.cnt["ld_c"])

        adaF = sbuf(st, "adaF", [128, 6, 8], F32)
        a1 = sbuf(st, "a1", [128, 8], F32)
        a2 = sbuf(st, "a2", [128, 8], F32)
        g1bc = sbuf(st, "g1bc", [128, D], F32)
        g2bc = sbuf(st, "g2bc", [128, D], F32)
        gnbc = sbuf(st, "gnbc", [128, 128], F32)
        sg8 = sbuf(st, "sg8", [128, 128], F32)
        gqs = sbuf(st, "gqs", [128, 1], F32)
        gks = sbuf(st, "gks", [128, 1], F32)
        nlam = sbuf(st, "nlam", [128, 1], F32)
        brbc = sbuf(st, "brbc", [128, NE], F32)
        b1s = sbuf(st, "b1s", [128, NE * 16], F32)
        bufA = sbuf(st, "bufA", [128, 8, S_TOK], BF16)
        bufB = sbuf(st, "bufB", [128, 8, S_TOK], BF16)
        T_ada = Tok("ada")
        T_a1 = Tok("a1")
        T_a2 = Tok("a2")
        T_g1 = Tok("g1")
        T_g2 = Tok("g2")
        T_misc = Tok("misc")
        T_in0 = Tok("in0")
        T_hT = [Tok("hT%d" % g) for g in range(4)]
        T_mixA = [Tok("mixA%d" % i) for i in range(NT)]
        T_mixD = [Tok("mixD%d" % i) for i in range(NT)]
        hT = bufA
        mixT = bufB

        def norm_transpose(stack, tag, tile_srcs, dstT, a_sc, sh_idx, T_a, T_dst_groups, src_is_dram, nxn=5, split=False, q="sp"):
            xt_r = Ring([sbuf(stack, "%s_xt%d" % (tag, i), [128, D], F32) for i in range(2)], tag + "xt") if src_is_dram else None
            xn_r = Ring([sbuf(stack, "%s_xn%d" % (tag, i), [128, D], BF16) for i in range(nxn)], tag + "xn")
            ssq = sbuf(stack, tag + "_ssq", [128, 64], F32)
            pT = [psum(stack, "%s_pT%d" % (tag, i), [128, 512], BF16) for i in range(2)]
            T_pT = [Tok(tag + "pT0"), Tok(tag + "pT1")]
            T_ssq = Tok(tag + "ssq")
            npt = [0]
            ntile = len(tile_srcs)

            def pre(i):
                src, tsrc = tile_srcs[i]
                if src_is_dram:
                    xt, txt = xt_r.next()
                    S.dma(q, "ld_" + txt.name, lambda e: e.dma_start(out=xt[:], in_=src), writes=[txt])
                    xin, tin = xt[:], [txt]
                else:
                    xin, tin = src, list(tsrc)
                xn, txn = xn_r.next()
                S.op("act", lambda e: e.activation(out=xn[:], in_=xin, func=AF.Square, accum_out=ssq[:, i:i + 1]), reads=tin, writes=[txn, T_ssq])
                S.op("act", lambda e: e.activation(out=ssq[:, 32 + i:33 + i], in_=ssq[:, i:i + 1], func=AF.Ln, scale=1.0 / D, bias=epsb[:, 0:1]),
                     reads=[T_ssq, T_eps], writes=[T_ssq])
                S.op("act", lambda e: e.activation(out=ssq[:, 32 + i:33 + i], in_=ssq[:, 32 + i:33 + i], func=AF.Exp, scale=-0.5), reads=[T_ssq], writes=[T_ssq])
                S.op("dve", lambda e: e.tensor_scalar(out=xn[:], in0=xin, scalar1=ssq[:, 32 + i:33 + i], scalar2=None, op0=ALU.mult),
                     reads=tin + [T_ssq], writes=[txn])
                return (xn, txn)

            def post(g, xns):
                for k in range(8):
                    p, tp = pT[npt[0] % 2], T_pT[npt[0] % 2]
                    npt[0] += 1
                    for ii in range(4):
                        xn, txn = xns[ii]
                        S.op("pe", lambda e: e.transpose(p[:, ii * 128:(ii + 1) * 128], xn[:, k * 128:(k + 1) * 128], identb[:]),
                             reads=[txn, T_c], writes=[tp], signal=(ii == 3))
                    S.op("act", lambda e: e.activation(out=dstT[:, k, g * 512:(g + 1) * 512], in_=p[:], func=AF.Identity,
                                                       scale=a_sc[:, k:k + 1], bias=adaF[:, sh_idx, k:k + 1]),
                         reads=[tp, T_a, T_ada], writes=[T_dst_groups[g]])
            if split:
                allx = [pre(i) for i in range(ntile)]
                yield
                for g in range(ntile // 4):
                    post(g, allx[g * 4:g * 4 + 4])
            else:
                for g in range(ntile // 4):
                    xns = [pre(g * 4 + ii) for ii in range(4)]
                    post(g, xns)
                    yield

        epsb = sbuf(st, "epsb", [128, 4], F32)
        T_eps = Tok("eps")
        S.op("dve", lambda e: e.memset(epsb[:, 0:1], EPS), writes=[T_eps])
        S.op("dve", lambda e: e.memset(epsb[:, 1:2], 64 * EPS), writes=[T_eps])
        S.op("dve", lambda e: e.memset(epsb[:, 2:3], 1.0), writes=[T_eps])

        with ExitStack() as sa:
            cTs = sbuf(sa, "cTs", [128, 8], F32)
            cact = sbuf(sa, "cact", [128, 8], F32)
            cbc = sbuf(sa, "cbc", [128, 8, 128], F32)
            wa = [sbuf(sa, "wa%d" % i, [128, 8, 1024], F32) for i in range(2)]
            T_wa = [Tok("wa0"), Tok("wa1")]
            badaFs = sbuf(sa, "badaFs", [128, 48], F32)
            bgrow = sbuf(sa, "bgrow", [128, 2, 1024], F32)
            gmixs = sbuf(sa, "gmixs", [128, 8], F32)
            gffns = sbuf(sa, "gffns", [128, 8], F32)
            lam4bc = sbuf(sa, "lam4bc", [128, 256], F32)
            lamtmp = sbuf(sa, "lamtmp", [128, 128], F32)
            lams = sbuf(sa, "lams", [128, 2], F32)
            subgbc = sbuf(sa, "subgbc", [128, 128], F32)
            pA = psum(sa, "pA", [128, 512], F32)
            pG = psum(sa, "pG", [128, 512], F32)
            T_pA, T_pG = Tok("pA"), Tok("pG")
            T_in = Tok("smallin")
            smalls = [(cTs[:], cT[:, :]), (badaFs[:], badaF[:, :]), (gmixs[:], gmixF[:, :]), (gffns[:], gffnF[:, :]),
                      (gqs[:], gq[:, :]), (gks[:], gk[:, :]), (b1s[:], b1F[:, :]),
                      (lam4bc[:], lam4[0:1, :].partition_broadcast(128)),
                      (subgbc[:], subg[0:1, :].partition_broadcast(128)),
                      (gnbc[:], hgn[0:1, :].partition_broadcast(128)),
                      (brbc[:], b_r[0:1, :].partition_broadcast(128)),
                      (bgrow[:, 0, :], bada_row[0:1, 2 * D:3 * D].partition_broadcast(128)),
                      (bgrow[:, 1, :], bada_row[0:1, 5 * D:6 * D].partition_broadcast(128))]
            for d_, s_ in smalls:
                S.dma("sp", "ld_s", lambda e, d_=d_, s_=s_: e.dma_start(out=d_, in_=s_), writes=[])
            T_in.w = ("ld_s", S.cnt["ld_s"])
            T_in0.w = T_in.w

            T_cact = Tok("cact")
            S.op("act", lambda e: e.activation(out=cact[:], in_=cTs[:], func=AF.Silu), reads=[T_in], writes=[T_cact])
            T_cbc = Tok("cbc")
            S.op("dve", lambda e: e.tensor_copy(out=cbc[:], in_=cact[:].unsqueeze(2).to_broadcast([128, 8, 128])),
                 reads=[T_cact], writes=[T_cbc])
            S.op("dve", lambda e: e.tensor_scalar(out=sg8[:], in0=subgbc[:], scalar1=1.0 - LAMBDA_INIT, scalar2=None, op0=ALU.mult),
                 reads=[T_in], writes=[T_misc])
            b1v = b1s[:].rearrange("p (e j) -> p e j", j=16)
            S.op("dve", lambda e: e.tensor_scalar(out=b1v[:, :, 8:16], in0=b1v[:, :, 8:16], scalar1=1.0, scalar2=None, op0=ALU.add),
                 reads=[T_in], writes=[T_misc])
            S.op("dve", lambda e: e.tensor_tensor(out=lamtmp[:, 0:64], in0=lam4bc[:, 0:64], in1=lam4bc[:, 64:128], op=ALU.mult),
                 reads=[T_in], writes=[T_misc])
            S.op("dve", lambda e: e.tensor_tensor(out=lamtmp[:, 64:128], in0=lam4bc[:, 128:192], in1=lam4bc[:, 192:256], op=ALU.mult),
                 reads=[T_in], writes=[T_misc])
            S.op("dve", lambda e: e.reduce_sum(out=lams[:], in_=lamtmp[:].rearrange("p (a b) -> p a b", a=2), axis=AX.X),
                 reads=[T_misc], writes=[T_misc])
            S.op("act", lambda e: e.activation(out=lams[:], in_=lams[:], func=AF.Exp), reads=[T_misc], writes=[T_misc])
            S.op("dve", lambda e: e.tensor_tensor(out=nlam[:], in0=lams[:, 1:2], in1=lams[:, 0:1], op=ALU.subtract),
                 reads=[T_misc], writes=[T_misc])
            S.op("dve", lambda e: e.tensor_scalar(out=nlam[:], in0=nlam[:], scalar1=-LAMBDA_INIT, scalar2=None, op0=ALU.add),
                 reads=[T_misc], writes=[T_misc])

            srcsB = [(x[i * 128:(i + 1) * 128, :], None) for i in range(NT)]
            genB = norm_transpose(sa, "B", srcsB, hT, a1, 0, T_a1, T_hT, True, nxn=NT, split=True, q="act")
            next(genB)
            wav = w_ada.rearrange("(k p) n -> p k n", p=128)
            for j in range(6):
                wb, tw = wa[j % 2], T_wa[j % 2]
                S.dma("sp", "ld_wa%d" % (j % 2), lambda e, wb=wb, j=j: e.dma_start(out=wb[:, 0:4, :], in_=wav[:, 0:4, j * D:(j + 1) * D]),
                      writes=[tw])
                S.dma("sp", "ld_wa%d" % (j % 2), lambda e, wb=wb, j=j: e.dma_start(out=wb[:, 4:8, :], in_=wav[:, 4:8, j * D:(j + 1) * D]),
                      writes=[])
                tw.w = ("ld_wa%d" % (j % 2), S.cnt["ld_wa%d" % (j % 2)])
                if j in (2, 5):
                    gdst = g1bc if j == 2 else g2bc
                    tg = T_g1 if j == 2 else T_g2
                    for half in range(2):
                        for k in range(8):
                            S.op("pe", lambda e, wb=wb, k=k, half=half: e.matmul(
                                pG[:], lhsT=cbc[:, k, :], rhs=wb[:, k, half * 512:(half + 1) * 512], start=(k == 0), stop=(k == 7)),
                                reads=[T_cbc, tw], writes=[T_pG], signal=(k == 7))
                        S.op("dve", lambda e, gdst=gdst, half=half, j=j: e.tensor_tensor(
                            out=gdst[:, half * 512:(half + 1) * 512], in0=pG[:], in1=bgrow[:, 0 if j == 2 else 1, half * 512:(half + 1) * 512],
                            op=ALU.add), reads=[T_pG, T_in], writes=[tg])
                else:
                    for m in range(8):
                        for k in range(8):
                            S.op("pe", lambda e, wb=wb, k=k, m=m: e.matmul(
                                pA[:, m:m + 1], lhsT=wb[:, k, m * 128:(m + 1) * 128], rhs=cact[:, k:k + 1], start=(k == 0), stop=(k == 7)),
                                reads=[T_cact, tw], writes=[T_pA], signal=(k == 7))
                    S.op("dve", lambda e, j=j: e.tensor_tensor(out=adaF[:, j, :], in0=pA[:, 0:8], in1=badaFs[:, j * 8:(j + 1) * 8], op=ALU.add),
                         reads=[T_pA, T_in], writes=[T_ada])
            S.op("dve", lambda e: e.scalar_tensor_tensor(out=a1[:], in0=adaF[:, 1, :], scalar=1.0, in1=gmixs[:], op0=ALU.add, op1=ALU.mult),
                 reads=[T_ada, T_in], writes=[T_a1])
            S.op("dve", lambda e: e.scalar_tensor_tensor(out=a2[:], in0=adaF[:, 4, :], scalar=1.0, in1=gffns[:], op0=ALU.add, op1=ALU.mult),
                 reads=[T_ada, T_in], writes=[T_a2])
            for _ in genB:
                pass
            if "ada" in dbg:
                dump("adaF", adaF[:].rearrange("p a b -> p (a b)"), T_ada, [128, 48], F32)
                dump("g1bc", g1bc[:], T_g1, [128, D], F32)
                dump("g2bc", g2bc[:], T_g2, [128, D], F32)
                dump("nlam", nlam[:], T_misc, [128, 1], F32)

        S.barrier()
        if "hT" in dbg:
            for g in range(4):
                dump("hT%d" % g, hT[:, :, g * 512:(g + 1) * 512], T_hT[g], [128, 8, 512], BF16)

        if stop_after == "B":
            S.final_wait("sp", out_toks)
            return nc, dbg_outs

        with ExitStack() as s1:
            wblk2 = sbuf(s1, "wblk2", [128, 8, 1536], BF16)
            T_wblk2 = Tok("wblk2")
            wst_r = Ring([sbuf(s1, "wst%d" % i, [128, 2048], F32) for i in range(2)], "wst")

            def load_wblk(src2d, col0, W, dst, T_dst, dcol0=0, eng="pool"):
                srcv = src2d.rearrange("(k p) n -> p k n", p=128)
                for k in range(8):
                    stg, tst = wst_r.next()
                    S.dma("sp", "ld_" + tst.name, lambda e, stg=stg, k=k: e.dma_start(out=stg[:, 0:W], in_=srcv[:, k, col0:col0 + W]), writes=[tst])
                    en = eng if eng != "alt" else ("act" if k % 2 == 0 else "dve")
                    if en == "act":
                        S.op("act", lambda e, stg=stg, k=k: e.copy(out=dst[:, k, dcol0:dcol0 + W], in_=stg[:, 0:W]), reads=[tst], writes=[T_dst])
                    else:
                        S.op(en, lambda e, stg=stg, k=k: e.tensor_copy(out=dst[:, k, dcol0:dcol0 + W], in_=stg[:, 0:W]), reads=[tst], writes=[T_dst])

            with ExitStack() as sh:
                wblk = sbuf(sh, "wblk", [128, 8, 2048], BF16)
                T_wblk = Tok("wblk")
                load_wblk(w_in, 0, 2048, wblk, T_wblk, eng="alt")
                lblbc = sbuf(sh, "lblbc", [128, 1024], F32)
                lbbc = sbuf(sh, "lbbc", [128, 512], F32)
                omlbc = sbuf(sh, "omlbc", [128, 512], F32)
                T_lb = Tok("lb")
                S.dma("sp", "ld_lb", lambda e: e.dma_start(out=lblbc[:], in_=lbl[0:1, :].partition_broadcast(128)), writes=[T_lb])
                S.op("dve", lambda e: e.tensor_tensor(out=lbbc[:], in0=lblbc[:, 0:512], in1=lblbc[:, 512:1024], op=ALU.subtract), reads=[T_lb], writes=[T_lb])
                S.op("act", lambda e: e.activation(out=lbbc[:], in_=lbbc[:], func=AF.Sigmoid), reads=[T_lb], writes=[T_lb])
                S.op("dve", lambda e: e.tensor_scalar(out=omlbc[:], in0=lbbc[:], scalar1=-1.0, scalar2=1.0, op0=ALU.mult, op1=ALU.add), reads=[T_lb], writes=[T_lb])
                load_wblk(w_in, 2048, 1536, wblk2, T_wblk2, eng="pool")
                pq = psum(sh, "pq", [128, 512], F32); pf = psum(sh, "pf", [128, 512], F32)
                pi_ = psum(sh, "pi", [128, 512], F32); pog = psum(sh, "pog", [128, 512], F32)
                pT = psum(sh, "pT", [128, 1024], BF16); pT2 = psum(sh, "pT2", [128, 1024], BF16)
                po = psum(sh, "po", [128, 512], F32); pkv = psum(sh, "pkv", [128, 512], F32)
                psc = pf
                T_pq, T_pf, T_pi, T_pog, T_pT, T_pT2, T_po, T_pkv = [Tok(n) for n in "pq pf pi pog pT pT2 po pkv".split()]
                T_psc = T_pf
                def f32t(n): return sbuf(sh, n, [128, 512], F32), Tok(n)
                def bft(n, w=512): return sbuf(sh, n, [128, w], BF16), Tok(n)
                fs, T_fs = f32t("fs"); lf, T_lf = f32t("lf"); kk, T_kk = f32t("kk"); qs, T_qs = f32t("qs")
                eb, T_eb = f32t("eb"); enb, T_enb = f32t("enb")
                Sst, T_Sst = f32t("Sst"); tmpkv, T_tmpkv = f32t("tmpkv"); onf, T_onf = f32t("onf")
                qt, T_qt = bft("qt"); Stb, T_Stb = bft("Stb"); oa, T_oa = bft("oa")
                ogs_r = Ring([sbuf(sh, "ogs%d" % a, [128, 512], F32) for a in range(2)], "ogs")
                kt_r = Ring([sbuf(sh, "kt%d" % a, [128, 512], BF16) for a in range(2)], "kt")
                vb_r = Ring([sbuf(sh, "vb%d" % a, [128, 512], BF16) for a in range(2)], "vb")
                qkT_r = Ring([sbuf(sh, "qkT%d" % a, [128, 1024], BF16) for a in range(2)], "qkT")
                scm_r = Ring([sbuf(sh, "scm%d" % a, [128, 512], BF16) for a in range(2)], "scm")
                ebm_r = Ring([sbuf(sh, "ebm%d" % a, [128, 3, 4], F32) for a in range(2)], "ebm")
                bm3 = sbuf(sh, "bm3", [128, 3, 4], F32); T_bm3 = Tok("bm3")
                ssq4 = sbuf(sh, "ssq4", [128, 4], F32); T_ssq4 = Tok("ssq4")
                v4 = lambda ap: ap.rearrange("p (h v) -> p h v", h=4)
                hctx = {}

                def front(i):
                    tc0, tc1 = i * 128, (i + 1) * 128
                    g = i // 4
                    ogs, T_ogs = ogs_r.next(); kt, T_kt = kt_r.next(); vb, T_vb = vb_r.next()
                    qkT, T_qkT = qkT_r.next(); scm, T_scm = scm_r.next(); ebm, T_ebm = ebm_r.next()
                    hctx[i] = (ogs, T_ogs, kt, T_kt, vb, T_vb, qkT, T_qkT, scm, T_scm, ebm, T_ebm)
                    for (p, tp, c0) in [(pf, T_pf, 512), (pq, T_pq, 0), (pi_, T_pi, 1024), (pog, T_pog, 1536)]:
                        for k in range(8):
                            S.op("pe", lambda e, p=p, k=k, c0=c0: e.matmul(p[:], lhsT=hT[:, k, tc0:tc1], rhs=wblk[:, k, c0:c0 + 512], start=(k == 0), stop=(k == 7)),
                                 reads=[T_hT[g], T_wblk], writes=[tp], signal=(k == 7))
                        yield
                    S.op("act", lambda e: e.activation(out=fs[:], in_=pf[:], func=AF.Exp, scale=-1.0), reads=[T_pf], writes=[T_fs]); yield
                    S.op("act", lambda e: e.activation(out=fs[:], in_=fs[:], func=AF.Identity, bias=epsb[:, 2:3]), reads=[T_fs, T_eps], writes=[T_fs]); yield
                    S.op("dve", lambda e: e.reciprocal(out=fs[:], in_=fs[:]), reads=[T_fs], writes=[T_fs]); yield
                    S.op("dve", lambda e: e.tensor_tensor(out=fs[:], in0=fs[:], in1=omlbc[:], op=ALU.mult), reads=[T_fs, T_lb], writes=[T_fs]); yield
                    S.op("dve", lambda e: e.tensor_tensor(out=fs[:], in0=fs[:], in1=lbbc[:], op=ALU.add), reads=[T_fs, T_lb], writes=[T_fs]); yield
                    S.op("act", lambda e: e.activation(out=lf[:], in_=fs[:], func=AF.Ln), reads=[T_fs], writes=[T_lf]); yield
                    S.op("act", lambda e: e.activation(out=kk[:], in_=fs[:], func=AF.Identity, scale=-1.0, bias=epsb[:, 2:3]), reads=[T_fs, T_eps], writes=[T_kk]); yield
                    S.op("pe", lambda e: e.matmul(pf[:], lhsT=amat[:, 0:128], rhs=lf[:], start=True, stop=True), reads=[T_lf, T_c], writes=[T_pf]); yield
                    S.op("act", lambda e: e.activation(out=qs[:], in_=pq[:], func=AF.Exp, scale=-1.0), reads=[T_pq], writes=[T_qs]); yield
                    S.op("act", lambda e: e.activation(out=qs[:], in_=qs[:], func=AF.Identity, bias=epsb[:, 2:3]), reads=[T_qs, T_eps], writes=[T_qs]); yield
                    S.op("dve", lambda e: e.reciprocal(out=qs[:], in_=qs[:]), reads=[T_qs], writes=[T_qs]); yield
                    S.op("dve", lambda e: e.tensor_tensor(out=qs[:], in0=qs[:], in1=pq[:], op=ALU.mult), reads=[T_qs, T_pq], writes=[T_qs]); yield
                    for h in range(4):
                        S.op("pe", lambda e, h=h: e.matmul(pq[:, 2 * h:2 * h + 2], lhsT=lf[:, h * 128:(h + 1) * 128], rhs=amat[:, 128:130], start=True, stop=True),
                             reads=[T_lf, T_c], writes=[T_pq], signal=(h == 3))
                    yield
                    S.op("act", lambda e: e.copy(out=vb[:], in_=pi_[:]), reads=[T_pi], writes=[T_vb]); yield
                    S.op("act", lambda e: e.activation(out=ogs[:], in_=pog[:], func=AF.Exp, scale=-1.0), reads=[T_pog], writes=[T_ogs]); yield
                    S.op("act", lambda e: e.activation(out=ogs[:], in_=ogs[:], func=AF.Identity, bias=epsb[:, 2:3]), reads=[T_ogs, T_eps], writes=[T_ogs]); yield
                    S.op("dve", lambda e: e.reciprocal(out=ogs[:], in_=ogs[:]), reads=[T_ogs], writes=[T_ogs]); yield
                    S.op("dve", lambda e: e.tensor_tensor(out=ogs[:], in0=ogs[:], in1=pog[:], op=ALU.mult), reads=[T_ogs, T_pog], writes=[T_ogs]); yield
                    S.op("act", lambda e: e.activation(out=eb[:], in_=pf[:], func=AF.Exp), reads=[T_pf], writes=[T_eb]); yield
                    S.op("act", lambda e: e.activation(out=enb[:], in_=pf[:], func=AF.Exp, scale=-1.0), reads=[T_pf], writes=[T_enb]); yield
                    S.op("dve", lambda e: e.tensor_tensor(out=qt[:], in0=qs[:], in1=eb[:], op=ALU.mult), reads=[T_qs, T_eb], writes=[T_qt]); yield
                    S.op("dve", lambda e: e.tensor_tensor(out=kt[:], in0=kk[:], in1=enb[:], op=ALU.mult), reads=[T_kk, T_enb], writes=[T_kt]); yield
                    S.op("dve", lambda e: e.tensor_copy(out=bm3[:, 0:2, :], in_=pq[:, 0:8].rearrange("p (h t) -> p t h", t=2)), reads=[T_pq], writes=[T_bm3]); yield
                    S.op("dve", lambda e: e.tensor_tensor(out=bm3[:, 2, :], in0=bm3[:, 1, :], in1=bm3[:, 0, :], op=ALU.subtract), reads=[T_bm3], writes=[T_bm3]); yield
                    S.op("act", lambda e: e.activation(out=ebm[:].rearrange("p a b -> p (a b)"), in_=bm3[:].rearrange("p a b -> p (a b)"), func=AF.Exp),
                         reads=[T_bm3], writes=[T_ebm]); yield
                    for h in range(4):
                        S.op("pe", lambda e, h=h: e.transpose(pT[:, h * 128:(h + 1) * 128], qt[:, h * 128:(h + 1) * 128], identb[:]),
                             reads=[T_qt, T_c], writes=[T_pT], signal=False)
                    for h in range(4):
                        S.op("pe", lambda e, h=h: e.transpose(pT[:, 512 + h * 128:512 + (h + 1) * 128], kt[:, h * 128:(h + 1) * 128], identb[:]),
                             reads=[T_kt, T_c], writes=[T_pT], signal=(h == 3))
                    yield
                    S.op("act", lambda e: e.copy(out=qkT[:], in_=pT[:]), reads=[T_pT], writes=[T_qkT]); yield
                    for h in range(4):
                        S.op("pe", lambda e, h=h: e.matmul(psc[:, h * 128:(h + 1) * 128], lhsT=qkT[:, 512 + h * 128:512 + (h + 1) * 128],
                                                           rhs=qkT[:, h * 128:(h + 1) * 128], start=True, stop=True),
                             reads=[T_qkT], writes=[T_psc], signal=(h == 3))
                    yield
                    S.op("dve", lambda e: e.tensor_tensor(out=v4(scm[:]), in0=v4(psc[:]), in1=tri[:].unsqueeze(1).to_broadcast([128, 4, 128]), op=ALU.mult),
                         reads=[T_psc, T_c], writes=[T_scm]); yield

                def back(i):
                    tc0, tc1 = i * 128, (i + 1) * 128
                    (ogs, T_ogs, kt, T_kt, vb, T_vb, qkT, T_qkT, scm, T_scm, ebm, T_ebm) = hctx.pop(i)
                    if i > 0:
                        S.op("dve", lambda e: e.tensor_tensor(out=v4(Stb[:]), in0=v4(Sst[:]), in1=ebm[:, 0, :].unsqueeze(2).to_broadcast([128, 4, 128]), op=ALU.mult),
                             reads=[T_Sst, T_ebm], writes=[T_Stb]); yield
                    for h in range(4):
                        hs0, hs1 = h * 128, (h + 1) * 128
                        S.op("pe", lambda e, hs0=hs0, hs1=hs1: e.matmul(pkv[:, hs0:hs1], lhsT=kt[:, hs0:hs1], rhs=vb[:, hs0:hs1], start=True, stop=True),
                             reads=[T_kt, T_vb], writes=[T_pkv], signal=(h == 3))
                    yield
                    S.op("dve", lambda e: e.tensor_tensor(out=v4(tmpkv[:]), in0=v4(pkv[:]), in1=ebm[:, 2, :].unsqueeze(2).to_broadcast([128, 4, 128]), op=ALU.mult),
                         reads=[T_pkv, T_ebm], writes=[T_tmpkv]); yield
                    for h in range(4):
                        hs0, hs1 = h * 128, (h + 1) * 128
                        S.op("pe", lambda e, hs0=hs0, hs1=hs1: e.matmul(po[:, hs0:hs1], lhsT=scm[:, hs0:hs1], rhs=vb[:, hs0:hs1], start=True, stop=(i == 0)),
                             reads=[T_scm, T_vb], writes=[T_po], signal=(i == 0 and h == 3))
                        if i > 0:
                            S.op("pe", lambda e, hs0=hs0, hs1=hs1: e.matmul(po[:, hs0:hs1], lhsT=qkT[:, hs0:hs1], rhs=Stb[:, hs0:hs1], start=False, stop=True),
                                 reads=[T_qkT, T_Stb], writes=[T_po], signal=(h == 3))
                    yield
                    if i == 0:
                        S.op("dve", lambda e: e.tensor_copy(out=Sst[:], in_=tmpkv[:]), reads=[T_tmpkv], writes=[T_Sst]); yield
                    else:
                        S.op("dve", lambda e: e.tensor_tensor(out=v4(Sst[:]), in0=v4(Sst[:]), in1=ebm[:, 1, :].unsqueeze(2).to_broadcast([128, 4, 128]), op=ALU.mult),
                             reads=[T_Sst, T_ebm, T_Stb], writes=[T_Sst]); yield
                        S.op("dve", lambda e: e.tensor_tensor(out=Sst[:], in0=Sst[:], in1=tmpkv[:], op=ALU.add), reads=[T_Sst, T_tmpkv], writes=[T_Sst]); yield
                    S.op("act", lambda e: e.activation(out=onf[:], in_=po[:], func=AF.Square), reads=[T_po], writes=[T_onf]); yield
                    S.op("dve", lambda e: e.reduce_sum(out=ssq4[:], in_=v4(onf[:]), axis=AX.X), reads=[T_onf], writes=[T_ssq4]); yield
                    S.op("act", lambda e: e.activation(out=ssq4[:], in_=ssq4[:], func=AF.Ln, scale=1.0 / 128, bias=epsb[:, 0:1]), reads=[T_ssq4, T_eps], writes=[T_ssq4]); yield
                    S.op("act", lambda e: e.activation(out=ssq4[:], in_=ssq4[:], func=AF.Exp, scale=-0.5), reads=[T_ssq4], writes=[T_ssq4]); yield
                    S.op("dve", lambda e: e.tensor_tensor(out=v4(onf[:]), in0=v4(po[:]), in1=ssq4[:].unsqueeze(2).to_broadcast([128, 4, 128]), op=ALU.mult),
                         reads=[T_po, T_ssq4], writes=[T_onf]); yield
                    S.op("pool", lambda e: e.tensor_tensor(out=v4(onf[:]), in0=v4(onf[:]), in1=gnbc[:].unsqueeze(1).to_broadcast([128, 4, 128]), op=ALU.mult),
                         reads=[T_onf, T_in0], writes=[T_onf]); yield
                    S.op("pool", lambda e: e.tensor_tensor(out=oa[:], in0=onf[:], in1=ogs[:], op=ALU.mult), reads=[T_onf, T_ogs], writes=[T_oa]); yield
                    for h in range(4):
                        S.op("pe", lambda e, h=h: e.transpose(pT2[:, h * 128:(h + 1) * 128], oa[:, h * 128:(h + 1) * 128], identb[:]),
                             reads=[T_oa, T_c], writes=[T_pT2], signal=(h == 3))
                    yield
                    S.op("act", lambda e: e.copy(out=mixT[:, 0:4, tc0:tc1], in_=pT2[:, 0:512].rearrange("p (h v) -> p h v", h=4)),
                         reads=[T_pT2], writes=[T_mixA[i]]); yield

                interleave(front(0))
                for i in range(NT):
                    interleave(front(i + 1) if i + 1 < NT else None, back(i))
                if "oa" in dbg:
                    T_all = Tok("mixAall")
                    T_all.w = T_mixA[NT - 1].w
                    dump("oaT", mixT[:, 0:4, :], T_all, [128, 4, S_TOK], BF16)
            S.barrier()
            if stop_after == "C1":
                S.final_wait("sp", out_toks)
                return nc, dbg_outs

            with ExitStack() as sd:
                pp = [psum(sd, "pp%d" % i, [128, 512], F32) for i in range(2)]
                T_pp = [Tok("pp0"), Tok("pp1")]
                pss = psum(sd, "pss", [128, 512], F32); T_pss = Tok("pss")
                pOb = [psum(sd, "pO%d" % i, [128, 512], F32) for i in range(4)]
                T_pOb = [[Tok("pO%d_%d" % (a, i)) for i in range(2)] for a in range(2)]
                T_pO = [[T_pOb[a][i // 2] for i in range(4)] for a in range(2)]

                def pOap(a, ql, w):
                    c0 = (ql % 2) * 256
                    return pOb[a * 2 + ql // 2][:, c0:c0 + w]
                pTo = psum(sd, "pTo", [128, 512], BF16); T_pTo = Tok("pTo")
                qn = [sbuf(sd, "qnT%d" % i, [128, S_TOK], BF16) for i in range(2)]
                kn = [sbuf(sd, "knT%d" % i, [128, S_TOK], BF16) for i in range(2)]
                T_qn = [[Tok("qn%d_%d" % (i, t)) for t in range(8)] for i in range(2)]
                T_kn = [[Tok("kn%d_%d" % (i, t)) for t in range(8)] for i in range(2)]
                Vext = sbuf(sd, "Vext", [128, NT, 4, 130], BF16); T_V = Tok("Vext")
                sq_r = Ring([sbuf(sd, "sq%d" % i, [128, 512], BF16) for i in range(2)], "sq")
                rs_r = Ring([sbuf(sd, "rs%d" % i, [128, 512], F32) for i in range(2)], "rs")
                PTall = [sbuf(sd, "PTall%d" % i, [128, NT, 512], BF16) for i in range(2)]
                T_PT = [[Tok("PT%d_%d" % (i, j)) for j in range(NT)] for i in range(2)]
                o1 = sbuf(sd, "o1", [128, 4, 128], F32); T_o1 = [Tok("o1_%d" % i) for i in range(4)]
                od_r = Ring([sbuf(sd, "od%d" % i, [128, 128], F32) for i in range(2)], "od")
                odn_r = Ring([sbuf(sd, "odn%d" % i, [128, 128], BF16) for i in range(2)], "odn")
                jk = sbuf(sd, "jk", [128, 128], BF16); T_jk = Tok("jk")
                rr_r = Ring([sbuf(sd, "rr%d" % i, [128, 4], F32) for i in range(8)], "rr")
                npp = [0]

                def nextpp():
                    a = npp[0] % 2
                    npp[0] += 1
                    return pp[a], T_pp[a]
                S.op("dve", lambda e: e.memset(Vext[:, :, :, 128:130], 1.0), writes=[T_V])
                for i in range(NT):
                    p, tp = nextpp()
                    for k in range(8):
                        S.op("pe", lambda e, p=p, k=k, i=i: e.matmul(p[:], lhsT=hT[:, k, i * 128:(i + 1) * 128], rhs=wblk2[:, k, 1024:1536], start=(k == 0), stop=(k == 7)),
                             reads=[T_hT[i // 4], T_wblk2], writes=[tp], signal=(k == 7))
                    S.op("act", lambda e, p=p, i=i: e.copy(out=Vext[:, i, :, 0:128], in_=p[:].rearrange("p (h v) -> p h v", h=4)), reads=[tp], writes=[T_V])

                T_pssA, T_pssB = Tok("pssA"), Tok("pssB")

                def proj_chunk(h, n):
                    t8, which = divmod(n, 2)
                    hb = h % 2
                    if which == 0:
                        dst, T_dst, c0, gs = qn[hb], T_qn[hb][t8], h * 128, gqs
                    else:
                        dst, T_dst, c0, gs = kn[hb], T_kn[hb][t8], 512 + h * 128, gks
                    p = pss[:, 0:256]
                    pb = pss[:, 256:512]
                    for k in range(8):
                        S.op("pe", lambda e, k=k: e.matmul(p, lhsT=wblk2[:, k, c0:c0 + 128], rhs=hT[:, k, t8 * 256:(t8 + 1) * 256], start=(k == 0), stop=(k == 7)),
                             reads=[T_hT[t8 // 2], T_wblk2], writes=[T_pssA], signal=(k == 7))
                    yield
                    sq, tsq = sq_r.next()
                    S.op("act", lambda e: e.activation(out=sq[:, 0:256], in_=p, func=AF.Square), reads=[T_pssA], writes=[tsq]); yield
                    S.op("pe", lambda e: e.matmul(pb, lhsT=bones[:], rhs=sq[:, 0:256], start=True, stop=True), reads=[tsq, T_c], writes=[T_pssB]); yield
                    rs, trs = rs_r.next()
                    S.op("act", lambda e: e.activation(out=rs[:, 0:256], in_=pb, func=AF.Ln, bias=epsb[:, 1:2]), reads=[T_pssB, T_eps], writes=[trs]); yield
                    S.op("act", lambda e: e.activation(out=rs[:, 0:256], in_=rs[:, 0:256], func=AF.Exp, scale=-0.5), reads=[trs], writes=[trs]); yield
                    S.op("dve", lambda e: e.scalar_tensor_tensor(out=dst[:, t8 * 256:(t8 + 1) * 256], in0=p, scalar=gs[:, 0:1], in1=rs[:, 0:256], op0=ALU.mult, op1=ALU.mult),
                         reads=[T_pssA, trs, T_in0], writes=[T_dst]); yield

                def proj_two(h, n):
                    yield from proj_chunk(h, 2 * n)
                    yield from proj_chunk(h, 2 * n + 1)

                def att_geom(g, j):
                    if j < 4 * g:
                        return 4 * g * 128, 512, False
                    return j * 128, (4 * g + 4 - j) * 128, True

                def att_qk(h, g, c, b):
                    hb = h % 2
                    cs0, cs1 = c * 64, (c + 1) * 64
                    nj = 4 * g + 4

                    def qk(j):
                        q0, nq, diag = att_geom(g, j)
                        p, tp = nextpp()
                        S.op("pe", lambda e: e.matmul(p[:, 0:nq], lhsT=kn[hb][cs0:cs1, j * 128:(j + 1) * 128], rhs=qn[hb][cs0:cs1, q0:q0 + nq], start=True, stop=True),
                             reads=[T_kn[hb][j // 2], T_qn[hb][2 * g], T_qn[hb][2 * g + 1]], writes=[tp])
                        return p, tp
                    cur = qk(0)
                    yield
                    for j in range(nj):
                        q0, nq, diag = att_geom(g, j)
                        p, tp = cur
                        PT, tPT = PTall[b][:, j, :], T_PT[b][j]
                        S.op("act", lambda e: e.activation(out=PT[:, 0:nq], in_=p[:, 0:nq], func=AF.Exp, scale=8.0), reads=[tp], writes=[tPT])
                        yield
                        if j + 1 < nj:
                            cur = qk(j + 1)
                            yield
                        if diag:
                            S.op("dve", lambda e: e.tensor_tensor(out=PT[:, 0:128], in0=PT[:, 0:128], in1=tri[:], op=ALU.mult), reads=[tPT, T_c], writes=[tPT])
                            yield

                def att_pv(h, g, c, a, b):
                    for ql in range(4):
                        qi = 4 * g + ql
                        for j in range(qi + 1):
                            q0, nq, diag = att_geom(g, j)
                            off = qi * 128 - q0
                            S.op("pe", lambda e: e.matmul(pOap(a, ql, 129), lhsT=PTall[b][:, j, off:off + 128], rhs=Vext[:, j, h, 0:129], start=(j == 0), stop=(j == qi)),
                                 reads=[T_PT[b][j], T_V], writes=[T_pO[a][ql]], signal=(j == qi))
                            if j % 2 == 1:
                                yield
                        yield

                def att_epi(h, g, c, a):
                    for ql in range(4):
                        qi = 4 * g + ql
                        rr, trr = rr_r.next()
                        S.op("dve", lambda e: e.reciprocal(out=rr[:, 0:1], in_=pOap(a, ql, 129)[:, 128:129]), reads=[T_pO[a][ql]], writes=[trr]); yield
                        if c == 0:
                            S.op("dve", lambda e: e.tensor_scalar(out=o1[:, ql, :], in0=pOap(a, ql, 128), scalar1=rr[:, 0:1], scalar2=None, op0=ALU.mult),
                                 reads=[T_pO[a][ql], trr], writes=[T_o1[ql]]); yield
                        else:
                            S.op("dve", lambda e: e.tensor_tensor(out=rr[:, 1:2], in0=rr[:, 0:1], in1=nlam[:], op=ALU.mult), reads=[trr, T_misc], writes=[trr]); yield
                            od, tod = od_r.next()
                            S.op("dve", lambda e: e.scalar_tensor_tensor(out=od[:], in0=pOap(a, ql, 128), scalar=rr[:, 1:2], in1=o1[:, ql, :], op0=ALU.mult, op1=ALU.add),
                                 reads=[T_pO[a][ql], trr, T_o1[ql]], writes=[tod]); yield
                            S.op("act", lambda e: e.activation(out=jk[:], in_=od[:], func=AF.Square, accum_out=rr[:, 2:3]), reads=[tod], writes=[T_jk, trr]); yield
                            S.op("act", lambda e: e.activation(out=rr[:, 3:4], in_=rr[:, 2:3], func=AF.Ln, scale=1.0 / 128, bias=epsb[:, 0:1]),
                                 reads=[trr, T_eps], writes=[trr]); yield
                            S.op("act", lambda e: e.activation(out=rr[:, 3:4], in_=rr[:, 3:4], func=AF.Exp, scale=-0.5), reads=[trr], writes=[trr]); yield
                            odn, todn = odn_r.next()
                            S.op("dve", lambda e: e.scalar_tensor_tensor(out=odn[:], in0=od[:], scalar=rr[:, 3:4], in1=sg8[:], op0=ALU.mult, op1=ALU.mult),
                                 reads=[tod, trr, T_misc], writes=[todn]); yield
                            S.op("pe", lambda e: e.transpose(pTo[:, 0:128], odn[:], identb[:]), reads=[todn, T_c], writes=[T_pTo]); yield
                            S.op("act", lambda e: e.copy(out=mixT[:, 4 + h, qi * 128:(qi + 1) * 128], in_=pTo[:, 0:128]), reads=[T_pTo], writes=[T_mixD[qi]]); yield

                for n in range(8):
                    interleave(proj_two(0, n))
                its = [(h, g, c) for h in range(4) for g in range(4) for c in range(2)]
                NI = len(its)
                for n in range(NI + 2):
                    gens = []
                    if n < NI:
                        h, g, c = its[n]
                        gens.append(att_qk(h, g, c, n % 2))
                    if 1 <= n <= NI:
                        h1, g1, c1 = its[n - 1]
                        gens.append(att_pv(h1, g1, c1, (n - 1) % 2, (n - 1) % 2))
                    if 2 <= n <= NI + 1:
                        h2, g2, c2 = its[n - 2]
                        gens.append(att_epi(h2, g2, c2, (n - 2) % 2))
                    if n < NI and its[n][0] < 3:
                        gens.append(proj_two(its[n][0] + 1, n % 8))
                    interleave(*gens)
                if "od" in dbg:
                    T_all = Tok("mixDall")
                    T_all.w = ("act", S.cnt["act"])
                    dump("odT", mixT[:, 4:8, :], T_all, [128, 4, S_TOK], BF16)
            S.barrier()
            if stop_after == "C2":
                S.final_wait("sp", out_toks)
                return nc, dbg_outs

        with ExitStack() as s2:
            acc = sbuf(s2, "acc", [128, NT, D], F32)
            T_acc = [[Tok("acc%d_%d" % (i, hf)) for hf in range(2)] for i in range(NT)]
            with ExitStack() as so:
                wst2_r = Ring([sbuf(so, "wst2_%d" % i, [128, 1024], F32) for i in range(3)], "wst2")
                wg = sbuf(so, "wg", [128, 8, 1024], BF16); T_wg = Tok("wg")
                wo = sbuf(so, "wo", [128, 8, 512], BF16); T_wo = Tok("wo")
                pz = [[psum(so, "pz%d_%d" % (a, b), [128, 512], F32) for b in range(4)] for a in range(2)]
                T_pz = [[Tok("pz%d_%d" % (a, b)) for b in range(4)] for a in range(2)]
                sa_r = Ring([sbuf(so, "sa%d" % i, [128, 512], F32) for i in range(2)], "sa")
                sd_r = Ring([sbuf(so, "sdg%d" % i, [128, 512], F32) for i in range(2)], "sdg")
                t1_r = Ring([sbuf(so, "t1_%d" % i, [128, 512], F32) for i in range(2)], "t1")
                t2_r = Ring([sbuf(so, "t2_%d" % i, [128, 512], F32) for i in range(2)], "t2")
                xh_r = Ring([sbuf(so, "xh%d" % i, [128, 512], F32) for i in range(2)], "xh")

                def load_w2(src2d, col0, W, dst, T_dst, dcol0):
                    srcv = src2d.rearrange("(k p) n -> p k n", p=128)
                    for k in range(8):
                        stg, tst = wst2_r.next()
                        S.dma("sp", "ld_" + tst.name, lambda e, stg=stg, k=k: e.dma_start(out=stg[:, 0:W], in_=srcv[:, k, col0:col0 + W]), writes=[tst])
                        if k % 2 == 0:
                            S.op("act", lambda e, stg=stg, k=k: e.copy(out=dst[:, k, dcol0:dcol0 + W], in_=stg[:, 0:W]), reads=[tst], writes=[T_dst])
                        else:
                            S.op("dve", lambda e, stg=stg, k=k: e.tensor_copy(out=dst[:, k, dcol0:dcol0 + W], in_=stg[:, 0:W]), reads=[tst], writes=[T_dst])
                for hf in range(2):
                    load_w2(w_in, 3584 + hf * 512, 512, wg, T_wg, 0)
                    load_w2(w_in, 4608 + hf * 512, 512, wg, T_wg, 512)
                    load_w2(w_out, hf * 512, 512, wo, T_wo, 0)
                    for i in range(NT):
                        tc0, tc1 = i * 128, (i + 1) * 128
                        pzz, tzz = pz[i % 2], T_pz[i % 2]
                        for k in range(8):
                            S.op("pe", lambda e, k=k, p=pzz[0]: e.matmul(p[:], lhsT=hT[:, k, tc0:tc1], rhs=wg[:, k, 0:512], start=(k == 0), stop=(k == 7)),
                                 reads=[T_hT[i // 4], T_wg], writes=[tzz[0]], signal=(k == 7))
                        for k in range(8):
                            S.op("pe", lambda e, k=k, p=pzz[1]: e.matmul(p[:], lhsT=hT[:, k, tc0:tc1], rhs=wg[:, k, 512:1024], start=(k == 0), stop=(k == 7)),
                                 reads=[T_hT[i // 4], T_wg], writes=[tzz[1]], signal=(k == 7))
                        for k in range(4):
                            S.op("pe", lambda e, k=k, p=pzz[2]: e.matmul(p[:], lhsT=mixT[:, k, tc0:tc1], rhs=wo[:, k, :], start=(k == 0), stop=(k == 3)),
                                 reads=[T_mixA[i], T_wo], writes=[tzz[2]], signal=(k == 3))
                        for k in range(4, 8):
                            S.op("pe", lambda e, k=k, p=pzz[3]: e.matmul(p[:], lhsT=mixT[:, k, tc0:tc1], rhs=wo[:, k, :], start=(k == 4), stop=(k == 7)),
                                 reads=[T_mixD[i], T_wo], writes=[tzz[3]], signal=(k == 7))
                        sa, tsa = sa_r.next(); sdg, tsd = sd_r.next(); t1, tt1 = t1_r.next(); t2, tt2 = t2_r.next(); xh, txh = xh_r.next()
                        S.dma("sp", "ld_" + txh.name, lambda e, xh=xh, i=i, hf=hf: e.dma_start(out=xh[:], in_=x[i * 128:(i + 1) * 128, hf * 512:(hf + 1) * 512]), writes=[txh])
                        S.op("act", lambda e, sa=sa, p=pzz[0]: e.activation(out=sa[:], in_=p[:], func=AF.Sigmoid), reads=[tzz[0]], writes=[tsa])
                        S.op("act", lambda e, sdg=sdg, p=pzz[1]: e.activation(out=sdg[:], in_=p[:], func=AF.Sigmoid), reads=[tzz[1]], writes=[tsd])
                        S.op("dve", lambda e, t1=t1, sa=sa, p=pzz[2]: e.tensor_tensor(out=t1[:], in0=sa[:], in1=p[:], op=ALU.mult), reads=[tsa, tzz[2]], writes=[tt1])
                        S.op("dve", lambda e, t2=t2, sdg=sdg, p=pzz[3]: e.tensor_tensor(out=t2[:], in0=sdg[:], in1=p[:], op=ALU.mult), reads=[tsd, tzz[3]], writes=[tt2])
                        S.op("pool", lambda e, t1=t1, t2=t2: e.tensor_tensor(out=t1[:], in0=t1[:], in1=t2[:], op=ALU.add), reads=[tt1, tt2], writes=[tt1])
                        S.op("dve", lambda e, t1=t1, hf=hf: e.tensor_tensor(out=t1[:], in0=t1[:], in1=g1bc[:, hf * 512:(hf + 1) * 512], op=ALU.mult), reads=[tt1, T_g1], writes=[tt1])
                        S.op("dve", lambda e, t1=t1, xh=xh, i=i, hf=hf: e.tensor_tensor(out=acc[:, i, hf * 512:(hf + 1) * 512], in0=t1[:], in1=xh[:], op=ALU.add),
                             reads=[tt1, txh], writes=[T_acc[i][hf]])
            S.barrier()
            if "x1" in dbg:
                for i in range(NT):
                    tt = Tok("x1d%d" % i)
                    tt.w = T_acc[i][1].w
                    dump("x1_%d" % i, acc[:, i, :], tt, [128, D], F32)
            if stop_after == "C3":
                S.final_wait("sp", out_toks)
                return nc, dbg_outs

            with ExitStack() as sf:
                NSLOT = 8
                ring2 = None

                def slot_ap(sl):
                    base = bufB[:, sl, :] if sl < 8 else ring2[:, sl - 8, :]
                    return base.rearrange("p (k c) -> p k c", k=8)
                T_slot = [Tok("slot%d" % i) for i in range(NSLOT)]
                w2b = sbuf(sf, "w2b", [128, 8, D], BF16); T_w2b = Tok("w2b")
                stg_r = Ring([sbuf(sf, "stg%d" % i, [128, 1024], F32) for i in range(4)], "stg")
                wrb = sbuf(sf, "wrb", [128, 8, NE], BF16); T_wrb = Tok("wrb")
                b2g = sbuf(sf, "b2g", [NE, D], BF16); T_b2g = Tok("b2g")
                gates = sbuf(sf, "gates", [128, 8, NE], F32); T_gates = [Tok("gates%d" % i) for i in range(8)]
                h2T = bufA
                T_h2T = [Tok("h2T0"), Tok("h2T1")]
                T_actT = [[Tok("actT%d_%d" % (j, tg)) for tg in range(2)] for j in range(8)]
                with ExitStack() as sw:
                    wrs = sbuf(sw, "wrs", [128, 8, NE], F32)
                    S.dma("sp", "ld_wrs", lambda e: e.dma_start(out=wrs[:], in_=w_r.rearrange("(k p) n -> p k n", p=128)), writes=[T_wrb])
                    S.op("dve", lambda e: e.tensor_copy(out=wrb[:], in_=wrs[:]), reads=[T_wrb], writes=[T_wrb])
                    b2f = sbuf(sw, "b2f", [NE, D], F32)
                    S.dma("sp", "ld_b2g", lambda e: e.dma_start(out=b2f[:], in_=b2[:, :]), writes=[T_b2g])
                    S.op("dve", lambda e: e.tensor_tensor(out=b2g[:], in0=b2f[:], in1=g2bc[0:NE, :], op=ALU.mult), reads=[T_b2g, T_g2], writes=[T_b2g])
                S.barrier()
                slot_ctr = [0]
                for th in range(2):
                    tiles = list(range(th * 8, th * 8 + 8))
                    with ExitStack() as sn:
                        srcs = [(acc[:, i, :], T_acc[i]) for i in tiles]
                        for _ in norm_transpose(sn, "D%d" % th, srcs, h2T, a2, 3, T_a2, T_h2T, False, nxn=4):
                            pass
                    S.barrier()
                    with ExitStack() as se:
                        pgb = [psum(se, "pgb%d_%d" % (th, a), [128, 512], F32) for a in range(2)]; T_pgb = [Tok("pgb0"), Tok("pgb1")]
                        plb = [psum(se, "plb%d_%d" % (th, a), [128, 512], F32) for a in range(2)]; T_plb = [Tok("plb0"), Tok("plb1")]
                        pyb = [psum(se, "pyb%d_%d" % (th, a), [128, 512], F32) for a in range(2)]; T_pyb = [Tok("pyb0"), Tok("pyb1")]
                        gm_r = Ring([sbuf(se, "gm%d_%d" % (th, a), [128, 512], F32) for a in range(2)], "gm")
                        sg_r = Ring([sbuf(se, "sg%d_%d" % (th, a), [128, 512], F32) for a in range(1)], "sg")
                        lm_r = Ring([sbuf(se, "lm%d_%d" % (th, a), [128, 512], F32) for a in range(1)], "lm")
                        tt_r = Ring([sbuf(se, "tt%d_%d" % (th, a), [128, 512], F32) for a in range(1)], "tt")
                        h2m = sbuf(se, "h2m%d" % th, [128, 8, 1024], BF16)
                        T_h2m = [Tok("h2m0"), Tok("h2m1")]
                        pmk = [psum(se, "pmk%d_%d" % (th, a), [128, 512], F32) for a in range(2)]
                        T_pmk = [Tok("pmk0"), Tok("pmk1")]
                        gTall = g1bc[0:NE, :]
                        T_gTall = Tok("gTall")

                        maskbf = sbuf(se, "maskbf%d" % th, [128, 1024], BF16)
                        T_maskbf = [Tok("maskbf0"), Tok("maskbf1")]

                        def mask_h2(e_):
                            for tg in range(2):
                                S.op("pe", lambda e: e.matmul(pmk[tg][:], lhsT=identf[0:NE, e_:e_ + 1].to_broadcast([NE, 128]), rhs=gTall[:, tg * 512:(tg + 1) * 512],
                                                              start=True, stop=True), reads=[T_gTall, T_c], writes=[T_pmk[tg]])
                            for tg in range(2):
                                S.op("act", lambda e: e.copy(out=maskbf[:, tg * 512:(tg + 1) * 512], in_=pmk[tg][:]), reads=[T_pmk[tg]], writes=[T_maskbf[tg]])
                            for tg in range(2):
                                for k in range(8):
                                    S.op("pool" if (tg == 0 or k >= 4) else "dve", lambda e: e.tensor_tensor(
                                        out=h2m[:, k, tg * 512:(tg + 1) * 512], in0=h2T[:, k, tg * 512:(tg + 1) * 512],
                                        in1=maskbf[:, tg * 512:(tg + 1) * 512], op=ALU.mult),
                                         reads=[T_h2T[tg], T_maskbf[tg]], writes=[T_h2m[tg]])
                        n1 = 0
                        n2 = 0
                        if th == 0:
                            print("sbuf bytes remaining in FFN expert scope:", nc.sbuf_bytes_remaining)
                        ne_run = NE if stop_after == "all" else int(stop_after[1:]) if stop_after.startswith("E") else NE
                        pend = []

                        def flush_casts():
                            while pend:
                                pend.pop(0)()

                        def load_piece(qidx):
                            e_, j_ = divmod(qidx, 8)
                            if e_ >= ne_run:
                                return
                            w1v = w1p[e_].rearrange("(k p) n -> p k n", p=128)
                            sl = qidx % NSLOT
                            sap = slot_ap(sl)
                            for part in range(2):
                                stg, tst = stg_r.next()
                                cc0 = part * D + j_ * 128
                                S.dma("sp", "ld_" + tst.name, lambda e, stg=stg, cc0=cc0: e.dma_start(
                                    out=stg[:].rearrange("p (k c) -> p k c", k=8), in_=w1v[:, :, cc0:cc0 + 128]), writes=[tst])
                                pend.append(lambda stg=stg, tst=tst, sap=sap, part=part, sl=sl: S.op("act", lambda e: e.copy(
                                    out=sap[:, :, part * 128:(part + 1) * 128], in_=stg[:].rearrange("p (k c) -> p k c", k=8)),
                                    reads=[tst], writes=[T_slot[sl]]))

                        def load_w2chunk(e_, k):
                            if e_ >= ne_run:
                                return
                            stg, tst = stg_r.next()
                            S.dma("sp", "ld_" + tst.name, lambda e, stg=stg, k=k: e.dma_start(out=stg[:], in_=w2[e_, k * 128:(k + 1) * 128, :]), writes=[tst])
                            pend.append(lambda stg=stg, tst=tst, k=k: S.op("dve" if k % 4 != 3 else "pool", lambda e: e.tensor_tensor(
                                out=w2b[:, k, :], in0=stg[:], in1=g2bc[:], op=ALU.mult), reads=[tst, T_g2], writes=[T_w2b]))
                        for qidx in range(NSLOT):
                            load_piece(qidx)
                            flush_casts()
                        rtmp = []
                        for ch in range(2):
                            rtmp.append(dict(
                                lg=sbuf(se, "lg%d_%d" % (th, ch), [128, NE], F32), T_lg=Tok("lg"),
                                top8=sbuf(se, "top8_%d_%d" % (th, ch), [128, 8], F32), T_top8=Tok("top8"),
                                msk=sbuf(se, "msk%d_%d" % (th, ch), [128, NE], F32), T_msk=Tok("msk"),
                                ex=sbuf(se, "ex%d_%d" % (th, ch), [128, NE], F32), T_ex=Tok("ex"),
                                sm=sbuf(se, "sm%d_%d" % (th, ch), [128, 4], F32), T_sm=Tok("sm"),
                                gTb=sbuf(se, "gTb%d_%d" % (th, ch), [NE, 128], BF16), T_gT=Tok("gT")))

                        def router_tile(il, ch):
                            i = tiles[il]
                            c0, c1 = il * 128, (il + 1) * 128
                            R = rtmp[ch]
                            lg, top8, msk, ex, sm = R["lg"], R["top8"], R["msk"], R["ex"], R["sm"]
                            gT = gTall[:, c0:c1]
                            T_lg, T_top8, T_msk, T_ex, T_sm, T_gT = R["T_lg"], R["T_top8"], R["T_msk"], R["T_ex"], R["T_sm"], R["T_gT"]
                            plog, T_plog = pgb[ch], T_pgb[ch]
                            pgT, T_pgT = plb[ch], T_plb[ch]
                            pbk, T_pbk = pyb[ch], T_pyb[ch]
                            for k in range(8):
                                S.op("pe", lambda e, k=k: e.matmul(plog[:, 0:NE], lhsT=h2T[:, k, c0:c1], rhs=wrb[:, k, :], start=(k == 0), stop=(k == 7)),
                                     reads=[T_h2T[il // 4], T_wrb], writes=[T_plog], signal=(k == 7))
                            yield
                            S.op("dve", lambda e: e.tensor_tensor(out=lg[:], in0=plog[:, 0:NE], in1=brbc[:], op=ALU.add), reads=[T_plog, T_in0], writes=[T_lg]); yield
                            S.op("dve", lambda e: e.max(out=top8[:], in_=lg[:]), reads=[T_lg], writes=[T_top8]); yield
                            S.op("dve", lambda e: e.tensor_scalar(out=msk[:], in0=lg[:], scalar1=top8[:, 3:4], scalar2=None, op0=ALU.is_ge), reads=[T_lg, T_top8], writes=[T_msk]); yield
                            S.op("dve", lambda e: e.tensor_scalar(out=sm[:, 0:1], in0=top8[:, 0:1], scalar1=-1.0, scalar2=None, op0=ALU.mult), reads=[T_top8], writes=[T_sm]); yield
                            S.op("act", lambda e: e.activation(out=ex[:], in_=lg[:], func=AF.Exp, bias=sm[:, 0:1]), reads=[T_lg, T_sm], writes=[T_ex]); yield
                            S.op("dve", lambda e: e.tensor_tensor(out=ex[:], in0=ex[:], in1=msk[:], op=ALU.mult), reads=[T_ex, T_msk], writes=[T_ex]); yield
                            S.op("dve", lambda e: e.reduce_sum(out=sm[:, 1:2], in_=ex[:], axis=AX.X), reads=[T_ex], writes=[T_sm]); yield
                            S.op("dve", lambda e: e.reciprocal(out=sm[:, 2:3], in_=sm[:, 1:2]), reads=[T_sm], writes=[T_sm]); yield
                            S.op("dve", lambda e: e.tensor_scalar(out=gates[:, il, :], in0=ex[:], scalar1=sm[:, 2:3], scalar2=None, op0=ALU.mult),
                                 reads=[T_ex, T_sm], writes=[T_gates[il]]); yield
                            S.op("pe", lambda e: e.matmul(pgT[0:NE, 0:128], lhsT=gates[:, il, :], rhs=identf[:], start=True, stop=True),
                                 reads=[T_gates[il], T_c], writes=[T_pgT]); yield
                            gTb = R["gTb"]
                            S.op("act", lambda e: e.copy(out=gT, in_=pgT[0:NE, 0:128]), reads=[T_pgT], writes=[T_gT, T_gTall]); yield
                            S.op("act", lambda e: e.copy(out=gTb[:], in_=pgT[0:NE, 0:128]), reads=[T_pgT], writes=[T_gT]); yield
                            for nh in range(2):
                                S.op("pe", lambda e: e.matmul(pbk[:], lhsT=gTb[:], rhs=b2g[:, nh * 512:(nh + 1) * 512], start=True, stop=True),
                                     reads=[T_gT, T_b2g], writes=[T_pbk]); yield
                                S.op("dve", lambda e: e.tensor_tensor(out=acc[:, i, nh * 512:(nh + 1) * 512], in0=acc[:, i, nh * 512:(nh + 1) * 512], in1=pbk[:], op=ALU.add),
                                     reads=[T_pbk, T_acc[i][nh]], writes=[T_acc[i][nh]]); yield
                        for il2 in range(4):
                            interleave(router_tile(2 * il2, 0), router_tile(2 * il2 + 1, 1))
                        S.op("dve", lambda e: e.tensor_scalar(out=gTall, in0=gTall, scalar1=0.0, scalar2=None, op0=ALU.is_gt),
                             reads=[rtmp[0]["T_gT"], rtmp[1]["T_gT"], T_gTall], writes=[T_gTall])
                        mask_h2(0)
                        if "gates" in dbg and th == 0:
                            tt_ = Tok("gd"); tt_.w = T_gates[7].w
                            dump("gates", gates[:].rearrange("p a b -> p (a b)"), tt_, [128, 8 * NE], F32)
                        for ex_i in range(ne_run):
                            slots = [(ex_i * 8 + j) % NSLOT for j in range(8)]
                            for j in range(8):
                                sl = slots[j]
                                sap = slot_ap(sl)
                                for tg in range(2):
                                    a = n1 % 2
                                    n1 += 1
                                    for k in range(8):
                                        S.op("pe", lambda e, k=k, a=a, sap=sap, tg=tg: e.matmul(pgb[a][:], lhsT=sap[:, k, 0:128], rhs=h2m[:, k, tg * 512:(tg + 1) * 512],
                                                                                          start=(k == 0), stop=(k == 7)),
                                             reads=[T_slot[sl], T_h2m[tg]], writes=[T_pgb[a]], signal=(k == 7))
                                    for k in range(8):
                                        S.op("pe", lambda e, k=k, a=a, sap=sap, tg=tg: e.matmul(plb[a][:], lhsT=sap[:, k, 128:256], rhs=h2m[:, k, tg * 512:(tg + 1) * 512],
                                                                                          start=(k == 0), stop=(k == 7)),
                                             reads=[T_slot[sl], T_h2m[tg]], writes=[T_plb[a]], signal=(k == 7))
                                    gm, tgm = gm_r.next(); sg, tsg = sg_r.next(); lm, tlm = lm_r.next(); tt, ttt = tt_r.next()
                                    bg = b1s[:, ex_i * 16 + j:ex_i * 16 + j + 1]
                                    bl = b1s[:, ex_i * 16 + 8 + j:ex_i * 16 + 8 + j + 1]
                                    S.op("dve", lambda e, gm=gm, a=a, bg=bg: e.tensor_scalar(out=gm[:], in0=pgb[a][:], scalar1=bg, scalar2=7.0, op0=ALU.add, op1=ALU.min),
                                         reads=[T_pgb[a], T_misc], writes=[tgm])
                                    S.op("act", lambda e, gm=gm, sg=sg: e.activation(out=sg[:], in_=gm[:], func=AF.Sigmoid, scale=1.702), reads=[tgm], writes=[tsg])
                                    S.op("dve", lambda e, lm=lm, a=a, bl=bl: e.tensor_scalar(out=lm[:], in0=plb[a][:], scalar1=bl, scalar2=8.0, op0=ALU.add, op1=ALU.min),
                                         reads=[T_plb[a], T_misc], writes=[tlm])
                                    S.op("pool", lambda e, gm=gm, sg=sg, tt=tt: e.tensor_tensor(out=tt[:], in0=gm[:], in1=sg[:], op=ALU.mult), reads=[tgm, tsg], writes=[ttt])
                                    S.op("dve", lambda e, lm=lm, tt=tt, j=j, tg=tg: e.scalar_tensor_tensor(
                                        out=bufA[:, j, 1024 + tg * 512:1024 + (tg + 1) * 512], in0=lm[:], scalar=-6.0, in1=tt[:], op0=ALU.max, op1=ALU.mult),
                                        reads=[tlm, ttt], writes=[T_actT[j][tg]])
                                flush_casts()
                                if j < 4:
                                    load_w2chunk(ex_i, 2 * j)
                                    load_w2chunk(ex_i, 2 * j + 1)
                                load_piece(ex_i * 8 + j + NSLOT)
                            flush_casts()
                            if ex_i + 1 < ne_run:
                                mask_h2(ex_i + 1)
                            for il, i in enumerate(tiles):
                                for nh in range(2):
                                    a = n2 % 2
                                    n2 += 1
                                    for k in range(8):
                                        S.op("pe", lambda e, k=k, a=a, il=il, nh=nh: e.matmul(
                                            pyb[a][:], lhsT=bufA[:, k, 1024 + il * 128:1024 + (il + 1) * 128], rhs=w2b[:, k, nh * 512:(nh + 1) * 512],
                                            start=(k == 0), stop=(k == 7)),
                                            reads=[T_actT[k][il // 4], T_w2b], writes=[T_pyb[a]], signal=(k == 7))
                                    S.op("dve", lambda e, a=a, il=il, i=i, nh=nh: e.scalar_tensor_tensor(
                                        out=acc[:, i, nh * 512:(nh + 1) * 512], in0=pyb[a][:], scalar=gates[:, il, ex_i:ex_i + 1], in1=acc[:, i, nh * 512:(nh + 1) * 512],
                                        op0=ALU.mult, op1=ALU.add),
                                        reads=[T_pyb[a], T_gates[il], T_acc[i][nh]], writes=[T_acc[i][nh]])
                        for i in tiles:
                            to = Tok("out%d" % i)
                            S.dma("sp", "st_out", lambda e, i=i: e.dma_start(out=out[i * 128:(i + 1) * 128, :], in_=acc[:, i, :]), reads=[T_acc[i][0], T_acc[i][1]], writes=[to])
                            out_toks.append(to)
                    S.barrier()

        S.final_wait("sp", out_toks)
    return nc, dbg_outs


def _consts():
    s = np.arange(128)
    mid = 63
    amat = np.zeros((128, 130), np.float32)
    amat[:, :128] = (s[:, None] <= s[None, :]).astype(np.float32) - (s[:, None] <= mid).astype(np.float32)
    amat[:, 128] = (s <= mid).astype(np.float32)
    amat[:, 129] = 1.0
    tri = (s[None, :] >= s[:, None]).astype(np.float32)
    bones = np.zeros((128, 128), np.float32)
    bones[:64, :64] = 1.0
    bones[64:, 64:] = 1.0
    return {
        "identb": np.eye(128, dtype=np.float32).astype(NPBF),
        "identf": np.eye(128, dtype=np.float32),
        "amat": amat,
        "tri": tri,
        "bones": bones.astype(NPBF),
    }


def _prep_shared(inp):
    f = lambda a: np.ascontiguousarray(np.asarray(a, dtype=np.float32))
    w1 = f(inp["w1"])[0]
    w1p = np.ascontiguousarray(np.concatenate([w1[:, :, 0::2], w1[:, :, 1::2]], axis=2))
    b1 = f(inp["b1"])[0]
    b1p = np.concatenate([b1[:, 0::2], b1[:, 1::2]], axis=1)
    b1F = np.ascontiguousarray(b1p.reshape(NE, 16, 128).transpose(2, 0, 1).reshape(128, NE * 16))
    sh = {
        "w_ada": f(inp["w_ada"])[0],
        "badaF": np.ascontiguousarray(f(inp["b_ada"])[0].reshape(48, 128).T),
        "bada_row": f(inp["b_ada"])[0].reshape(1, -1),
        "gmixF": np.ascontiguousarray(f(inp["mix_norm_g"])[0].reshape(8, 128).T),
        "gffnF": np.ascontiguousarray(f(inp["ffn_norm_g"])[0].reshape(8, 128).T),
        "w_in": f(inp["w_in"])[0],
        "lbl": f(inp["hg_lower_bound_logits"]).reshape(1, 1024),
        "hgn": f(inp["hg_out_norm_g"]).reshape(1, 128),
        "gq": np.tile(f(inp["da_q_norm_g"]).reshape(64), 2).reshape(128, 1),
        "gk": np.tile(f(inp["da_k_norm_g"]).reshape(64), 2).reshape(128, 1),
        "lam4": np.concatenate([f(inp["da_lambda_q1"]).reshape(64), f(inp["da_lambda_k1"]).reshape(64),
                                f(inp["da_lambda_q2"]).reshape(64), f(inp["da_lambda_k2"]).reshape(64)]).reshape(1, 256),
        "subg": f(inp["da_subln_g"]).reshape(1, 128),
        "w_out": f(inp["w_out"])[0],
        "w_r": f(inp["w_router"])[0],
        "b_r": f(inp["b_router"]).reshape(1, NE),
        "w1p": w1p,
        "b1F": b1F,
        "w2": f(inp["w2"])[0],
        "b2": f(inp["b2"])[0],
    }
    sh.update(_consts())
    return sh


def _prep_core(inp, sh, b):
    m = dict(sh)
    m["x"] = np.ascontiguousarray(np.asarray(inp["x"], dtype=np.float32)[b])
    m["cT"] = np.ascontiguousarray(np.asarray(inp["c"], dtype=np.float32)[b].reshape(8, 128).T)
    return m


def kernel(**inputs):
    nc, _ = build_nc()
    sh = _prep_shared(inputs)
    in_maps = [_prep_core(inputs, sh, b) for b in range(8)]
    res = run_bass_kernel_spmd(nc, in_maps, core_ids=list(range(8)))
    return np.stack([np.asarray(r["out"], dtype=np.float32) for r in res.results], axis=0)
```

```python
import numpy as np
import ml_dtypes
from contextlib import ExitStack
import concourse.bass as bass
import concourse.mybir as mybir
from concourse.bass_utils import run_bass_kernel_spmd

F32 = mybir.dt.float32
BF16 = mybir.dt.bfloat16
AF = mybir.ActivationFunctionType
ALU = mybir.AluOpType
AX = mybir.AxisListType
NPBF = ml_dtypes.bfloat16

S_TOK = 2048
D = 1024
NT = 16
NE = 32
EPS = 1e-6
LAMBDA_INIT = 0.2


class Tok:
    __slots__ = ("name", "w", "r")

    def __init__(self, name=""):
        self.name = name
        self.w = None
        self.r = []


class Sched:
    def __init__(self, nc, stack, same_engine_sync=True):
        self.nc = nc
        self.stack = stack
        self.eng = {"pe": nc.tensor, "act": nc.scalar, "dve": nc.vector, "pool": nc.gpsimd, "sp": nc.sync}
        self.sems = {}
        self.cnt = {}
        self.waited = {e: {} for e in self.eng}
        self.same = same_engine_sync
        self.n_inst = 0
        self.n_wait = 0

    def sem(self, key):
        if key not in self.sems:
            self.sems[key] = self.stack.enter_context(self.nc.semaphore("s_%s" % key))
            self.cnt[key] = 0
        return self.sems[key]

    def _deps(self, reads, writes):
        deps = {}

        def add(t):
            if t is None:
                return
            k, v = t
            if deps.get(k, 0) < v:
                deps[k] = v
        for t in reads:
            add(t.w)
        for t in writes:
            add(t.w)
            for r in t.r:
                add(r)
        return deps

    def _emit_waits(self, e, deps):
        eng = self.eng[e]
        for k, v in deps.items():
            if k == e and (e == "pe" or not self.same):
                continue
            if self.waited[e].get(k, 0) >= v:
                continue
            eng.wait_ge(self.sems[k], v)
            self.waited[e][k] = v
            self.n_wait += 1

    def op(self, e, fn, reads=(), writes=(), signal=True):
        self.sem(e)
        deps = self._deps(reads, writes)
        self._emit_waits(e, deps)
        ins = fn(self.eng[e])
        self.n_inst += 1
        if signal:
            self.cnt[e] += 1
            ins.then_inc(self.sems[e], 1)
            tk = (e, self.cnt[e])
        else:
            tk = (e, self.cnt[e] + 1)
        for t in writes:
            t.w = tk
            t.r = []
        for t in reads:
            if len(t.r) > 64:
                best = {}
                for k, v in t.r:
                    if best.get(k, 0) < v:
                        best[k] = v
                t.r = list(best.items())
            t.r.append(tk)
        return tk

    def dma(self, q, semkey, fn, reads=(), writes=()):
        self.sem(semkey)
        deps = self._deps(reads, writes)
        self._emit_waits(q, deps) if False else None
        eng = self.eng[q]
        for k, v in deps.items():
            if self.waited[q].get(k, 0) >= v:
                continue
            eng.wait_ge(self.sems[k], v)
            self.waited[q][k] = v
            self.n_wait += 1
        ins = fn(eng)
        self.n_inst += 1
        self.cnt[semkey] += 16
        ins.then_inc(self.sems[semkey], 16)
        tk = (semkey, self.cnt[semkey])
        for t in writes:
            t.w = tk
            t.r = []
        for t in reads:
            t.r.append(tk)
        return tk

    def barrier(self):
        for e in self.eng:
            for k, v in self.cnt.items():
                if v == 0 or k == e and e == "pe":
                    continue
                if self.waited[e].get(k, 0) >= v:
                    continue
                self.eng[e].wait_ge(self.sems[k], v)
                self.waited[e][k] = v
                self.n_wait += 1

    def final_wait(self, q, toks):
        deps = {}
        for t in toks:
            if t.w is not None:
                k, v = t.w
                deps[k] = max(deps.get(k, 0), v)
        for k, v in deps.items():
            self.eng[q].wait_ge(self.sems[k], v)


class Ring:
    def __init__(self, bufs, name):
        self.bufs = bufs
        self.toks = [Tok("%s%d" % (name, i)) for i in range(len(bufs))]
        self.i = 0

    def next(self):
        b, t = self.bufs[self.i], self.toks[self.i]
        self.i = (self.i + 1) % len(self.bufs)
        return b, t


def interleave(*gens):
    gens = [g for g in gens if g is not None]
    while gens:
        for g in list(gens):
            try:
                next(g)
            except StopIteration:
                gens.remove(g)


def build_nc(stop_after="all", dbg=()):
    nc = bass.Bass("TRN2", target_bir_lowering=False)

    def din(name, shape, dt=F32):
        return nc.dram_tensor(name, list(shape), dt, kind="ExternalInput").ap()

    x = din("x", [S_TOK, D])
    cT = din("cT", [128, 8])
    w_ada = din("w_ada", [D, 6 * D])
    badaF = din("badaF", [128, 48])
    bada_row = din("bada_row", [1, 6 * D])
    gmixF = din("gmixF", [128, 8])
    gffnF = din("gffnF", [128, 8])
    w_in = din("w_in", [D, 5632])
    lbl = din("lbl", [1, 1024])
    hgn = din("hgn", [1, 128])
    gq = din("gq", [128, 1])
    gk = din("gk", [128, 1])
    lam4 = din("lam4", [1, 256])
    subg = din("subg", [1, 128])
    w_out = din("w_out", [D, D])
    w_r = din("w_r", [D, NE])
    b_r = din("b_r", [1, NE])
    w1p = din("w1p", [NE, D, 2 * D])
    b1F = din("b1F", [128, NE * 16])
    w2 = din("w2", [NE, D, D])
    b2 = din("b2", [NE, D])
    identb_d = din("identb", [128, 128], BF16)
    identf_d = din("identf", [128, 128])
    amat_d = din("amat", [128, 130])
    tri_d = din("tri", [128, 128])
    bones_d = din("bones", [128, 128], BF16)
    out = nc.dram_tensor("out", [S_TOK, D], F32, kind="ExternalOutput").ap()
    dbg_outs = {}

    with ExitStack() as st:
        S = Sched(nc, st)
        out_toks = []

        def sbuf(stack, name, shape, dt):
            return stack.enter_context(nc.sbuf_tensor("sb_" + name, list(shape), dt))

        def psum(stack, name, shape, dt):
            return stack.enter_context(nc.psum_tensor("ps_" + name, list(shape), dt))

        ld_i = [0]

        def load(dst, src, tok, q="sp"):
            ld_i[0] += 1
            return S.dma(q, "ld_%s" % tok.name, lambda e: e.dma_start(out=dst, in_=src), writes=[tok])

        def dump(name, ap, tok, shape, dt):
            d = nc.dram_tensor("dbg_" + name, list(shape), dt, kind="ExternalOutput").ap()
            t = Tok("dbgo_" + name)
            S.dma("sp", "dbg_" + name, lambda e: e.dma_start(out=d, in_=ap), reads=[tok], writes=[t])
            out_toks.append(t)
            dbg_outs[name] = d

        identb = sbuf(st, "identb", [128, 128], BF16)
        identf = sbuf(st, "identf", [128, 128], F32)
        amat = sbuf(st, "amat", [128, 130], F32)
        tri = sbuf(st, "tri", [128, 128], F32)
        bones = sbuf(st, "bones", [128, 128], BF16)
        T_c = Tok("consts")
        for (d_, s_, nm) in [(identb, identb_d, "c0"), (identf, identf_d, "c1"), (amat, amat_d, "c2"), (tri, tri_d, "c3"),
                             (bones, bones_d, "c4")]:
            S.dma("sp", "ld_c", lambda e, d_=d_, s_=s_: e.dma_start(out=d_[:], in_=s_[:, :]), writes=[])
        T_c.w = ("ld_c", S.cnt["ld_c"])

        adaF = sbuf(st, "adaF", [128, 6, 8], F32)
        a1 = sbuf(st, "a1", [128, 8], F32)
        a2 = sbuf(st, "a2", [128, 8], F32)
        g1bc = sbuf(st, "g1bc", [128, D], F32)
        g2bc = sbuf(st, "g2bc", [128, D], F32)
        gnbc = sbuf(st, "gnbc", [128, 128], F32)
        sg8 = sbuf(st, "sg8", [128, 128], F32)
        gqs = sbuf(st, "gqs", [128, 1], F32)
        gks = sbuf(st, "gks", [128, 1], F32)
        nlam = sbuf(st, "nlam", [128, 1], F32)
        brbc = sbuf(st, "brbc", [128, NE], F32)
        b1s = sbuf(st, "b1s", [128, NE * 16], F32)
        bufA = sbuf(st, "bufA", [128, 8, S_TOK], BF16)
        bufB = sbuf(st, "bufB", [128, 8, S_TOK], BF16)
        T_ada = Tok("ada")
        T_a1 = Tok("a1")
        T_a2 = Tok("a2")
        T_g1 = Tok("g1")
        T_g2 = Tok("g2")
        T_misc = Tok("misc")
        T_in0 = Tok("in0")
        T_hT = [Tok("hT%d" % g) for g in range(4)]
        T_mixA = [Tok("mixA%d" % i) for i in range(NT)]
        T_mixD = [Tok("mixD%d" % i) for i in range(NT)]
        hT = bufA
        mixT = bufB

        def norm_transpose(stack, tag, tile_srcs, dstT, a_sc, sh_idx, T_a, T_dst_groups, src_is_dram, nxn=5, split=False, q="sp"):
            xt_r = Ring([sbuf(stack, "%s_xt%d" % (tag, i), [128, D], F32) for i in range(2)], tag + "xt") if src_is_dram else None
            xn_r = Ring([sbuf(stack, "%s_xn%d" % (tag, i), [128, D], BF16) for i in range(nxn)], tag + "xn")
            ssq = sbuf(stack, tag + "_ssq", [128, 64], F32)
            pT = [psum(stack, "%s_pT%d" % (tag, i), [128, 512], BF16) for i in range(2)]
            T_pT = [Tok(tag + "pT0"), Tok(tag + "pT1")]
            T_ssq = Tok(tag + "ssq")
            npt = [0]
            ntile = len(tile_srcs)

            def pre(i):
                src, tsrc = tile_srcs[i]
                if src_is_dram:
                    xt, txt = xt_r.next()
                    S.dma(q, "ld_" + txt.name, lambda e: e.dma_start(out=xt[:], in_=src), writes=[txt])
                    xin, tin = xt[:], [txt]
                else:
                    xin, tin = src, list(tsrc)
                xn, txn = xn_r.next()
                S.op("act", lambda e: e.activation(out=xn[:], in_=xin, func=AF.Square, accum_out=ssq[:, i:i + 1]), reads=tin, writes=[txn, T_ssq])
                S.op("act", lambda e: e.activation(out=ssq[:, 32 + i:33 + i], in_=ssq[:, i:i + 1], func=AF.Ln, scale=1.0 / D, bias=epsb[:, 0:1]),
                     reads=[T_ssq, T_eps], writes=[T_ssq])
                S.op("act", lambda e: e.activation(out=ssq[:, 32 + i:33 + i], in_=ssq[:, 32 + i:33 + i], func=AF.Exp, scale=-0.5), reads=[T_ssq], writes=[T_ssq])
                S.op("dve", lambda e: e.tensor_scalar(out=xn[:], in0=xin, scalar1=ssq[:, 32 + i:33 + i], scalar2=None, op0=ALU.mult),
                     reads=tin + [T_ssq], writes=[txn])
                return (xn, txn)

            def post(g, xns):
                for k in range(8):
                    p, tp = pT[npt[0] % 2], T_pT[npt[0] % 2]
                    npt[0] += 1
                    for ii in range(4):
                        xn, txn = xns[ii]
                        S.op("pe", lambda e: e.transpose(p[:, ii * 128:(ii + 1) * 128], xn[:, k * 128:(k + 1) * 128], identb[:]),
                             reads=[txn, T_c], writes=[tp], signal=(ii == 3))
                    S.op("act", lambda e: e.activation(out=dstT[:, k, g * 512:(g + 1) * 512], in_=p[:], func=AF.Identity,
                                                       scale=a_sc[:, k:k + 1], bias=adaF[:, sh_idx, k:k + 1]),
                         reads=[tp, T_a, T_ada], writes=[T_dst_groups[g]])
            if split:
                allx = [pre(i) for i in range(ntile)]
                yield
                for g in range(ntile // 4):
                    post(g, allx[g * 4:g * 4 + 4])
            else:
                for g in range(ntile // 4):
                    xns = [pre(g * 4 + ii) for ii in range(4)]
                    post(g, xns)
                    yield

        epsb = sbuf(st, "epsb", [128, 4], F32)
        T_eps = Tok("eps")
        S.op("dve", lambda e: e.memset(epsb[:, 0:1], EPS), writes=[T_eps])
        S.op("dve", lambda e: e.memset(epsb[:, 1:2], 64 * EPS), writes=[T_eps])
        S.op("dve", lambda e: e.memset(epsb[:, 2:3], 1.0), writes=[T_eps])

        with ExitStack() as sa:
            cTs = sbuf(sa, "cTs", [128, 8], F32)
            cact = sbuf(sa, "cact", [128, 8], F32)
            cbc = sbuf(sa, "cbc", [128, 8, 128], F32)
            wa = [sbuf(sa, "wa%d" % i, [128, 8, 1024], F32) for i in range(2)]
            T_wa = [Tok("wa0"), Tok("wa1")]
            badaFs = sbuf(sa, "badaFs", [128, 48], F32)
            bgrow = sbuf(sa, "bgrow", [128, 2, 1024], F32)
            gmixs = sbuf(sa, "gmixs", [128, 8], F32)
            gffns = sbuf(sa, "gffns", [128, 8], F32)
            lam4bc = sbuf(sa, "lam4bc", [128, 256], F32)
            lamtmp = sbuf(sa, "lamtmp", [128, 128], F32)
            lams = sbuf(sa, "lams", [128, 2], F32)
            subgbc = sbuf(sa, "subgbc", [128, 128], F32)
            pA = psum(sa, "pA", [128, 512], F32)
            pG = psum(sa, "pG", [128, 512], F32)
            T_pA, T_pG = Tok("pA"), Tok("pG")
            T_in = Tok("smallin")
            smalls = [(cTs[:], cT[:, :]), (badaFs[:], badaF[:, :]), (gmixs[:], gmixF[:, :]), (gffns[:], gffnF[:, :]),
                      (gqs[:], gq[:, :]), (gks[:], gk[:, :]), (b1s[:], b1F[:, :]),
                      (lam4bc[:], lam4[0:1, :].partition_broadcast(128)),
                      (subgbc[:], subg[0:1, :].partition_broadcast(128)),
                      (gnbc[:], hgn[0:1, :].partition_broadcast(128)),
                      (brbc[:], b_r[0:1, :].partition_broadcast(128)),
                      (bgrow[:, 0, :], bada_row[0:1, 2 * D:3 * D].partition_broadcast(128)),
                      (bgrow[:, 1, :], bada_row[0:1, 5 * D:6 * D].partition_broadcast(128))]
            for d_, s_ in smalls:
                S.dma("sp", "ld_s", lambda e, d_=d_, s_=s_: e.dma_start(out=d_, in_=s_), writes=[])
            T_in.w = ("ld_s", S.cnt["ld_s"])
            T_in0.w = T_in.w

            T_cact = Tok("cact")
            S.op("act", lambda e: e.activation(out=cact[:], in_=cTs[:], func=AF.Silu), reads=[T_in], writes=[T_cact])
            T_cbc = Tok("cbc")
            S.op("dve", lambda e: e.tensor_copy(out=cbc[:], in_=cact[:].unsqueeze(2).to_broadcast([128, 8, 128])),
                 reads=[T_cact], writes=[T_cbc])
            S.op("dve", lambda e: e.tensor_scalar(out=sg8[:], in0=subgbc[:], scalar1=1.0 - LAMBDA_INIT, scalar2=None, op0=ALU.mult),
                 reads=[T_in], writes=[T_misc])
            b1v = b1s[:].rearrange("p (e j) -> p e j", j=16)
            S.op("dve", lambda e: e.tensor_scalar(out=b1v[:, :, 8:16], in0=b1v[:, :, 8:16], scalar1=1.0, scalar2=None, op0=ALU.add),
                 reads=[T_in], writes=[T_misc])
            S.op("dve", lambda e: e.tensor_tensor(out=lamtmp[:, 0:64], in0=lam4bc[:, 0:64], in1=lam4bc[:, 64:128], op=ALU.mult),
                 reads=[T_in], writes=[T_misc])
            S.op("dve", lambda e: e.tensor_tensor(out=lamtmp[:, 64:128], in0=lam4bc[:, 128:192], in1=lam4bc[:, 192:256], op=ALU.mult),
                 reads=[T_in], writes=[T_misc])
            S.op("dve", lambda e: e.reduce_sum(out=lams[:], in_=lamtmp[:].rearrange("p (a b) -> p a b", a=2), axis=AX.X),
                 reads=[T_misc], writes=[T_misc])
            S.op("act", lambda e: e.activation(out=lams[:], in_=lams[:], func=AF.Exp), reads=[T_misc], writes=[T_misc])
            S.op("dve", lambda e: e.tensor_tensor(out=nlam[:], in0=lams[:, 1:2], in1=lams[:, 0:1], op=ALU.subtract),
                 reads=[T_misc], writes=[T_misc])
            S.op("dve", lambda e: e.tensor_scalar(out=nlam[:], in0=nlam[:], scalar1=-LAMBDA_INIT, scalar2=None, op0=ALU.add),
                 reads=[T_misc], writes=[T_misc])

            srcsB = [(x[i * 128:(i + 1) * 128, :], None) for i in range(NT)]
            genB = norm_transpose(sa, "B", srcsB, hT, a1, 0, T_a1, T_hT, True, nxn=NT, split=True, q="act")
            next(genB)
            wav = w_ada.rearrange("(k p) n -> p k n", p=128)
            for j in range(6):
                wb, tw = wa[j % 2], T_wa[j % 2]
                S.dma("sp", "ld_wa%d" % (j % 2), lambda e, wb=wb, j=j: e.dma_start(out=wb[:, 0:4, :], in_=wav[:, 0:4, j * D:(j + 1) * D]),
                      writes=[tw])
                S.dma("sp", "ld_wa%d" % (j % 2), lambda e, wb=wb, j=j: e.dma_start(out=wb[:, 4:8, :], in_=wav[:, 4:8, j * D:(j + 1) * D]),
                      writes=[])
                tw.w = ("ld_wa%d" % (j % 2), S.cnt["ld_wa%d" % (j % 2)])
                if j in (2, 5):
                    gdst = g1bc if j == 2 else g2bc
                    tg = T_g1 if j == 2 else T_g2
                    for half in range(2):
                        for k in range(8):
                            S.op("pe", lambda e, wb=wb, k=k, half=half: e.matmul(
                                pG[:], lhsT=cbc[:, k, :], rhs=wb[:, k, half * 512:(half + 1) * 512], start=(k == 0), stop=(k == 7)),
                                reads=[T_cbc, tw], writes=[T_pG], signal=(k == 7))
                        S.op("dve", lambda e, gdst=gdst, half=half, j=j: e.tensor_tensor(
                            out=gdst[:, half * 512:(half + 1) * 512], in0=pG[:], in1=bgrow[:, 0 if j == 2 else 1, half * 512:(half + 1) * 512],
                            op=ALU.add), reads=[T_pG, T_in], writes=[tg])
                else:
                    for m in range(8):
                        for k in range(8):
                            S.op("pe", lambda e, wb=wb, k=k, m=m: e.matmul(
                                pA[:, m:m + 1], lhsT=wb[:, k, m * 128:(m + 1) * 128], rhs=cact[:, k:k + 1], start=(k == 0), stop=(k == 7)),
                                reads=[T_cact, tw], writes=[T_pA], signal=(k == 7))
                    S.op("dve", lambda e, j=j: e.tensor_tensor(out=adaF[:, j, :], in0=pA[:, 0:8], in1=badaFs[:, j * 8:(j + 1) * 8], op=ALU.add),
                         reads=[T_pA, T_in], writes=[T_ada])
            S.op("dve", lambda e: e.scalar_tensor_tensor(out=a1[:], in0=adaF[:, 1, :], scalar=1.0, in1=gmixs[:], op0=ALU.add, op1=ALU.mult),
                 reads=[T_ada, T_in], writes=[T_a1])
            S.op("dve", lambda e: e.scalar_tensor_tensor(out=a2[:], in0=adaF[:, 4, :], scalar=1.0, in1=gffns[:], op0=ALU.add, op1=ALU.mult),
                 reads=[T_ada, T_in], writes=[T_a2])
            for _ in genB:
                pass
            if "ada" in dbg:
                dump("adaF", adaF[:].rearrange("p a b -> p (a b)"), T_ada, [128, 48], F32)
                dump("g1bc", g1bc[:], T_g1, [128, D], F32)
                dump("g2bc", g2bc[:], T_g2, [128, D], F32)
                dump("nlam", nlam[:], T_misc, [128, 1], F32)

        S.barrier()
        if "hT" in dbg:
            for g in range(4):
                dump("hT%d" % g, hT[:, :, g * 512:(g + 1) * 512], T_hT[g], [128, 8, 512], BF16)

        if stop_after == "B":
            S.final_wait("sp", out_toks)
            return nc, dbg_outs

        with ExitStack() as s1:
            wblk2 = sbuf(s1, "wblk2", [128, 8, 1536], BF16)
            T_wblk2 = Tok("wblk2")
            wst_r = Ring([sbuf(s1, "wst%d" % i, [128, 2048], F32) for i in range(2)], "wst")

            def load_wblk(src2d, col0, W, dst, T_dst, dcol0=0, eng="pool"):
                srcv = src2d.rearrange("(k p) n -> p k n", p=128)
                for k in range(8):
                    stg, tst = wst_r.next()
                    S.dma("sp", "ld_" + tst.name, lambda e, stg=stg, k=k: e.dma_start(out=stg[:, 0:W], in_=srcv[:, k, col0:col0 + W]), writes=[tst])
                    en = eng if eng != "alt" else ("act" if k % 2 == 0 else "dve")
                    if en == "act":
                        S.op("act", lambda e, stg=stg, k=k: e.copy(out=dst[:, k, dcol0:dcol0 + W], in_=stg[:, 0:W]), reads=[tst], writes=[T_dst])
                    else:
                        S.op(en, lambda e, stg=stg, k=k: e.tensor_copy(out=dst[:, k, dcol0:dcol0 + W], in_=stg[:, 0:W]), reads=[tst], writes=[T_dst])

            with ExitStack() as sh:
                wblk = sbuf(sh, "wblk", [128, 8, 2048], BF16)
                T_wblk = Tok("wblk")
                load_wblk(w_in, 0, 2048, wblk, T_wblk, eng="alt")
                lblbc = sbuf(sh, "lblbc", [128, 1024], F32)
                lbbc = sbuf(sh, "lbbc", [128, 512], F32)
                omlbc = sbuf(sh, "omlbc", [128, 512], F32)
                T_lb = Tok("lb")
                S.dma("sp", "ld_lb", lambda e: e.dma_start(out=lblbc[:], in_=lbl[0:1, :].partition_broadcast(128)), writes=[T_lb])
                S.op("dve", lambda e: e.tensor_tensor(out=lbbc[:], in0=lblbc[:, 0:512], in1=lblbc[:, 512:1024], op=ALU.subtract), reads=[T_lb], writes=[T_lb])
                S.op("act", lambda e: e.activation(out=lbbc[:], in_=lbbc[:], func=AF.Sigmoid), reads=[T_lb], writes=[T_lb])
                S.op("dve", lambda e: e.tensor_scalar(out=omlbc[:], in0=lbbc[:], scalar1=-1.0, scalar2=1.0, op0=ALU.mult, op1=ALU.add), reads=[T_lb], writes=[T_lb])
                load_wblk(w_in, 2048, 1536, wblk2, T_wblk2, eng="pool")
                pq = psum(sh, "pq", [128, 512], F32); pf = psum(sh, "pf", [128, 512], F32)
                pi_ = psum(sh, "pi", [128, 512], F32); pog = psum(sh, "pog", [128, 512], F32)
                pT = psum(sh, "pT", [128, 1024], BF16); pT2 = psum(sh, "pT2", [128, 1024], BF16)
                po = psum(sh, "po", [128, 512], F32); pkv = psum(sh, "pkv", [128, 512], F32)
                psc = pf
                T_pq, T_pf, T_pi, T_pog, T_pT, T_pT2, T_po, T_pkv = [Tok(n) for n in "pq pf pi pog pT pT2 po pkv".split()]
                T_psc = T_pf
                def f32t(n): return sbuf(sh, n, [128, 512], F32), Tok(n)
                def bft(n, w=512): return sbuf(sh, n, [128, w], BF16), Tok(n)
                fs, T_fs = f32t("fs"); lf, T_lf = f32t("lf"); kk, T_kk = f32t("kk"); qs, T_qs = f32t("qs")
                eb, T_eb = f32t("eb"); enb, T_enb = f32t("enb")
                Sst, T_Sst = f32t("Sst"); tmpkv, T_tmpkv = f32t("tmpkv"); onf, T_onf = f32t("onf")
                qt, T_qt = bft("qt"); Stb, T_Stb = bft("Stb"); oa, T_oa = bft("oa")
                ogs_r = Ring([sbuf(sh, "ogs%d" % a, [128, 512], F32) for a in range(2)], "ogs")
                kt_r = Ring([sbuf(sh, "kt%d" % a, [128, 512], BF16) for a in range(2)], "kt")
                vb_r = Ring([sbuf(sh, "vb%d" % a, [128, 512], BF16) for a in range(2)], "vb")
                qkT_r = Ring([sbuf(sh, "qkT%d" % a, [128, 1024], BF16) for a in range(2)], "qkT")
                scm_r = Ring([sbuf(sh, "scm%d" % a, [128, 512], BF16) for a in range(2)], "scm")
                ebm_r = Ring([sbuf(sh, "ebm%d" % a, [128, 3, 4], F32) for a in range(2)], "ebm")
                bm3 = sbuf(sh, "bm3", [128, 3, 4], F32); T_bm3 = Tok("bm3")
                ssq4 = sbuf(sh, "ssq4", [128, 4], F32); T_ssq4 = Tok("ssq4")
                v4 = lambda ap: ap.rearrange("p (h v) -> p h v", h=4)
                hctx = {}

                def front(i):
                    tc0, tc1 = i * 128, (i + 1) * 128
                    g = i // 4
                    ogs, T_ogs = ogs_r.next(); kt, T_kt = kt_r.next(); vb, T_vb = vb_r.next()
                    qkT, T_qkT = qkT_r.next(); scm, T_scm = scm_r.next(); ebm, T_ebm = ebm_r.next()
                    hctx[i] = (ogs, T_ogs, kt, T_kt, vb, T_vb, qkT, T_qkT, scm, T_scm, ebm, T_ebm)
                    for (p, tp, c0) in [(pf, T_pf, 512), (pq, T_pq, 0), (pi_, T_pi, 1024), (pog, T_pog, 1536)]:
                        for k in range(8):
                            S.op("pe", lambda e, p=p, k=k, c0=c0: e.matmul(p[:], lhsT=hT[:, k, tc0:tc1], rhs=wblk[:, k, c0:c0 + 512], start=(k == 0), stop=(k == 7)),
                                 reads=[T_hT[g], T_wblk], writes=[tp], signal=(k == 7))
                        yield
                    S.op("act", lambda e: e.activation(out=fs[:], in_=pf[:], func=AF.Exp, scale=-1.0), reads=[T_pf], writes=[T_fs]); yield
                    S.op("act", lambda e: e.activation(out=fs[:], in_=fs[:], func=AF.Identity, bias=epsb[:, 2:3]), reads=[T_fs, T_eps], writes=[T_fs]); yield
                    S.op("dve", lambda e: e.reciprocal(out=fs[:], in_=fs[:]), reads=[T_fs], writes=[T_fs]); yield
                    S.op("dve", lambda e: e.tensor_tensor(out=fs[:], in0=fs[:], in1=omlbc[:], op=ALU.mult), reads=[T_fs, T_lb], writes=[T_fs]); yield
                    S.op("dve", lambda e: e.tensor_tensor(out=fs[:], in0=fs[:], in1=lbbc[:], op=ALU.add), reads=[T_fs, T_lb], writes=[T_fs]); yield
                    S.op("act", lambda e: e.activation(out=lf[:], in_=fs[:], func=AF.Ln), reads=[T_fs], writes=[T_lf]); yield
                    S.op("act", lambda e: e.activation(out=kk[:], in_=fs[:], func=AF.Identity, scale=-1.0, bias=epsb[:, 2:3]), reads=[T_fs, T_eps], writes=[T_kk]); yield
                    S.op("pe", lambda e: e.matmul(pf[:], lhsT=amat[:, 0:128], rhs=lf[:], start=True, stop=True), reads=[T_lf, T_c], writes=[T_pf]); yield
                    S.op("act", lambda e: e.activation(out=qs[:], in_=pq[:], func=AF.Exp, scale=-1.0), reads=[T_pq], writes=[T_qs]); yield
                    S.op("act", lambda e: e.activation(out=qs[:], in_=qs[:], func=AF.Identity, bias=epsb[:, 2:3]), reads=[T_qs, T_eps], writes=[T_qs]); yield
                    S.op("dve", lambda e: e.reciprocal(out=qs[:], in_=qs[:]), reads=[T_qs], writes=[T_qs]); yield
                    S.op("dve", lambda e: e.tensor_tensor(out=qs[:], in0=qs[:], in1=pq[:], op=ALU.mult), reads=[T_qs, T_pq], writes=[T_qs]); yield
                    for h in range(4):
                        S.op("pe", lambda e, h=h: e.matmul(pq[:, 2 * h:2 * h + 2], lhsT=lf[:, h * 128:(h + 1) * 128], rhs=amat[:, 128:130], start=True, stop=True),
                             reads=[T_lf, T_c], writes=[T_pq], signal=(h == 3))
                    yield
                    S.op("act", lambda e: e.copy(out=vb[:], in_=pi_[:]), reads=[T_pi], writes=[T_vb]); yield
                    S.op("act", lambda e: e.activation(out=ogs[:], in_=pog[:], func=AF.Exp, scale=-1.0), reads=[T_pog], writes=[T_ogs]); yield
                    S.op("act", lambda e: e.activation(out=ogs[:], in_=ogs[:], func=AF.Identity, bias=epsb[:, 2:3]), reads=[T_ogs, T_eps], writes=[T_ogs]); yield
                    S.op("dve", lambda e: e.reciprocal(out=ogs[:], in_=ogs[:]), reads=[T_ogs], writes=[T_ogs]); yield
                    S.op("dve", lambda e: e.tensor_tensor(out=ogs[:], in0=ogs[:], in1=pog[:], op=ALU.mult), reads=[T_ogs, T_pog], writes=[T_ogs]); yield
                    S.op("act", lambda e: e.activation(out=eb[:], in_=pf[:], func=AF.Exp), reads=[T_pf], writes=[T_eb]); yield
                    S.op("act", lambda e: e.activation(out=enb[:], in_=pf[:], func=AF.Exp, scale=-1.0), reads=[T_pf], writes=[T_enb]); yield
                    S.op("dve", lambda e: e.tensor_tensor(out=qt[:], in0=qs[:], in1=eb[:], op=ALU.mult), reads=[T_qs, T_eb], writes=[T_qt]); yield
                    S.op("dve", lambda e: e.tensor_tensor(out=kt[:], in0=kk[:], in1=enb[:], op=ALU.mult), reads=[T_kk, T_enb], writes=[T_kt]); yield
                    S.op("dve", lambda e: e.tensor_copy(out=bm3[:, 0:2, :], in_=pq[:, 0:8].rearrange("p (h t) -> p t h", t=2)), reads=[T_pq], writes=[T_bm3]); yield
                    S.op("dve", lambda e: e.tensor_tensor(out=bm3[:, 2, :], in0=bm3[:, 1, :], in1=bm3[:, 0, :], op=ALU.subtract), reads=[T_bm3], writes=[T_bm3]); yield
                    S.op("act", lambda e: e.activation(out=ebm[:].rearrange("p a b -> p (a b)"), in_=bm3[:].rearrange("p a b -> p (a b)"), func=AF.Exp),
                         reads=[T_bm3], writes=[T_ebm]); yield
                    for h in range(4):
                        S.op("pe", lambda e, h=h: e.transpose(pT[:, h * 128:(h + 1) * 128], qt[:, h * 128:(h + 1) * 128], identb[:]),
                             reads=[T_qt, T_c], writes=[T_pT], signal=False)
                    for h in range(4):
                        S.op("pe", lambda e, h=h: e.transpose(pT[:, 512 + h * 128:512 + (h + 1) * 128], kt[:, h * 128:(h + 1) * 128], identb[:]),
                             reads=[T_kt, T_c], writes=[T_pT], signal=(h == 3))
                    yield
                    S.op("act", lambda e: e.copy(out=qkT[:], in_=pT[:]), reads=[T_pT], writes=[T_qkT]); yield
                    for h in range(4):
                        S.op("pe", lambda e, h=h: e.matmul(psc[:, h * 128:(h + 1) * 128], lhsT=qkT[:, 512 + h * 128:512 + (h + 1) * 128],
                                                           rhs=qkT[:, h * 128:(h + 1) * 128], start=True, stop=True),
                             reads=[T_qkT], writes=[T_psc], signal=(h == 3))
                    yield
                    S.op("dve", lambda e: e.tensor_tensor(out=v4(scm[:]), in0=v4(psc[:]), in1=tri[:].unsqueeze(1).to_broadcast([128, 4, 128]), op=ALU.mult),
                         reads=[T_psc, T_c], writes=[T_scm]); yield

                def back(i):
                    tc0, tc1 = i * 128, (i + 1) * 128
                    (ogs, T_ogs, kt, T_kt, vb, T_vb, qkT, T_qkT, scm, T_scm, ebm, T_ebm) = hctx.pop(i)
                    if i > 0:
                        S.op("dve", lambda e: e.tensor_tensor(out=v4(Stb[:]), in0=v4(Sst[:]), in1=ebm[:, 0, :].unsqueeze(2).to_broadcast([128, 4, 128]), op=ALU.mult),
                             reads=[T_Sst, T_ebm], writes=[T_Stb]); yield
                    for h in range(4):
                        hs0, hs1 = h * 128, (h + 1) * 128
                        S.op("pe", lambda e, hs0=hs0, hs1=hs1: e.matmul(pkv[:, hs0:hs1], lhsT=kt[:, hs0:hs1], rhs=vb[:, hs0:hs1], start=True, stop=True),
                             reads=[T_kt, T_vb], writes=[T_pkv], signal=(h == 3))
                    yield
                    S.op("dve", lambda e: e.tensor_tensor(out=v4(tmpkv[:]), in0=v4(pkv[:]), in1=ebm[:, 2, :].unsqueeze(2).to_broadcast([128, 4, 128]), op=ALU.mult),
                         reads=[T_pkv, T_ebm], writes=[T_tmpkv]); yield
                    for h in range(4):
                        hs0, hs1 = h * 128, (h + 1) * 128
                        S.op("pe", lambda e, hs0=hs0, hs1=hs1: e.matmul(po[:, hs0:hs1], lhsT=scm[:, hs0:hs1], rhs=vb[:, hs0:hs1], start=True, stop=(i == 0)),
                             reads=[T_scm, T_vb], writes=[T_po], signal=(i == 0 and h == 3))
                        if i > 0:
                            S.op("pe", lambda e, hs0=hs0, hs1=hs1: e.matmul(po[:, hs0:hs1], lhsT=qkT[:, hs0:hs1], rhs=Stb[:, hs0:hs1], start=False, stop=True),
                                 reads=[T_qkT, T_Stb], writes=[T_po], signal=(h == 3))
                    yield
                    if i == 0:
                        S.op("dve", lambda e: e.tensor_copy(out=Sst[:], in_=tmpkv[:]), reads=[T_tmpkv], writes=[T_Sst]); yield
                    else:
                        S.op("dve", lambda e: e.tensor_tensor(out=v4(Sst[:]), in0=v4(Sst[:]), in1=ebm[:, 1, :].unsqueeze(2).to_broadcast([128, 4, 128]), op=ALU.mult),
                             reads=[T_Sst, T_ebm, T_Stb], writes=[T_Sst]); yield
                        S.op("dve", lambda e: e.tensor_tensor(out=Sst[:], in0=Sst[:], in1=tmpkv[:], op=ALU.add), reads=[T_Sst, T_tmpkv], writes=[T_Sst]); yield
                    S.op("act", lambda e: e.activation(out=onf[:], in_=po[:], func=AF.Square), reads=[T_po], writes=[T_onf]); yield
                    S.op("dve", lambda e: e.reduce_sum(out=ssq4[:], in_=v4(onf[:]), axis=AX.X), reads=[T_onf], writes=[T_ssq4]); yield
                    S.op("act", lambda e: e.activation(out=ssq4[:], in_=ssq4[:], func=AF.Ln, scale=1.0 / 128, bias=epsb[:, 0:1]), reads=[T_ssq4, T_eps], writes=[T_ssq4]); yield
                    S.op("act", lambda e: e.activation(out=ssq4[:], in_=ssq4[:], func=AF.Exp, scale=-0.5), reads=[T_ssq4], writes=[T_ssq4]); yield
                    S.op("dve", lambda e: e.tensor_tensor(out=v4(onf[:]), in0=v4(po[:]), in1=ssq4[:].unsqueeze(2).to_broadcast([128, 4, 128]), op=ALU.mult),
                         reads=[T_po, T_ssq4], writes=[T_onf]); yield
                    S.op("pool", lambda e: e.tensor_tensor(out=v4(onf[:]), in0=v4(onf[:]), in1=gnbc[:].unsqueeze(1).to_broadcast([128, 4, 128]), op=ALU.mult),
                         reads=[T_onf, T_in0], writes=[T_onf]); yield
                    S.op("pool", lambda e: e.tensor_tensor(out=oa[:], in0=onf[:], in1=ogs[:], op=ALU.mult), reads=[T_onf, T_ogs], writes=[T_oa]); yield
                    for h in range(4):
                        S.op("pe", lambda e, h=h: e.transpose(pT2[:, h * 128:(h + 1) * 128], oa[:, h * 128:(h + 1) * 128], identb[:]),
                             reads=[T_oa, T_c], writes=[T_pT2], signal=(h == 3))
                    yield
                    S.op("act", lambda e: e.copy(out=mixT[:, 0:4, tc0:tc1], in_=pT2[:, 0:512].rearrange("p (h v) -> p h v", h=4)),
                         reads=[T_pT2], writes=[T_mixA[i]]); yield

                interleave(front(0))
                for i in range(NT):
                    interleave(front(i + 1) if i + 1 < NT else None, back(i))
                if "oa" in dbg:
                    T_all = Tok("mixAall")
                    T_all.w = T_mixA[NT - 1].w
                    dump("oaT", mixT[:, 0:4, :], T_all, [128, 4, S_TOK], BF16)
            S.barrier()
            if stop_after == "C1":
                S.final_wait("sp", out_toks)
                return nc, dbg_outs

            with ExitStack() as sd:
                pp = [psum(sd, "pp%d" % i, [128, 512], F32) for i in range(2)]
                T_pp = [Tok("pp0"), Tok("pp1")]
                pss = psum(sd, "pss", [128, 512], F32); T_pss = Tok("pss")
                pOb = [psum(sd, "pO%d" % i, [128, 512], F32) for i in range(4)]
                T_pOb = [[Tok("pO%d_%d" % (a, i)) for i in range(2)] for a in range(2)]
                T_pO = [[T_pOb[a][i // 2] for i in range(4)] for a in range(2)]

                def pOap(a, ql, w):
                    c0 = (ql % 2) * 256
                    return pOb[a * 2 + ql // 2][:, c0:c0 + w]
                pTo = psum(sd, "pTo", [128, 512], BF16); T_pTo = Tok("pTo")
                qn = [sbuf(sd, "qnT%d" % i, [128, S_TOK], BF16) for i in range(2)]
                kn = [sbuf(sd, "knT%d" % i, [128, S_TOK], BF16) for i in range(2)]
                T_qn = [[Tok("qn%d_%d" % (i, t)) for t in range(8)] for i in range(2)]
                T_kn = [[Tok("kn%d_%d" % (i, t)) for t in range(8)] for i in range(2)]
                Vext = sbuf(sd, "Vext", [128, NT, 4, 130], BF16); T_V = Tok("Vext")
                sq_r = Ring([sbuf(sd, "sq%d" % i, [128, 512], BF16) for i in range(2)], "sq")
                rs_r = Ring([sbuf(sd, "rs%d" % i, [128, 512], F32) for i in range(2)], "rs")
                PTall = [sbuf(sd, "PTall%d" % i, [128, NT, 512], BF16) for i in range(2)]
                T_PT = [[Tok("PT%d_%d" % (i, j)) for j in range(NT)] for i in range(2)]
                o1 = sbuf(sd, "o1", [128, 4, 128], F32); T_o1 = [Tok("o1_%d" % i) for i in range(4)]
                od_r = Ring([sbuf(sd, "od%d" % i, [128, 128], F32) for i in range(2)], "od")
                odn_r = Ring([sbuf(sd, "odn%d" % i, [128, 128], BF16) for i in range(2)], "odn")
                jk = sbuf(sd, "jk", [128, 128], BF16); T_jk = Tok("jk")
                rr_r = Ring([sbuf(sd, "rr%d" % i, [128, 4], F32) for i in range(8)], "rr")
                npp = [0]

                def nextpp():
                    a = npp[0] % 2
                    npp[0] += 1
                    return pp[a], T_pp[a]
                S.op("dve", lambda e: e.memset(Vext[:, :, :, 128:130], 1.0), writes=[T_V])
                for i in range(NT):
                    p, tp = nextpp()
                    for k in range(8):
                        S.op("pe", lambda e, p=p, k=k, i=i: e.matmul(p[:], lhsT=hT[:, k, i * 128:(i + 1) * 128], rhs=wblk2[:, k, 1024:1536], start=(k == 0), stop=(k == 7)),
                             reads=[T_hT[i // 4], T_wblk2], writes=[tp], signal=(k == 7))
                    S.op("act", lambda e, p=p, i=i: e.copy(out=Vext[:, i, :, 0:128], in_=p[:].rearrange("p (h v) -> p h v", h=4)), reads=[tp], writes=[T_V])

                T_pssA, T_pssB = Tok("pssA"), Tok("pssB")

                def proj_chunk(h, n):
                    t8, which = divmod(n, 2)
                    hb = h % 2
                    if which == 0:
                        dst, T_dst, c0, gs = qn[hb], T_qn[hb][t8], h * 128, gqs
                    else:
                        dst, T_dst, c0, gs = kn[hb], T_kn[hb][t8], 512 + h * 128, gks
                    p = pss[:, 0:256]
                    pb = pss[:, 256:512]
                    for k in range(8):
                        S.op("pe", lambda e, k=k: e.matmul(p, lhsT=wblk2[:, k, c0:c0 + 128], rhs=hT[:, k, t8 * 256:(t8 + 1) * 256], start=(k == 0), stop=(k == 7)),
                             reads=[T_hT[t8 // 2], T_wblk2], writes=[T_pssA], signal=(k == 7))
                    yield
                    sq, tsq = sq_r.next()
                    S.op("act", lambda e: e.activation(out=sq[:, 0:256], in_=p, func=AF.Square), reads=[T_pssA], writes=[tsq]); yield
                    S.op("pe", lambda e: e.matmul(pb, lhsT=bones[:], rhs=sq[:, 0:256], start=True, stop=True), reads=[tsq, T_c], writes=[T_pssB]); yield
                    rs, trs = rs_r.next()
                    S.op("act", lambda e: e.activation(out=rs[:, 0:256], in_=pb, func=AF.Ln, bias=epsb[:, 1:2]), reads=[T_pssB, T_eps], writes=[trs]); yield
                    S.op("act", lambda e: e.activation(out=rs[:, 0:256], in_=rs[:, 0:256], func=AF.Exp, scale=-0.5), reads=[trs], writes=[trs]); yield
                    S.op("dve", lambda e: e.scalar_tensor_tensor(out=dst[:, t8 * 256:(t8 + 1) * 256], in0=p, scalar=gs[:, 0:1], in1=rs[:, 0:256], op0=ALU.mult, op1=ALU.mult),
                         reads=[T_pssA, trs, T_in0], writes=[T_dst]); yield

                def proj_two(h, n):
                    yield from proj_chunk(h, 2 * n)
                    yield from proj_chunk(h, 2 * n + 1)

                def att_geom(g, j):
                    if j < 4 * g:
                        return 4 * g * 128, 512, False
                    return j * 128, (4 * g + 4 - j) * 128, True

                def att_qk(h, g, c, b):
                    hb = h % 2
                    cs0, cs1 = c * 64, (c + 1) * 64
                    nj = 4 * g + 4

                    def qk(j):
                        q0, nq, diag = att_geom(g, j)
                        p, tp = nextpp()
                        S.op("pe", lambda e: e.matmul(p[:, 0:nq], lhsT=kn[hb][cs0:cs1, j * 128:(j + 1) * 128], rhs=qn[hb][cs0:cs1, q0:q0 + nq], start=True, stop=True),
                             reads=[T_kn[hb][j // 2], T_qn[hb][2 * g], T_qn[hb][2 * g + 1]], writes=[tp])
                        return p, tp
                    cur = qk(0)
                    yield
                    for j in range(nj):
                        q0, nq, diag = att_geom(g, j)
                        p, tp = cur
                        PT, tPT = PTall[b][:, j, :], T_PT[b][j]
                        S.op("act", lambda e: e.activation(out=PT[:, 0:nq], in_=p[:, 0:nq], func=AF.Exp, scale=8.0), reads=[tp], writes=[tPT])
                        yield
                        if j + 1 < nj:
                            cur = qk(j + 1)
                            yield
                        if diag:
                            S.op("dve", lambda e: e.tensor_tensor(out=PT[:, 0:128], in0=PT[:, 0:128], in1=tri[:], op=ALU.mult), reads=[tPT, T_c], writes=[tPT])
                            yield

                def att_pv(h, g, c, a, b):
                    for ql in range(4):
                        qi = 4 * g + ql
                        for j in range(qi + 1):
                            q0, nq, diag = att_geom(g, j)
                            off = qi * 128 - q0
                            S.op("pe", lambda e: e.matmul(pOap(a, ql, 129), lhsT=PTall[b][:, j, off:off + 128], rhs=Vext[:, j, h, 0:129], start=(j == 0), stop=(j == qi)),
                                 reads=[T_PT[b][j], T_V], writes=[T_pO[a][ql]], signal=(j == qi))
                            if j % 2 == 1:
                                yield
                        yield

                def att_epi(h, g, c, a):
                    for ql in range(4):
                        qi = 4 * g + ql
                        rr, trr = rr_r.next()
                        S.op("dve", lambda e: e.reciprocal(out=rr[:, 0:1], in_=pOap(a, ql, 129)[:, 128:129]), reads=[T_pO[a][ql]], writes=[trr]); yield
                        if c == 0:
                            S.op("dve", lambda e: e.tensor_scalar(out=o1[:, ql, :], in0=pOap(a, ql, 128), scalar1=rr[:, 0:1], scalar2=None, op0=ALU.mult),
                                 reads=[T_pO[a][ql], trr], writes=[T_o1[ql]]); yield
                        else:
                            S.op("dve", lambda e: e.tensor_tensor(out=rr[:, 1:2], in0=rr[:, 0:1], in1=nlam[:], op=ALU.mult), reads=[trr, T_misc], writes=[trr]); yield
                            od, tod = od_r.next()
                            S.op("dve", lambda e: e.scalar_tensor_tensor(out=od[:], in0=pOap(a, ql, 128), scalar=rr[:, 1:2], in1=o1[:, ql, :], op0=ALU.mult, op1=ALU.add),
                                 reads=[T_pO[a][ql], trr, T_o1[ql]], writes=[tod]); yield
                            S.op("act", lambda e: e.activation(out=jk[:], in_=od[:], func=AF.Square, accum_out=rr[:, 2:3]), reads=[tod], writes=[T_jk, trr]); yield
                            S.op("act", lambda e: e.activation(out=rr[:, 3:4], in_=rr[:, 2:3], func=AF.Ln, scale=1.0 / 128, bias=epsb[:, 0:1]),
                                 reads=[trr, T_eps], writes=[trr]); yield
                            S.op("act", lambda e: e.activation(out=rr[:, 3:4], in_=rr[:, 3:4], func=AF.Exp, scale=-0.5), reads=[trr], writes=[trr]); yield
                            odn, todn = odn_r.next()
                            S.op("dve", lambda e: e.scalar_tensor_tensor(out=odn[:], in0=od[:], scalar=rr[:, 3:4], in1=sg8[:], op0=ALU.mult, op1=ALU.mult),
                                 reads=[tod, trr, T_misc], writes=[todn]); yield
                            S.op("pe", lambda e: e.transpose(pTo[:, 0:128], odn[:], identb[:]), reads=[todn, T_c], writes=[T_pTo]); yield
                            S.op("act", lambda e: e.copy(out=mixT[:, 4 + h, qi * 128:(qi + 1) * 128], in_=pTo[:, 0:128]), reads=[T_pTo], writes=[T_mixD[qi]]); yield

                for n in range(8):
                    interleave(proj_two(0, n))
                its = [(h, g, c) for h in range(4) for g in range(4) for c in range(2)]
                NI = len(its)
                for n in range(NI + 2):
                    gens = []
                    if n < NI:
                        h, g, c = its[n]
                        gens.append(att_qk(h, g, c, n % 2))
                    if 1 <= n <= NI:
                        h1, g1, c1 = its[n - 1]
                        gens.append(att_pv(h1, g1, c1, (n - 1) % 2, (n - 1) % 2))
                    if 2 <= n <= NI + 1:
                        h2, g2, c2 = its[n - 2]
                        gens.append(att_epi(h2, g2, c2, (n - 2) % 2))
                    if n < NI and its[n][0] < 3:
                        gens.append(proj_two(its[n][0] + 1, n % 8))
                    interleave(*gens)
                if "od" in dbg:
                    T_all = Tok("mixDall")
                    T_all.w = ("act", S.cnt["act"])
                    dump("odT", mixT[:, 4:8, :], T_all, [128, 4, S_TOK], BF16)
            S.barrier()
            if stop_after == "C2":
                S.final_wait("sp", out_toks)
                return nc, dbg_outs

        with ExitStack() as s2:
            acc = sbuf(s2, "acc", [128, NT, D], F32)
            T_acc = [[Tok("acc%d_%d" % (i, hf)) for hf in range(2)] for i in range(NT)]
            with ExitStack() as so:
                wst2_r = Ring([sbuf(so, "wst2_%d" % i, [128, 1024], F32) for i in range(3)], "wst2")
                wg = sbuf(so, "wg", [128, 8, 1024], BF16); T_wg = Tok("wg")
                wo = sbuf(so, "wo", [128, 8, 512], BF16); T_wo = Tok("wo")
                pz = [[psum(so, "pz%d_%d" % (a, b), [128, 512], F32) for b in range(4)] for a in range(2)]
                T_pz = [[Tok("pz%d_%d" % (a, b)) for b in range(4)] for a in range(2)]
                sa_r = Ring([sbuf(so, "sa%d" % i, [128, 512], F32) for i in range(2)], "sa")
                sd_r = Ring([sbuf(so, "sdg%d" % i, [128, 512], F32) for i in range(2)], "sdg")
                t1_r = Ring([sbuf(so, "t1_%d" % i, [128, 512], F32) for i in range(2)], "t1")
                t2_r = Ring([sbuf(so, "t2_%d" % i, [128, 512], F32) for i in range(2)], "t2")
                xh_r = Ring([sbuf(so, "xh%d" % i, [128, 512], F32) for i in range(2)], "xh")

                def load_w2(src2d, col0, W, dst, T_dst, dcol0):
                    srcv = src2d.rearrange("(k p) n -> p k n", p=128)
                    for k in range(8):
                        stg, tst = wst2_r.next()
                        S.dma("sp", "ld_" + tst.name, lambda e, stg=stg, k=k: e.dma_start(out=stg[:, 0:W], in_=srcv[:, k, col0:col0 + W]), writes=[tst])
                        if k % 2 == 0:
                            S.op("act", lambda e, stg=stg, k=k: e.copy(out=dst[:, k, dcol0:dcol0 + W], in_=stg[:, 0:W]), reads=[tst], writes=[T_dst])
                        else:
                            S.op("dve", lambda e, stg=stg, k=k: e.tensor_copy(out=dst[:, k, dcol0:dcol0 + W], in_=stg[:, 0:W]), reads=[tst], writes=[T_dst])
                for hf in range(2):
                    load_w2(w_in, 3584 + hf * 512, 512, wg, T_wg, 0)
                    load_w2(w_in, 4608 + hf * 512, 512, wg, T_wg, 512)
                    load_w2(w_out, hf * 512, 512, wo, T_wo, 0)
                    for i in range(NT):
                        tc0, tc1 = i * 128, (i + 1) * 128
                        pzz, tzz = pz[i % 2], T_pz[i % 2]
                        for k in range(8):
                            S.op("pe", lambda e, k=k, p=pzz[0]: e.matmul(p[:], lhsT=hT[:, k, tc0:tc1], rhs=wg[:, k, 0:512], start=(k == 0), stop=(k == 7)),
                                 reads=[T_hT[i // 4], T_wg], writes=[tzz[0]], signal=(k == 7))
                        for k in range(8):
                            S.op("pe", lambda e, k=k, p=pzz[1]: e.matmul(p[:], lhsT=hT[:, k, tc0:tc1], rhs=wg[:, k, 512:1024], start=(k == 0), stop=(k == 7)),
                                 reads=[T_hT[i // 4], T_wg], writes=[tzz[1]], signal=(k == 7))
                        for k in range(4):
                            S.op("pe", lambda e, k=k, p=pzz[2]: e.matmul(p[:], lhsT=mixT[:, k, tc0:tc1], rhs=wo[:, k, :], start=(k == 0), stop=(k == 3)),
                                 reads=[T_mixA[i], T_wo], writes=[tzz[2]], signal=(k == 3))
                        for k in range(4, 8):
                            S.op("pe", lambda e, k=k, p=pzz[3]: e.matmul(p[:], lhsT=mixT[:, k, tc0:tc1], rhs=wo[:, k, :], start=(k == 4), stop=(k == 7)),
                                 reads=[T_mixD[i], T_wo], writes=[tzz[3]], signal=(k == 7))
                        sa, tsa = sa_r.next(); sdg, tsd = sd_r.next(); t1, tt1 = t1_r.next(); t2, tt2 = t2_r.next(); xh, txh = xh_r.next()
                        S.dma("sp", "ld_" + txh.name, lambda e, xh=xh, i=i, hf=hf: e.dma_start(out=xh[:], in_=x[i * 128:(i + 1) * 128, hf * 512:(hf + 1) * 512]), writes=[txh])
                        S.op("act", lambda e, sa=sa, p=pzz[0]: e.activation(out=sa[:], in_=p[:], func=AF.Sigmoid), reads=[tzz[0]], writes=[tsa])
                        S.op("act", lambda e, sdg=sdg, p=pzz[1]: e.activation(out=sdg[:], in_=p[:], func=AF.Sigmoid), reads=[tzz[1]], writes=[tsd])
                        S.op("dve", lambda e, t1=t1, sa=sa, p=pzz[2]: e.tensor_tensor(out=t1[:], in0=sa[:], in1=p[:], op=ALU.mult), reads=[tsa, tzz[2]], writes=[tt1])
                        S.op("dve", lambda e, t2=t2, sdg=sdg, p=pzz[3]: e.tensor_tensor(out=t2[:], in0=sdg[:], in1=p[:], op=ALU.mult), reads=[tsd, tzz[3]], writes=[tt2])
                        S.op("pool", lambda e, t1=t1, t2=t2: e.tensor_tensor(out=t1[:], in0=t1[:], in1=t2[:], op=ALU.add), reads=[tt1, tt2], writes=[tt1])
                        S.op("dve", lambda e, t1=t1, hf=hf: e.tensor_tensor(out=t1[:], in0=t1[:], in1=g1bc[:, hf * 512:(hf + 1) * 512], op=ALU.mult), reads=[tt1, T_g1], writes=[tt1])
                        S.op("dve", lambda e, t1=t1, xh=xh, i=i, hf=hf: e.tensor_tensor(out=acc[:, i, hf * 512:(hf + 1) * 512], in0=t1[:], in1=xh[:], op=ALU.add),
                             reads=[tt1, txh], writes=[T_acc[i][hf]])
            S.barrier()
            if "x1" in dbg:
                for i in range(NT):
                    tt = Tok("x1d%d" % i)
                    tt.w = T_acc[i][1].w
                    dump("x1_%d" % i, acc[:, i, :], tt, [128, D], F32)
            if stop_after == "C3":
                S.final_wait("sp", out_toks)
                return nc, dbg_outs

            with ExitStack() as sf:
                NSLOT = 10
                ring2 = sbuf(sf, "ring2", [128, NSLOT - 8, 2048], BF16)

                def slot_ap(sl):
                    base = bufB[:, sl, :] if sl < 8 else ring2[:, sl - 8, :]
                    return base.rearrange("p (k c) -> p k c", k=8)
                T_slot = [Tok("slot%d" % i) for i in range(NSLOT)]
                w2b = sbuf(sf, "w2b", [128, 8, D], BF16); T_w2b = Tok("w2b")
                stg_r = Ring([sbuf(sf, "stg%d" % i, [128, 1024], F32) for i in range(4)], "stg")
                wrb = sbuf(sf, "wrb", [128, 8, NE], BF16); T_wrb = Tok("wrb")
                wrs = sbuf(sf, "wrs", [128, 8, NE], F32)
                b2g = sbuf(sf, "b2g", [NE, D], F32); T_b2g = Tok("b2g")
                gates = sbuf(sf, "gates", [128, 8, NE], F32); T_gates = [Tok("gates%d" % i) for i in range(8)]
                h2T = bufA
                T_h2T = [Tok("h2T0"), Tok("h2T1")]
                T_actT = [[Tok("actT%d_%d" % (j, tg)) for tg in range(2)] for j in range(8)]
                S.dma("sp", "ld_wrs", lambda e: e.dma_start(out=wrs[:], in_=w_r.rearrange("(k p) n -> p k n", p=128)), writes=[T_wrb])
                S.op("dve", lambda e: e.tensor_copy(out=wrb[:], in_=wrs[:]), reads=[T_wrb], writes=[T_wrb])
                S.dma("sp", "ld_b2g", lambda e: e.dma_start(out=b2g[:], in_=b2[:, :]), writes=[T_b2g])
                S.op("dve", lambda e: e.tensor_tensor(out=b2g[:], in0=b2g[:], in1=g2bc[0:NE, :], op=ALU.mult), reads=[T_b2g, T_g2], writes=[T_b2g])
                slot_ctr = [0]
                for th in range(2):
                    tiles = list(range(th * 8, th * 8 + 8))
                    with ExitStack() as sn:
                        srcs = [(acc[:, i, :], T_acc[i]) for i in tiles]
                        for _ in norm_transpose(sn, "D%d" % th, srcs, h2T, a2, 3, T_a2, T_h2T, False, nxn=4):
                            pass
                    S.barrier()
                    with ExitStack() as se:
                        pgb = [psum(se, "pgb%d_%d" % (th, a), [128, 512], F32) for a in range(2)]; T_pgb = [Tok("pgb0"), Tok("pgb1")]
                        plb = [psum(se, "plb%d_%d" % (th, a), [128, 512], F32) for a in range(2)]; T_plb = [Tok("plb0"), Tok("plb1")]
                        pyb = [psum(se, "pyb%d_%d" % (th, a), [128, 512], F32) for a in range(2)]; T_pyb = [Tok("pyb0"), Tok("pyb1")]
                        gm_r = Ring([sbuf(se, "gm%d_%d" % (th, a), [128, 512], F32) for a in range(2)], "gm")
                        sg_r = Ring([sbuf(se, "sg%d_%d" % (th, a), [128, 512], F32) for a in range(2)], "sg")
                        lm_r = Ring([sbuf(se, "lm%d_%d" % (th, a), [128, 512], F32) for a in range(1)], "lm")
                        tt_r = Ring([sbuf(se, "tt%d_%d" % (th, a), [128, 512], F32) for a in range(1)], "tt")
                        n1 = 0
                        n2 = 0
                        if th == 0:
                            print("sbuf bytes remaining in FFN expert scope:", nc.sbuf_bytes_remaining)
                        ne_run = NE if stop_after == "all" else int(stop_after[1:]) if stop_after.startswith("E") else NE
                        pend = []

                        def flush_casts():
                            while pend:
                                pend.pop(0)()

                        def load_piece(qidx):
                            e_, j_ = divmod(qidx, 8)
                            if e_ >= ne_run:
                                return
                            w1v = w1p[e_].rearrange("(k p) n -> p k n", p=128)
                            sl = qidx % NSLOT
                            sap = slot_ap(sl)
                            for part in range(2):
                                stg, tst = stg_r.next()
                                cc0 = part * D + j_ * 128
                                S.dma("sp", "ld_" + tst.name, lambda e, stg=stg, cc0=cc0: e.dma_start(
                                    out=stg[:].rearrange("p (k c) -> p k c", k=8), in_=w1v[:, :, cc0:cc0 + 128]), writes=[tst])
                                pend.append(lambda stg=stg, tst=tst, sap=sap, part=part, sl=sl: S.op("act", lambda e: e.copy(
                                    out=sap[:, :, part * 128:(part + 1) * 128], in_=stg[:].rearrange("p (k c) -> p k c", k=8)),
                                    reads=[tst], writes=[T_slot[sl]]))

                        def load_w2chunk(e_, k):
                            if e_ >= ne_run:
                                return
                            stg, tst = stg_r.next()
                            S.dma("sp", "ld_" + tst.name, lambda e, stg=stg, k=k: e.dma_start(out=stg[:], in_=w2[e_, k * 128:(k + 1) * 128, :]), writes=[tst])
                            pend.append(lambda stg=stg, tst=tst, k=k: S.op("dve" if k % 4 != 3 else "pool", lambda e: e.tensor_tensor(
                                out=w2b[:, k, :], in0=stg[:], in1=g2bc[:], op=ALU.mult), reads=[tst, T_g2], writes=[T_w2b]))
                        for qidx in range(NSLOT):
                            load_piece(qidx)
                            flush_casts()
                        rtmp = []
                        for ch in range(2):
                            rtmp.append(dict(
                                lg=sbuf(se, "lg%d_%d" % (th, ch), [128, NE], F32), T_lg=Tok("lg"),
                                top8=sbuf(se, "top8_%d_%d" % (th, ch), [128, 8], F32), T_top8=Tok("top8"),
                                msk=sbuf(se, "msk%d_%d" % (th, ch), [128, NE], F32), T_msk=Tok("msk"),
                                ex=sbuf(se, "ex%d_%d" % (th, ch), [128, NE], F32), T_ex=Tok("ex"),
                                sm=sbuf(se, "sm%d_%d" % (th, ch), [128, 4], F32), T_sm=Tok("sm"),
                                gT=sbuf(se, "gT%d_%d" % (th, ch), [NE, 128], F32), T_gT=Tok("gT")))

                        def router_tile(il, ch):
                            i = tiles[il]
                            c0, c1 = il * 128, (il + 1) * 128
                            R = rtmp[ch]
                            lg, top8, msk, ex, sm, gT = R["lg"], R["top8"], R["msk"], R["ex"], R["sm"], R["gT"]
                            T_lg, T_top8, T_msk, T_ex, T_sm, T_gT = R["T_lg"], R["T_top8"], R["T_msk"], R["T_ex"], R["T_sm"], R["T_gT"]
                            plog, T_plog = pgb[ch], T_pgb[ch]
                            pgT, T_pgT = plb[ch], T_plb[ch]
                            pbk, T_pbk = pyb[ch], T_pyb[ch]
                            for k in range(8):
                                S.op("pe", lambda e, k=k: e.matmul(plog[:, 0:NE], lhsT=h2T[:, k, c0:c1], rhs=wrb[:, k, :], start=(k == 0), stop=(k == 7)),
                                     reads=[T_h2T[il // 4], T_wrb], writes=[T_plog], signal=(k == 7))
                            yield
                            S.op("dve", lambda e: e.tensor_tensor(out=lg[:], in0=plog[:, 0:NE], in1=brbc[:], op=ALU.add), reads=[T_plog, T_in0], writes=[T_lg]); yield
                            S.op("dve", lambda e: e.max(out=top8[:], in_=lg[:]), reads=[T_lg], writes=[T_top8]); yield
                            S.op("dve", lambda e: e.tensor_scalar(out=msk[:], in0=lg[:], scalar1=top8[:, 3:4], scalar2=None, op0=ALU.is_ge), reads=[T_lg, T_top8], writes=[T_msk]); yield
                            S.op("dve", lambda e: e.tensor_scalar(out=sm[:, 0:1], in0=top8[:, 0:1], scalar1=-1.0, scalar2=None, op0=ALU.mult), reads=[T_top8], writes=[T_sm]); yield
                            S.op("act", lambda e: e.activation(out=ex[:], in_=lg[:], func=AF.Exp, bias=sm[:, 0:1]), reads=[T_lg, T_sm], writes=[T_ex]); yield
                            S.op("dve", lambda e: e.tensor_tensor(out=ex[:], in0=ex[:], in1=msk[:], op=ALU.mult), reads=[T_ex, T_msk], writes=[T_ex]); yield
                            S.op("dve", lambda e: e.reduce_sum(out=sm[:, 1:2], in_=ex[:], axis=AX.X), reads=[T_ex], writes=[T_sm]); yield
                            S.op("dve", lambda e: e.reciprocal(out=sm[:, 2:3], in_=sm[:, 1:2]), reads=[T_sm], writes=[T_sm]); yield
                            S.op("dve", lambda e: e.tensor_scalar(out=gates[:, il, :], in0=ex[:], scalar1=sm[:, 2:3], scalar2=None, op0=ALU.mult),
                                 reads=[T_ex, T_sm], writes=[T_gates[il]]); yield
                            S.op("pe", lambda e: e.matmul(pgT[0:NE, 0:128], lhsT=gates[:, il, :], rhs=identf[:], start=True, stop=True),
                                 reads=[T_gates[il], T_c], writes=[T_pgT]); yield
                            S.op("act", lambda e: e.copy(out=gT[:], in_=pgT[0:NE, 0:128]), reads=[T_pgT], writes=[T_gT]); yield
                            for nh in range(2):
                                S.op("pe", lambda e: e.matmul(pbk[:], lhsT=gT[:], rhs=b2g[:, nh * 512:(nh + 1) * 512], start=True, stop=True),
                                     reads=[T_gT, T_b2g], writes=[T_pbk]); yield
                                S.op("dve", lambda e: e.tensor_tensor(out=acc[:, i, nh * 512:(nh + 1) * 512], in0=acc[:, i, nh * 512:(nh + 1) * 512], in1=pbk[:], op=ALU.add),
                                     reads=[T_pbk, T_acc[i][nh]], writes=[T_acc[i][nh]]); yield
                        for il2 in range(4):
                            interleave(router_tile(2 * il2, 0), router_tile(2 * il2 + 1, 1))
                        if "gates" in dbg and th == 0:
                            tt_ = Tok("gd"); tt_.w = T_gates[7].w
                            dump("gates", gates[:].rearrange("p a b -> p (a b)"), tt_, [128, 8 * NE], F32)
                        for ex_i in range(ne_run):
                            slots = [(ex_i * 8 + j) % NSLOT for j in range(8)]
                            for j in range(8):
                                sl = slots[j]
                                sap = slot_ap(sl)
                                for tg in range(2):
                                    a = n1 % 2
                                    n1 += 1
                                    for k in range(8):
                                        S.op("pe", lambda e, k=k, a=a, sap=sap, tg=tg: e.matmul(pgb[a][:], lhsT=sap[:, k, 0:128], rhs=h2T[:, k, tg * 512:(tg + 1) * 512],
                                                                                          start=(k == 0), stop=(k == 7)),
                                             reads=[T_slot[sl], T_h2T[tg]], writes=[T_pgb[a]] + ([T_plb[a]] if k == 0 else []), signal=(k == 7))
                                    for k in range(8):
                                        S.op("pe", lambda e, k=k, a=a, sap=sap, tg=tg: e.matmul(plb[a][:], lhsT=sap[:, k, 128:256], rhs=h2T[:, k, tg * 512:(tg + 1) * 512],
                                                                                          start=(k == 0), stop=(k == 7)),
                                             reads=[T_slot[sl], T_h2T[tg]], writes=[T_plb[a]], signal=(k == 7))
                                    gm, tgm = gm_r.next(); sg, tsg = sg_r.next(); lm, tlm = lm_r.next(); tt, ttt = tt_r.next()
                                    bg = b1s[:, ex_i * 16 + j:ex_i * 16 + j + 1]
                                    bl = b1s[:, ex_i * 16 + 8 + j:ex_i * 16 + 8 + j + 1]
                                    S.op("dve", lambda e, gm=gm, a=a, bg=bg: e.tensor_scalar(out=gm[:], in0=pgb[a][:], scalar1=bg, scalar2=7.0, op0=ALU.add, op1=ALU.min),
                                         reads=[T_pgb[a], T_misc], writes=[tgm])
                                    S.op("act", lambda e, gm=gm, sg=sg: e.activation(out=sg[:], in_=gm[:], func=AF.Sigmoid, scale=1.702), reads=[tgm], writes=[tsg])
                                    S.op("dve", lambda e, lm=lm, a=a, bl=bl: e.tensor_scalar(out=lm[:], in0=plb[a][:], scalar1=bl, scalar2=8.0, op0=ALU.add, op1=ALU.min),
                                         reads=[T_plb[a], T_misc], writes=[tlm])
                                    S.op("pool", lambda e, gm=gm, sg=sg, tt=tt: e.tensor_tensor(out=tt[:], in0=gm[:], in1=sg[:], op=ALU.mult), reads=[tgm, tsg], writes=[ttt])
                                    S.op("dve", lambda e, lm=lm, tt=tt, j=j, tg=tg: e.scalar_tensor_tensor(
                                        out=bufA[:, j, 1024 + tg * 512:1024 + (tg + 1) * 512], in0=lm[:], scalar=-6.0, in1=tt[:], op0=ALU.max, op1=ALU.mult),
                                        reads=[tlm, ttt], writes=[T_actT[j][tg]])
                                flush_casts()
                                if j < 4:
                                    load_w2chunk(ex_i, 2 * j)
                                    load_w2chunk(ex_i, 2 * j + 1)
                                load_piece(ex_i * 8 + j + NSLOT)
                            flush_casts()
                            for il, i in enumerate(tiles):
                                for nh in range(2):
                                    a = n2 % 2
                                    n2 += 1
                                    for k in range(8):
                                        S.op("pe", lambda e, k=k, a=a, il=il, nh=nh: e.matmul(
                                            pyb[a][:], lhsT=bufA[:, k, 1024 + il * 128:1024 + (il + 1) * 128], rhs=w2b[:, k, nh * 512:(nh + 1) * 512],
                                            start=(k == 0), stop=(k == 7)),
                                            reads=[T_actT[k][il // 4], T_w2b], writes=[T_pyb[a]], signal=(k == 7))
                                    S.op("dve", lambda e, a=a, il=il, i=i, nh=nh: e.scalar_tensor_tensor(
                                        out=acc[:, i, nh * 512:(nh + 1) * 512], in0=pyb[a][:], scalar=gates[:, il, ex_i:ex_i + 1], in1=acc[:, i, nh * 512:(nh + 1) * 512],
                                        op0=ALU.mult, op1=ALU.add),
                                        reads=[T_pyb[a], T_gates[il], T_acc[i][nh]], writes=[T_acc[i][nh]])
                        for i in tiles:
                            to = Tok("out%d" % i)
                            S.dma("sp", "st_out", lambda e, i=i: e.dma_start(out=out[i * 128:(i + 1) * 128, :], in_=acc[:, i, :]), reads=[T_acc[i][0], T_acc[i][1]], writes=[to])
                            out_toks.append(to)
                    S.barrier()

        S.final_wait("sp", out_toks)
    return nc, dbg_outs


def _consts():
    s = np.arange(128)
    mid = 63
    amat = np.zeros((128, 130), np.float32)
    amat[:, :128] = (s[:, None] <= s[None, :]).astype(np.float32) - (s[:, None] <= mid).astype(np.float32)
    amat[:, 128] = (s <= mid).astype(np.float32)
    amat[:, 129] = 1.0
    tri = (s[None, :] >= s[:, None]).astype(np.float32)
    bones = np.zeros((128, 128), np.float32)
    bones[:64, :64] = 1.0
    bones[64:, 64:] = 1.0
    return {
        "identb": np.eye(128, dtype=np.float32).astype(NPBF),
        "identf": np.eye(128, dtype=np.float32),
        "amat": amat,
        "tri": tri,
        "bones": bones.astype(NPBF),
    }


def _prep_shared(inp):
    f = lambda a: np.ascontiguousarray(np.asarray(a, dtype=np.float32))
    w1 = f(inp["w1"])[0]
    w1p = np.ascontiguousarray(np.concatenate([w1[:, :, 0::2], w1[:, :, 1::2]], axis=2))
    b1 = f(inp["b1"])[0]
    b1p = np.concatenate([b1[:, 0::2], b1[:, 1::2]], axis=1)
    b1F = np.ascontiguousarray(b1p.reshape(NE, 16, 128).transpose(2, 0, 1).reshape(128, NE * 16))
    sh = {
        "w_ada": f(inp["w_ada"])[0],
        "badaF": np.ascontiguousarray(f(inp["b_ada"])[0].reshape(48, 128).T),
        "bada_row": f(inp["b_ada"])[0].reshape(1, -1),
        "gmixF": np.ascontiguousarray(f(inp["mix_norm_g"])[0].reshape(8, 128).T),
        "gffnF": np.ascontiguousarray(f(inp["ffn_norm_g"])[0].reshape(8, 128).T),
        "w_in": f(inp["w_in"])[0],
        "lbl": f(inp["hg_lower_bound_logits"]).reshape(1, 1024),
        "hgn": f(inp["hg_out_norm_g"]).reshape(1, 128),
        "gq": np.tile(f(inp["da_q_norm_g"]).reshape(64), 2).reshape(128, 1),
        "gk": np.tile(f(inp["da_k_norm_g"]).reshape(64), 2).reshape(128, 1),
        "lam4": np.concatenate([f(inp["da_lambda_q1"]).reshape(64), f(inp["da_lambda_k1"]).reshape(64),
                                f(inp["da_lambda_q2"]).reshape(64), f(inp["da_lambda_k2"]).reshape(64)]).reshape(1, 256),
        "subg": f(inp["da_subln_g"]).reshape(1, 128),
        "w_out": f(inp["w_out"])[0],
        "w_r": f(inp["w_router"])[0],
        "b_r": f(inp["b_router"]).reshape(1, NE),
        "w1p": w1p,
        "b1F": b1F,
        "w2": f(inp["w2"])[0],
        "b2": f(inp["b2"])[0],
    }
    sh.update(_consts())
    return sh


def _prep_core(inp, sh, b):
    m = dict(sh)
    m["x"] = np.ascontiguousarray(np.asarray(inp["x"], dtype=np.float32)[b])
    m["cT"] = np.ascontiguousarray(np.asarray(inp["c"], dtype=np.float32)[b].reshape(8, 128).T)
    return m


def kernel(**inputs):
    nc, _ = build_nc()
    sh = _prep_shared(inputs)
    in_maps = [_prep_core(inputs, sh, b) for b in range(8)]
    res = run_bass_kernel_spmd(nc, in_maps, core_ids=list(range(8)))
    return np.stack([np.asarray(r["out"], dtype=np.float32) for r in res.results], axis=0)
```

```python
import numpy as np
import ml_dtypes
from contextlib import ExitStack
import concourse.bass as bass
import concourse.mybir as mybir
from concourse.bass_utils import run_bass_kernel_spmd

F32 = mybir.dt.float32
BF16 = mybir.dt.bfloat16
AF = mybir.ActivationFunctionType
ALU = mybir.AluOpType
AX = mybir.AxisListType
NPBF = ml_dtypes.bfloat16

S_TOK = 2048
D = 1024
NT = 16
NE = 32
EPS = 1e-6
LAMBDA_INIT = 0.2


class Tok:
    __slots__ = ("name", "w", "r")

    def __init__(self, name=""):
        self.name = name
        self.w = None
        self.r = []


class Sched:
    def __init__(self, nc, stack, same_engine_sync=True):
        self.nc = nc
        self.stack = stack
        self.eng = {"pe": nc.tensor, "act": nc.scalar, "dve": nc.vector, "pool": nc.gpsimd, "sp": nc.sync}
        self.sems = {}
        self.cnt = {}
        self.waited = {e: {} for e in self.eng}
        self.same = same_engine_sync
        self.n_inst = 0
        self.n_wait = 0

    def sem(self, key):
        if key not in self.sems:
            self.sems[key] = self.stack.enter_context(self.nc.semaphore("s_%s" % key))
            self.cnt[key] = 0
        return self.sems[key]

    def _deps(self, reads, writes):
        deps = {}

        def add(t):
            if t is None:
                return
            k, v = t
            if deps.get(k, 0) < v:
                deps[k] = v
        for t in reads:
            add(t.w)
        for t in writes:
            add(t.w)
            for r in t.r:
                add(r)
        return deps

    def _emit_waits(self, e, deps):
        eng = self.eng[e]
        for k, v in deps.items():
            if k == e and (e == "pe" or not self.same):
                continue
            if self.waited[e].get(k, 0) >= v:
                continue
            eng.wait_ge(self.sems[k], v)
            self.waited[e][k] = v
            self.n_wait += 1

    def op(self, e, fn, reads=(), writes=(), signal=True):
        self.sem(e)
        deps = self._deps(reads, writes)
        self._emit_waits(e, deps)
        ins = fn(self.eng[e])
        self.n_inst += 1
        if signal:
            self.cnt[e] += 1
            ins.then_inc(self.sems[e], 1)
            tk = (e, self.cnt[e])
        else:
            tk = (e, self.cnt[e] + 1)
        for t in writes:
            t.w = tk
            t.r = []
        for t in reads:
            if len(t.r) > 64:
                best = {}
                for k, v in t.r:
                    if best.get(k, 0) < v:
                        best[k] = v
                t.r = list(best.items())
            t.r.append(tk)
        return tk

    def dma(self, q, semkey, fn, reads=(), writes=()):
        self.sem(semkey)
        deps = self._deps(reads, writes)
        self._emit_waits(q, deps) if False else None
        eng = self.eng[q]
        for k, v in deps.items():
            if self.waited[q].get(k, 0) >= v:
                continue
            eng.wait_ge(self.sems[k], v)
            self.waited[q][k] = v
            self.n_wait += 1
        ins = fn(eng)
        self.n_inst += 1
        self.cnt[semkey] += 16
        ins.then_inc(self.sems[semkey], 16)
        tk = (semkey, self.cnt[semkey])
        for t in writes:
            t.w = tk
            t.r = []
        for t in reads:
            t.r.append(tk)
        return tk

    def barrier(self):
        for e in self.eng:
            for k, v in self.cnt.items():
                if v == 0 or k == e and e == "pe":
                    continue
                if self.waited[e].get(k, 0) >= v:
                    continue
                self.eng[e].wait_ge(self.sems[k], v)
                self.waited[e][k] = v
                self.n_wait += 1

    def final_wait(self, q, toks):
        deps = {}
        for t in toks:
            if t.w is not None:
                k, v = t.w
                deps[k] = max(deps.get(k, 0), v)
        for k, v in deps.items():
            self.eng[q].wait_ge(self.sems[k], v)


class Ring:
    def __init__(self, bufs, name):
        self.bufs = bufs
        self.toks = [Tok("%s%d" % (name, i)) for i in range(len(bufs))]
        self.i = 0

    def next(self):
        b, t = self.bufs[self.i], self.toks[self.i]
        self.i = (self.i + 1) % len(self.bufs)
        return b, t


def interleave(*gens):
    gens = [g for g in gens if g is not None]
    while gens:
        for g in list(gens):
            try:
                next(g)
            except StopIteration:
                gens.remove(g)


def build_nc(stop_after="all", dbg=()):
    nc = bass.Bass("TRN2", target_bir_lowering=False)

    def din(name, shape, dt=F32):
        return nc.dram_tensor(name, list(shape), dt, kind="ExternalInput").ap()

    x = din("x", [S_TOK, D])
    cT = din("cT", [128, 8])
    w_ada = din("w_ada", [D, 6 * D])
    badaF = din("badaF", [128, 48])
    bada_row = din("bada_row", [1, 6 * D])
    gmixF = din("gmixF", [128, 8])
    gffnF = din("gffnF", [128, 8])
    w_in = din("w_in", [D, 5632])
    lbl = din("lbl", [1, 1024])
    hgn = din("hgn", [1, 128])
    gq = din("gq", [128, 1])
    gk = din("gk", [128, 1])
    lam4 = din("lam4", [1, 256])
    subg = din("subg", [1, 128])
    w_out = din("w_out", [D, D])
    w_r = din("w_r", [D, NE])
    b_r = din("b_r", [1, NE])
    w1p = din("w1p", [NE, D, 2 * D])
    b1F = din("b1F", [128, NE * 16])
    w2 = din("w2", [NE, D, D])
    b2 = din("b2", [NE, D])
    identb_d = din("identb", [128, 128], BF16)
    identf_d = din("identf", [128, 128])
    amat_d = din("amat", [128, 130])
    tri_d = din("tri", [128, 128])
    bones_d = din("bones", [128, 128], BF16)
    out = nc.dram_tensor("out", [S_TOK, D], F32, kind="ExternalOutput").ap()
    dbg_outs = {}

    with ExitStack() as st:
        S = Sched(nc, st)
        out_toks = []

        def sbuf(stack, name, shape, dt):
            return stack.enter_context(nc.sbuf_tensor("sb_" + name, list(shape), dt))

        def psum(stack, name, shape, dt):
            return stack.enter_context(nc.psum_tensor("ps_" + name, list(shape), dt))

        ld_i = [0]

        def load(dst, src, tok, q="sp"):
            ld_i[0] += 1
            return S.dma(q, "ld_%s" % tok.name, lambda e: e.dma_start(out=dst, in_=src), writes=[tok])

        def dump(name, ap, tok, shape, dt):
            d = nc.dram_tensor("dbg_" + name, list(shape), dt, kind="ExternalOutput").ap()
            t = Tok("dbgo_" + name)
            S.dma("sp", "dbg_" + name, lambda e: e.dma_start(out=d, in_=ap), reads=[tok], writes=[t])
            out_toks.append(t)
            dbg_outs[name] = d

        identb = sbuf(st, "identb", [128, 128], BF16)
        identf = sbuf(st, "identf", [128, 128], F32)
        amat = sbuf(st, "amat", [128, 130], F32)
        tri = sbuf(st, "tri", [128, 128], F32)
        bones = sbuf(st, "bones", [128, 128], BF16)
        T_c = Tok("consts")
        for (d_, s_, nm) in [(identb, identb_d, "c0"), (identf, identf_d, "c1"), (amat, amat_d, "c2"), (tri, tri_d, "c3"),
                             (bones, bones_d, "c4")]:
            S.dma("sp", "ld_c", lambda e, d_=d_, s_=s_: e.dma_start(out=d_[:], in_=s_[:, :]), writes=[])
        T_c.w = ("ld_c", S.cnt["ld_c"])

        adaF = sbuf(st, "adaF", [128, 6, 8], F32)
        a1 = sbuf(st, "a1", [128, 8], F32)
        a2 = sbuf(st, "a2", [128, 8], F32)
        g1bc = sbuf(st, "g1bc", [128, D], F32)
        g2bc = sbuf(st, "g2bc", [128, D], F32)
        gnbc = sbuf(st, "gnbc", [128, 128], F32)
        sg8 = sbuf(st, "sg8", [128, 128], F32)
        gqs = sbuf(st, "gqs", [128, 1], F32)
        gks = sbuf(st, "gks", [128, 1], F32)
        nlam = sbuf(st, "nlam", [128, 1], F32)
        brbc = sbuf(st, "brbc", [128, NE], F32)
        b1s = sbuf(st, "b1s", [128, NE * 16], F32)
        bufA = sbuf(st, "bufA", [128, 8, S_TOK], BF16)
        bufB = sbuf(st, "bufB", [128, 8, S_TOK], BF16)
        T_ada = Tok("ada")
        T_a1 = Tok("a1")
        T_a2 = Tok("a2")
        T_g1 = Tok("g1")
        T_g2 = Tok("g2")
        T_misc = Tok("misc")
        T_in0 = Tok("in0")
        T_hT = [Tok("hT%d" % g) for g in range(4)]
        T_mixA = [Tok("mixA%d" % i) for i in range(NT)]
        T_mixD = [Tok("mixD%d" % i) for i in range(NT)]
        hT = bufA
        mixT = bufB

        def norm_transpose(stack, tag, tile_srcs, dstT, a_sc, sh_idx, T_a, T_dst_groups, src_is_dram, nxn=5, split=False, q="sp"):
            xt_r = Ring([sbuf(stack, "%s_xt%d" % (tag, i), [128, D], F32) for i in range(2)], tag + "xt") if src_is_dram else None
            xn_r = Ring([sbuf(stack, "%s_xn%d" % (tag, i), [128, D], BF16) for i in range(nxn)], tag + "xn")
            ssq = sbuf(stack, tag + "_ssq", [128, 64], F32)
            pT = [psum(stack, "%s_pT%d" % (tag, i), [128, 512], BF16) for i in range(2)]
            T_pT = [Tok(tag + "pT0"), Tok(tag + "pT1")]
            T_ssq = Tok(tag + "ssq")
            npt = [0]
            ntile = len(tile_srcs)

            def pre(i):
                src, tsrc = tile_srcs[i]
                if src_is_dram:
                    xt, txt = xt_r.next()
                    S.dma(q, "ld_" + txt.name, lambda e: e.dma_start(out=xt[:], in_=src), writes=[txt])
                    xin, tin = xt[:], [txt]
                else:
                    xin, tin = src, list(tsrc)
                xn, txn = xn_r.next()
                S.op("act", lambda e: e.activation(out=xn[:], in_=xin, func=AF.Square, accum_out=ssq[:, i:i + 1]), reads=tin, writes=[txn, T_ssq])
                S.op("act", lambda e: e.activation(out=ssq[:, 32 + i:33 + i], in_=ssq[:, i:i + 1], func=AF.Ln, scale=1.0 / D, bias=epsb[:, 0:1]),
                     reads=[T_ssq, T_eps], writes=[T_ssq])
                S.op("act", lambda e: e.activation(out=ssq[:, 32 + i:33 + i], in_=ssq[:, 32 + i:33 + i], func=AF.Exp, scale=-0.5), reads=[T_ssq], writes=[T_ssq])
                S.op("dve", lambda e: e.tensor_scalar(out=xn[:], in0=xin, scalar1=ssq[:, 32 + i:33 + i], scalar2=None, op0=ALU.mult),
                     reads=tin + [T_ssq], writes=[txn])
                return (xn, txn)

            def post(g, xns):
                for k in range(8):
                    p, tp = pT[npt[0] % 2], T_pT[npt[0] % 2]
                    npt[0] += 1
                    for ii in range(4):
                        xn, txn = xns[ii]
                        S.op("pe", lambda e: e.transpose(p[:, ii * 128:(ii + 1) * 128], xn[:, k * 128:(k + 1) * 128], identb[:]),
                             reads=[txn, T_c], writes=[tp], signal=(ii == 3))
                    S.op("act", lambda e: e.activation(out=dstT[:, k, g * 512:(g + 1) * 512], in_=p[:], func=AF.Identity,
                                                       scale=a_sc[:, k:k + 1], bias=adaF[:, sh_idx, k:k + 1]),
                         reads=[tp, T_a, T_ada], writes=[T_dst_groups[g]])
            if split:
                allx = [pre(i) for i in range(ntile)]
                yield
                for g in range(ntile // 4):
                    post(g, allx[g * 4:g * 4 + 4])
            else:
                for g in range(ntile // 4):
                    xns = [pre(g * 4 + ii) for ii in range(4)]
                    post(g, xns)
                    yield

        epsb = sbuf(st, "epsb", [128, 4], F32)
        T_eps = Tok("eps")
        S.op("dve", lambda e: e.memset(epsb[:, 0:1], EPS), writes=[T_eps])
        S.op("dve", lambda e: e.memset(epsb[:, 1:2], 64 * EPS), writes=[T_eps])
        S.op("dve", lambda e: e.memset(epsb[:, 2:3], 1.0), writes=[T_eps])

        with ExitStack() as sa:
            cTs = sbuf(sa, "cTs", [128, 8], F32)
            cact = sbuf(sa, "cact", [128, 8], F32)
            cbc = sbuf(sa, "cbc", [128, 8, 128], F32)
            wa = [sbuf(sa, "wa%d" % i, [128, 8, 1024], F32) for i in range(2)]
            T_wa = [Tok("wa0"), Tok("wa1")]
            badaFs = sbuf(sa, "badaFs", [128, 48], F32)
            bgrow = sbuf(sa, "bgrow", [128, 2, 1024], F32)
            gmixs = sbuf(sa, "gmixs", [128, 8], F32)
            gffns = sbuf(sa, "gffns", [128, 8], F32)
            lam4bc = sbuf(sa, "lam4bc", [128, 256], F32)
            lamtmp = sbuf(sa, "lamtmp", [128, 128], F32)
            lams = sbuf(sa, "lams", [128, 2], F32)
            subgbc = sbuf(sa, "subgbc", [128, 128], F32)
            pA = psum(sa, "pA", [128, 512], F32)
            pG = psum(sa, "pG", [128, 512], F32)
            T_pA, T_pG = Tok("pA"), Tok("pG")
            T_in = Tok("smallin")
            smalls = [(cTs[:], cT[:, :]), (badaFs[:], badaF[:, :]), (gmixs[:], gmixF[:, :]), (gffns[:], gffnF[:, :]),
                      (gqs[:], gq[:, :]), (gks[:], gk[:, :]), (b1s[:], b1F[:, :]),
                      (lam4bc[:], lam4[0:1, :].partition_broadcast(128)),
                      (subgbc[:], subg[0:1, :].partition_broadcast(128)),
                      (gnbc[:], hgn[0:1, :].partition_broadcast(128)),
                      (brbc[:], b_r[0:1, :].partition_broadcast(128)),
                      (bgrow[:, 0, :], bada_row[0:1, 2 * D:3 * D].partition_broadcast(128)),
                      (bgrow[:, 1, :], bada_row[0:1, 5 * D:6 * D].partition_broadcast(128))]
            for d_, s_ in smalls:
                S.dma("sp", "ld_s", lambda e, d_=d_, s_=s_: e.dma_start(out=d_, in_=s_), writes=[])
            T_in.w = ("ld_s", S.cnt["ld_s"])
            T_in0.w = T_in.w

            T_cact = Tok("cact")
            S.op("act", lambda e: e.activation(out=cact[:], in_=cTs[:], func=AF.Silu), reads=[T_in], writes=[T_cact])
            T_cbc = Tok("cbc")
            S.op("dve", lambda e: e.tensor_copy(out=cbc[:], in_=cact[:].unsqueeze(2).to_broadcast([128, 8, 128])),
                 reads=[T_cact], writes=[T_cbc])
            S.op("dve", lambda e: e.tensor_scalar(out=sg8[:], in0=subgbc[:], scalar1=1.0 - LAMBDA_INIT, scalar2=None, op0=ALU.mult),
                 reads=[T_in], writes=[T_misc])
            b1v = b1s[:].rearrange("p (e j) -> p e j", j=16)
            S.op("dve", lambda e: e.tensor_scalar(out=b1v[:, :, 8:16], in0=b1v[:, :, 8:16], scalar1=1.0, scalar2=None, op0=ALU.add),
                 reads=[T_in], writes=[T_misc])
            S.op("dve", lambda e: e.tensor_tensor(out=lamtmp[:, 0:64], in0=lam4bc[:, 0:64], in1=lam4bc[:, 64:128], op=ALU.mult),
                 reads=[T_in], writes=[T_misc])
            S.op("dve", lambda e: e.tensor_tensor(out=lamtmp[:, 64:128], in0=lam4bc[:, 128:192], in1=lam4bc[:, 192:256], op=ALU.mult),
                 reads=[T_in], writes=[T_misc])
            S.op("dve", lambda e: e.reduce_sum(out=lams[:], in_=lamtmp[:].rearrange("p (a b) -> p a b", a=2), axis=AX.X),
                 reads=[T_misc], writes=[T_misc])
            S.op("act", lambda e: e.activation(out=lams[:], in_=lams[:], func=AF.Exp), reads=[T_misc], writes=[T_misc])
            S.op("dve", lambda e: e.tensor_tensor(out=nlam[:], in0=lams[:, 1:2], in1=lams[:, 0:1], op=ALU.subtract),
                 reads=[T_misc], writes=[T_misc])
            S.op("dve", lambda e: e.tensor_scalar(out=nlam[:], in0=nlam[:], scalar1=-LAMBDA_INIT, scalar2=None, op0=ALU.add),
                 reads=[T_misc], writes=[T_misc])

            srcsB = [(x[i * 128:(i + 1) * 128, :], None) for i in range(NT)]
            genB = norm_transpose(sa, "B", srcsB, hT, a1, 0, T_a1, T_hT, True, nxn=NT, split=True, q="act")
            next(genB)
            wav = w_ada.rearrange("(k p) n -> p k n", p=128)
            for j in range(6):
                wb, tw = wa[j % 2], T_wa[j % 2]
                S.dma("sp", "ld_wa%d" % (j % 2), lambda e, wb=wb, j=j: e.dma_start(out=wb[:, 0:4, :], in_=wav[:, 0:4, j * D:(j + 1) * D]),
                      writes=[tw])
                S.dma("sp", "ld_wa%d" % (j % 2), lambda e, wb=wb, j=j: e.dma_start(out=wb[:, 4:8, :], in_=wav[:, 4:8, j * D:(j + 1) * D]),
                      writes=[])
                tw.w = ("ld_wa%d" % (j % 2), S.cnt["ld_wa%d" % (j % 2)])
                if j in (2, 5):
                    gdst = g1bc if j == 2 else g2bc
                    tg = T_g1 if j == 2 else T_g2
                    for half in range(2):
                        for k in range(8):
                            S.op("pe", lambda e, wb=wb, k=k, half=half: e.matmul(
                                pG[:], lhsT=cbc[:, k, :], rhs=wb[:, k, half * 512:(half + 1) * 512], start=(k == 0), stop=(k == 7)),
                                reads=[T_cbc, tw], writes=[T_pG], signal=(k == 7))
                        S.op("dve", lambda e, gdst=gdst, half=half, j=j: e.tensor_tensor(
                            out=gdst[:, half * 512:(half + 1) * 512], in0=pG[:], in1=bgrow[:, 0 if j == 2 else 1, half * 512:(half + 1) * 512],
                            op=ALU.add), reads=[T_pG, T_in], writes=[tg])
                else:
                    for m in range(8):
                        for k in range(8):
                            S.op("pe", lambda e, wb=wb, k=k, m=m: e.matmul(
                                pA[:, m:m + 1], lhsT=wb[:, k, m * 128:(m + 1) * 128], rhs=cact[:, k:k + 1], start=(k == 0), stop=(k == 7)),
                                reads=[T_cact, tw], writes=[T_pA], signal=(k == 7))
                    S.op("dve", lambda e, j=j: e.tensor_tensor(out=adaF[:, j, :], in0=pA[:, 0:8], in1=badaFs[:, j * 8:(j + 1) * 8], op=ALU.add),
                         reads=[T_pA, T_in], writes=[T_ada])
            S.op("dve", lambda e: e.scalar_tensor_tensor(out=a1[:], in0=adaF[:, 1, :], scalar=1.0, in1=gmixs[:], op0=ALU.add, op1=ALU.mult),
                 reads=[T_ada, T_in], writes=[T_a1])
            S.op("dve", lambda e: e.scalar_tensor_tensor(out=a2[:], in0=adaF[:, 4, :], scalar=1.0, in1=gffns[:], op0=ALU.add, op1=ALU.mult),
                 reads=[T_ada, T_in], writes=[T_a2])
            for _ in genB:
                pass
            if "ada" in dbg:
                dump("adaF", adaF[:].rearrange("p a b -> p (a b)"), T_ada, [128, 48], F32)
                dump("g1bc", g1bc[:], T_g1, [128, D], F32)
                dump("g2bc", g2bc[:], T_g2, [128, D], F32)
                dump("nlam", nlam[:], T_misc, [128, 1], F32)

        S.barrier()
        if "hT" in dbg:
            for g in range(4):
                dump("hT%d" % g, hT[:, :, g * 512:(g + 1) * 512], T_hT[g], [128, 8, 512], BF16)

        if stop_after == "B":
            S.final_wait("sp", out_toks)
            return nc, dbg_outs

        with ExitStack() as s1:
            wblk2 = sbuf(s1, "wblk2", [128, 8, 1536], BF16)
            T_wblk2 = Tok("wblk2")
            wst_r = Ring([sbuf(s1, "wst%d" % i, [128, 2048], F32) for i in range(2)], "wst")

            def load_wblk(src2d, col0, W, dst, T_dst, dcol0=0, eng="pool"):
                srcv = src2d.rearrange("(k p) n -> p k n", p=128)
                for k in range(8):
                    stg, tst = wst_r.next()
                    S.dma("sp", "ld_" + tst.name, lambda e, stg=stg, k=k: e.dma_start(out=stg[:, 0:W], in_=srcv[:, k, col0:col0 + W]), writes=[tst])
                    en = eng if eng != "alt" else ("act" if k % 2 == 0 else "dve")
                    if en == "act":
                        S.op("act", lambda e, stg=stg, k=k: e.copy(out=dst[:, k, dcol0:dcol0 + W], in_=stg[:, 0:W]), reads=[tst], writes=[T_dst])
                    else:
                        S.op(en, lambda e, stg=stg, k=k: e.tensor_copy(out=dst[:, k, dcol0:dcol0 + W], in_=stg[:, 0:W]), reads=[tst], writes=[T_dst])

            with ExitStack() as sh:
                wblk = sbuf(sh, "wblk", [128, 8, 2048], BF16)
                T_wblk = Tok("wblk")
                load_wblk(w_in, 0, 2048, wblk, T_wblk, eng="alt")
                lblbc = sbuf(sh, "lblbc", [128, 1024], F32)
                lbbc = sbuf(sh, "lbbc", [128, 512], F32)
                omlbc = sbuf(sh, "omlbc", [128, 512], F32)
                T_lb = Tok("lb")
                S.dma("sp", "ld_lb", lambda e: e.dma_start(out=lblbc[:], in_=lbl[0:1, :].partition_broadcast(128)), writes=[T_lb])
                S.op("dve", lambda e: e.tensor_tensor(out=lbbc[:], in0=lblbc[:, 0:512], in1=lblbc[:, 512:1024], op=ALU.subtract), reads=[T_lb], writes=[T_lb])
                S.op("act", lambda e: e.activation(out=lbbc[:], in_=lbbc[:], func=AF.Sigmoid), reads=[T_lb], writes=[T_lb])
                S.op("dve", lambda e: e.tensor_scalar(out=omlbc[:], in0=lbbc[:], scalar1=-1.0, scalar2=1.0, op0=ALU.mult, op1=ALU.add), reads=[T_lb], writes=[T_lb])
                load_wblk(w_in, 2048, 1536, wblk2, T_wblk2, eng="pool")
                pq = psum(sh, "pq", [128, 512], F32); pf = psum(sh, "pf", [128, 512], F32)
                pi_ = psum(sh, "pi", [128, 512], F32); pog = psum(sh, "pog", [128, 512], F32)
                pT = psum(sh, "pT", [128, 1024], BF16); pT2 = psum(sh, "pT2", [128, 1024], BF16)
                po = psum(sh, "po", [128, 512], F32); pkv = psum(sh, "pkv", [128, 512], F32)
                psc = pf
                T_pq, T_pf, T_pi, T_pog, T_pT, T_pT2, T_po, T_pkv = [Tok(n) for n in "pq pf pi pog pT pT2 po pkv".split()]
                T_psc = T_pf
                def f32t(n): return sbuf(sh, n, [128, 512], F32), Tok(n)
                def bft(n, w=512): return sbuf(sh, n, [128, w], BF16), Tok(n)
                fs, T_fs = f32t("fs"); lf, T_lf = f32t("lf"); kk, T_kk = f32t("kk"); qs, T_qs = f32t("qs")
                eb, T_eb = f32t("eb"); enb, T_enb = f32t("enb")
                Sst, T_Sst = f32t("Sst"); tmpkv, T_tmpkv = f32t("tmpkv"); onf, T_onf = f32t("onf")
                qt, T_qt = bft("qt"); Stb, T_Stb = bft("Stb"); oa, T_oa = bft("oa")
                ogs_r = Ring([sbuf(sh, "ogs%d" % a, [128, 512], F32) for a in range(2)], "ogs")
                kt_r = Ring([sbuf(sh, "kt%d" % a, [128, 512], BF16) for a in range(2)], "kt")
                vb_r = Ring([sbuf(sh, "vb%d" % a, [128, 512], BF16) for a in range(2)], "vb")
                qkT_r = Ring([sbuf(sh, "qkT%d" % a, [128, 1024], BF16) for a in range(2)], "qkT")
                scm_r = Ring([sbuf(sh, "scm%d" % a, [128, 512], BF16) for a in range(2)], "scm")
                ebm_r = Ring([sbuf(sh, "ebm%d" % a, [128, 3, 4], F32) for a in range(2)], "ebm")
                bm3 = sbuf(sh, "bm3", [128, 3, 4], F32); T_bm3 = Tok("bm3")
                ssq4 = sbuf(sh, "ssq4", [128, 4], F32); T_ssq4 = Tok("ssq4")
                v4 = lambda ap: ap.rearrange("p (h v) -> p h v", h=4)
                hctx = {}

                def front(i):
                    tc0, tc1 = i * 128, (i + 1) * 128
                    g = i // 4
                    ogs, T_ogs = ogs_r.next(); kt, T_kt = kt_r.next(); vb, T_vb = vb_r.next()
                    qkT, T_qkT = qkT_r.next(); scm, T_scm = scm_r.next(); ebm, T_ebm = ebm_r.next()
                    hctx[i] = (ogs, T_ogs, kt, T_kt, vb, T_vb, qkT, T_qkT, scm, T_scm, ebm, T_ebm)
                    for (p, tp, c0) in [(pf, T_pf, 512), (pq, T_pq, 0), (pi_, T_pi, 1024), (pog, T_pog, 1536)]:
                        for k in range(8):
                            S.op("pe", lambda e, p=p, k=k, c0=c0: e.matmul(p[:], lhsT=hT[:, k, tc0:tc1], rhs=wblk[:, k, c0:c0 + 512], start=(k == 0), stop=(k == 7)),
                                 reads=[T_hT[g], T_wblk], writes=[tp], signal=(k == 7))
                        yield
                    S.op("act", lambda e: e.activation(out=fs[:], in_=pf[:], func=AF.Exp, scale=-1.0), reads=[T_pf], writes=[T_fs]); yield
                    S.op("act", lambda e: e.activation(out=fs[:], in_=fs[:], func=AF.Identity, bias=epsb[:, 2:3]), reads=[T_fs, T_eps], writes=[T_fs]); yield
                    S.op("dve", lambda e: e.reciprocal(out=fs[:], in_=fs[:]), reads=[T_fs], writes=[T_fs]); yield
                    S.op("dve", lambda e: e.tensor_tensor(out=fs[:], in0=fs[:], in1=omlbc[:], op=ALU.mult), reads=[T_fs, T_lb], writes=[T_fs]); yield
                    S.op("dve", lambda e: e.tensor_tensor(out=fs[:], in0=fs[:], in1=lbbc[:], op=ALU.add), reads=[T_fs, T_lb], writes=[T_fs]); yield
                    S.op("act", lambda e: e.activation(out=lf[:], in_=fs[:], func=AF.Ln), reads=[T_fs], writes=[T_lf]); yield
                    S.op("act", lambda e: e.activation(out=kk[:], in_=fs[:], func=AF.Identity, scale=-1.0, bias=epsb[:, 2:3]), reads=[T_fs, T_eps], writes=[T_kk]); yield
                    S.op("pe", lambda e: e.matmul(pf[:], lhsT=amat[:, 0:128], rhs=lf[:], start=True, stop=True), reads=[T_lf, T_c], writes=[T_pf]); yield
                    S.op("act", lambda e: e.activation(out=qs[:], in_=pq[:], func=AF.Exp, scale=-1.0), reads=[T_pq], writes=[T_qs]); yield
                    S.op("act", lambda e: e.activation(out=qs[:], in_=qs[:], func=AF.Identity, bias=epsb[:, 2:3]), reads=[T_qs, T_eps], writes=[T_qs]); yield
                    S.op("dve", lambda e: e.reciprocal(out=qs[:], in_=qs[:]), reads=[T_qs], writes=[T_qs]); yield
                    S.op("dve", lambda e: e.tensor_tensor(out=qs[:], in0=qs[:], in1=pq[:], op=ALU.mult), reads=[T_qs, T_pq], writes=[T_qs]); yield
                    for h in range(4):
                        S.op("pe", lambda e, h=h: e.matmul(pq[:, 2 * h:2 * h + 2], lhsT=lf[:, h * 128:(h + 1) * 128], rhs=amat[:, 128:130], start=True, stop=True),
                             reads=[T_lf, T_c], writes=[T_pq], signal=(h == 3))
                    yield
                    S.op("act", lambda e: e.copy(out=vb[:], in_=pi_[:]), reads=[T_pi], writes=[T_vb]); yield
                    S.op("act", lambda e: e.activation(out=ogs[:], in_=pog[:], func=AF.Exp, scale=-1.0), reads=[T_pog], writes=[T_ogs]); yield
                    S.op("act", lambda e: e.activation(out=ogs[:], in_=ogs[:], func=AF.Identity, bias=epsb[:, 2:3]), reads=[T_ogs, T_eps], writes=[T_ogs]); yield
                    S.op("dve", lambda e: e.reciprocal(out=ogs[:], in_=ogs[:]), reads=[T_ogs], writes=[T_ogs]); yield
                    S.op("dve", lambda e: e.tensor_tensor(out=ogs[:], in0=ogs[:], in1=pog[:], op=ALU.mult), reads=[T_ogs, T_pog], writes=[T_ogs]); yield
                    S.op("act", lambda e: e.activation(out=eb[:], in_=pf[:], func=AF.Exp), reads=[T_pf], writes=[T_eb]); yield
                    S.op("act", lambda e: e.activation(out=enb[:], in_=pf[:], func=AF.Exp, scale=-1.0), reads=[T_pf], writes=[T_enb]); yield
                    S.op("dve", lambda e: e.tensor_tensor(out=qt[:], in0=qs[:], in1=eb[:], op=ALU.mult), reads=[T_qs, T_eb], writes=[T_qt]); yield
                    S.op("dve", lambda e: e.tensor_tensor(out=kt[:], in0=kk[:], in1=enb[:], op=ALU.mult), reads=[T_kk, T_enb], writes=[T_kt]); yield
                    S.op("dve", lambda e: e.tensor_copy(out=bm3[:, 0:2, :], in_=pq[:, 0:8].rearrange("p (h t) -> p t h", t=2)), reads=[T_pq], writes=[T_bm3]); yield
                    S.op("dve", lambda e: e.tensor_tensor(out=bm3[:, 2, :], in0=bm3[:, 1, :], in1=bm3[:, 0, :], op=ALU.subtract), reads=[T_bm3], writes=[T_bm3]); yield
                    S.op("act", lambda e: e.activation(out=ebm[:].rearrange("p a b -> p (a b)"), in_=bm3[:].rearrange("p a b -> p (a b)"), func=AF.Exp),
                         reads=[T_bm3], writes=[T_ebm]); yield
                    for h in range(4):
                        S.op("pe", lambda e, h=h: e.transpose(pT[:, h * 128:(h + 1) * 128], qt[:, h * 128:(h + 1) * 128], identb[:]),
                             reads=[T_qt, T_c], writes=[T_pT], signal=False)
                    for h in range(4):
                        S.op("pe", lambda e, h=h: e.transpose(pT[:, 512 + h * 128:512 + (h + 1) * 128], kt[:, h * 128:(h + 1) * 128], identb[:]),
                             reads=[T_kt, T_c], writes=[T_pT], signal=(h == 3))
                    yield
                    S.op("act", lambda e: e.copy(out=qkT[:], in_=pT[:]), reads=[T_pT], writes=[T_qkT]); yield
                    for h in range(4):
                        S.op("pe", lambda e, h=h: e.matmul(psc[:, h * 128:(h + 1) * 128], lhsT=qkT[:, 512 + h * 128:512 + (h + 1) * 128],
                                                           rhs=qkT[:, h * 128:(h + 1) * 128], start=True, stop=True),
                             reads=[T_qkT], writes=[T_psc], signal=(h == 3))
                    yield
                    S.op("dve", lambda e: e.tensor_tensor(out=v4(scm[:]), in0=v4(psc[:]), in1=tri[:].unsqueeze(1).to_broadcast([128, 4, 128]), op=ALU.mult),
                         reads=[T_psc, T_c], writes=[T_scm]); yield

                def back(i):
                    tc0, tc1 = i * 128, (i + 1) * 128
                    (ogs, T_ogs, kt, T_kt, vb, T_vb, qkT, T_qkT, scm, T_scm, ebm, T_ebm) = hctx.pop(i)
                    if i > 0:
                        S.op("dve", lambda e: e.tensor_tensor(out=v4(Stb[:]), in0=v4(Sst[:]), in1=ebm[:, 0, :].unsqueeze(2).to_broadcast([128, 4, 128]), op=ALU.mult),
                             reads=[T_Sst, T_ebm], writes=[T_Stb]); yield
                    for h in range(4):
                        hs0, hs1 = h * 128, (h + 1) * 128
                        S.op("pe", lambda e, hs0=hs0, hs1=hs1: e.matmul(pkv[:, hs0:hs1], lhsT=kt[:, hs0:hs1], rhs=vb[:, hs0:hs1], start=True, stop=True),
                             reads=[T_kt, T_vb], writes=[T_pkv], signal=(h == 3))
                    yield
                    S.op("dve", lambda e: e.tensor_tensor(out=v4(tmpkv[:]), in0=v4(pkv[:]), in1=ebm[:, 2, :].unsqueeze(2).to_broadcast([128, 4, 128]), op=ALU.mult),
                         reads=[T_pkv, T_ebm], writes=[T_tmpkv]); yield
                    for h in range(4):
                        hs0, hs1 = h * 128, (h + 1) * 128
                        S.op("pe", lambda e, hs0=hs0, hs1=hs1: e.matmul(po[:, hs0:hs1], lhsT=scm[:, hs0:hs1], rhs=vb[:, hs0:hs1], start=True, stop=(i == 0)),
                             reads=[T_scm, T_vb], writes=[T_po], signal=(i == 0 and h == 3))
                        if i > 0:
                            S.op("pe", lambda e, hs0=hs0, hs1=hs1: e.matmul(po[:, hs0:hs1], lhsT=qkT[:, hs0:hs1], rhs=Stb[:, hs0:hs1], start=False, stop=True),
                                 reads=[T_qkT, T_Stb], writes=[T_po], signal=(h == 3))
                    yield
                    if i == 0:
                        S.op("dve", lambda e: e.tensor_copy(out=Sst[:], in_=tmpkv[:]), reads=[T_tmpkv], writes=[T_Sst]); yield
                    else:
                        S.op("dve", lambda e: e.tensor_tensor(out=v4(Sst[:]), in0=v4(Sst[:]), in1=ebm[:, 1, :].unsqueeze(2).to_broadcast([128, 4, 128]), op=ALU.mult),
                             reads=[T_Sst, T_ebm, T_Stb], writes=[T_Sst]); yield
                        S.op("dve", lambda e: e.tensor_tensor(out=Sst[:], in0=Sst[:], in1=tmpkv[:], op=ALU.add), reads=[T_Sst, T_tmpkv], writes=[T_Sst]); yield
                    S.op("act", lambda e: e.activation(out=onf[:], in_=po[:], func=AF.Square), reads=[T_po], writes=[T_onf]); yield
                    S.op("dve", lambda e: e.reduce_sum(out=ssq4[:], in_=v4(onf[:]), axis=AX.X), reads=[T_onf], writes=[T_ssq4]); yield
                    S.op("act", lambda e: e.activation(out=ssq4[:], in_=ssq4[:], func=AF.Ln, scale=1.0 / 128, bias=epsb[:, 0:1]), reads=[T_ssq4, T_eps], writes=[T_ssq4]); yield
                    S.op("act", lambda e: e.activation(out=ssq4[:], in_=ssq4[:], func=AF.Exp, scale=-0.5), reads=[T_ssq4], writes=[T_ssq4]); yield
                    S.op("dve", lambda e: e.tensor_tensor(out=v4(onf[:]), in0=v4(po[:]), in1=ssq4[:].unsqueeze(2).to_broadcast([128, 4, 128]), op=ALU.mult),
                         reads=[T_po, T_ssq4], writes=[T_onf]); yield
                    S.op("pool", lambda e: e.tensor_tensor(out=v4(onf[:]), in0=v4(onf[:]), in1=gnbc[:].unsqueeze(1).to_broadcast([128, 4, 128]), op=ALU.mult),
                         reads=[T_onf, T_in0], writes=[T_onf]); yield
                    S.op("pool", lambda e: e.tensor_tensor(out=oa[:], in0=onf[:], in1=ogs[:], op=ALU.mult), reads=[T_onf, T_ogs], writes=[T_oa]); yield
                    for h in range(4):
                        S.op("pe", lambda e, h=h: e.transpose(pT2[:, h * 128:(h + 1) * 128], oa[:, h * 128:(h + 1) * 128], identb[:]),
                             reads=[T_oa, T_c], writes=[T_pT2], signal=(h == 3))
                    yield
                    S.op("act", lambda e: e.copy(out=mixT[:, 0:4, tc0:tc1], in_=pT2[:, 0:512].rearrange("p (h v) -> p h v", h=4)),
                         reads=[T_pT2], writes=[T_mixA[i]]); yield

                interleave(front(0))
                for i in range(NT):
                    interleave(front(i + 1) if i + 1 < NT else None, back(i))
                if "oa" in dbg:
                    T_all = Tok("mixAall")
                    T_all.w = T_mixA[NT - 1].w
                    dump("oaT", mixT[:, 0:4, :], T_all, [128, 4, S_TOK], BF16)
            S.barrier()
            if stop_after == "C1":
                S.final_wait("sp", out_toks)
                return nc, dbg_outs

            with ExitStack() as sd:
                pp = [psum(sd, "pp%d" % i, [128, 512], F32) for i in range(2)]
                T_pp = [Tok("pp0"), Tok("pp1")]
                pss = psum(sd, "pss", [128, 512], F32); T_pss = Tok("pss")
                pOb = [psum(sd, "pO%d" % i, [128, 512], F32) for i in range(4)]
                T_pOb = [[Tok("pO%d_%d" % (a, i)) for i in range(2)] for a in range(2)]
                T_pO = [[T_pOb[a][i // 2] for i in range(4)] for a in range(2)]

                def pOap(a, ql, w):
                    c0 = (ql % 2) * 256
                    return pOb[a * 2 + ql // 2][:, c0:c0 + w]
                pTo = psum(sd, "pTo", [128, 512], BF16); T_pTo = Tok("pTo")
                qn = [sbuf(sd, "qnT%d" % i, [128, S_TOK], BF16) for i in range(2)]
                kn = [sbuf(sd, "knT%d" % i, [128, S_TOK], BF16) for i in range(2)]
                T_qn = [[Tok("qn%d_%d" % (i, t)) for t in range(8)] for i in range(2)]
                T_kn = [[Tok("kn%d_%d" % (i, t)) for t in range(8)] for i in range(2)]
                Vext = sbuf(sd, "Vext", [128, NT, 4, 130], BF16); T_V = Tok("Vext")
                sq_r = Ring([sbuf(sd, "sq%d" % i, [128, 512], BF16) for i in range(2)], "sq")
                rs_r = Ring([sbuf(sd, "rs%d" % i, [128, 512], F32) for i in range(2)], "rs")
                PTall = [sbuf(sd, "PTall%d" % i, [128, NT, 512], BF16) for i in range(2)]
                T_PT = [[Tok("PT%d_%d" % (i, j)) for j in range(NT)] for i in range(2)]
                o1 = sbuf(sd, "o1", [128, 4, 128], F32); T_o1 = [Tok("o1_%d" % i) for i in range(4)]
                od_r = Ring([sbuf(sd, "od%d" % i, [128, 128], F32) for i in range(2)], "od")
                odn_r = Ring([sbuf(sd, "odn%d" % i, [128, 128], BF16) for i in range(2)], "odn")
                jk = sbuf(sd, "jk", [128, 128], BF16); T_jk = Tok("jk")
                rr_r = Ring([sbuf(sd, "rr%d" % i, [128, 4], F32) for i in range(8)], "rr")
                npp = [0]

                def nextpp():
                    a = npp[0] % 2
                    npp[0] += 1
                    return pp[a], T_pp[a]
                S.op("dve", lambda e: e.memset(Vext[:, :, :, 128:130], 1.0), writes=[T_V])
                for i in range(NT):
                    p, tp = nextpp()
                    for k in range(8):
                        S.op("pe", lambda e, p=p, k=k, i=i: e.matmul(p[:], lhsT=hT[:, k, i * 128:(i + 1) * 128], rhs=wblk2[:, k, 1024:1536], start=(k == 0), stop=(k == 7)),
                             reads=[T_hT[i // 4], T_wblk2], writes=[tp], signal=(k == 7))
                    S.op("act", lambda e, p=p, i=i: e.copy(out=Vext[:, i, :, 0:128], in_=p[:].rearrange("p (h v) -> p h v", h=4)), reads=[tp], writes=[T_V])

                T_pssA, T_pssB = Tok("pssA"), Tok("pssB")

                def proj_chunk(h, n):
                    t8, which = divmod(n, 2)
                    hb = h % 2
                    if which == 0:
                        dst, T_dst, c0, gs = qn[hb], T_qn[hb][t8], h * 128, gqs
                    else:
                        dst, T_dst, c0, gs = kn[hb], T_kn[hb][t8], 512 + h * 128, gks
                    p = pss[:, 0:256]
                    pb = pss[:, 256:512]
                    for k in range(8):
                        S.op("pe", lambda e, k=k: e.matmul(p, lhsT=wblk2[:, k, c0:c0 + 128], rhs=hT[:, k, t8 * 256:(t8 + 1) * 256], start=(k == 0), stop=(k == 7)),
                             reads=[T_hT[t8 // 2], T_wblk2], writes=[T_pssA], signal=(k == 7))
                    yield
                    sq, tsq = sq_r.next()
                    S.op("act", lambda e: e.activation(out=sq[:, 0:256], in_=p, func=AF.Square), reads=[T_pssA], writes=[tsq]); yield
                    S.op("pe", lambda e: e.matmul(pb, lhsT=bones[:], rhs=sq[:, 0:256], start=True, stop=True), reads=[tsq, T_c], writes=[T_pssB]); yield
                    rs, trs = rs_r.next()
                    S.op("act", lambda e: e.activation(out=rs[:, 0:256], in_=pb, func=AF.Ln, bias=epsb[:, 1:2]), reads=[T_pssB, T_eps], writes=[trs]); yield
                    S.op("act", lambda e: e.activation(out=rs[:, 0:256], in_=rs[:, 0:256], func=AF.Exp, scale=-0.5), reads=[trs], writes=[trs]); yield
                    S.op("dve", lambda e: e.scalar_tensor_tensor(out=dst[:, t8 * 256:(t8 + 1) * 256], in0=p, scalar=gs[:, 0:1], in1=rs[:, 0:256], op0=ALU.mult, op1=ALU.mult),
                         reads=[T_pssA, trs, T_in0], writes=[T_dst]); yield

                def proj_two(h, n):
                    yield from proj_chunk(h, 2 * n)
                    yield from proj_chunk(h, 2 * n + 1)

                def att_geom(g, j):
                    if j < 4 * g:
                        return 4 * g * 128, 512, False
                    return j * 128, (4 * g + 4 - j) * 128, True

                def att_qk(h, g, c, b):
                    hb = h % 2
                    cs0, cs1 = c * 64, (c + 1) * 64
                    nj = 4 * g + 4

                    def qk(j):
                        q0, nq, diag = att_geom(g, j)
                        p, tp = nextpp()
                        S.op("pe", lambda e: e.matmul(p[:, 0:nq], lhsT=kn[hb][cs0:cs1, j * 128:(j + 1) * 128], rhs=qn[hb][cs0:cs1, q0:q0 + nq], start=True, stop=True),
                             reads=[T_kn[hb][j // 2], T_qn[hb][2 * g], T_qn[hb][2 * g + 1]], writes=[tp])
                        return p, tp
                    cur = qk(0)
                    yield
                    for j in range(nj):
                        q0, nq, diag = att_geom(g, j)
                        p, tp = cur
                        PT, tPT = PTall[b][:, j, :], T_PT[b][j]
                        S.op("act", lambda e: e.activation(out=PT[:, 0:nq], in_=p[:, 0:nq], func=AF.Exp, scale=8.0), reads=[tp], writes=[tPT])
                        yield
                        if j + 1 < nj:
                            cur = qk(j + 1)
                            yield
                        if diag:
                            S.op("dve", lambda e: e.tensor_tensor(out=PT[:, 0:128], in0=PT[:, 0:128], in1=tri[:], op=ALU.mult), reads=[tPT, T_c], writes=[tPT])
                            yield

                def att_pv(h, g, c, a, b):
                    for ql in range(4):
                        qi = 4 * g + ql
                        for j in range(qi + 1):
                            q0, nq, diag = att_geom(g, j)
                            off = qi * 128 - q0
                            S.op("pe", lambda e: e.matmul(pOap(a, ql, 129), lhsT=PTall[b][:, j, off:off + 128], rhs=Vext[:, j, h, 0:129], start=(j == 0), stop=(j == qi)),
                                 reads=[T_PT[b][j], T_V], writes=[T_pO[a][ql]], signal=(j == qi))
                            if j % 2 == 1:
                                yield
                        yield

                def att_epi(h, g, c, a):
                    for ql in range(4):
                        qi = 4 * g + ql
                        rr, trr = rr_r.next()
                        S.op("dve", lambda e: e.reciprocal(out=rr[:, 0:1], in_=pOap(a, ql, 129)[:, 128:129]), reads=[T_pO[a][ql]], writes=[trr]); yield
                        if c == 0:
                            S.op("dve", lambda e: e.tensor_scalar(out=o1[:, ql, :], in0=pOap(a, ql, 128), scalar1=rr[:, 0:1], scalar2=None, op0=ALU.mult),
                                 reads=[T_pO[a][ql], trr], writes=[T_o1[ql]]); yield
                        else:
                            S.op("dve", lambda e: e.tensor_tensor(out=rr[:, 1:2], in0=rr[:, 0:1], in1=nlam[:], op=ALU.mult), reads=[trr, T_misc], writes=[trr]); yield
                            od, tod = od_r.next()
                            S.op("dve", lambda e: e.scalar_tensor_tensor(out=od[:], in0=pOap(a, ql, 128), scalar=rr[:, 1:2], in1=o1[:, ql, :], op0=ALU.mult, op1=ALU.add),
                                 reads=[T_pO[a][ql], trr, T_o1[ql]], writes=[tod]); yield
                            S.op("act", lambda e: e.activation(out=jk[:], in_=od[:], func=AF.Square, accum_out=rr[:, 2:3]), reads=[tod], writes=[T_jk, trr]); yield
                            S.op("act", lambda e: e.activation(out=rr[:, 3:4], in_=rr[:, 2:3], func=AF.Ln, scale=1.0 / 128, bias=epsb[:, 0:1]),
                                 reads=[trr, T_eps], writes=[trr]); yield
                            S.op("act", lambda e: e.activation(out=rr[:, 3:4], in_=rr[:, 3:4], func=AF.Exp, scale=-0.5), reads=[trr], writes=[trr]); yield
                            odn, todn = odn_r.next()
                            S.op("dve", lambda e: e.scalar_tensor_tensor(out=odn[:], in0=od[:], scalar=rr[:, 3:4], in1=sg8[:], op0=ALU.mult, op1=ALU.mult),
                                 reads=[tod, trr, T_misc], writes=[todn]); yield
                            S.op("pe", lambda e: e.transpose(pTo[:, 0:128], odn[:], identb[:]), reads=[todn, T_c], writes=[T_pTo]); yield
                            S.op("act", lambda e: e.copy(out=mixT[:, 4 + h, qi * 128:(qi + 1) * 128], in_=pTo[:, 0:128]), reads=[T_pTo], writes=[T_mixD[qi]]); yield

                for n in range(8):
                    interleave(proj_two(0, n))
                its = [(h, g, c) for h in range(4) for g in range(4) for c in range(2)]
                NI = len(its)
                for n in range(NI + 2):
                    gens = []
                    if n < NI:
                        h, g, c = its[n]
                        gens.append(att_qk(h, g, c, n % 2))
                    if 1 <= n <= NI:
                        h1, g1, c1 = its[n - 1]
                        gens.append(att_pv(h1, g1, c1, (n - 1) % 2, (n - 1) % 2))
                    if 2 <= n <= NI + 1:
                        h2, g2, c2 = its[n - 2]
                        gens.append(att_epi(h2, g2, c2, (n - 2) % 2))
                    if n < NI and its[n][0] < 3:
                        gens.append(proj_two(its[n][0] + 1, n % 8))
                    interleave(*gens)
                if "od" in dbg:
                    T_all = Tok("mixDall")
                    T_all.w = ("act", S.cnt["act"])
                    dump("odT", mixT[:, 4:8, :], T_all, [128, 4, S_TOK], BF16)
            S.barrier()
            if stop_after == "C2":
                S.final_wait("sp", out_toks)
                return nc, dbg_outs

        with ExitStack() as s2:
            acc = sbuf(s2, "acc", [128, NT, D], F32)
            T_acc = [[Tok("acc%d_%d" % (i, hf)) for hf in range(2)] for i in range(NT)]
            with ExitStack() as so:
                wst2_r = Ring([sbuf(so, "wst2_%d" % i, [128, 1024], F32) for i in range(3)], "wst2")
                wg = sbuf(so, "wg", [128, 8, 1024], BF16); T_wg = Tok("wg")
                wo = sbuf(so, "wo", [128, 8, 512], BF16); T_wo = Tok("wo")
                pz = [[psum(so, "pz%d_%d" % (a, b), [128, 512], F32) for b in range(4)] for a in range(2)]
                T_pz = [[Tok("pz%d_%d" % (a, b)) for b in range(4)] for a in range(2)]
                sa_r = Ring([sbuf(so, "sa%d" % i, [128, 512], F32) for i in range(2)], "sa")
                sd_r = Ring([sbuf(so, "sdg%d" % i, [128, 512], F32) for i in range(2)], "sdg")
                t1_r = Ring([sbuf(so, "t1_%d" % i, [128, 512], F32) for i in range(2)], "t1")
                t2_r = Ring([sbuf(so, "t2_%d" % i, [128, 512], F32) for i in range(2)], "t2")
                xh_r = Ring([sbuf(so, "xh%d" % i, [128, 512], F32) for i in range(2)], "xh")

                def load_w2(src2d, col0, W, dst, T_dst, dcol0):
                    srcv = src2d.rearrange("(k p) n -> p k n", p=128)
                    for k in range(8):
                        stg, tst = wst2_r.next()
                        S.dma("sp", "ld_" + tst.name, lambda e, stg=stg, k=k: e.dma_start(out=stg[:, 0:W], in_=srcv[:, k, col0:col0 + W]), writes=[tst])
                        if k % 2 == 0:
                            S.op("act", lambda e, stg=stg, k=k: e.copy(out=dst[:, k, dcol0:dcol0 + W], in_=stg[:, 0:W]), reads=[tst], writes=[T_dst])
                        else:
                            S.op("dve", lambda e, stg=stg, k=k: e.tensor_copy(out=dst[:, k, dcol0:dcol0 + W], in_=stg[:, 0:W]), reads=[tst], writes=[T_dst])
                for hf in range(2):
                    load_w2(w_in, 3584 + hf * 512, 512, wg, T_wg, 0)
                    load_w2(w_in, 4608 + hf * 512, 512, wg, T_wg, 512)
                    load_w2(w_out, hf * 512, 512, wo, T_wo, 0)
                    for i in range(NT):
                        tc0, tc1 = i * 128, (i + 1) * 128
                        pzz, tzz = pz[i % 2], T_pz[i % 2]
                        for k in range(8):
                            S.op("pe", lambda e, k=k, p=pzz[0]: e.matmul(p[:], lhsT=hT[:, k, tc0:tc1], rhs=wg[:, k, 0:512], start=(k == 0), stop=(k == 7)),
                                 reads=[T_hT[i // 4], T_wg], writes=[tzz[0]], signal=(k == 7))
                        for k in range(8):
                            S.op("pe", lambda e, k=k, p=pzz[1]: e.matmul(p[:], lhsT=hT[:, k, tc0:tc1], rhs=wg[:, k, 512:1024], start=(k == 0), stop=(k == 7)),
                                 reads=[T_hT[i // 4], T_wg], writes=[tzz[1]], signal=(k == 7))
                        for k in range(4):
                            S.op("pe", lambda e, k=k, p=pzz[2]: e.matmul(p[:], lhsT=mixT[:, k, tc0:tc1], rhs=wo[:, k, :], start=(k == 0), stop=(k == 3)),
                                 reads=[T_mixA[i], T_wo], writes=[tzz[2]], signal=(k == 3))
                        for k in range(4, 8):
                            S.op("pe", lambda e, k=k, p=pzz[3]: e.matmul(p[:], lhsT=mixT[:, k, tc0:tc1], rhs=wo[:, k, :], start=(k == 4), stop=(k == 7)),
                                 reads=[T_mixD[i], T_wo], writes=[tzz[3]], signal=(k == 7))
                        sa, tsa = sa_r.next(); sdg, tsd = sd_r.next(); t1, tt1 = t1_r.next(); t2, tt2 = t2_r.next(); xh, txh = xh_r.next()
                        S.dma("sp", "ld_" + txh.name, lambda e, xh=xh, i=i, hf=hf: e.dma_start(out=xh[:], in_=x[i * 128:(i + 1) * 128, hf * 512:(hf + 1) * 512]), writes=[txh])
                        S.op("act", lambda e, sa=sa, p=pzz[0]: e.activation(out=sa[:], in_=p[:], func=AF.Sigmoid), reads=[tzz[0]], writes=[tsa])
                        S.op("act", lambda e, sdg=sdg, p=pzz[1]: e.activation(out=sdg[:], in_=p[:], func=AF.Sigmoid), reads=[tzz[1]], writes=[tsd])
                        S.op("dve", lambda e, t1=t1, sa=sa, p=pzz[2]: e.tensor_tensor(out=t1[:], in0=sa[:], in1=p[:], op=ALU.mult), reads=[tsa, tzz[2]], writes=[tt1])
                        S.op("dve", lambda e, t2=t2, sdg=sdg, p=pzz[3]: e.tensor_tensor(out=t2[:], in0=sdg[:], in1=p[:], op=ALU.mult), reads=[tsd, tzz[3]], writes=[tt2])
                        S.op("pool", lambda e, t1=t1, t2=t2: e.tensor_tensor(out=t1[:], in0=t1[:], in1=t2[:], op=ALU.add), reads=[tt1, tt2], writes=[tt1])
                        S.op("dve", lambda e, t1=t1, hf=hf: e.tensor_tensor(out=t1[:], in0=t1[:], in1=g1bc[:, hf * 512:(hf + 1) * 512], op=ALU.mult), reads=[tt1, T_g1], writes=[tt1])
                        S.op("dve", lambda e, t1=t1, xh=xh, i=i, hf=hf: e.tensor_tensor(out=acc[:, i, hf * 512:(hf + 1) * 512], in0=t1[:], in1=xh[:], op=ALU.add),
                             reads=[tt1, txh], writes=[T_acc[i][hf]])
            S.barrier()
            if "x1" in dbg:
                for i in range(NT):
                    tt = Tok("x1d%d" % i)
                    tt.w = T_acc[i][1].w
                    dump("x1_%d" % i, acc[:, i, :], tt, [128, D], F32)
            if stop_after == "C3":
                S.final_wait("sp", out_toks)
                return nc, dbg_outs

            with ExitStack() as sf:
                NSLOT = 10
                ring2 = sbuf(sf, "ring2", [128, NSLOT - 8, 2048], BF16)

                def slot_ap(sl):
                    base = bufB[:, sl, :] if sl < 8 else ring2[:, sl - 8, :]
                    return base.rearrange("p (k c) -> p k c", k=8)
                T_slot = [Tok("slot%d" % i) for i in range(NSLOT)]
                w2b = sbuf(sf, "w2b", [128, 8, D], BF16); T_w2b = Tok("w2b")
                stg_r = Ring([sbuf(sf, "stg%d" % i, [128, 1024], F32) for i in range(4)], "stg")
                wrb = sbuf(sf, "wrb", [128, 8, NE], BF16); T_wrb = Tok("wrb")
                wrs = sbuf(sf, "wrs", [128, 8, NE], F32)
                b2g = sbuf(sf, "b2g", [NE, D], F32); T_b2g = Tok("b2g")
                gates = sbuf(sf, "gates", [128, 8, NE], F32); T_gates = [Tok("gates%d" % i) for i in range(8)]
                h2T = bufA
                T_h2T = [Tok("h2T0"), Tok("h2T1")]
                T_actT = [[Tok("actT%d_%d" % (j, tg)) for tg in range(2)] for j in range(8)]
                S.dma("sp", "ld_wrs", lambda e: e.dma_start(out=wrs[:], in_=w_r.rearrange("(k p) n -> p k n", p=128)), writes=[T_wrb])
                S.op("dve", lambda e: e.tensor_copy(out=wrb[:], in_=wrs[:]), reads=[T_wrb], writes=[T_wrb])
                S.dma("sp", "ld_b2g", lambda e: e.dma_start(out=b2g[:], in_=b2[:, :]), writes=[T_b2g])
                S.op("dve", lambda e: e.tensor_tensor(out=b2g[:], in0=b2g[:], in1=g2bc[0:NE, :], op=ALU.mult), reads=[T_b2g, T_g2], writes=[T_b2g])
                slot_ctr = [0]
                for th in range(2):
                    tiles = list(range(th * 8, th * 8 + 8))
                    with ExitStack() as sn:
                        srcs = [(acc[:, i, :], T_acc[i]) for i in tiles]
                        for _ in norm_transpose(sn, "D%d" % th, srcs, h2T, a2, 3, T_a2, T_h2T, False, nxn=4):
                            pass
                    S.barrier()
                    with ExitStack() as se:
                        pgb = [psum(se, "pgb%d_%d" % (th, a), [128, 512], F32) for a in range(3)]; T_pgb = [Tok("pgb%d" % a) for a in range(3)]
                        plb = [psum(se, "plb%d_%d" % (th, a), [128, 512], F32) for a in range(3)]; T_plb = [Tok("plb%d" % a) for a in range(3)]
                        pyb = [psum(se, "pyb%d_%d" % (th, a), [128, 512], F32) for a in range(2)]; T_pyb = [Tok("pyb0"), Tok("pyb1")]
                        gm_r = Ring([sbuf(se, "gm%d_%d" % (th, a), [128, 512], F32) for a in range(2)], "gm")
                        sg_r = Ring([sbuf(se, "sg%d_%d" % (th, a), [128, 512], F32) for a in range(2)], "sg")
                        lm_r = Ring([sbuf(se, "lm%d_%d" % (th, a), [128, 512], F32) for a in range(1)], "lm")
                        tt_r = Ring([sbuf(se, "tt%d_%d" % (th, a), [128, 512], F32) for a in range(1)], "tt")
                        n1 = 0
                        n2 = 0
                        if th == 0:
                            print("sbuf bytes remaining in FFN expert scope:", nc.sbuf_bytes_remaining)
                        ne_run = NE if stop_after == "all" else int(stop_after[1:]) if stop_after.startswith("E") else NE
                        pend = []

                        def flush_casts():
                            while pend:
                                pend.pop(0)()

                        def load_piece(qidx):
                            e_, j_ = divmod(qidx, 8)
                            if e_ >= ne_run:
                                return
                            w1v = w1p[e_].rearrange("(k p) n -> p k n", p=128)
                            sl = qidx % NSLOT
                            sap = slot_ap(sl)
                            for part in range(2):
                                stg, tst = stg_r.next()
                                cc0 = part * D + j_ * 128
                                S.dma("sp", "ld_" + tst.name, lambda e, stg=stg, cc0=cc0: e.dma_start(
                                    out=stg[:].rearrange("p (k c) -> p k c", k=8), in_=w1v[:, :, cc0:cc0 + 128]), writes=[tst])
                                pend.append(lambda stg=stg, tst=tst, sap=sap, part=part, sl=sl: S.op("act", lambda e: e.copy(
                                    out=sap[:, :, part * 128:(part + 1) * 128], in_=stg[:].rearrange("p (k c) -> p k c", k=8)),
                                    reads=[tst], writes=[T_slot[sl]]))

                        def load_w2chunk(e_, k):
                            if e_ >= ne_run:
                                return
                            stg, tst = stg_r.next()
                            S.dma("sp", "ld_" + tst.name, lambda e, stg=stg, k=k: e.dma_start(out=stg[:], in_=w2[e_, k * 128:(k + 1) * 128, :]), writes=[tst])
                            pend.append(lambda stg=stg, tst=tst, k=k: S.op("dve" if k % 4 != 3 else "pool", lambda e: e.tensor_tensor(
                                out=w2b[:, k, :], in0=stg[:], in1=g2bc[:], op=ALU.mult), reads=[tst, T_g2], writes=[T_w2b]))
                        for qidx in range(NSLOT):
                            load_piece(qidx)
                            flush_casts()
                        rtmp = []
                        for ch in range(2):
                            rtmp.append(dict(
                                lg=sbuf(se, "lg%d_%d" % (th, ch), [128, NE], F32), T_lg=Tok("lg"),
                                top8=sbuf(se, "top8_%d_%d" % (th, ch), [128, 8], F32), T_top8=Tok("top8"),
                                msk=sbuf(se, "msk%d_%d" % (th, ch), [128, NE], F32), T_msk=Tok("msk"),
                                ex=sbuf(se, "ex%d_%d" % (th, ch), [128, NE], F32), T_ex=Tok("ex"),
                                sm=sbuf(se, "sm%d_%d" % (th, ch), [128, 4], F32), T_sm=Tok("sm"),
                                gT=sbuf(se, "gT%d_%d" % (th, ch), [NE, 128], F32), T_gT=Tok("gT")))

                        def router_tile(il, ch):
                            i = tiles[il]
                            c0, c1 = il * 128, (il + 1) * 128
                            R = rtmp[ch]
                            lg, top8, msk, ex, sm, gT = R["lg"], R["top8"], R["msk"], R["ex"], R["sm"], R["gT"]
                            T_lg, T_top8, T_msk, T_ex, T_sm, T_gT = R["T_lg"], R["T_top8"], R["T_msk"], R["T_ex"], R["T_sm"], R["T_gT"]
                            plog, T_plog = pgb[ch], T_pgb[ch]
                            pgT, T_pgT = plb[ch], T_plb[ch]
                            pbk, T_pbk = pyb[ch], T_pyb[ch]
                            for k in range(8):
                                S.op("pe", lambda e, k=k: e.matmul(plog[:, 0:NE], lhsT=h2T[:, k, c0:c1], rhs=wrb[:, k, :], start=(k == 0), stop=(k == 7)),
                                     reads=[T_h2T[il // 4], T_wrb], writes=[T_plog], signal=(k == 7))
                            yield
                            S.op("dve", lambda e: e.tensor_tensor(out=lg[:], in0=plog[:, 0:NE], in1=brbc[:], op=ALU.add), reads=[T_plog, T_in0], writes=[T_lg]); yield
                            S.op("dve", lambda e: e.max(out=top8[:], in_=lg[:]), reads=[T_lg], writes=[T_top8]); yield
                            S.op("dve", lambda e: e.tensor_scalar(out=msk[:], in0=lg[:], scalar1=top8[:, 3:4], scalar2=None, op0=ALU.is_ge), reads=[T_lg, T_top8], writes=[T_msk]); yield
                            S.op("dve", lambda e: e.tensor_scalar(out=sm[:, 0:1], in0=top8[:, 0:1], scalar1=-1.0, scalar2=None, op0=ALU.mult), reads=[T_top8], writes=[T_sm]); yield
                            S.op("act", lambda e: e.activation(out=ex[:], in_=lg[:], func=AF.Exp, bias=sm[:, 0:1]), reads=[T_lg, T_sm], writes=[T_ex]); yield
                            S.op("dve", lambda e: e.tensor_tensor(out=ex[:], in0=ex[:], in1=msk[:], op=ALU.mult), reads=[T_ex, T_msk], writes=[T_ex]); yield
                            S.op("dve", lambda e: e.reduce_sum(out=sm[:, 1:2], in_=ex[:], axis=AX.X), reads=[T_ex], writes=[T_sm]); yield
                            S.op("dve", lambda e: e.reciprocal(out=sm[:, 2:3], in_=sm[:, 1:2]), reads=[T_sm], writes=[T_sm]); yield
                            S.op("dve", lambda e: e.tensor_scalar(out=gates[:, il, :], in0=ex[:], scalar1=sm[:, 2:3], scalar2=None, op0=ALU.mult),
                                 reads=[T_ex, T_sm], writes=[T_gates[il]]); yield
                            S.op("pe", lambda e: e.matmul(pgT[0:NE, 0:128], lhsT=gates[:, il, :], rhs=identf[:], start=True, stop=True),
                                 reads=[T_gates[il], T_c], writes=[T_pgT]); yield
                            S.op("act", lambda e: e.copy(out=gT[:], in_=pgT[0:NE, 0:128]), reads=[T_pgT], writes=[T_gT]); yield
                            for nh in range(2):
                                S.op("pe", lambda e: e.matmul(pbk[:], lhsT=gT[:], rhs=b2g[:, nh * 512:(nh + 1) * 512], start=True, stop=True),
                                     reads=[T_gT, T_b2g], writes=[T_pbk]); yield
                                S.op("dve", lambda e: e.tensor_tensor(out=acc[:, i, nh * 512:(nh + 1) * 512], in0=acc[:, i, nh * 512:(nh + 1) * 512], in1=pbk[:], op=ALU.add),
                                     reads=[T_pbk, T_acc[i][nh]], writes=[T_acc[i][nh]]); yield
                        for il2 in range(4):
                            interleave(router_tile(2 * il2, 0), router_tile(2 * il2 + 1, 1))
                        if "gates" in dbg and th == 0:
                            tt_ = Tok("gd"); tt_.w = T_gates[7].w
                            dump("gates", gates[:].rearrange("p a b -> p (a b)"), tt_, [128, 8 * NE], F32)
                        for ex_i in range(ne_run):
                            slots = [(ex_i * 8 + j) % NSLOT for j in range(8)]
                            for j in range(8):
                                sl = slots[j]
                                sap = slot_ap(sl)
                                for tg in range(2):
                                    a = n1 % 3
                                    hoist = [T_plb[a]] + ([T_pgb[(a + 1) % 3], T_plb[(a + 1) % 3]] if n1 % 2 == 0 else [])
                                    n1 += 1
                                    for k in range(8):
                                        S.op("pe", lambda e, k=k, a=a, sap=sap, tg=tg: e.matmul(pgb[a][:], lhsT=sap[:, k, 0:128], rhs=h2T[:, k, tg * 512:(tg + 1) * 512],
                                                                                          start=(k == 0), stop=(k == 7)),
                                             reads=[T_slot[sl], T_h2T[tg]], writes=[T_pgb[a]] + (hoist if k == 0 else []), signal=(k == 7))
                                    for k in range(8):
                                        S.op("pe", lambda e, k=k, a=a, sap=sap, tg=tg: e.matmul(plb[a][:], lhsT=sap[:, k, 128:256], rhs=h2T[:, k, tg * 512:(tg + 1) * 512],
                                                                                          start=(k == 0), stop=(k == 7)),
                                             reads=[T_slot[sl], T_h2T[tg]], writes=[T_plb[a]], signal=(k == 7))
                                    gm, tgm = gm_r.next(); sg, tsg = sg_r.next(); lm, tlm = lm_r.next(); tt, ttt = tt_r.next()
                                    bg = b1s[:, ex_i * 16 + j:ex_i * 16 + j + 1]
                                    bl = b1s[:, ex_i * 16 + 8 + j:ex_i * 16 + 8 + j + 1]
                                    S.op("dve", lambda e, gm=gm, a=a, bg=bg: e.tensor_scalar(out=gm[:], in0=pgb[a][:], scalar1=bg, scalar2=7.0, op0=ALU.add, op1=ALU.min),
                                         reads=[T_pgb[a], T_misc], writes=[tgm])
                                    S.op("act", lambda e, gm=gm, sg=sg: e.activation(out=sg[:], in_=gm[:], func=AF.Sigmoid, scale=1.702), reads=[tgm], writes=[tsg])
                                    S.op("dve", lambda e, lm=lm, a=a, bl=bl: e.tensor_scalar(out=lm[:], in0=plb[a][:], scalar1=bl, scalar2=8.0, op0=ALU.add, op1=ALU.min),
                                         reads=[T_plb[a], T_misc], writes=[tlm])
                                    S.op("pool", lambda e, gm=gm, sg=sg, tt=tt: e.tensor_tensor(out=tt[:], in0=gm[:], in1=sg[:], op=ALU.mult), reads=[tgm, tsg], writes=[ttt])
                                    S.op("dve", lambda e, lm=lm, tt=tt, j=j, tg=tg: e.scalar_tensor_tensor(
                                        out=bufA[:, j, 1024 + tg * 512:1024 + (tg + 1) * 512], in0=lm[:], scalar=-6.0, in1=tt[:], op0=ALU.max, op1=ALU.mult),
                                        reads=[tlm, ttt], writes=[T_actT[j][tg]])
                                flush_casts()
                                if j < 4:
                                    load_w2chunk(ex_i, 2 * j)
                                    load_w2chunk(ex_i, 2 * j + 1)
                                load_piece(ex_i * 8 + j + NSLOT)
                            flush_casts()
                            for il, i in enumerate(tiles):
                                for nh in range(2):
                                    a = n2 % 2
                                    n2 += 1
                                    for k in range(8):
                                        S.op("pe", lambda e, k=k, a=a, il=il, nh=nh: e.matmul(
                                            pyb[a][:], lhsT=bufA[:, k, 1024 + il * 128:1024 + (il + 1) * 128], rhs=w2b[:, k, nh * 512:(nh + 1) * 512],
                                            start=(k == 0), stop=(k == 7)),
                                            reads=[T_actT[k][il // 4], T_w2b], writes=[T_pyb[a]], signal=(k == 7))
                                    S.op("dve", lambda e, a=a, il=il, i=i, nh=nh: e.scalar_tensor_tensor(
                                        out=acc[:, i, nh * 512:(nh + 1) * 512], in0=pyb[a][:], scalar=gates[:, il, ex_i:ex_i + 1], in1=acc[:, i, nh * 512:(nh + 1) * 512],
                                        op0=ALU.mult, op1=ALU.add),
                                        reads=[T_pyb[a], T_gates[il], T_acc[i][nh]], writes=[T_acc[i][nh]])
                        for i in tiles:
                            to = Tok("out%d" % i)
                            S.dma("sp", "st_out", lambda e, i=i: e.dma_start(out=out[i * 128:(i + 1) * 128, :], in_=acc[:, i, :]), reads=[T_acc[i][0], T_acc[i][1]], writes=[to])
                            out_toks.append(to)
                    S.barrier()

        S.final_wait("sp", out_toks)
    return nc, dbg_outs


def _consts():
    s = np.arange(128)
    mid = 63
    amat = np.zeros((128, 130), np.float32)
    amat[:, :128] = (s[:, None] <= s[None, :]).astype(np.float32) - (s[:, None] <= mid).astype(np.float32)
    amat[:, 128] = (s <= mid).astype(np.float32)
    amat[:, 129] = 1.0
    tri = (s[None, :] >= s[:, None]).astype(np.float32)
    bones = np.zeros((128, 128), np.float32)
    bones[:64, :64] = 1.0
    bones[64:, 64:] = 1.0
    return {
        "identb": np.eye(128, dtype=np.float32).astype(NPBF),
        "identf": np.eye(128, dtype=np.float32),
        "amat": amat,
        "tri": tri,
        "bones": bones.astype(NPBF),
    }


def _prep_shared(inp):
    f = lambda a: np.ascontiguousarray(np.asarray(a, dtype=np.float32))
    w1 = f(inp["w1"])[0]
    w1p = np.ascontiguousarray(np.concatenate([w1[:, :, 0::2], w1[:, :, 1::2]], axis=2))
    b1 = f(inp["b1"])[0]
    b1p = np.concatenate([b1[:, 0::2], b1[:, 1::2]], axis=1)
    b1F = np.ascontiguousarray(b1p.reshape(NE, 16, 128).transpose(2, 0, 1).reshape(128, NE * 16))
    sh = {
        "w_ada": f(inp["w_ada"])[0],
        "badaF": np.ascontiguousarray(f(inp["b_ada"])[0].reshape(48, 128).T),
        "bada_row": f(inp["b_ada"])[0].reshape(1, -1),
        "gmixF": np.ascontiguousarray(f(inp["mix_norm_g"])[0].reshape(8, 128).T),
        "gffnF": np.ascontiguousarray(f(inp["ffn_norm_g"])[0].reshape(8, 128).T),
        "w_in": f(inp["w_in"])[0],
        "lbl": f(inp["hg_lower_bound_logits"]).reshape(1, 1024),
        "hgn": f(inp["hg_out_norm_g"]).reshape(1, 128),
        "gq": np.tile(f(inp["da_q_norm_g"]).reshape(64), 2).reshape(128, 1),
        "gk": np.tile(f(inp["da_k_norm_g"]).reshape(64), 2).reshape(128, 1),
        "lam4": np.concatenate([f(inp["da_lambda_q1"]).reshape(64), f(inp["da_lambda_k1"]).reshape(64),
                                f(inp["da_lambda_q2"]).reshape(64), f(inp["da_lambda_k2"]).reshape(64)]).reshape(1, 256),
        "subg": f(inp["da_subln_g"]).reshape(1, 128),
        "w_out": f(inp["w_out"])[0],
        "w_r": f(inp["w_router"])[0],
        "b_r": f(inp["b_router"]).reshape(1, NE),
        "w1p": w1p,
        "b1F": b1F,
        "w2": f(inp["w2"])[0],
        "b2": f(inp["b2"])[0],
    }
    sh.update(_consts())
    return sh


def _prep_core(inp, sh, b):
    m = dict(sh)
    m["x"] = np.ascontiguousarray(np.asarray(inp["x"], dtype=np.float32)[b])
    m["cT"] = np.ascontiguousarray(np.asarray(inp["c"], dtype=np.float32)[b].reshape(8, 128).T)
    return m


def kernel(**inputs):
    nc, _ = build_nc()
    sh = _prep_shared(inputs)
    in_maps = [_prep_core(inputs, sh, b) for b in range(8)]
    res = run_bass_kernel_spmd(nc, in_maps, core_ids=list(range(8)))
    return np.stack([np.asarray(r["out"], dtype=np.float32) for r in res.results], axis=0)
```

```python
import numpy as np
import ml_dtypes
from contextlib import ExitStack
import concourse.bass as bass
import concourse.mybir as mybir
from concourse.bass_utils import run_bass_kernel_spmd

F32 = mybir.dt.float32
BF16 = mybir.dt.bfloat16
AF = mybir.ActivationFunctionType
ALU = mybir.AluOpType
AX = mybir.AxisListType
NPBF = ml_dtypes.bfloat16

S_TOK = 2048
D = 1024
NT = 16
NE = 32
EPS = 1e-6
LAMBDA_INIT = 0.2


class Tok:
    __slots__ = ("name", "w", "r")

    def __init__(self, name=""):
        self.name = name
        self.w = None
        self.r = []


class Sched:
    def __init__(self, nc, stack, same_engine_sync=True):
        self.nc = nc
        self.stack = stack
        self.eng = {"pe": nc.tensor, "act": nc.scalar, "dve": nc.vector, "pool": nc.gpsimd, "sp": nc.sync}
        self.sems = {}
        self.cnt = {}
        self.waited = {e: {} for e in self.eng}
        self.same = same_engine_sync
        self.n_inst = 0
        self.n_wait = 0

    def sem(self, key):
        if key not in self.sems:
            self.sems[key] = self.stack.enter_context(self.nc.semaphore("s_%s" % key))
            self.cnt[key] = 0
        return self.sems[key]

    def _deps(self, reads, writes):
        deps = {}

        def add(t):
            if t is None:
                return
            k, v = t
            if deps.get(k, 0) < v:
                deps[k] = v
        for t in reads:
            add(t.w)
        for t in writes:
            add(t.w)
            for r in t.r:
                add(r)
        return deps

    def _emit_waits(self, e, deps):
        eng = self.eng[e]
        for k, v in deps.items():
            if k == e and (e == "pe" or not self.same):
                continue
            if self.waited[e].get(k, 0) >= v:
                continue
            eng.wait_ge(self.sems[k], v)
            self.waited[e][k] = v
            self.n_wait += 1

    def op(self, e, fn, reads=(), writes=(), signal=True):
        self.sem(e)
        deps = self._deps(reads, writes)
        self._emit_waits(e, deps)
        ins = fn(self.eng[e])
        self.n_inst += 1
        if signal:
            self.cnt[e] += 1
            ins.then_inc(self.sems[e], 1)
            tk = (e, self.cnt[e])
        else:
            tk = (e, self.cnt[e] + 1)
        for t in writes:
            t.w = tk
            t.r = []
        for t in reads:
            if len(t.r) > 64:
                best = {}
                for k, v in t.r:
                    if best.get(k, 0) < v:
                        best[k] = v
                t.r = list(best.items())
            t.r.append(tk)
        return tk

    def dma(self, q, semkey, fn, reads=(), writes=()):
        self.sem(semkey)
        deps = self._deps(reads, writes)
        self._emit_waits(q, deps) if False else None
        eng = self.eng[q]
        for k, v in deps.items():
            if self.waited[q].get(k, 0) >= v:
                continue
            eng.wait_ge(self.sems[k], v)
            self.waited[q][k] = v
            self.n_wait += 1
        ins = fn(eng)
        self.n_inst += 1
        self.cnt[semkey] += 16
        ins.then_inc(self.sems[semkey], 16)
        tk = (semkey, self.cnt[semkey])
        for t in writes:
            t.w = tk
            t.r = []
        for t in reads:
            t.r.append(tk)
        return tk

    def barrier(self):
        for e in self.eng:
            for k, v in self.cnt.items():
                if v == 0 or k == e and e == "pe":
                    continue
                if self.waited[e].get(k, 0) >= v:
                    continue
                self.eng[e].wait_ge(self.sems[k], v)
                self.waited[e][k] = v
                self.n_wait += 1

    def final_wait(self, q, toks):
        deps = {}
        for t in toks:
            if t.w is not None:
                k, v = t.w
                deps[k] = max(deps.get(k, 0), v)
        for k, v in deps.items():
            self.eng[q].wait_ge(self.sems[k], v)


class Ring:
    def __init__(self, bufs, name):
        self.bufs = bufs
        self.toks = [Tok("%s%d" % (name, i)) for i in range(len(bufs))]
        self.i = 0

    def next(self):
        b, t = self.bufs[self.i], self.toks[self.i]
        self.i = (self.i + 1) % len(self.bufs)
        return b, t


def interleave(*gens):
    gens = [g for g in gens if g is not None]
    while gens:
        for g in list(gens):
            try:
                next(g)
            except StopIteration:
                gens.remove(g)


def build_nc(stop_after="all", dbg=()):
    nc = bass.Bass("TRN2", target_bir_lowering=False)

    def din(name, shape, dt=F32):
        return nc.dram_tensor(name, list(shape), dt, kind="ExternalInput").ap()

    x = din("x", [S_TOK, D])
    cT = din("cT", [128, 8])
    w_ada = din("w_ada", [D, 6 * D])
    badaF = din("badaF", [128, 48])
    bada_row = din("bada_row", [1, 6 * D])
    gmixF = din("gmixF", [128, 8])
    gffnF = din("gffnF", [128, 8])
    w_in = din("w_in", [D, 5632])
    lbl = din("lbl", [1, 1024])
    hgn = din("hgn", [1, 128])
    gq = din("gq", [128, 1])
    gk = din("gk", [128, 1])
    lam4 = din("lam4", [1, 256])
    subg = din("subg", [1, 128])
    w_out = din("w_out", [D, D])
    w_r = din("w_r", [D, NE])
    b_r = din("b_r", [1, NE])
    w1p = din("w1p", [NE, D, 2 * D])
    b1F = din("b1F", [128, NE * 16])
    w2 = din("w2", [NE, D, D])
    b2 = din("b2", [NE, D])
    identb_d = din("identb", [128, 128], BF16)
    identf_d = din("identf", [128, 128])
    amat_d = din("amat", [128, 130])
    tri_d = din("tri", [128, 128])
    bones_d = din("bones", [128, 128], BF16)
    out = nc.dram_tensor("out", [S_TOK, D], F32, kind="ExternalOutput").ap()
    dbg_outs = {}

    with ExitStack() as st:
        S = Sched(nc, st)
        out_toks = []

        def sbuf(stack, name, shape, dt):
            return stack.enter_context(nc.sbuf_tensor("sb_" + name, list(shape), dt))

        def psum(stack, name, shape, dt):
            return stack.enter_context(nc.psum_tensor("ps_" + name, list(shape), dt))

        ld_i = [0]

        def load(dst, src, tok, q="sp"):
            ld_i[0] += 1
            return S.dma(q, "ld_%s" % tok.name, lambda e: e.dma_start(out=dst, in_=src), writes=[tok])

        def dump(name, ap, tok, shape, dt):
            d = nc.dram_tensor("dbg_" + name, list(shape), dt, kind="ExternalOutput").ap()
            t = Tok("dbgo_" + name)
            S.dma("sp", "dbg_" + name, lambda e: e.dma_start(out=d, in_=ap), reads=[tok], writes=[t])
            out_toks.append(t)
            dbg_outs[name] = d

        identb = sbuf(st, "identb", [128, 128], BF16)
        identf = sbuf(st, "identf", [128, 128], F32)
        amat = sbuf(st, "amat", [128, 130], F32)
        tri = sbuf(st, "tri", [128, 128], F32)
        bones = sbuf(st, "bones", [128, 128], BF16)
        T_c = Tok("consts")
        for (d_, s_, nm) in [(identb, identb_d, "c0"), (identf, identf_d, "c1"), (amat, amat_d, "c2"), (tri, tri_d, "c3"),
                             (bones, bones_d, "c4")]:
            S.dma("sp", "ld_c", lambda e, d_=d_, s_=s_: e.dma_start(out=d_[:], in_=s_[:, :]), writes=[])
        T_c.w = ("ld_c", S.cnt["ld_c"])

        adaF = sbuf(st, "adaF", [128, 6, 8], F32)
        a1 = sbuf(st, "a1", [128, 8], F32)
        a2 = sbuf(st, "a2", [128, 8], F32)
        g1bc = sbuf(st, "g1bc", [128, D], F32)
        g2bc = sbuf(st, "g2bc", [128, D], F32)
        gnbc = sbuf(st, "gnbc", [128, 128], F32)
        sg8 = sbuf(st, "sg8", [128, 128], F32)
        gqs = sbuf(st, "gqs", [128, 1], F32)
        gks = sbuf(st, "gks", [128, 1], F32)
        nlam = sbuf(st, "nlam", [128, 1], F32)
        brbc = sbuf(st, "brbc", [128, NE], F32)
        b1s = sbuf(st, "b1s", [128, NE * 16], F32)
        bufA = sbuf(st, "bufA", [128, 8, S_TOK], BF16)
        bufB = sbuf(st, "bufB", [128, 8, S_TOK], BF16)
        T_ada = Tok("ada")
        T_a1 = Tok("a1")
        T_a2 = Tok("a2")
        T_g1 = Tok("g1")
        T_g2 = Tok("g2")
        T_misc = Tok("misc")
        T_in0 = Tok("in0")
        T_hT = [Tok("hT%d" % g) for g in range(4)]
        T_mixA = [Tok("mixA%d" % i) for i in range(NT)]
        T_mixD = [Tok("mixD%d" % i) for i in range(NT)]
        hT = bufA
        mixT = bufB

        def norm_transpose(stack, tag, tile_srcs, dstT, a_sc, sh_idx, T_a, T_dst_groups, src_is_dram, nxn=5, split=False, q="sp"):
            xt_r = Ring([sbuf(stack, "%s_xt%d" % (tag, i), [128, D], F32) for i in range(2)], tag + "xt") if src_is_dram else None
            xn_r = Ring([sbuf(stack, "%s_xn%d" % (tag, i), [128, D], BF16) for i in range(nxn)], tag + "xn")
            ssq = sbuf(stack, tag + "_ssq", [128, 64], F32)
            pT = [psum(stack, "%s_pT%d" % (tag, i), [128, 512], BF16) for i in range(2)]
            T_pT = [Tok(tag + "pT0"), Tok(tag + "pT1")]
            T_ssq = Tok(tag + "ssq")
            npt = [0]
            ntile = len(tile_srcs)

            def pre(i):
                src, tsrc = tile_srcs[i]
                if src_is_dram:
                    xt, txt = xt_r.next()
                    S.dma(q, "ld_" + txt.name, lambda e: e.dma_start(out=xt[:], in_=src), writes=[txt])
                    xin, tin = xt[:], [txt]
                else:
                    xin, tin = src, list(tsrc)
                xn, txn = xn_r.next()
                S.op("act", lambda e: e.activation(out=xn[:], in_=xin, func=AF.Square, accum_out=ssq[:, i:i + 1]), reads=tin, writes=[txn, T_ssq])
                S.op("act", lambda e: e.activation(out=ssq[:, 32 + i:33 + i], in_=ssq[:, i:i + 1], func=AF.Ln, scale=1.0 / D, bias=epsb[:, 0:1]),
                     reads=[T_ssq, T_eps], writes=[T_ssq])
                S.op("act", lambda e: e.activation(out=ssq[:, 32 + i:33 + i], in_=ssq[:, 32 + i:33 + i], func=AF.Exp, scale=-0.5), reads=[T_ssq], writes=[T_ssq])
                S.op("dve", lambda e: e.tensor_scalar(out=xn[:], in0=xin, scalar1=ssq[:, 32 + i:33 + i], scalar2=None, op0=ALU.mult),
                     reads=tin + [T_ssq], writes=[txn])
                return (xn, txn)

            def post(g, xns):
                for k in range(8):
                    p, tp = pT[npt[0] % 2], T_pT[npt[0] % 2]
                    npt[0] += 1
                    for ii in range(4):
                        xn, txn = xns[ii]
                        S.op("pe", lambda e: e.transpose(p[:, ii * 128:(ii + 1) * 128], xn[:, k * 128:(k + 1) * 128], identb[:]),
                             reads=[txn, T_c], writes=[tp], signal=(ii == 3))
                    S.op("act", lambda e: e.activation(out=dstT[:, k, g * 512:(g + 1) * 512], in_=p[:], func=AF.Identity,
                                                       scale=a_sc[:, k:k + 1], bias=adaF[:, sh_idx, k:k + 1]),
                         reads=[tp, T_a, T_ada], writes=[T_dst_groups[g]])
            if split:
                allx = [pre(i) for i in range(ntile)]
                yield
                for g in range(ntile // 4):
                    post(g, allx[g * 4:g * 4 + 4])
            else:
                for g in range(ntile // 4):
                    xns = [pre(g * 4 + ii) for ii in range(4)]
                    post(g, xns)
                    yield

        epsb = sbuf(st, "epsb", [128, 4], F32)
        T_eps = Tok("eps")
        S.op("dve", lambda e: e.memset(epsb[:, 0:1], EPS), writes=[T_eps])
        S.op("dve", lambda e: e.memset(epsb[:, 1:2], 64 * EPS), writes=[T_eps])
        S.op("dve", lambda e: e.memset(epsb[:, 2:3], 1.0), writes=[T_eps])

        with ExitStack() as sa:
            cTs = sbuf(sa, "cTs", [128, 8], F32)
            cact = sbuf(sa, "cact", [128, 8], F32)
            cbc = sbuf(sa, "cbc", [128, 8, 128], F32)
            wa = [sbuf(sa, "wa%d" % i, [128, 8, 1024], F32) for i in range(2)]
            T_wa = [Tok("wa0"), Tok("wa1")]
            badaFs = sbuf(sa, "badaFs", [128, 48], F32)
            bgrow = sbuf(sa, "bgrow", [128, 2, 1024], F32)
            gmixs = sbuf(sa, "gmixs", [128, 8], F32)
            gffns = sbuf(sa, "gffns", [128, 8], F32)
            lam4bc = sbuf(sa, "lam4bc", [128, 256], F32)
            lamtmp = sbuf(sa, "lamtmp", [128, 128], F32)
            lams = sbuf(sa, "lams", [128, 2], F32)
            subgbc = sbuf(sa, "subgbc", [128, 128], F32)
            pA = psum(sa, "pA", [128, 512], F32)
            pG = psum(sa, "pG", [128, 512], F32)
            T_pA, T_pG = Tok("pA"), Tok("pG")
            T_in = Tok("smallin")
            smalls = [(cTs[:], cT[:, :]), (badaFs[:], badaF[:, :]), (gmixs[:], gmixF[:, :]), (gffns[:], gffnF[:, :]),
                      (gqs[:], gq[:, :]), (gks[:], gk[:, :]), (b1s[:], b1F[:, :]),
                      (lam4bc[:], lam4[0:1, :].partition_broadcast(128)),
                      (subgbc[:], subg[0:1, :].partition_broadcast(128)),
                      (gnbc[:], hgn[0:1, :].partition_broadcast(128)),
                      (brbc[:], b_r[0:1, :].partition_broadcast(128)),
                      (bgrow[:, 0, :], bada_row[0:1, 2 * D:3 * D].partition_broadcast(128)),
                      (bgrow[:, 1, :], bada_row[0:1, 5 * D:6 * D].partition_broadcast(128))]
            for d_, s_ in smalls:
                S.dma("sp", "ld_s", lambda e, d_=d_, s_=s_: e.dma_start(out=d_, in_=s_), writes=[])
            T_in.w = ("ld_s", S.cnt["ld_s"])
            T_in0.w = T_in.w

            T_cact = Tok("cact")
            S.op("act", lambda e: e.activation(out=cact[:], in_=cTs[:], func=AF.Silu), reads=[T_in], writes=[T_cact])
            T_cbc = Tok("cbc")
            S.op("dve", lambda e: e.tensor_copy(out=cbc[:], in_=cact[:].unsqueeze(2).to_broadcast([128, 8, 128])),
                 reads=[T_cact], writes=[T_cbc])
            S.op("dve", lambda e: e.tensor_scalar(out=sg8[:], in0=subgbc[:], scalar1=1.0 - LAMBDA_INIT, scalar2=None, op0=ALU.mult),
                 reads=[T_in], writes=[T_misc])
            b1v = b1s[:].rearrange("p (e j) -> p e j", j=16)
            S.op("dve", lambda e: e.tensor_scalar(out=b1v[:, :, 8:16], in0=b1v[:, :, 8:16], scalar1=1.0, scalar2=None, op0=ALU.add),
                 reads=[T_in], writes=[T_misc])
            S.op("dve", lambda e: e.tensor_tensor(out=lamtmp[:, 0:64], in0=lam4bc[:, 0:64], in1=lam4bc[:, 64:128], op=ALU.mult),
                 reads=[T_in], writes=[T_misc])
            S.op("dve", lambda e: e.tensor_tensor(out=lamtmp[:, 64:128], in0=lam4bc[:, 128:192], in1=lam4bc[:, 192:256], op=ALU.mult),
                 reads=[T_in], writes=[T_misc])
            S.op("dve", lambda e: e.reduce_sum(out=lams[:], in_=lamtmp[:].rearrange("p (a b) -> p a b", a=2), axis=AX.X),
                 reads=[T_misc], writes=[T_misc])
            S.op("act", lambda e: e.activation(out=lams[:], in_=lams[:], func=AF.Exp), reads=[T_misc], writes=[T_misc])
            S.op("dve", lambda e: e.tensor_tensor(out=nlam[:], in0=lams[:, 1:2], in1=lams[:, 0:1], op=ALU.subtract),
                 reads=[T_misc], writes=[T_misc])
            S.op("dve", lambda e: e.tensor_scalar(out=nlam[:], in0=nlam[:], scalar1=-LAMBDA_INIT, scalar2=None, op0=ALU.add),
                 reads=[T_misc], writes=[T_misc])

            srcsB = [(x[i * 128:(i + 1) * 128, :], None) for i in range(NT)]
            genB = norm_transpose(sa, "B", srcsB, hT, a1, 0, T_a1, T_hT, True, nxn=NT, split=True, q="act")
            next(genB)
            wav = w_ada.rearrange("(k p) n -> p k n", p=128)
            for j in range(6):
                wb, tw = wa[j % 2], T_wa[j % 2]
                S.dma("sp", "ld_wa%d" % (j % 2), lambda e, wb=wb, j=j: e.dma_start(out=wb[:, 0:4, :], in_=wav[:, 0:4, j * D:(j + 1) * D]),
                      writes=[tw])
                S.dma("sp", "ld_wa%d" % (j % 2), lambda e, wb=wb, j=j: e.dma_start(out=wb[:, 4:8, :], in_=wav[:, 4:8, j * D:(j + 1) * D]),
                      writes=[])
                tw.w = ("ld_wa%d" % (j % 2), S.cnt["ld_wa%d" % (j % 2)])
                if j in (2, 5):
                    gdst = g1bc if j == 2 else g2bc
                    tg = T_g1 if j == 2 else T_g2
                    for half in range(2):
                        for k in range(8):
                            S.op("pe", lambda e, wb=wb, k=k, half=half: e.matmul(
                                pG[:], lhsT=cbc[:, k, :], rhs=wb[:, k, half * 512:(half + 1) * 512], start=(k == 0), stop=(k == 7)),
                                reads=[T_cbc, tw], writes=[T_pG], signal=(k == 7))
                        S.op("dve", lambda e, gdst=gdst, half=half, j=j: e.tensor_tensor(
                            out=gdst[:, half * 512:(half + 1) * 512], in0=pG[:], in1=bgrow[:, 0 if j == 2 else 1, half * 512:(half + 1) * 512],
                            op=ALU.add), reads=[T_pG, T_in], writes=[tg])
                else:
                    for m in range(8):
                        for k in range(8):
                            S.op("pe", lambda e, wb=wb, k=k, m=m: e.matmul(
                                pA[:, m:m + 1], lhsT=wb[:, k, m * 128:(m + 1) * 128], rhs=cact[:, k:k + 1], start=(k == 0), stop=(k == 7)),
                                reads=[T_cact, tw], writes=[T_pA], signal=(k == 7))
                    S.op("dve", lambda e, j=j: e.tensor_tensor(out=adaF[:, j, :], in0=pA[:, 0:8], in1=badaFs[:, j * 8:(j + 1) * 8], op=ALU.add),
                         reads=[T_pA, T_in], writes=[T_ada])
            S.op("dve", lambda e: e.scalar_tensor_tensor(out=a1[:], in0=adaF[:, 1, :], scalar=1.0, in1=gmixs[:], op0=ALU.add, op1=ALU.mult),
                 reads=[T_ada, T_in], writes=[T_a1])
            S.op("dve", lambda e: e.scalar_tensor_tensor(out=a2[:], in0=adaF[:, 4, :], scalar=1.0, in1=gffns[:], op0=ALU.add, op1=ALU.mult),
                 reads=[T_ada, T_in], writes=[T_a2])
            for _ in genB:
                pass
            if "ada" in dbg:
                dump("adaF", adaF[:].rearrange("p a b -> p (a b)"), T_ada, [128, 48], F32)
                dump("g1bc", g1bc[:], T_g1, [128, D], F32)
                dump("g2bc", g2bc[:], T_g2, [128, D], F32)
                dump("nlam", nlam[:], T_misc, [128, 1], F32)

        S.barrier()
        if "hT" in dbg:
            for g in range(4):
                dump("hT%d" % g, hT[:, :, g * 512:(g + 1) * 512], T_hT[g], [128, 8, 512], BF16)

        if stop_after == "B":
            S.final_wait("sp", out_toks)
            return nc, dbg_outs

        with ExitStack() as s1:
            wblk2 = sbuf(s1, "wblk2", [128, 8, 1536], BF16)
            T_wblk2 = Tok("wblk2")
            wst_r = Ring([sbuf(s1, "wst%d" % i, [128, 2048], F32) for i in range(2)], "wst")

            def load_wblk(src2d, col0, W, dst, T_dst, dcol0=0, eng="pool"):
                srcv = src2d.rearrange("(k p) n -> p k n", p=128)
                for k in range(8):
                    stg, tst = wst_r.next()
                    S.dma("sp", "ld_" + tst.name, lambda e, stg=stg, k=k: e.dma_start(out=stg[:, 0:W], in_=srcv[:, k, col0:col0 + W]), writes=[tst])
                    en = eng if eng != "alt" else ("act" if k % 2 == 0 else "dve")
                    if en == "act":
                        S.op("act", lambda e, stg=stg, k=k: e.copy(out=dst[:, k, dcol0:dcol0 + W], in_=stg[:, 0:W]), reads=[tst], writes=[T_dst])
                    else:
                        S.op(en, lambda e, stg=stg, k=k: e.tensor_copy(out=dst[:, k, dcol0:dcol0 + W], in_=stg[:, 0:W]), reads=[tst], writes=[T_dst])

            with ExitStack() as sh:
                wblk = sbuf(sh, "wblk", [128, 8, 2048], BF16)
                T_wblk = Tok("wblk")
                load_wblk(w_in, 0, 2048, wblk, T_wblk, eng="alt")
                lblbc = sbuf(sh, "lblbc", [128, 1024], F32)
                lbbc = sbuf(sh, "lbbc", [128, 512], F32)
                omlbc = sbuf(sh, "omlbc", [128, 512], F32)
                T_lb = Tok("lb")
                S.dma("sp", "ld_lb", lambda e: e.dma_start(out=lblbc[:], in_=lbl[0:1, :].partition_broadcast(128)), writes=[T_lb])
                S.op("dve", lambda e: e.tensor_tensor(out=lbbc[:], in0=lblbc[:, 0:512], in1=lblbc[:, 512:1024], op=ALU.subtract), reads=[T_lb], writes=[T_lb])
                S.op("act", lambda e: e.activation(out=lbbc[:], in_=lbbc[:], func=AF.Sigmoid), reads=[T_lb], writes=[T_lb])
                S.op("dve", lambda e: e.tensor_scalar(out=omlbc[:], in0=lbbc[:], scalar1=-1.0, scalar2=1.0, op0=ALU.mult, op1=ALU.add), reads=[T_lb], writes=[T_lb])
                load_wblk(w_in, 2048, 1536, wblk2, T_wblk2, eng="pool")
                pq = psum(sh, "pq", [128, 512], F32); pf = psum(sh, "pf", [128, 512], F32)
                pi_ = psum(sh, "pi", [128, 512], F32); pog = psum(sh, "pog", [128, 512], F32)
                pT = psum(sh, "pT", [128, 1024], BF16); pT2 = psum(sh, "pT2", [128, 1024], BF16)
                po = psum(sh, "po", [128, 512], F32); pkv = psum(sh, "pkv", [128, 512], F32)
                psc = pf
                T_pq, T_pf, T_pi, T_pog, T_pT, T_pT2, T_po, T_pkv = [Tok(n) for n in "pq pf pi pog pT pT2 po pkv".split()]
                T_psc = T_pf
                def f32t(n): return sbuf(sh, n, [128, 512], F32), Tok(n)
                def bft(n, w=512): return sbuf(sh, n, [128, w], BF16), Tok(n)
                fs, T_fs = f32t("fs"); lf, T_lf = f32t("lf"); kk, T_kk = f32t("kk"); qs, T_qs = f32t("qs")
                eb, T_eb = f32t("eb"); enb, T_enb = f32t("enb")
                Sst, T_Sst = f32t("Sst"); tmpkv, T_tmpkv = f32t("tmpkv"); onf, T_onf = f32t("onf")
                qt, T_qt = bft("qt"); Stb, T_Stb = bft("Stb"); oa, T_oa = bft("oa")
                ogs_r = Ring([sbuf(sh, "ogs%d" % a, [128, 512], F32) for a in range(2)], "ogs")
                kt_r = Ring([sbuf(sh, "kt%d" % a, [128, 512], BF16) for a in range(2)], "kt")
                vb_r = Ring([sbuf(sh, "vb%d" % a, [128, 512], BF16) for a in range(2)], "vb")
                qkT_r = Ring([sbuf(sh, "qkT%d" % a, [128, 1024], BF16) for a in range(2)], "qkT")
                scm_r = Ring([sbuf(sh, "scm%d" % a, [128, 512], BF16) for a in range(2)], "scm")
                ebm_r = Ring([sbuf(sh, "ebm%d" % a, [128, 3, 4], F32) for a in range(2)], "ebm")
                bm3 = sbuf(sh, "bm3", [128, 3, 4], F32); T_bm3 = Tok("bm3")
                ssq4 = sbuf(sh, "ssq4", [128, 4], F32); T_ssq4 = Tok("ssq4")
                v4 = lambda ap: ap.rearrange("p (h v) -> p h v", h=4)
                hctx = {}

                def front(i):
                    tc0, tc1 = i * 128, (i + 1) * 128
                    g = i // 4
                    ogs, T_ogs = ogs_r.next(); kt, T_kt = kt_r.next(); vb, T_vb = vb_r.next()
                    qkT, T_qkT = qkT_r.next(); scm, T_scm = scm_r.next(); ebm, T_ebm = ebm_r.next()
                    hctx[i] = (ogs, T_ogs, kt, T_kt, vb, T_vb, qkT, T_qkT, scm, T_scm, ebm, T_ebm)
                    for (p, tp, c0) in [(pf, T_pf, 512), (pq, T_pq, 0), (pi_, T_pi, 1024), (pog, T_pog, 1536)]:
                        for k in range(8):
                            S.op("pe", lambda e, p=p, k=k, c0=c0: e.matmul(p[:], lhsT=hT[:, k, tc0:tc1], rhs=wblk[:, k, c0:c0 + 512], start=(k == 0), stop=(k == 7)),
                                 reads=[T_hT[g], T_wblk], writes=[tp], signal=(k == 7))
                        yield
                    S.op("act", lambda e: e.activation(out=fs[:], in_=pf[:], func=AF.Exp, scale=-1.0), reads=[T_pf], writes=[T_fs]); yield
                    S.op("act", lambda e: e.activation(out=fs[:], in_=fs[:], func=AF.Identity, bias=epsb[:, 2:3]), reads=[T_fs, T_eps], writes=[T_fs]); yield
                    S.op("dve", lambda e: e.reciprocal(out=fs[:], in_=fs[:]), reads=[T_fs], writes=[T_fs]); yield
                    S.op("dve", lambda e: e.tensor_tensor(out=fs[:], in0=fs[:], in1=omlbc[:], op=ALU.mult), reads=[T_fs, T_lb], writes=[T_fs]); yield
                    S.op("dve", lambda e: e.tensor_tensor(out=fs[:], in0=fs[:], in1=lbbc[:], op=ALU.add), reads=[T_fs, T_lb], writes=[T_fs]); yield
                    S.op("act", lambda e: e.activation(out=lf[:], in_=fs[:], func=AF.Ln), reads=[T_fs], writes=[T_lf]); yield
                    S.op("act", lambda e: e.activation(out=kk[:], in_=fs[:], func=AF.Identity, scale=-1.0, bias=epsb[:, 2:3]), reads=[T_fs, T_eps], writes=[T_kk]); yield
                    S.op("pe", lambda e: e.matmul(pf[:], lhsT=amat[:, 0:128], rhs=lf[:], start=True, stop=True), reads=[T_lf, T_c], writes=[T_pf]); yield
                    S.op("act", lambda e: e.activation(out=qs[:], in_=pq[:], func=AF.Exp, scale=-1.0), reads=[T_pq], writes=[T_qs]); yield
                    S.op("act", lambda e: e.activation(out=qs[:], in_=qs[:], func=AF.Identity, bias=epsb[:, 2:3]), reads=[T_qs, T_eps], writes=[T_qs]); yield
                    S.op("dve", lambda e: e.reciprocal(out=qs[:], in_=qs[:]), reads=[T_qs], writes=[T_qs]); yield
                    S.op("dve", lambda e: e.tensor_tensor(out=qs[:], in0=qs[:], in1=pq[:], op=ALU.mult), reads=[T_qs, T_pq], writes=[T_qs]); yield
                    for h in range(4):
                        S.op("pe", lambda e, h=h: e.matmul(pq[:, 2 * h:2 * h + 2], lhsT=lf[:, h * 128:(h + 1) * 128], rhs=amat[:, 128:130], start=True, stop=True),
                             reads=[T_lf, T_c], writes=[T_pq], signal=(h == 3))
                    yield
                    S.op("act", lambda e: e.copy(out=vb[:], in_=pi_[:]), reads=[T_pi], writes=[T_vb]); yield
                    S.op("act", lambda e: e.activation(out=ogs[:], in_=pog[:], func=AF.Exp, scale=-1.0), reads=[T_pog], writes=[T_ogs]); yield
                    S.op("act", lambda e: e.activation(out=ogs[:], in_=ogs[:], func=AF.Identity, bias=epsb[:, 2:3]), reads=[T_ogs, T_eps], writes=[T_ogs]); yield
                    S.op("dve", lambda e: e.reciprocal(out=ogs[:], in_=ogs[:]), reads=[T_ogs], writes=[T_ogs]); yield
                    S.op("dve", lambda e: e.tensor_tensor(out=ogs[:], in0=ogs[:], in1=pog[:], op=ALU.mult), reads=[T_ogs, T_pog], writes=[T_ogs]); yield
                    S.op("act", lambda e: e.activation(out=eb[:], in_=pf[:], func=AF.Exp), reads=[T_pf], writes=[T_eb]); yield
                    S.op("act", lambda e: e.activation(out=enb[:], in_=pf[:], func=AF.Exp, scale=-1.0), reads=[T_pf], writes=[T_enb]); yield
                    S.op("dve", lambda e: e.tensor_tensor(out=qt[:], in0=qs[:], in1=eb[:], op=ALU.mult), reads=[T_qs, T_eb], writes=[T_qt]); yield
                    S.op("dve", lambda e: e.tensor_tensor(out=kt[:], in0=kk[:], in1=enb[:], op=ALU.mult), reads=[T_kk, T_enb], writes=[T_kt]); yield
                    S.op("dve", lambda e: e.tensor_copy(out=bm3[:, 0:2, :], in_=pq[:, 0:8].rearrange("p (h t) -> p t h", t=2)), reads=[T_pq], writes=[T_bm3]); yield
                    S.op("dve", lambda e: e.tensor_tensor(out=bm3[:, 2, :], in0=bm3[:, 1, :], in1=bm3[:, 0, :], op=ALU.subtract), reads=[T_bm3], writes=[T_bm3]); yield
                    S.op("act", lambda e: e.activation(out=ebm[:].rearrange("p a b -> p (a b)"), in_=bm3[:].rearrange("p a b -> p (a b)"), func=AF.Exp),
                         reads=[T_bm3], writes=[T_ebm]); yield
                    for h in range(4):
                        S.op("pe", lambda e, h=h: e.transpose(pT[:, h * 128:(h + 1) * 128], qt[:, h * 128:(h + 1) * 128], identb[:]),
                             reads=[T_qt, T_c], writes=[T_pT], signal=False)
                    for h in range(4):
                        S.op("pe", lambda e, h=h: e.transpose(pT[:, 512 + h * 128:512 + (h + 1) * 128], kt[:, h * 128:(h + 1) * 128], identb[:]),
                             reads=[T_kt, T_c], writes=[T_pT], signal=(h == 3))
                    yield
                    S.op("act", lambda e: e.copy(out=qkT[:], in_=pT[:]), reads=[T_pT], writes=[T_qkT]); yield
                    for h in range(4):
                        S.op("pe", lambda e, h=h: e.matmul(psc[:, h * 128:(h + 1) * 128], lhsT=qkT[:, 512 + h * 128:512 + (h + 1) * 128],
                                                           rhs=qkT[:, h * 128:(h + 1) * 128], start=True, stop=True),
                             reads=[T_qkT], writes=[T_psc], signal=(h == 3))
                    yield
                    S.op("dve", lambda e: e.tensor_tensor(out=v4(scm[:]), in0=v4(psc[:]), in1=tri[:].unsqueeze(1).to_broadcast([128, 4, 128]), op=ALU.mult),
                         reads=[T_psc, T_c], writes=[T_scm]); yield

                def back(i):
                    tc0, tc1 = i * 128, (i + 1) * 128
                    (ogs, T_ogs, kt, T_kt, vb, T_vb, qkT, T_qkT, scm, T_scm, ebm, T_ebm) = hctx.pop(i)
                    if i > 0:
                        S.op("dve", lambda e: e.tensor_tensor(out=v4(Stb[:]), in0=v4(Sst[:]), in1=ebm[:, 0, :].unsqueeze(2).to_broadcast([128, 4, 128]), op=ALU.mult),
                             reads=[T_Sst, T_ebm], writes=[T_Stb]); yield
                    for h in range(4):
                        hs0, hs1 = h * 128, (h + 1) * 128
                        S.op("pe", lambda e, hs0=hs0, hs1=hs1: e.matmul(pkv[:, hs0:hs1], lhsT=kt[:, hs0:hs1], rhs=vb[:, hs0:hs1], start=True, stop=True),
                             reads=[T_kt, T_vb], writes=[T_pkv], signal=(h == 3))
                    yield
                    S.op("dve", lambda e: e.tensor_tensor(out=v4(tmpkv[:]), in0=v4(pkv[:]), in1=ebm[:, 2, :].unsqueeze(2).to_broadcast([128, 4, 128]), op=ALU.mult),
                         reads=[T_pkv, T_ebm], writes=[T_tmpkv]); yield
                    for h in range(4):
                        hs0, hs1 = h * 128, (h + 1) * 128
                        S.op("pe", lambda e, hs0=hs0, hs1=hs1: e.matmul(po[:, hs0:hs1], lhsT=scm[:, hs0:hs1], rhs=vb[:, hs0:hs1], start=True, stop=(i == 0)),
                             reads=[T_scm, T_vb], writes=[T_po], signal=(i == 0 and h == 3))
                        if i > 0:
                            S.op("pe", lambda e, hs0=hs0, hs1=hs1: e.matmul(po[:, hs0:hs1], lhsT=qkT[:, hs0:hs1], rhs=Stb[:, hs0:hs1], start=False, stop=True),
                                 reads=[T_qkT, T_Stb], writes=[T_po], signal=(h == 3))
                    yield
                    if i == 0:
                        S.op("dve", lambda e: e.tensor_copy(out=Sst[:], in_=tmpkv[:]), reads=[T_tmpkv], writes=[T_Sst]); yield
                    else:
                        S.op("dve", lambda e: e.tensor_tensor(out=v4(Sst[:]), in0=v4(Sst[:]), in1=ebm[:, 1, :].unsqueeze(2).to_broadcast([128, 4, 128]), op=ALU.mult),
                             reads=[T_Sst, T_ebm, T_Stb], writes=[T_Sst]); yield
                        S.op("dve", lambda e: e.tensor_tensor(out=Sst[:], in0=Sst[:], in1=tmpkv[:], op=ALU.add), reads=[T_Sst, T_tmpkv], writes=[T_Sst]); yield
                    S.op("act", lambda e: e.activation(out=onf[:], in_=po[:], func=AF.Square), reads=[T_po], writes=[T_onf]); yield
                    S.op("dve", lambda e: e.reduce_sum(out=ssq4[:], in_=v4(onf[:]), axis=AX.X), reads=[T_onf], writes=[T_ssq4]); yield
                    S.op("act", lambda e: e.activation(out=ssq4[:], in_=ssq4[:], func=AF.Ln, scale=1.0 / 128, bias=epsb[:, 0:1]), reads=[T_ssq4, T_eps], writes=[T_ssq4]); yield
                    S.op("act", lambda e: e.activation(out=ssq4[:], in_=ssq4[:], func=AF.Exp, scale=-0.5), reads=[T_ssq4], writes=[T_ssq4]); yield
                    S.op("dve", lambda e: e.tensor_tensor(out=v4(onf[:]), in0=v4(po[:]), in1=ssq4[:].unsqueeze(2).to_broadcast([128, 4, 128]), op=ALU.mult),
                         reads=[T_po, T_ssq4], writes=[T_onf]); yield
                    S.op("pool", lambda e: e.tensor_tensor(out=v4(onf[:]), in0=v4(onf[:]), in1=gnbc[:].unsqueeze(1).to_broadcast([128, 4, 128]), op=ALU.mult),
                         reads=[T_onf, T_in0], writes=[T_onf]); yield
                    S.op("pool", lambda e: e.tensor_tensor(out=oa[:], in0=onf[:], in1=ogs[:], op=ALU.mult), reads=[T_onf, T_ogs], writes=[T_oa]); yield
                    for h in range(4):
                        S.op("pe", lambda e, h=h: e.transpose(pT2[:, h * 128:(h + 1) * 128], oa[:, h * 128:(h + 1) * 128], identb[:]),
                             reads=[T_oa, T_c], writes=[T_pT2], signal=(h == 3))
                    yield
                    S.op("act", lambda e: e.copy(out=mixT[:, 0:4, tc0:tc1], in_=pT2[:, 0:512].rearrange("p (h v) -> p h v", h=4)),
                         reads=[T_pT2], writes=[T_mixA[i]]); yield

                interleave(front(0))
                for i in range(NT):
                    interleave(front(i + 1) if i + 1 < NT else None, back(i))
                if "oa" in dbg:
                    T_all = Tok("mixAall")
                    T_all.w = T_mixA[NT - 1].w
                    dump("oaT", mixT[:, 0:4, :], T_all, [128, 4, S_TOK], BF16)
            S.barrier()
            if stop_after == "C1":
                S.final_wait("sp", out_toks)
                return nc, dbg_outs

            with ExitStack() as sd:
                pp = [psum(sd, "pp%d" % i, [128, 512], F32) for i in range(2)]
                T_pp = [Tok("pp0"), Tok("pp1")]
                pss = psum(sd, "pss", [128, 512], F32); T_pss = Tok("pss")
                pOb = [psum(sd, "pO%d" % i, [128, 512], F32) for i in range(4)]
                T_pOb = [[Tok("pO%d_%d" % (a, i)) for i in range(2)] for a in range(2)]
                T_pO = [[T_pOb[a][i // 2] for i in range(4)] for a in range(2)]

                def pOap(a, ql, w):
                    c0 = (ql % 2) * 256
                    return pOb[a * 2 + ql // 2][:, c0:c0 + w]
                pTo = psum(sd, "pTo", [128, 512], BF16); T_pTo = Tok("pTo")
                qn = [sbuf(sd, "qnT%d" % i, [128, S_TOK], BF16) for i in range(2)]
                kn = [sbuf(sd, "knT%d" % i, [128, S_TOK], BF16) for i in range(2)]
                T_qn = [[Tok("qn%d_%d" % (i, t)) for t in range(8)] for i in range(2)]
                T_kn = [[Tok("kn%d_%d" % (i, t)) for t in range(8)] for i in range(2)]
                Vext = sbuf(sd, "Vext", [128, NT, 4, 130], BF16); T_V = Tok("Vext")
                sq_r = Ring([sbuf(sd, "sq%d" % i, [128, 512], BF16) for i in range(2)], "sq")
                rs_r = Ring([sbuf(sd, "rs%d" % i, [128, 512], F32) for i in range(2)], "rs")
                PTall = [sbuf(sd, "PTall%d" % i, [128, NT, 512], BF16) for i in range(2)]
                T_PT = [[Tok("PT%d_%d" % (i, j)) for j in range(NT)] for i in range(2)]
                o1 = sbuf(sd, "o1", [128, 4, 128], F32); T_o1 = [Tok("o1_%d" % i) for i in range(4)]
                od_r = Ring([sbuf(sd, "od%d" % i, [128, 128], F32) for i in range(2)], "od")
                odn_r = Ring([sbuf(sd, "odn%d" % i, [128, 128], BF16) for i in range(2)], "odn")
                jk = sbuf(sd, "jk", [128, 128], BF16); T_jk = Tok("jk")
                rr_r = Ring([sbuf(sd, "rr%d" % i, [128, 4], F32) for i in range(8)], "rr")
                npp = [0]

                def nextpp():
                    a = npp[0] % 2
                    npp[0] += 1
                    return pp[a], T_pp[a]
                S.op("dve", lambda e: e.memset(Vext[:, :, :, 128:130], 1.0), writes=[T_V])
                for i in range(NT):
                    p, tp = nextpp()
                    for k in range(8):
                        S.op("pe", lambda e, p=p, k=k, i=i: e.matmul(p[:], lhsT=hT[:, k, i * 128:(i + 1) * 128], rhs=wblk2[:, k, 1024:1536], start=(k == 0), stop=(k == 7)),
                             reads=[T_hT[i // 4], T_wblk2], writes=[tp], signal=(k == 7))
                    S.op("act", lambda e, p=p, i=i: e.copy(out=Vext[:, i, :, 0:128], in_=p[:].rearrange("p (h v) -> p h v", h=4)), reads=[tp], writes=[T_V])

                T_pssA, T_pssB = Tok("pssA"), Tok("pssB")

                def proj_chunk(h, n):
                    t8, which = divmod(n, 2)
                    hb = h % 2
                    if which == 0:
                        dst, T_dst, c0, gs = qn[hb], T_qn[hb][t8], h * 128, gqs
                    else:
                        dst, T_dst, c0, gs = kn[hb], T_kn[hb][t8], 512 + h * 128, gks
                    p = pss[:, 0:256]
                    pb = pss[:, 256:512]
                    for k in range(8):
                        S.op("pe", lambda e, k=k: e.matmul(p, lhsT=wblk2[:, k, c0:c0 + 128], rhs=hT[:, k, t8 * 256:(t8 + 1) * 256], start=(k == 0), stop=(k == 7)),
                             reads=[T_hT[t8 // 2], T_wblk2], writes=[T_pssA], signal=(k == 7))
                    yield
                    sq, tsq = sq_r.next()
                    S.op("act", lambda e: e.activation(out=sq[:, 0:256], in_=p, func=AF.Square), reads=[T_pssA], writes=[tsq]); yield
                    S.op("pe", lambda e: e.matmul(pb, lhsT=bones[:], rhs=sq[:, 0:256], start=True, stop=True), reads=[tsq, T_c], writes=[T_pssB]); yield
                    rs, trs = rs_r.next()
                    S.op("act", lambda e: e.activation(out=rs[:, 0:256], in_=pb, func=AF.Ln, bias=epsb[:, 1:2]), reads=[T_pssB, T_eps], writes=[trs]); yield
                    S.op("act", lambda e: e.activation(out=rs[:, 0:256], in_=rs[:, 0:256], func=AF.Exp, scale=-0.5), reads=[trs], writes=[trs]); yield
                    S.op("dve", lambda e: e.scalar_tensor_tensor(out=dst[:, t8 * 256:(t8 + 1) * 256], in0=p, scalar=gs[:, 0:1], in1=rs[:, 0:256], op0=ALU.mult, op1=ALU.mult),
                         reads=[T_pssA, trs, T_in0], writes=[T_dst]); yield

                def proj_two(h, n):
                    yield from proj_chunk(h, 2 * n)
                    yield from proj_chunk(h, 2 * n + 1)

                def att_geom(g, j):
                    if j < 4 * g:
                        return 4 * g * 128, 512, False
                    return j * 128, (4 * g + 4 - j) * 128, True

                def att_qk(h, g, c, b):
                    hb = h % 2
                    cs0, cs1 = c * 64, (c + 1) * 64
                    nj = 4 * g + 4

                    def qk(j):
                        q0, nq, diag = att_geom(g, j)
                        p, tp = nextpp()
                        S.op("pe", lambda e: e.matmul(p[:, 0:nq], lhsT=kn[hb][cs0:cs1, j * 128:(j + 1) * 128], rhs=qn[hb][cs0:cs1, q0:q0 + nq], start=True, stop=True),
                             reads=[T_kn[hb][j // 2], T_qn[hb][2 * g], T_qn[hb][2 * g + 1]], writes=[tp])
                        return p, tp
                    cur = qk(0)
                    yield
                    for j in range(nj):
                        q0, nq, diag = att_geom(g, j)
                        p, tp = cur
                        PT, tPT = PTall[b][:, j, :], T_PT[b][j]
                        S.op("act", lambda e: e.activation(out=PT[:, 0:nq], in_=p[:, 0:nq], func=AF.Exp, scale=8.0), reads=[tp], writes=[tPT])
                        yield
                        if j + 1 < nj:
                            cur = qk(j + 1)
                            yield
                        if diag:
                            S.op("dve", lambda e: e.tensor_tensor(out=PT[:, 0:128], in0=PT[:, 0:128], in1=tri[:], op=ALU.mult), reads=[tPT, T_c], writes=[tPT])
                            yield

                def att_pv(h, g, c, a, b):
                    for ql in range(4):
                        qi = 4 * g + ql
                        for j in range(qi + 1):
                            q0, nq, diag = att_geom(g, j)
                            off = qi * 128 - q0
                            S.op("pe", lambda e: e.matmul(pOap(a, ql, 129), lhsT=PTall[b][:, j, off:off + 128], rhs=Vext[:, j, h, 0:129], start=(j == 0), stop=(j == qi)),
                                 reads=[T_PT[b][j], T_V], writes=[T_pO[a][ql]], signal=(j == qi))
                            if j % 2 == 1:
                                yield
                        yield

                def att_epi(h, g, c, a):
                    for ql in range(4):
                        qi = 4 * g + ql
                        rr, trr = rr_r.next()
                        S.op("dve", lambda e: e.reciprocal(out=rr[:, 0:1], in_=pOap(a, ql, 129)[:, 128:129]), reads=[T_pO[a][ql]], writes=[trr]); yield
                        if c == 0:
                            S.op("dve", lambda e: e.tensor_scalar(out=o1[:, ql, :], in0=pOap(a, ql, 128), scalar1=rr[:, 0:1], scalar2=None, op0=ALU.mult),
                                 reads=[T_pO[a][ql], trr], writes=[T_o1[ql]]); yield
                        else:
                            S.op("dve", lambda e: e.tensor_tensor(out=rr[:, 1:2], in0=rr[:, 0:1], in1=nlam[:], op=ALU.mult), reads=[trr, T_misc], writes=[trr]); yield
                            od, tod = od_r.next()
                            S.op("dve", lambda e: e.scalar_tensor_tensor(out=od[:], in0=pOap(a, ql, 128), scalar=rr[:, 1:2], in1=o1[:, ql, :], op0=ALU.mult, op1=ALU.add),
                                 reads=[T_pO[a][ql], trr, T_o1[ql]], writes=[tod]); yield
                            S.op("act", lambda e: e.activation(out=jk[:], in_=od[:], func=AF.Square, accum_out=rr[:, 2:3]), reads=[tod], writes=[T_jk, trr]); yield
                            S.op("act", lambda e: e.activation(out=rr[:, 3:4], in_=rr[:, 2:3], func=AF.Ln, scale=1.0 / 128, bias=epsb[:, 0:1]),
                                 reads=[trr, T_eps], writes=[trr]); yield
                            S.op("act", lambda e: e.activation(out=rr[:, 3:4], in_=rr[:, 3:4], func=AF.Exp, scale=-0.5), reads=[trr], writes=[trr]); yield
                            odn, todn = odn_r.next()
                            S.op("dve", lambda e: e.scalar_tensor_tensor(out=odn[:], in0=od[:], scalar=rr[:, 3:4], in1=sg8[:], op0=ALU.mult, op1=ALU.mult),
                                 reads=[tod, trr, T_misc], writes=[todn]); yield
                            S.op("pe", lambda e: e.transpose(pTo[:, 0:128], odn[:], identb[:]), reads=[todn, T_c], writes=[T_pTo]); yield
                            S.op("act", lambda e: e.copy(out=mixT[:, 4 + h, qi * 128:(qi + 1) * 128], in_=pTo[:, 0:128]), reads=[T_pTo], writes=[T_mixD[qi]]); yield

                for n in range(8):
                    interleave(proj_two(0, n))
                its = [(h, g, c) for h in range(4) for g in range(4) for c in range(2)]
                NI = len(its)
                for n in range(NI + 2):
                    gens = []
                    if n < NI:
                        h, g, c = its[n]
                        gens.append(att_qk(h, g, c, n % 2))
                    if 1 <= n <= NI:
                        h1, g1, c1 = its[n - 1]
                        gens.append(att_pv(h1, g1, c1, (n - 1) % 2, (n - 1) % 2))
                    if 2 <= n <= NI + 1:
                        h2, g2, c2 = its[n - 2]
                        gens.append(att_epi(h2, g2, c2, (n - 2) % 2))
                    if n < NI and its[n][0] < 3:
                        gens.append(proj_two(its[n][0] + 1, n % 8))
                    interleave(*gens)
                if "od" in dbg:
                    T_all = Tok("mixDall")
                    T_all.w = ("act", S.cnt["act"])
                    dump("odT", mixT[:, 4:8, :], T_all, [128, 4, S_TOK], BF16)
            S.barrier()
            if stop_after == "C2":
                S.final_wait("sp", out_toks)
                return nc, dbg_outs

        with ExitStack() as s2:
            acc = sbuf(s2, "acc", [128, NT, D], F32)
            T_acc = [[Tok("acc%d_%d" % (i, hf)) for hf in range(2)] for i in range(NT)]
            with ExitStack() as so:
                wst2_r = Ring([sbuf(so, "wst2_%d" % i, [128, 1024], F32) for i in range(3)], "wst2")
                wg = sbuf(so, "wg", [128, 8, 1024], BF16); T_wg = Tok("wg")
                wo = sbuf(so, "wo", [128, 8, 512], BF16); T_wo = Tok("wo")
                pz = [[psum(so, "pz%d_%d" % (a, b), [128, 512], F32) for b in range(4)] for a in range(2)]
                T_pz = [[Tok("pz%d_%d" % (a, b)) for b in range(4)] for a in range(2)]
                sa_r = Ring([sbuf(so, "sa%d" % i, [128, 512], F32) for i in range(2)], "sa")
                sd_r = Ring([sbuf(so, "sdg%d" % i, [128, 512], F32) for i in range(2)], "sdg")
                t1_r = Ring([sbuf(so, "t1_%d" % i, [128, 512], F32) for i in range(2)], "t1")
                t2_r = Ring([sbuf(so, "t2_%d" % i, [128, 512], F32) for i in range(2)], "t2")
                xh_r = Ring([sbuf(so, "xh%d" % i, [128, 512], F32) for i in range(2)], "xh")

                def load_w2(src2d, col0, W, dst, T_dst, dcol0):
                    srcv = src2d.rearrange("(k p) n -> p k n", p=128)
                    for k in range(8):
                        stg, tst = wst2_r.next()
                        S.dma("sp", "ld_" + tst.name, lambda e, stg=stg, k=k: e.dma_start(out=stg[:, 0:W], in_=srcv[:, k, col0:col0 + W]), writes=[tst])
                        if k % 2 == 0:
                            S.op("act", lambda e, stg=stg, k=k: e.copy(out=dst[:, k, dcol0:dcol0 + W], in_=stg[:, 0:W]), reads=[tst], writes=[T_dst])
                        else:
                            S.op("dve", lambda e, stg=stg, k=k: e.tensor_copy(out=dst[:, k, dcol0:dcol0 + W], in_=stg[:, 0:W]), reads=[tst], writes=[T_dst])
                for hf in range(2):
                    load_w2(w_in, 3584 + hf * 512, 512, wg, T_wg, 0)
                    load_w2(w_in, 4608 + hf * 512, 512, wg, T_wg, 512)
                    load_w2(w_out, hf * 512, 512, wo, T_wo, 0)
                    for i in range(NT):
                        tc0, tc1 = i * 128, (i + 1) * 128
                        pzz, tzz = pz[i % 2], T_pz[i % 2]
                        for k in range(8):
                            S.op("pe", lambda e, k=k, p=pzz[0]: e.matmul(p[:], lhsT=hT[:, k, tc0:tc1], rhs=wg[:, k, 0:512], start=(k == 0), stop=(k == 7)),
                                 reads=[T_hT[i // 4], T_wg], writes=[tzz[0]], signal=(k == 7))
                        for k in range(8):
                            S.op("pe", lambda e, k=k, p=pzz[1]: e.matmul(p[:], lhsT=hT[:, k, tc0:tc1], rhs=wg[:, k, 512:1024], start=(k == 0), stop=(k == 7)),
                                 reads=[T_hT[i // 4], T_wg], writes=[tzz[1]], signal=(k == 7))
                        for k in range(4):
                            S.op("pe", lambda e, k=k, p=pzz[2]: e.matmul(p[:], lhsT=mixT[:, k, tc0:tc1], rhs=wo[:, k, :], start=(k == 0), stop=(k == 3)),
                                 reads=[T_mixA[i], T_wo], writes=[tzz[2]], signal=(k == 3))
                        for k in range(4, 8):
                            S.op("pe", lambda e, k=k, p=pzz[3]: e.matmul(p[:], lhsT=mixT[:, k, tc0:tc1], rhs=wo[:, k, :], start=(k == 4), stop=(k == 7)),
                                 reads=[T_mixD[i], T_wo], writes=[tzz[3]], signal=(k == 7))
                        sa, tsa = sa_r.next(); sdg, tsd = sd_r.next(); t1, tt1 = t1_r.next(); t2, tt2 = t2_r.next(); xh, txh = xh_r.next()
                        S.dma("sp", "ld_" + txh.name, lambda e, xh=xh, i=i, hf=hf: e.dma_start(out=xh[:], in_=x[i * 128:(i + 1) * 128, hf * 512:(hf + 1) * 512]), writes=[txh])
                        S.op("act", lambda e, sa=sa, p=pzz[0]: e.activation(out=sa[:], in_=p[:], func=AF.Sigmoid), reads=[tzz[0]], writes=[tsa])
                        S.op("act", lambda e, sdg=sdg, p=pzz[1]: e.activation(out=sdg[:], in_=p[:], func=AF.Sigmoid), reads=[tzz[1]], writes=[tsd])
                        S.op("dve", lambda e, t1=t1, sa=sa, p=pzz[2]: e.tensor_tensor(out=t1[:], in0=sa[:], in1=p[:], op=ALU.mult), reads=[tsa, tzz[2]], writes=[tt1])
                        S.op("dve", lambda e, t2=t2, sdg=sdg, p=pzz[3]: e.tensor_tensor(out=t2[:], in0=sdg[:], in1=p[:], op=ALU.mult), reads=[tsd, tzz[3]], writes=[tt2])
                        S.op("pool", lambda e, t1=t1, t2=t2: e.tensor_tensor(out=t1[:], in0=t1[:], in1=t2[:], op=ALU.add), reads=[tt1, tt2], writes=[tt1])
                        S.op("dve", lambda e, t1=t1, hf=hf: e.tensor_tensor(out=t1[:], in0=t1[:], in1=g1bc[:, hf * 512:(hf + 1) * 512], op=ALU.mult), reads=[tt1, T_g1], writes=[tt1])
                        S.op("dve", lambda e, t1=t1, xh=xh, i=i, hf=hf: e.tensor_tensor(out=acc[:, i, hf * 512:(hf + 1) * 512], in0=t1[:], in1=xh[:], op=ALU.add),
                             reads=[tt1, txh], writes=[T_acc[i][hf]])
            S.barrier()
            if "x1" in dbg:
                for i in range(NT):
                    tt = Tok("x1d%d" % i)
                    tt.w = T_acc[i][1].w
                    dump("x1_%d" % i, acc[:, i, :], tt, [128, D], F32)
            if stop_after == "C3":
                S.final_wait("sp", out_toks)
                return nc, dbg_outs

            with ExitStack() as sf:
                NSLOT = 10
                ring2 = sbuf(sf, "ring2", [128, NSLOT - 8, 2048], BF16)

                def slot_ap(sl):
                    base = bufB[:, sl, :] if sl < 8 else ring2[:, sl - 8, :]
                    return base.rearrange("p (k c) -> p k c", k=8)
                T_slot = [Tok("slot%d" % i) for i in range(NSLOT)]
                w2b = sbuf(sf, "w2b", [128, 8, D], BF16); T_w2b = Tok("w2b")
                stg_r = Ring([sbuf(sf, "stg%d" % i, [128, 1024], F32) for i in range(4)], "stg")
                wrb = sbuf(sf, "wrb", [128, 8, NE], BF16); T_wrb = Tok("wrb")
                wrs = sbuf(sf, "wrs", [128, 8, NE], F32)
                b2g = sbuf(sf, "b2g", [NE, D], F32); T_b2g = Tok("b2g")
                gates = sbuf(sf, "gates", [128, 8, NE], F32); T_gates = [Tok("gates%d" % i) for i in range(8)]
                h2T = bufA
                T_h2T = [Tok("h2T0"), Tok("h2T1")]
                T_actT = [[Tok("actT%d_%d" % (j, tg)) for tg in range(2)] for j in range(8)]
                S.dma("sp", "ld_wrs", lambda e: e.dma_start(out=wrs[:], in_=w_r.rearrange("(k p) n -> p k n", p=128)), writes=[T_wrb])
                S.op("dve", lambda e: e.tensor_copy(out=wrb[:], in_=wrs[:]), reads=[T_wrb], writes=[T_wrb])
                S.dma("sp", "ld_b2g", lambda e: e.dma_start(out=b2g[:], in_=b2[:, :]), writes=[T_b2g])
                S.op("dve", lambda e: e.tensor_tensor(out=b2g[:], in0=b2g[:], in1=g2bc[0:NE, :], op=ALU.mult), reads=[T_b2g, T_g2], writes=[T_b2g])
                slot_ctr = [0]
                for th in range(2):
                    tiles = list(range(th * 8, th * 8 + 8))
                    with ExitStack() as sn:
                        srcs = [(acc[:, i, :], T_acc[i]) for i in tiles]
                        for _ in norm_transpose(sn, "D%d" % th, srcs, h2T, a2, 3, T_a2, T_h2T, False, nxn=4):
                            pass
                    S.barrier()
                    with ExitStack() as se:
                        pgb = [psum(se, "pgb%d_%d" % (th, a), [128, 512], F32) for a in range(2)]; T_pgb = [Tok("pgb0"), Tok("pgb1")]
                        plb = [psum(se, "plb%d_%d" % (th, a), [128, 512], F32) for a in range(2)]; T_plb = [Tok("plb0"), Tok("plb1")]
                        pyb = [psum(se, "pyb%d_%d" % (th, a), [128, 512], F32) for a in range(4)]; T_pyb = [Tok("pyb%d" % a) for a in range(4)]
                        gm_r = Ring([sbuf(se, "gm%d_%d" % (th, a), [128, 512], F32) for a in range(2)], "gm")
                        sg_r = Ring([sbuf(se, "sg%d_%d" % (th, a), [128, 512], F32) for a in range(2)], "sg")
                        lm_r = Ring([sbuf(se, "lm%d_%d" % (th, a), [128, 512], F32) for a in range(1)], "lm")
                        tt_r = Ring([sbuf(se, "tt%d_%d" % (th, a), [128, 512], F32) for a in range(1)], "tt")
                        n1 = 0
                        n2 = 0
                        if th == 0:
                            print("sbuf bytes remaining in FFN expert scope:", nc.sbuf_bytes_remaining)
                        ne_run = NE if stop_after == "all" else int(stop_after[1:]) if stop_after.startswith("E") else NE
                        pend = []

                        def flush_casts():
                            while pend:
                                pend.pop(0)()

                        def load_piece(qidx):
                            e_, j_ = divmod(qidx, 8)
                            if e_ >= ne_run:
                                return
                            w1v = w1p[e_].rearrange("(k p) n -> p k n", p=128)
                            sl = qidx % NSLOT
                            sap = slot_ap(sl)
                            for part in range(2):
                                stg, tst = stg_r.next()
                                cc0 = part * D + j_ * 128
                                S.dma("sp", "ld_" + tst.name, lambda e, stg=stg, cc0=cc0: e.dma_start(
                                    out=stg[:].rearrange("p (k c) -> p k c", k=8), in_=w1v[:, :, cc0:cc0 + 128]), writes=[tst])
                                pend.append(lambda stg=stg, tst=tst, sap=sap, part=part, sl=sl: S.op("act", lambda e: e.copy(
                                    out=sap[:, :, part * 128:(part + 1) * 128], in_=stg[:].rearrange("p (k c) -> p k c", k=8)),
                                    reads=[tst], writes=[T_slot[sl]]))

                        def load_w2chunk(e_, k):
                            if e_ >= ne_run:
                                return
                            stg, tst = stg_r.next()
                            S.dma("sp", "ld_" + tst.name, lambda e, stg=stg, k=k: e.dma_start(out=stg[:], in_=w2[e_, k * 128:(k + 1) * 128, :]), writes=[tst])
                            pend.append(lambda stg=stg, tst=tst, k=k: S.op("dve" if k % 4 != 3 else "pool", lambda e: e.tensor_tensor(
                                out=w2b[:, k, :], in0=stg[:], in1=g2bc[:], op=ALU.mult), reads=[tst, T_g2], writes=[T_w2b]))
                        for qidx in range(NSLOT):
                            load_piece(qidx)
                            flush_casts()
                        rtmp = []
                        for ch in range(2):
                            rtmp.append(dict(
                                lg=sbuf(se, "lg%d_%d" % (th, ch), [128, NE], F32), T_lg=Tok("lg"),
                                top8=sbuf(se, "top8_%d_%d" % (th, ch), [128, 8], F32), T_top8=Tok("top8"),
                                msk=sbuf(se, "msk%d_%d" % (th, ch), [128, NE], F32), T_msk=Tok("msk"),
                                ex=sbuf(se, "ex%d_%d" % (th, ch), [128, NE], F32), T_ex=Tok("ex"),
                                sm=sbuf(se, "sm%d_%d" % (th, ch), [128, 4], F32), T_sm=Tok("sm"),
                                gT=sbuf(se, "gT%d_%d" % (th, ch), [NE, 128], F32), T_gT=Tok("gT")))

                        def router_tile(il, ch):
                            i = tiles[il]
                            c0, c1 = il * 128, (il + 1) * 128
                            R = rtmp[ch]
                            lg, top8, msk, ex, sm, gT = R["lg"], R["top8"], R["msk"], R["ex"], R["sm"], R["gT"]
                            T_lg, T_top8, T_msk, T_ex, T_sm, T_gT = R["T_lg"], R["T_top8"], R["T_msk"], R["T_ex"], R["T_sm"], R["T_gT"]
                            plog, T_plog = pgb[ch], T_pgb[ch]
                            pgT, T_pgT = plb[ch], T_plb[ch]
                            pbk, T_pbk = pyb[ch], T_pyb[ch]
                            for k in range(8):
                                S.op("pe", lambda e, k=k: e.matmul(plog[:, 0:NE], lhsT=h2T[:, k, c0:c1], rhs=wrb[:, k, :], start=(k == 0), stop=(k == 7)),
                                     reads=[T_h2T[il // 4], T_wrb], writes=[T_plog], signal=(k == 7))
                            yield
                            S.op("dve", lambda e: e.tensor_tensor(out=lg[:], in0=plog[:, 0:NE], in1=brbc[:], op=ALU.add), reads=[T_plog, T_in0], writes=[T_lg]); yield
                            S.op("dve", lambda e: e.max(out=top8[:], in_=lg[:]), reads=[T_lg], writes=[T_top8]); yield
                            S.op("dve", lambda e: e.tensor_scalar(out=msk[:], in0=lg[:], scalar1=top8[:, 3:4], scalar2=None, op0=ALU.is_ge), reads=[T_lg, T_top8], writes=[T_msk]); yield
                            S.op("dve", lambda e: e.tensor_scalar(out=sm[:, 0:1], in0=top8[:, 0:1], scalar1=-1.0, scalar2=None, op0=ALU.mult), reads=[T_top8], writes=[T_sm]); yield
                            S.op("act", lambda e: e.activation(out=ex[:], in_=lg[:], func=AF.Exp, bias=sm[:, 0:1]), reads=[T_lg, T_sm], writes=[T_ex]); yield
                            S.op("dve", lambda e: e.tensor_tensor(out=ex[:], in0=ex[:], in1=msk[:], op=ALU.mult), reads=[T_ex, T_msk], writes=[T_ex]); yield
                            S.op("dve", lambda e: e.reduce_sum(out=sm[:, 1:2], in_=ex[:], axis=AX.X), reads=[T_ex], writes=[T_sm]); yield
                            S.op("dve", lambda e: e.reciprocal(out=sm[:, 2:3], in_=sm[:, 1:2]), reads=[T_sm], writes=[T_sm]); yield
                            S.op("dve", lambda e: e.tensor_scalar(out=gates[:, il, :], in0=ex[:], scalar1=sm[:, 2:3], scalar2=None, op0=ALU.mult),
                                 reads=[T_ex, T_sm], writes=[T_gates[il]]); yield
                            S.op("pe", lambda e: e.matmul(pgT[0:NE, 0:128], lhsT=gates[:, il, :], rhs=identf[:], start=True, stop=True),
                                 reads=[T_gates[il], T_c], writes=[T_pgT]); yield
                            S.op("act", lambda e: e.copy(out=gT[:], in_=pgT[0:NE, 0:128]), reads=[T_pgT], writes=[T_gT]); yield
                            for nh in range(2):
                                S.op("pe", lambda e: e.matmul(pbk[:], lhsT=gT[:], rhs=b2g[:, nh * 512:(nh + 1) * 512], start=True, stop=True),
                                     reads=[T_gT, T_b2g], writes=[T_pbk]); yield
                                S.op("dve", lambda e: e.tensor_tensor(out=acc[:, i, nh * 512:(nh + 1) * 512], in0=acc[:, i, nh * 512:(nh + 1) * 512], in1=pbk[:], op=ALU.add),
                                     reads=[T_pbk, T_acc[i][nh]], writes=[T_acc[i][nh]]); yield
                        for il2 in range(4):
                            interleave(router_tile(2 * il2, 0), router_tile(2 * il2 + 1, 1))
                        if "gates" in dbg and th == 0:
                            tt_ = Tok("gd"); tt_.w = T_gates[7].w
                            dump("gates", gates[:].rearrange("p a b -> p (a b)"), tt_, [128, 8 * NE], F32)
                        for ex_i in range(ne_run):
                            slots = [(ex_i * 8 + j) % NSLOT for j in range(8)]
                            for j in range(8):
                                sl = slots[j]
                                sap = slot_ap(sl)
                                for tg in range(2):
                                    a = n1 % 2
                                    n1 += 1
                                    for k in range(8):
                                        S.op("pe", lambda e, k=k, a=a, sap=sap, tg=tg: e.matmul(pgb[a][:], lhsT=sap[:, k, 0:128], rhs=h2T[:, k, tg * 512:(tg + 1) * 512],
                                                                                          start=(k == 0), stop=(k == 7)),
                                             reads=[T_slot[sl], T_h2T[tg]], writes=[T_pgb[a]] + ([T_plb[a]] if k == 0 else []), signal=(k == 7))
                                    for k in range(8):
                                        S.op("pe", lambda e, k=k, a=a, sap=sap, tg=tg: e.matmul(plb[a][:], lhsT=sap[:, k, 128:256], rhs=h2T[:, k, tg * 512:(tg + 1) * 512],
                                                                                          start=(k == 0), stop=(k == 7)),
                                             reads=[T_slot[sl], T_h2T[tg]], writes=[T_plb[a]], signal=(k == 7))
                                    gm, tgm = gm_r.next(); sg, tsg = sg_r.next(); lm, tlm = lm_r.next(); tt, ttt = tt_r.next()
                                    bg = b1s[:, ex_i * 16 + j:ex_i * 16 + j + 1]
                                    bl = b1s[:, ex_i * 16 + 8 + j:ex_i * 16 + 8 + j + 1]
                                    S.op("dve", lambda e, gm=gm, a=a, bg=bg: e.tensor_scalar(out=gm[:], in0=pgb[a][:], scalar1=bg, scalar2=7.0, op0=ALU.add, op1=ALU.min),
                                         reads=[T_pgb[a], T_misc], writes=[tgm])
                                    S.op("act", lambda e, gm=gm, sg=sg: e.activation(out=sg[:], in_=gm[:], func=AF.Sigmoid, scale=1.702), reads=[tgm], writes=[tsg])
                                    S.op("dve", lambda e, lm=lm, a=a, bl=bl: e.tensor_scalar(out=lm[:], in0=plb[a][:], scalar1=bl, scalar2=8.0, op0=ALU.add, op1=ALU.min),
                                         reads=[T_plb[a], T_misc], writes=[tlm])
                                    S.op("pool", lambda e, gm=gm, sg=sg, tt=tt: e.tensor_tensor(out=tt[:], in0=gm[:], in1=sg[:], op=ALU.mult), reads=[tgm, tsg], writes=[ttt])
                                    S.op("dve", lambda e, lm=lm, tt=tt, j=j, tg=tg: e.scalar_tensor_tensor(
                                        out=bufA[:, j, 1024 + tg * 512:1024 + (tg + 1) * 512], in0=lm[:], scalar=-6.0, in1=tt[:], op0=ALU.max, op1=ALU.mult),
                                        reads=[tlm, ttt], writes=[T_actT[j][tg]])
                                flush_casts()
                                if j < 4:
                                    load_w2chunk(ex_i, 2 * j)
                                    load_w2chunk(ex_i, 2 * j + 1)
                                load_piece(ex_i * 8 + j + NSLOT)
                            flush_casts()
                            for il, i in enumerate(tiles):
                                for nh in range(2):
                                    a = n2 % 4
                                    hoist2 = [T_pyb[(a + 1) % 4]] if n2 % 2 == 0 else []
                                    n2 += 1
                                    for k in range(8):
                                        S.op("pe", lambda e, k=k, a=a, il=il, nh=nh: e.matmul(
                                            pyb[a][:], lhsT=bufA[:, k, 1024 + il * 128:1024 + (il + 1) * 128], rhs=w2b[:, k, nh * 512:(nh + 1) * 512],
                                            start=(k == 0), stop=(k == 7)),
                                            reads=[T_actT[k][il // 4], T_w2b], writes=[T_pyb[a]] + (hoist2 if k == 0 else []), signal=(k == 7))
                                    S.op("dve", lambda e, a=a, il=il, i=i, nh=nh: e.scalar_tensor_tensor(
                                        out=acc[:, i, nh * 512:(nh + 1) * 512], in0=pyb[a][:], scalar=gates[:, il, ex_i:ex_i + 1], in1=acc[:, i, nh * 512:(nh + 1) * 512],
                                        op0=ALU.mult, op1=ALU.add),
                                        reads=[T_pyb[a], T_gates[il], T_acc[i][nh]], writes=[T_acc[i][nh]])
                        for i in tiles:
                            to = Tok("out%d" % i)
                            S.dma("sp", "st_out", lambda e, i=i: e.dma_start(out=out[i * 128:(i + 1) * 128, :], in_=acc[:, i, :]), reads=[T_acc[i][0], T_acc[i][1]], writes=[to])
                            out_toks.append(to)
                    S.barrier()

        S.final_wait("sp", out_toks)
    return nc, dbg_outs


def _consts():
    s = np.arange(128)
    mid = 63
    amat = np.zeros((128, 130), np.float32)
    amat[:, :128] = (s[:, None] <= s[None, :]).astype(np.float32) - (s[:, None] <= mid).astype(np.float32)
    amat[:, 128] = (s <= mid).astype(np.float32)
    amat[:, 129] = 1.0
    tri = (s[None, :] >= s[:, None]).astype(np.float32)
    bones = np.zeros((128, 128), np.float32)
    bones[:64, :64] = 1.0
    bones[64:, 64:] = 1.0
    return {
        "identb": np.eye(128, dtype=np.float32).astype(NPBF),
        "identf": np.eye(128, dtype=np.float32),
        "amat": amat,
        "tri": tri,
        "bones": bones.astype(NPBF),
    }


def _prep_shared(inp):
    f = lambda a: np.ascontiguousarray(np.asarray(a, dtype=np.float32))
    w1 = f(inp["w1"])[0]
    w1p = np.ascontiguousarray(np.concatenate([w1[:, :, 0::2], w1[:, :, 1::2]], axis=2))
    b1 = f(inp["b1"])[0]
    b1p = np.concatenate([b1[:, 0::2], b1[:, 1::2]], axis=1)
    b1F = np.ascontiguousarray(b1p.reshape(NE, 16, 128).transpose(2, 0, 1).reshape(128, NE * 16))
    sh = {
        "w_ada": f(inp["w_ada"])[0],
        "badaF": np.ascontiguousarray(f(inp["b_ada"])[0].reshape(48, 128).T),
        "bada_row": f(inp["b_ada"])[0].reshape(1, -1),
        "gmixF": np.ascontiguousarray(f(inp["mix_norm_g"])[0].reshape(8, 128).T),
        "gffnF": np.ascontiguousarray(f(inp["ffn_norm_g"])[0].reshape(8, 128).T),
        "w_in": f(inp["w_in"])[0],
        "lbl": f(inp["hg_lower_bound_logits"]).reshape(1, 1024),
        "hgn": f(inp["hg_out_norm_g"]).reshape(1, 128),
        "gq": np.tile(f(inp["da_q_norm_g"]).reshape(64), 2).reshape(128, 1),
        "gk": np.tile(f(inp["da_k_norm_g"]).reshape(64), 2).reshape(128, 1),
        "lam4": np.concatenate([f(inp["da_lambda_q1"]).reshape(64), f(inp["da_lambda_k1"]).reshape(64),
                                f(inp["da_lambda_q2"]).reshape(64), f(inp["da_lambda_k2"]).reshape(64)]).reshape(1, 256),
        "subg": f(inp["da_subln_g"]).reshape(1, 128),
        "w_out": f(inp["w_out"])[0],
        "w_r": f(inp["w_router"])[0],
        "b_r": f(inp["b_router"]).reshape(1, NE),
        "w1p": w1p,
        "b1F": b1F,
        "w2": f(inp["w2"])[0],
        "b2": f(inp["b2"])[0],
    }
    sh.update(_consts())
    return sh


def _prep_core(inp, sh, b):
    m = dict(sh)
    m["x"] = np.ascontiguousarray(np.asarray(inp["x"], dtype=np.float32)[b])
    m["cT"] = np.ascontiguousarray(np.asarray(inp["c"], dtype=np.float32)[b].reshape(8, 128).T)
    return m


def kernel(**inputs):
    nc, _ = build_nc()
    sh = _prep_shared(inputs)
    in_maps = [_prep_core(inputs, sh, b) for b in range(8)]
    res = run_bass_kernel_spmd(nc, in_maps, core_ids=list(range(8)))
    return np.stack([np.asarray(r["out"], dtype=np.float32) for r in res.results], axis=0)
```

```python
import numpy as np
import ml_dtypes
from contextlib import ExitStack
import concourse.bass as bass
import concourse.mybir as mybir
from concourse.bass_utils import run_bass_kernel_spmd

F32 = mybir.dt.float32
BF16 = mybir.dt.bfloat16
AF = mybir.ActivationFunctionType
ALU = mybir.AluOpType
AX = mybir.AxisListType
NPBF = ml_dtypes.bfloat16

S_TOK = 2048
D = 1024
NT = 16
NE = 32
EPS = 1e-6
LAMBDA_INIT = 0.2


class Tok:
    __slots__ = ("name", "w", "r")

    def __init__(self, name=""):
        self.name = name
        self.w = None
        self.r = []


class Sched:
    def __init__(self, nc, stack, same_engine_sync=True):
        self.nc = nc
        self.stack = stack
        self.eng = {"pe": nc.tensor, "act": nc.scalar, "dve": nc.vector, "pool": nc.gpsimd, "sp": nc.sync}
        self.sems = {}
        self.cnt = {}
        self.waited = {e: {} for e in self.eng}
        self.same = same_engine_sync
        self.n_inst = 0
        self.n_wait = 0

    def sem(self, key):
        if key not in self.sems:
            self.sems[key] = self.stack.enter_context(self.nc.semaphore("s_%s" % key))
            self.cnt[key] = 0
        return self.sems[key]

    def _deps(self, reads, writes):
        deps = {}

        def add(t):
            if t is None:
                return
            k, v = t
            if deps.get(k, 0) < v:
                deps[k] = v
        for t in reads:
            add(t.w)
        for t in writes:
            add(t.w)
            for r in t.r:
                add(r)
        return deps

    def _emit_waits(self, e, deps):
        eng = self.eng[e]
        for k, v in deps.items():
            if k == e and (e == "pe" or not self.same):
                continue
            if self.waited[e].get(k, 0) >= v:
                continue
            eng.wait_ge(self.sems[k], v)
            self.waited[e][k] = v
            self.n_wait += 1

    def op(self, e, fn, reads=(), writes=(), signal=True):
        self.sem(e)
        deps = self._deps(reads, writes)
        self._emit_waits(e, deps)
        ins = fn(self.eng[e])
        self.n_inst += 1
        if signal:
            self.cnt[e] += 1
            ins.then_inc(self.sems[e], 1)
            tk = (e, self.cnt[e])
        else:
            tk = (e, self.cnt[e] + 1)
        for t in writes:
            t.w = tk
            t.r = []
        for t in reads:
            if len(t.r) > 64:
                best = {}
                for k, v in t.r:
                    if best.get(k, 0) < v:
                        best[k] = v
                t.r = list(best.items())
            t.r.append(tk)
        return tk

    def dma(self, q, semkey, fn, reads=(), writes=()):
        self.sem(semkey)
        deps = self._deps(reads, writes)
        self._emit_waits(q, deps) if False else None
        eng = self.eng[q]
        for k, v in deps.items():
            if self.waited[q].get(k, 0) >= v:
                continue
            eng.wait_ge(self.sems[k], v)
            self.waited[q][k] = v
            self.n_wait += 1
        ins = fn(eng)
        self.n_inst += 1
        self.cnt[semkey] += 16
        ins.then_inc(self.sems[semkey], 16)
        tk = (semkey, self.cnt[semkey])
        for t in writes:
            t.w = tk
            t.r = []
        for t in reads:
            t.r.append(tk)
        return tk

    def barrier(self):
        for e in self.eng:
            for k, v in self.cnt.items():
                if v == 0 or k == e and e == "pe":
                    continue
                if self.waited[e].get(k, 0) >= v:
                    continue
                self.eng[e].wait_ge(self.sems[k], v)
                self.waited[e][k] = v
                self.n_wait += 1

    def final_wait(self, q, toks):
        deps = {}
        for t in toks:
            if t.w is not None:
                k, v = t.w
                deps[k] = max(deps.get(k, 0), v)
        for k, v in deps.items():
            self.eng[q].wait_ge(self.sems[k], v)


class Ring:
    def __init__(self, bufs, name):
        self.bufs = bufs
        self.toks = [Tok("%s%d" % (name, i)) for i in range(len(bufs))]
        self.i = 0

    def next(self):
        b, t = self.bufs[self.i], self.toks[self.i]
        self.i = (self.i + 1) % len(self.bufs)
        return b, t


def interleave(*gens):
    gens = [g for g in gens if g is not None]
    while gens:
        for g in list(gens):
            try:
                next(g)
            except StopIteration:
                gens.remove(g)


def build_nc(stop_after="all", dbg=()):
    nc = bass.Bass("TRN2", target_bir_lowering=False)

    def din(name, shape, dt=F32):
        return nc.dram_tensor(name, list(shape), dt, kind="ExternalInput").ap()

    x = din("x", [S_TOK, D])
    cT = din("cT", [128, 8])
    w_ada = din("w_ada", [D, 6 * D])
    badaF = din("badaF", [128, 48])
    bada_row = din("bada_row", [1, 6 * D])
    gmixF = din("gmixF", [128, 8])
    gffnF = din("gffnF", [128, 8])
    w_in = din("w_in", [D, 5632])
    lbl = din("lbl", [1, 1024])
    hgn = din("hgn", [1, 128])
    gq = din("gq", [128, 1])
    gk = din("gk", [128, 1])
    lam4 = din("lam4", [1, 256])
    subg = din("subg", [1, 128])
    w_out = din("w_out", [D, D])
    w_r = din("w_r", [D, NE])
    b_r = din("b_r", [1, NE])
    w1p = din("w1p", [NE, D, 2 * D])
    b1F = din("b1F", [128, NE * 16])
    w2 = din("w2", [NE, D, D])
    b2 = din("b2", [NE, D])
    identb_d = din("identb", [128, 128], BF16)
    identf_d = din("identf", [128, 128])
    amat_d = din("amat", [128, 130])
    tri_d = din("tri", [128, 128])
    bones_d = din("bones", [128, 128], BF16)
    out = nc.dram_tensor("out", [S_TOK, D], F32, kind="ExternalOutput").ap()
    dbg_outs = {}

    with ExitStack() as st:
        S = Sched(nc, st)
        out_toks = []

        def sbuf(stack, name, shape, dt):
            return stack.enter_context(nc.sbuf_tensor("sb_" + name, list(shape), dt))

        def psum(stack, name, shape, dt):
            return stack.enter_context(nc.psum_tensor("ps_" + name, list(shape), dt))

        ld_i = [0]

        def load(dst, src, tok, q="sp"):
            ld_i[0] += 1
            return S.dma(q, "ld_%s" % tok.name, lambda e: e.dma_start(out=dst, in_=src), writes=[tok])

        def dump(name, ap, tok, shape, dt):
            d = nc.dram_tensor("dbg_" + name, list(shape), dt, kind="ExternalOutput").ap()
            t = Tok("dbgo_" + name)
            S.dma("sp", "dbg_" + name, lambda e: e.dma_start(out=d, in_=ap), reads=[tok], writes=[t])
            out_toks.append(t)
            dbg_outs[name] = d

        identb = sbuf(st, "identb", [128, 128], BF16)
        identf = sbuf(st, "identf", [128, 128], F32)
        amat = sbuf(st, "amat", [128, 130], F32)
        tri = sbuf(st, "tri", [128, 128], F32)
        bones = sbuf(st, "bones", [128, 128], BF16)
        T_c = Tok("consts")
        for (d_, s_, nm) in [(identb, identb_d, "c0"), (identf, identf_d, "c1"), (amat, amat_d, "c2"), (tri, tri_d, "c3"),
                             (bones, bones_d, "c4")]:
            S.dma("sp", "ld_c", lambda e, d_=d_, s_=s_: e.dma_start(out=d_[:], in_=s_[:, :]), writes=[])
        T_c.w = ("ld_c", S.cnt["ld_c"])

        adaF = sbuf(st, "adaF", [128, 6, 8], F32)
        a1 = sbuf(st, "a1", [128, 8], F32)
        a2 = sbuf(st, "a2", [128, 8], F32)
        g1bc = sbuf(st, "g1bc", [128, D], F32)
        g2bc = sbuf(st, "g2bc", [128, D], F32)
        gnbc = sbuf(st, "gnbc", [128, 128], F32)
        sg8 = sbuf(st, "sg8", [128, 128], F32)
        gqs = sbuf(st, "gqs", [128, 1], F32)
        gks = sbuf(st, "gks", [128, 1], F32)
        nlam = sbuf(st, "nlam", [128, 1], F32)
        brbc = sbuf(st, "brbc", [128, NE], F32)
        b1s = sbuf(st, "b1s", [128, NE * 16], F32)
        bufA = sbuf(st, "bufA", [128, 8, S_TOK], BF16)
        bufB = sbuf(st, "bufB", [128, 8, S_TOK], BF16)
        T_ada = Tok("ada")
        T_a1 = Tok("a1")
        T_a2 = Tok("a2")
        T_g1 = Tok("g1")
        T_g2 = Tok("g2")
        T_misc = Tok("misc")
        T_in0 = Tok("in0")
        T_hT = [Tok("hT%d" % g) for g in range(4)]
        T_mixA = [Tok("mixA%d" % i) for i in range(NT)]
        T_mixD = [Tok("mixD%d" % i) for i in range(NT)]
        hT = bufA
        mixT = bufB

        def norm_transpose(stack, tag, tile_srcs, dstT, a_sc, sh_idx, T_a, T_dst_groups, src_is_dram, nxn=5, split=False, q="sp"):
            xt_r = Ring([sbuf(stack, "%s_xt%d" % (tag, i), [128, D], F32) for i in range(2)], tag + "xt") if src_is_dram else None
            xn_r = Ring([sbuf(stack, "%s_xn%d" % (tag, i), [128, D], BF16) for i in range(nxn)], tag + "xn")
            ssq = sbuf(stack, tag + "_ssq", [128, 64], F32)
            pT = [psum(stack, "%s_pT%d" % (tag, i), [128, 512], BF16) for i in range(2)]
            T_pT = [Tok(tag + "pT0"), Tok(tag + "pT1")]
            T_ssq = Tok(tag + "ssq")
            npt = [0]
            ntile = len(tile_srcs)

            def pre(i):
                src, tsrc = tile_srcs[i]
                if src_is_dram:
                    xt, txt = xt_r.next()
                    S.dma(q, "ld_" + txt.name, lambda e: e.dma_start(out=xt[:], in_=src), writes=[txt])
                    xin, tin = xt[:], [txt]
                else:
                    xin, tin = src, list(tsrc)
                xn, txn = xn_r.next()
                S.op("act", lambda e: e.activation(out=xn[:], in_=xin, func=AF.Square, accum_out=ssq[:, i:i + 1]), reads=tin, writes=[txn, T_ssq])
                S.op("act", lambda e: e.activation(out=ssq[:, 32 + i:33 + i], in_=ssq[:, i:i + 1], func=AF.Ln, scale=1.0 / D, bias=epsb[:, 0:1]),
                     reads=[T_ssq, T_eps], writes=[T_ssq])
                S.op("act", lambda e: e.activation(out=ssq[:, 32 + i:33 + i], in_=ssq[:, 32 + i:33 + i], func=AF.Exp, scale=-0.5), reads=[T_ssq], writes=[T_ssq])
                S.op("dve", lambda e: e.tensor_scalar(out=xn[:], in0=xin, scalar1=ssq[:, 32 + i:33 + i], scalar2=None, op0=ALU.mult),
                     reads=tin + [T_ssq], writes=[txn])
                return (xn, txn)

            def post(g, xns):
                for k in range(8):
                    p, tp = pT[npt[0] % 2], T_pT[npt[0] % 2]
                    npt[0] += 1
                    for ii in range(4):
                        xn, txn = xns[ii]
                        S.op("pe", lambda e: e.transpose(p[:, ii * 128:(ii + 1) * 128], xn[:, k * 128:(k + 1) * 128], identb[:]),
                             reads=[txn, T_c], writes=[tp], signal=(ii == 3))
                    S.op("act", lambda e: e.activation(out=dstT[:, k, g * 512:(g + 1) * 512], in_=p[:], func=AF.Identity,
                                                       scale=a_sc[:, k:k + 1], bias=adaF[:, sh_idx, k:k + 1]),
                         reads=[tp, T_a, T_ada], writes=[T_dst_groups[g]])
            if split:
                allx = [pre(i) for i in range(ntile)]
                yield
                for g in range(ntile // 4):
                    post(g, allx[g * 4:g * 4 + 4])
            else:
                for g in range(ntile // 4):
                    xns = [pre(g * 4 + ii) for ii in range(4)]
                    post(g, xns)
                    yield

        epsb = sbuf(st, "epsb", [128, 4], F32)
        T_eps = Tok("eps")
        S.op("dve", lambda e: e.memset(epsb[:, 0:1], EPS), writes=[T_eps])
        S.op("dve", lambda e: e.memset(epsb[:, 1:2], 64 * EPS), writes=[T_eps])
        S.op("dve", lambda e: e.memset(epsb[:, 2:3], 1.0), writes=[T_eps])

        with ExitStack() as sa:
            cTs = sbuf(sa, "cTs", [128, 8], F32)
            cact = sbuf(sa, "cact", [128, 8], F32)
            cbc = sbuf(sa, "cbc", [128, 8, 128], F32)
            wa = [sbuf(sa, "wa%d" % i, [128, 8, 1024], F32) for i in range(2)]
            T_wa = [Tok("wa0"), Tok("wa1")]
            badaFs = sbuf(sa, "badaFs", [128, 48], F32)
            bgrow = sbuf(sa, "bgrow", [128, 2, 1024], F32)
            gmixs = sbuf(sa, "gmixs", [128, 8], F32)
            gffns = sbuf(sa, "gffns", [128, 8], F32)
            lam4bc = sbuf(sa, "lam4bc", [128, 256], F32)
            lamtmp = sbuf(sa, "lamtmp", [128, 128], F32)
            lams = sbuf(sa, "lams", [128, 2], F32)
            subgbc = sbuf(sa, "subgbc", [128, 128], F32)
            pA = psum(sa, "pA", [128, 512], F32)
            pG = psum(sa, "pG", [128, 512], F32)
            T_pA, T_pG = Tok("pA"), Tok("pG")
            T_in = Tok("smallin")
            smalls = [(cTs[:], cT[:, :]), (badaFs[:], badaF[:, :]), (gmixs[:], gmixF[:, :]), (gffns[:], gffnF[:, :]),
                      (gqs[:], gq[:, :]), (gks[:], gk[:, :]), (b1s[:], b1F[:, :]),
                      (lam4bc[:], lam4[0:1, :].partition_broadcast(128)),
                      (subgbc[:], subg[0:1, :].partition_broadcast(128)),
                      (gnbc[:], hgn[0:1, :].partition_broadcast(128)),
                      (brbc[:], b_r[0:1, :].partition_broadcast(128)),
                      (bgrow[:, 0, :], bada_row[0:1, 2 * D:3 * D].partition_broadcast(128)),
                      (bgrow[:, 1, :], bada_row[0:1, 5 * D:6 * D].partition_broadcast(128))]
            for d_, s_ in smalls:
                S.dma("sp", "ld_s", lambda e, d_=d_, s_=s_: e.dma_start(out=d_, in_=s_), writes=[])
            T_in.w = ("ld_s", S.cnt["ld_s"])
            T_in0.w = T_in.w

            T_cact = Tok("cact")
            S.op("act", lambda e: e.activation(out=cact[:], in_=cTs[:], func=AF.Silu), reads=[T_in], writes=[T_cact])
            T_cbc = Tok("cbc")
            S.op("dve", lambda e: e.tensor_copy(out=cbc[:], in_=cact[:].unsqueeze(2).to_broadcast([128, 8, 128])),
                 reads=[T_cact], writes=[T_cbc])
            S.op("dve", lambda e: e.tensor_scalar(out=sg8[:], in0=subgbc[:], scalar1=1.0 - LAMBDA_INIT, scalar2=None, op0=ALU.mult),
                 reads=[T_in], writes=[T_misc])
            b1v = b1s[:].rearrange("p (e j) -> p e j", j=16)
            S.op("dve", lambda e: e.tensor_scalar(out=b1v[:, :, 8:16], in0=b1v[:, :, 8:16], scalar1=1.0, scalar2=None, op0=ALU.add),
                 reads=[T_in], writes=[T_misc])
            S.op("dve", lambda e: e.tensor_tensor(out=lamtmp[:, 0:64], in0=lam4bc[:, 0:64], in1=lam4bc[:, 64:128], op=ALU.mult),
                 reads=[T_in], writes=[T_misc])
            S.op("dve", lambda e: e.tensor_tensor(out=lamtmp[:, 64:128], in0=lam4bc[:, 128:192], in1=lam4bc[:, 192:256], op=ALU.mult),
                 reads=[T_in], writes=[T_misc])
            S.op("dve", lambda e: e.reduce_sum(out=lams[:], in_=lamtmp[:].rearrange("p (a b) -> p a b", a=2), axis=AX.X),
                 reads=[T_misc], writes=[T_misc])
            S.op("act", lambda e: e.activation(out=lams[:], in_=lams[:], func=AF.Exp), reads=[T_misc], writes=[T_misc])
            S.op("dve", lambda e: e.tensor_tensor(out=nlam[:], in0=lams[:, 1:2], in1=lams[:, 0:1], op=ALU.subtract),
                 reads=[T_misc], writes=[T_misc])
            S.op("dve", lambda e: e.tensor_scalar(out=nlam[:], in0=nlam[:], scalar1=-LAMBDA_INIT, scalar2=None, op0=ALU.add),
                 reads=[T_misc], writes=[T_misc])

            srcsB = [(x[i * 128:(i + 1) * 128, :], None) for i in range(NT)]
            genB = norm_transpose(sa, "B", srcsB, hT, a1, 0, T_a1, T_hT, True, nxn=NT, split=True, q="act")
            next(genB)
            wav = w_ada.rearrange("(k p) n -> p k n", p=128)
            for j in range(6):
                wb, tw = wa[j % 2], T_wa[j % 2]
                S.dma("sp", "ld_wa%d" % (j % 2), lambda e, wb=wb, j=j: e.dma_start(out=wb[:, 0:4, :], in_=wav[:, 0:4, j * D:(j + 1) * D]),
                      writes=[tw])
                S.dma("sp", "ld_wa%d" % (j % 2), lambda e, wb=wb, j=j: e.dma_start(out=wb[:, 4:8, :], in_=wav[:, 4:8, j * D:(j + 1) * D]),
                      writes=[])
                tw.w = ("ld_wa%d" % (j % 2), S.cnt["ld_wa%d" % (j % 2)])
                if j in (2, 5):
                    gdst = g1bc if j == 2 else g2bc
                    tg = T_g1 if j == 2 else T_g2
                    for half in range(2):
                        for k in range(8):
                            S.op("pe", lambda e, wb=wb, k=k, half=half: e.matmul(
                                pG[:], lhsT=cbc[:, k, :], rhs=wb[:, k, half * 512:(half + 1) * 512], start=(k == 0), stop=(k == 7)),
                                reads=[T_cbc, tw], writes=[T_pG], signal=(k == 7))
                        S.op("dve", lambda e, gdst=gdst, half=half, j=j: e.tensor_tensor(
                            out=gdst[:, half * 512:(half + 1) * 512], in0=pG[:], in1=bgrow[:, 0 if j == 2 else 1, half * 512:(half + 1) * 512],
                            op=ALU.add), reads=[T_pG, T_in], writes=[tg])
                else:
                    for m in range(8):
                        for k in range(8):
                            S.op("pe", lambda e, wb=wb, k=k, m=m: e.matmul(
                                pA[:, m:m + 1], lhsT=wb[:, k, m * 128:(m + 1) * 128], rhs=cact[:, k:k + 1], start=(k == 0), stop=(k == 7)),
                                reads=[T_cact, tw], writes=[T_pA], signal=(k == 7))
                    S.op("dve", lambda e, j=j: e.tensor_tensor(out=adaF[:, j, :], in0=pA[:, 0:8], in1=badaFs[:, j * 8:(j + 1) * 8], op=ALU.add),
                         reads=[T_pA, T_in], writes=[T_ada])
            S.op("dve", lambda e: e.scalar_tensor_tensor(out=a1[:], in0=adaF[:, 1, :], scalar=1.0, in1=gmixs[:], op0=ALU.add, op1=ALU.mult),
                 reads=[T_ada, T_in], writes=[T_a1])
            S.op("dve", lambda e: e.scalar_tensor_tensor(out=a2[:], in0=adaF[:, 4, :], scalar=1.0, in1=gffns[:], op0=ALU.add, op1=ALU.mult),
                 reads=[T_ada, T_in], writes=[T_a2])
            for _ in genB:
                pass
            if "ada" in dbg:
                dump("adaF", adaF[:].rearrange("p a b -> p (a b)"), T_ada, [128, 48], F32)
                dump("g1bc", g1bc[:], T_g1, [128, D], F32)
                dump("g2bc", g2bc[:], T_g2, [128, D], F32)
                dump("nlam", nlam[:], T_misc, [128, 1], F32)

        S.barrier()
        if "hT" in dbg:
            for g in range(4):
                dump("hT%d" % g, hT[:, :, g * 512:(g + 1) * 512], T_hT[g], [128, 8, 512], BF16)

        if stop_after == "B":
            S.final_wait("sp", out_toks)
            return nc, dbg_outs

        with ExitStack() as s1:
            wblk2 = sbuf(s1, "wblk2", [128, 8, 1536], BF16)
            T_wblk2 = Tok("wblk2")
            wst_r = Ring([sbuf(s1, "wst%d" % i, [128, 2048], F32) for i in range(2)], "wst")

            def load_wblk(src2d, col0, W, dst, T_dst, dcol0=0, eng="pool"):
                srcv = src2d.rearrange("(k p) n -> p k n", p=128)
                for k in range(8):
                    stg, tst = wst_r.next()
                    S.dma("sp", "ld_" + tst.name, lambda e, stg=stg, k=k: e.dma_start(out=stg[:, 0:W], in_=srcv[:, k, col0:col0 + W]), writes=[tst])
                    en = eng if eng != "alt" else ("act" if k % 2 == 0 else "dve")
                    if en == "act":
                        S.op("act", lambda e, stg=stg, k=k: e.copy(out=dst[:, k, dcol0:dcol0 + W], in_=stg[:, 0:W]), reads=[tst], writes=[T_dst])
                    else:
                        S.op(en, lambda e, stg=stg, k=k: e.tensor_copy(out=dst[:, k, dcol0:dcol0 + W], in_=stg[:, 0:W]), reads=[tst], writes=[T_dst])

            with ExitStack() as sh:
                wblk = sbuf(sh, "wblk", [128, 8, 2048], BF16)
                T_wblk = Tok("wblk")
                load_wblk(w_in, 0, 2048, wblk, T_wblk, eng="alt")
                lblbc = sbuf(sh, "lblbc", [128, 1024], F32)
                lbbc = sbuf(sh, "lbbc", [128, 512], F32)
                omlbc = sbuf(sh, "omlbc", [128, 512], F32)
                T_lb = Tok("lb")
                S.dma("sp", "ld_lb", lambda e: e.dma_start(out=lblbc[:], in_=lbl[0:1, :].partition_broadcast(128)), writes=[T_lb])
                S.op("dve", lambda e: e.tensor_tensor(out=lbbc[:], in0=lblbc[:, 0:512], in1=lblbc[:, 512:1024], op=ALU.subtract), reads=[T_lb], writes=[T_lb])
                S.op("act", lambda e: e.activation(out=lbbc[:], in_=lbbc[:], func=AF.Sigmoid), reads=[T_lb], writes=[T_lb])
                S.op("dve", lambda e: e.tensor_scalar(out=omlbc[:], in0=lbbc[:], scalar1=-1.0, scalar2=1.0, op0=ALU.mult, op1=ALU.add), reads=[T_lb], writes=[T_lb])
                load_wblk(w_in, 2048, 1536, wblk2, T_wblk2, eng="pool")
                pq = psum(sh, "pq", [128, 512], F32); pf = psum(sh, "pf", [128, 512], F32)
                pi_ = psum(sh, "pi", [128, 512], F32); pog = psum(sh, "pog", [128, 512], F32)
                pT = psum(sh, "pT", [128, 1024], BF16); pT2 = psum(sh, "pT2", [128, 1024], BF16)
                po = psum(sh, "po", [128, 512], F32); pkv = psum(sh, "pkv", [128, 512], F32)
                psc = pf
                T_pq, T_pf, T_pi, T_pog, T_pT, T_pT2, T_po, T_pkv = [Tok(n) for n in "pq pf pi pog pT pT2 po pkv".split()]
                T_psc = T_pf
                def f32t(n): return sbuf(sh, n, [128, 512], F32), Tok(n)
                def bft(n, w=512): return sbuf(sh, n, [128, w], BF16), Tok(n)
                fs, T_fs = f32t("fs"); lf, T_lf = f32t("lf"); kk, T_kk = f32t("kk"); qs, T_qs = f32t("qs")
                eb, T_eb = f32t("eb"); enb, T_enb = f32t("enb")
                Sst, T_Sst = f32t("Sst"); tmpkv, T_tmpkv = f32t("tmpkv"); onf, T_onf = f32t("onf")
                qt, T_qt = bft("qt"); Stb, T_Stb = bft("Stb"); oa, T_oa = bft("oa")
                ogs_r = Ring([sbuf(sh, "ogs%d" % a, [128, 512], F32) for a in range(2)], "ogs")
                kt_r = Ring([sbuf(sh, "kt%d" % a, [128, 512], BF16) for a in range(2)], "kt")
                vb_r = Ring([sbuf(sh, "vb%d" % a, [128, 512], BF16) for a in range(2)], "vb")
                qkT_r = Ring([sbuf(sh, "qkT%d" % a, [128, 1024], BF16) for a in range(2)], "qkT")
                scm_r = Ring([sbuf(sh, "scm%d" % a, [128, 512], BF16) for a in range(2)], "scm")
                ebm_r = Ring([sbuf(sh, "ebm%d" % a, [128, 3, 4], F32) for a in range(2)], "ebm")
                bm3 = sbuf(sh, "bm3", [128, 3, 4], F32); T_bm3 = Tok("bm3")
                ssq4 = sbuf(sh, "ssq4", [128, 4], F32); T_ssq4 = Tok("ssq4")
                v4 = lambda ap: ap.rearrange("p (h v) -> p h v", h=4)
                hctx = {}

                def front(i):
                    tc0, tc1 = i * 128, (i + 1) * 128
                    g = i // 4
                    ogs, T_ogs = ogs_r.next(); kt, T_kt = kt_r.next(); vb, T_vb = vb_r.next()
                    qkT, T_qkT = qkT_r.next(); scm, T_scm = scm_r.next(); ebm, T_ebm = ebm_r.next()
                    hctx[i] = (ogs, T_ogs, kt, T_kt, vb, T_vb, qkT, T_qkT, scm, T_scm, ebm, T_ebm)
                    for (p, tp, c0) in [(pf, T_pf, 512), (pq, T_pq, 0), (pi_, T_pi, 1024), (pog, T_pog, 1536)]:
                        for k in range(8):
                            S.op("pe", lambda e, p=p, k=k, c0=c0: e.matmul(p[:], lhsT=hT[:, k, tc0:tc1], rhs=wblk[:, k, c0:c0 + 512], start=(k == 0), stop=(k == 7)),
                                 reads=[T_hT[g], T_wblk], writes=[tp], signal=(k == 7))
                        yield
                    S.op("act", lambda e: e.activation(out=fs[:], in_=pf[:], func=AF.Exp, scale=-1.0), reads=[T_pf], writes=[T_fs]); yield
                    S.op("act", lambda e: e.activation(out=fs[:], in_=fs[:], func=AF.Identity, bias=epsb[:, 2:3]), reads=[T_fs, T_eps], writes=[T_fs]); yield
                    S.op("dve", lambda e: e.reciprocal(out=fs[:], in_=fs[:]), reads=[T_fs], writes=[T_fs]); yield
                    S.op("dve", lambda e: e.tensor_tensor(out=fs[:], in0=fs[:], in1=omlbc[:], op=ALU.mult), reads=[T_fs, T_lb], writes=[T_fs]); yield
                    S.op("dve", lambda e: e.tensor_tensor(out=fs[:], in0=fs[:], in1=lbbc[:], op=ALU.add), reads=[T_fs, T_lb], writes=[T_fs]); yield
                    S.op("act", lambda e: e.activation(out=lf[:], in_=fs[:], func=AF.Ln), reads=[T_fs], writes=[T_lf]); yield
                    S.op("act", lambda e: e.activation(out=kk[:], in_=fs[:], func=AF.Identity, scale=-1.0, bias=epsb[:, 2:3]), reads=[T_fs, T_eps], writes=[T_kk]); yield
                    S.op("pe", lambda e: e.matmul(pf[:], lhsT=amat[:, 0:128], rhs=lf[:], start=True, stop=True), reads=[T_lf, T_c], writes=[T_pf]); yield
                    S.op("act", lambda e: e.activation(out=qs[:], in_=pq[:], func=AF.Exp, scale=-1.0), reads=[T_pq], writes=[T_qs]); yield
                    S.op("act", lambda e: e.activation(out=qs[:], in_=qs[:], func=AF.Identity, bias=epsb[:, 2:3]), reads=[T_qs, T_eps], writes=[T_qs]); yield
                    S.op("dve", lambda e: e.reciprocal(out=qs[:], in_=qs[:]), reads=[T_qs], writes=[T_qs]); yield
                    S.op("dve", lambda e: e.tensor_tensor(out=qs[:], in0=qs[:], in1=pq[:], op=ALU.mult), reads=[T_qs, T_pq], writes=[T_qs]); yield
                    for h in range(4):
                        S.op("pe", lambda e, h=h: e.matmul(pq[:, 2 * h:2 * h + 2], lhsT=lf[:, h * 128:(h + 1) * 128], rhs=amat[:, 128:130], start=True, stop=True),
                             reads=[T_lf, T_c], writes=[T_pq], signal=(h == 3))
                    yield
                    S.op("act", lambda e: e.copy(out=vb[:], in_=pi_[:]), reads=[T_pi], writes=[T_vb]); yield
                    S.op("act", lambda e: e.activation(out=ogs[:], in_=pog[:], func=AF.Exp, scale=-1.0), reads=[T_pog], writes=[T_ogs]); yield
                    S.op("act", lambda e: e.activation(out=ogs[:], in_=ogs[:], func=AF.Identity, bias=epsb[:, 2:3]), reads=[T_ogs, T_eps], writes=[T_ogs]); yield
                    S.op("dve", lambda e: e.reciprocal(out=ogs[:], in_=ogs[:]), reads=[T_ogs], writes=[T_ogs]); yield
                    S.op("dve", lambda e: e.tensor_tensor(out=ogs[:], in0=ogs[:], in1=pog[:], op=ALU.mult), reads=[T_ogs, T_pog], writes=[T_ogs]); yield
                    S.op("act", lambda e: e.activation(out=eb[:], in_=pf[:], func=AF.Exp), reads=[T_pf], writes=[T_eb]); yield
                    S.op("act", lambda e: e.activation(out=enb[:], in_=pf[:], func=AF.Exp, scale=-1.0), reads=[T_pf], writes=[T_enb]); yield
                    S.op("dve", lambda e: e.tensor_tensor(out=qt[:], in0=qs[:], in1=eb[:], op=ALU.mult), reads=[T_qs, T_eb], writes=[T_qt]); yield
                    S.op("dve", lambda e: e.tensor_tensor(out=kt[:], in0=kk[:], in1=enb[:], op=ALU.mult), reads=[T_kk, T_enb], writes=[T_kt]); yield
                    S.op("dve", lambda e: e.tensor_copy(out=bm3[:, 0:2, :], in_=pq[:, 0:8].rearrange("p (h t) -> p t h", t=2)), reads=[T_pq], writes=[T_bm3]); yield
                    S.op("dve", lambda e: e.tensor_tensor(out=bm3[:, 2, :], in0=bm3[:, 1, :], in1=bm3[:, 0, :], op=ALU.subtract), reads=[T_bm3], writes=[T_bm3]); yield
                    S.op("act", lambda e: e.activation(out=ebm[:].rearrange("p a b -> p (a b)"), in_=bm3[:].rearrange("p a b -> p (a b)"), func=AF.Exp),
                         reads=[T_bm3], writes=[T_ebm]); yield
                    for h in range(4):
                        S.op("pe", lambda e, h=h: e.transpose(pT[:, h * 128:(h + 1) * 128], qt[:, h * 128:(h + 1) * 128], identb[:]),
                             reads=[T_qt, T_c], writes=[T_pT], signal=False)
                    for h in range(4):
                        S.op("pe", lambda e, h=h: e.transpose(pT[:, 512 + h * 128:512 + (h + 1) * 128], kt[:, h * 128:(h + 1) * 128], identb[:]),
                             reads=[T_kt, T_c], writes=[T_pT], signal=(h == 3))
                    yield
                    S.op("act", lambda e: e.copy(out=qkT[:], in_=pT[:]), reads=[T_pT], writes=[T_qkT]); yield
                    for h in range(4):
                        S.op("pe", lambda e, h=h: e.matmul(psc[:, h * 128:(h + 1) * 128], lhsT=qkT[:, 512 + h * 128:512 + (h + 1) * 128],
                                                           rhs=qkT[:, h * 128:(h + 1) * 128], start=True, stop=True),
                             reads=[T_qkT], writes=[T_psc], signal=(h == 3))
                    yield
                    S.op("dve", lambda e: e.tensor_tensor(out=v4(scm[:]), in0=v4(psc[:]), in1=tri[:].unsqueeze(1).to_broadcast([128, 4, 128]), op=ALU.mult),
                         reads=[T_psc, T_c], writes=[T_scm]); yield

                def back(i):
                    tc0, tc1 = i * 128, (i + 1) * 128
                    (ogs, T_ogs, kt, T_kt, vb, T_vb, qkT, T_qkT, scm, T_scm, ebm, T_ebm) = hctx.pop(i)
                    if i > 0:
                        S.op("dve", lambda e: e.tensor_tensor(out=v4(Stb[:]), in0=v4(Sst[:]), in1=ebm[:, 0, :].unsqueeze(2).to_broadcast([128, 4, 128]), op=ALU.mult),
                             reads=[T_Sst, T_ebm], writes=[T_Stb]); yield
                    for h in range(4):
                        hs0, hs1 = h * 128, (h + 1) * 128
                        S.op("pe", lambda e, hs0=hs0, hs1=hs1: e.matmul(pkv[:, hs0:hs1], lhsT=kt[:, hs0:hs1], rhs=vb[:, hs0:hs1], start=True, stop=True),
                             reads=[T_kt, T_vb], writes=[T_pkv], signal=(h == 3))
                    yield
                    S.op("dve", lambda e: e.tensor_tensor(out=v4(tmpkv[:]), in0=v4(pkv[:]), in1=ebm[:, 2, :].unsqueeze(2).to_broadcast([128, 4, 128]), op=ALU.mult),
                         reads=[T_pkv, T_ebm], writes=[T_tmpkv]); yield
                    for h in range(4):
                        hs0, hs1 = h * 128, (h + 1) * 128
                        S.op("pe", lambda e, hs0=hs0, hs1=hs1: e.matmul(po[:, hs0:hs1], lhsT=scm[:, hs0:hs1], rhs=vb[:, hs0:hs1], start=True, stop=(i == 0)),
                             reads=[T_scm, T_vb], writes=[T_po], signal=(i == 0 and h == 3))
                        if i > 0:
                            S.op("pe", lambda e, hs0=hs0, hs1=hs1: e.matmul(po[:, hs0:hs1], lhsT=qkT[:, hs0:hs1], rhs=Stb[:, hs0:hs1], start=False, stop=True),
                                 reads=[T_qkT, T_Stb], writes=[T_po], signal=(h == 3))
                    yield
                    if i == 0:
                        S.op("dve", lambda e: e.tensor_copy(out=Sst[:], in_=tmpkv[:]), reads=[T_tmpkv], writes=[T_Sst]); yield
                    else:
                        S.op("dve", lambda e: e.tensor_tensor(out=v4(Sst[:]), in0=v4(Sst[:]), in1=ebm[:, 1, :].unsqueeze(2).to_broadcast([128, 4, 128]), op=ALU.mult),
                             reads=[T_Sst, T_ebm, T_Stb], writes=[T_Sst]); yield
                        S.op("dve", lambda e: e.tensor_tensor(out=Sst[:], in0=Sst[:], in1=tmpkv[:], op=ALU.add), reads=[T_Sst, T_tmpkv], writes=[T_Sst]); yield
                    S.op("act", lambda e: e.activation(out=onf[:], in_=po[:], func=AF.Square), reads=[T_po], writes=[T_onf]); yield
                    S.op("dve", lambda e: e.reduce_sum(out=ssq4[:], in_=v4(onf[:]), axis=AX.X), reads=[T_onf], writes=[T_ssq4]); yield
                    S.op("act", lambda e: e.activation(out=ssq4[:], in_=ssq4[:], func=AF.Ln, scale=1.0 / 128, bias=epsb[:, 0:1]), reads=[T_ssq4, T_eps], writes=[T_ssq4]); yield
                    S.op("act", lambda e: e.activation(out=ssq4[:], in_=ssq4[:], func=AF.Exp, scale=-0.5), reads=[T_ssq4], writes=[T_ssq4]); yield
                    S.op("dve", lambda e: e.tensor_tensor(out=v4(onf[:]), in0=v4(po[:]), in1=ssq4[:].unsqueeze(2).to_broadcast([128, 4, 128]), op=ALU.mult),
                         reads=[T_po, T_ssq4], writes=[T_onf]); yield
                    S.op("pool", lambda e: e.tensor_tensor(out=v4(onf[:]), in0=v4(onf[:]), in1=gnbc[:].unsqueeze(1).to_broadcast([128, 4, 128]), op=ALU.mult),
                         reads=[T_onf, T_in0], writes=[T_onf]); yield
                    S.op("pool", lambda e: e.tensor_tensor(out=oa[:], in0=onf[:], in1=ogs[:], op=ALU.mult), reads=[T_onf, T_ogs], writes=[T_oa]); yield
                    for h in range(4):
                        S.op("pe", lambda e, h=h: e.transpose(pT2[:, h * 128:(h + 1) * 128], oa[:, h * 128:(h + 1) * 128], identb[:]),
                             reads=[T_oa, T_c], writes=[T_pT2], signal=(h == 3))
                    yield
                    S.op("act", lambda e: e.copy(out=mixT[:, 0:4, tc0:tc1], in_=pT2[:, 0:512].rearrange("p (h v) -> p h v", h=4)),
                         reads=[T_pT2], writes=[T_mixA[i]]); yield

                interleave(front(0))
                for i in range(NT):
                    interleave(front(i + 1) if i + 1 < NT else None, back(i))
                if "oa" in dbg:
                    T_all = Tok("mixAall")
                    T_all.w = T_mixA[NT - 1].w
                    dump("oaT", mixT[:, 0:4, :], T_all, [128, 4, S_TOK], BF16)
            S.barrier()
            if stop_after == "C1":
                S.final_wait("sp", out_toks)
                return nc, dbg_outs

            with ExitStack() as sd:
                pp = [psum(sd, "pp%d" % i, [128, 512], F32) for i in range(2)]
                T_pp = [Tok("pp0"), Tok("pp1")]
                pss = psum(sd, "pss", [128, 512], F32); T_pss = Tok("pss")
                pOb = [psum(sd, "pO%d" % i, [128, 512], F32) for i in range(4)]
                T_pOb = [[Tok("pO%d_%d" % (a, i)) for i in range(2)] for a in range(2)]
                T_pO = [[T_pOb[a][i // 2] for i in range(4)] for a in range(2)]

                def pOap(a, ql, w):
                    c0 = (ql % 2) * 256
                    return pOb[a * 2 + ql // 2][:, c0:c0 + w]
                pTo = psum(sd, "pTo", [128, 512], BF16); T_pTo = Tok("pTo")
                qn = [sbuf(sd, "qnT%d" % i, [128, S_TOK], BF16) for i in range(2)]
                kn = [sbuf(sd, "knT%d" % i, [128, S_TOK], BF16) for i in range(2)]
                T_qn = [[Tok("qn%d_%d" % (i, t)) for t in range(8)] for i in range(2)]
                T_kn = [[Tok("kn%d_%d" % (i, t)) for t in range(8)] for i in range(2)]
                Vext = sbuf(sd, "Vext", [128, NT, 4, 130], BF16); T_V = Tok("Vext")
                sq_r = Ring([sbuf(sd, "sq%d" % i, [128, 512], BF16) for i in range(2)], "sq")
                rs_r = Ring([sbuf(sd, "rs%d" % i, [128, 512], F32) for i in range(2)], "rs")
                PTall = [sbuf(sd, "PTall%d" % i, [128, NT, 512], BF16) for i in range(2)]
                T_PT = [[Tok("PT%d_%d" % (i, j)) for j in range(NT)] for i in range(2)]
                o1 = sbuf(sd, "o1", [128, 4, 128], F32); T_o1 = [Tok("o1_%d" % i) for i in range(4)]
                od_r = Ring([sbuf(sd, "od%d" % i, [128, 128], F32) for i in range(2)], "od")
                odn_r = Ring([sbuf(sd, "odn%d" % i, [128, 128], BF16) for i in range(2)], "odn")
                jk = sbuf(sd, "jk", [128, 128], BF16); T_jk = Tok("jk")
                rr_r = Ring([sbuf(sd, "rr%d" % i, [128, 4], F32) for i in range(8)], "rr")
                npp = [0]

                def nextpp():
                    a = npp[0] % 2
                    npp[0] += 1
                    return pp[a], T_pp[a]
                S.op("dve", lambda e: e.memset(Vext[:, :, :, 128:130], 1.0), writes=[T_V])
                for i in range(NT):
                    p, tp = nextpp()
                    for k in range(8):
                        S.op("pe", lambda e, p=p, k=k, i=i: e.matmul(p[:], lhsT=hT[:, k, i * 128:(i + 1) * 128], rhs=wblk2[:, k, 1024:1536], start=(k == 0), stop=(k == 7)),
                             reads=[T_hT[i // 4], T_wblk2], writes=[tp], signal=(k == 7))
                    S.op("act", lambda e, p=p, i=i: e.copy(out=Vext[:, i, :, 0:128], in_=p[:].rearrange("p (h v) -> p h v", h=4)), reads=[tp], writes=[T_V])

                T_pssA, T_pssB = Tok("pssA"), Tok("pssB")

                def proj_chunk(h, n):
                    t8, which = divmod(n, 2)
                    hb = h % 2
                    if which == 0:
                        dst, T_dst, c0, gs = qn[hb], T_qn[hb][t8], h * 128, gqs
                    else:
                        dst, T_dst, c0, gs = kn[hb], T_kn[hb][t8], 512 + h * 128, gks
                    p = pss[:, 0:256]
                    pb = pss[:, 256:512]
                    for k in range(8):
                        S.op("pe", lambda e, k=k: e.matmul(p, lhsT=wblk2[:, k, c0:c0 + 128], rhs=hT[:, k, t8 * 256:(t8 + 1) * 256], start=(k == 0), stop=(k == 7)),
                             reads=[T_hT[t8 // 2], T_wblk2], writes=[T_pssA], signal=(k == 7))
                    yield
                    sq, tsq = sq_r.next()
                    S.op("act", lambda e: e.activation(out=sq[:, 0:256], in_=p, func=AF.Square), reads=[T_pssA], writes=[tsq]); yield
                    S.op("pe", lambda e: e.matmul(pb, lhsT=bones[:], rhs=sq[:, 0:256], start=True, stop=True), reads=[tsq, T_c], writes=[T_pssB]); yield
                    rs, trs = rs_r.next()
                    S.op("act", lambda e: e.activation(out=rs[:, 0:256], in_=pb, func=AF.Ln, bias=epsb[:, 1:2]), reads=[T_pssB, T_eps], writes=[trs]); yield
                    S.op("act", lambda e: e.activation(out=rs[:, 0:256], in_=rs[:, 0:256], func=AF.Exp, scale=-0.5), reads=[trs], writes=[trs]); yield
                    S.op("dve", lambda e: e.scalar_tensor_tensor(out=dst[:, t8 * 256:(t8 + 1) * 256], in0=p, scalar=gs[:, 0:1], in1=rs[:, 0:256], op0=ALU.mult, op1=ALU.mult),
                         reads=[T_pssA, trs, T_in0], writes=[T_dst]); yield

                def proj_two(h, n):
                    yield from proj_chunk(h, 2 * n)
                    yield from proj_chunk(h, 2 * n + 1)

                def att_geom(g, j):
                    if j < 4 * g:
                        return 4 * g * 128, 512, False
                    return j * 128, (4 * g + 4 - j) * 128, True

                def att_qk(h, g, c, b):
                    hb = h % 2
                    cs0, cs1 = c * 64, (c + 1) * 64
                    nj = 4 * g + 4

                    def qk(j):
                        q0, nq, diag = att_geom(g, j)
                        p, tp = nextpp()
                        S.op("pe", lambda e: e.matmul(p[:, 0:nq], lhsT=kn[hb][cs0:cs1, j * 128:(j + 1) * 128], rhs=qn[hb][cs0:cs1, q0:q0 + nq], start=True, stop=True),
                             reads=[T_kn[hb][j // 2], T_qn[hb][2 * g], T_qn[hb][2 * g + 1]], writes=[tp])
                        return p, tp
                    cur = qk(0)
                    yield
                    for j in range(nj):
                        q0, nq, diag = att_geom(g, j)
                        p, tp = cur
                        PT, tPT = PTall[b][:, j, :], T_PT[b][j]
                        S.op("act", lambda e: e.activation(out=PT[:, 0:nq], in_=p[:, 0:nq], func=AF.Exp, scale=8.0), reads=[tp], writes=[tPT])
                        yield
                        if j + 1 < nj:
                            cur = qk(j + 1)
                            yield
                        if diag:
                            S.op("dve", lambda e: e.tensor_tensor(out=PT[:, 0:128], in0=PT[:, 0:128], in1=tri[:], op=ALU.mult), reads=[tPT, T_c], writes=[tPT])
                            yield

                def att_pv(h, g, c, a, b):
                    for ql in range(4):
                        qi = 4 * g + ql
                        for j in range(qi + 1):
                            q0, nq, diag = att_geom(g, j)
                            off = qi * 128 - q0
                            rd = [T_PT[b][j], T_V]
                            if ql == 0 and j == 0:
                                rd = [T_PT[b][jj] for jj in range(4 * g + 4)] + [T_V]
                            S.op("pe", lambda e: e.matmul(pOap(a, ql, 129), lhsT=PTall[b][:, j, off:off + 128], rhs=Vext[:, j, h, 0:129], start=(j == 0), stop=(j == qi)),
                                 reads=rd, writes=[T_pO[a][ql]], signal=(j == qi))
                            if j % 2 == 1:
                                yield
                        yield

                def att_epi(h, g, c, a):
                    for ql in range(4):
                        qi = 4 * g + ql
                        rr, trr = rr_r.next()
                        S.op("dve", lambda e: e.reciprocal(out=rr[:, 0:1], in_=pOap(a, ql, 129)[:, 128:129]), reads=[T_pO[a][ql]], writes=[trr]); yield
                        if c == 0:
                            S.op("dve", lambda e: e.tensor_scalar(out=o1[:, ql, :], in0=pOap(a, ql, 128), scalar1=rr[:, 0:1], scalar2=None, op0=ALU.mult),
                                 reads=[T_pO[a][ql], trr], writes=[T_o1[ql]]); yield
                        else:
                            S.op("dve", lambda e: e.tensor_tensor(out=rr[:, 1:2], in0=rr[:, 0:1], in1=nlam[:], op=ALU.mult), reads=[trr, T_misc], writes=[trr]); yield
                            od, tod = od_r.next()
                            S.op("dve", lambda e: e.scalar_tensor_tensor(out=od[:], in0=pOap(a, ql, 128), scalar=rr[:, 1:2], in1=o1[:, ql, :], op0=ALU.mult, op1=ALU.add),
                                 reads=[T_pO[a][ql], trr, T_o1[ql]], writes=[tod]); yield
                            S.op("act", lambda e: e.activation(out=jk[:], in_=od[:], func=AF.Square, accum_out=rr[:, 2:3]), reads=[tod], writes=[T_jk, trr]); yield
                            S.op("act", lambda e: e.activation(out=rr[:, 3:4], in_=rr[:, 2:3], func=AF.Ln, scale=1.0 / 128, bias=epsb[:, 0:1]),
                                 reads=[trr, T_eps], writes=[trr]); yield
                            S.op("act", lambda e: e.activation(out=rr[:, 3:4], in_=rr[:, 3:4], func=AF.Exp, scale=-0.5), reads=[trr], writes=[trr]); yield
                            odn, todn = odn_r.next()
                            S.op("dve", lambda e: e.scalar_tensor_tensor(out=odn[:], in0=od[:], scalar=rr[:, 3:4], in1=sg8[:], op0=ALU.mult, op1=ALU.mult),
                                 reads=[tod, trr, T_misc], writes=[todn]); yield
                            S.op("pe", lambda e: e.transpose(pTo[:, 0:128], odn[:], identb[:]), reads=[todn, T_c], writes=[T_pTo]); yield
                            S.op("act", lambda e: e.copy(out=mixT[:, 4 + h, qi * 128:(qi + 1) * 128], in_=pTo[:, 0:128]), reads=[T_pTo], writes=[T_mixD[qi]]); yield

                for n in range(8):
                    interleave(proj_two(0, n))
                its = [(h, g, c) for h in range(4) for g in range(4) for c in range(2)]
                NI = len(its)
                for n in range(NI + 2):
                    gens = []
                    if n < NI:
                        h, g, c = its[n]
                        gens.append(att_qk(h, g, c, n % 2))
                    if 1 <= n <= NI:
                        h1, g1, c1 = its[n - 1]
                        gens.append(att_pv(h1, g1, c1, (n - 1) % 2, (n - 1) % 2))
                    if 2 <= n <= NI + 1:
                        h2, g2, c2 = its[n - 2]
                        gens.append(att_epi(h2, g2, c2, (n - 2) % 2))
                    if n < NI and its[n][0] < 3:
                        gens.append(proj_two(its[n][0] + 1, n % 8))
                    interleave(*gens)
                if "od" in dbg:
                    T_all = Tok("mixDall")
                    T_all.w = ("act", S.cnt["act"])
                    dump("odT", mixT[:, 4:8, :], T_all, [128, 4, S_TOK], BF16)
            S.barrier()
            if stop_after == "C2":
                S.final_wait("sp", out_toks)
                return nc, dbg_outs

        with ExitStack() as s2:
            acc = sbuf(s2, "acc", [128, NT, D], F32)
            T_acc = [[Tok("acc%d_%d" % (i, hf)) for hf in range(2)] for i in range(NT)]
            with ExitStack() as so:
                wst2_r = Ring([sbuf(so, "wst2_%d" % i, [128, 1024], F32) for i in range(3)], "wst2")
                wg = sbuf(so, "wg", [128, 8, 1024], BF16); T_wg = Tok("wg")
                wo = sbuf(so, "wo", [128, 8, 512], BF16); T_wo = Tok("wo")
                pz = [[psum(so, "pz%d_%d" % (a, b), [128, 512], F32) for b in range(4)] for a in range(2)]
                T_pz = [[Tok("pz%d_%d" % (a, b)) for b in range(4)] for a in range(2)]
                sa_r = Ring([sbuf(so, "sa%d" % i, [128, 512], F32) for i in range(2)], "sa")
                sd_r = Ring([sbuf(so, "sdg%d" % i, [128, 512], F32) for i in range(2)], "sdg")
                t1_r = Ring([sbuf(so, "t1_%d" % i, [128, 512], F32) for i in range(2)], "t1")
                t2_r = Ring([sbuf(so, "t2_%d" % i, [128, 512], F32) for i in range(2)], "t2")
                xh_r = Ring([sbuf(so, "xh%d" % i, [128, 512], F32) for i in range(2)], "xh")

                def load_w2(src2d, col0, W, dst, T_dst, dcol0):
                    srcv = src2d.rearrange("(k p) n -> p k n", p=128)
                    for k in range(8):
                        stg, tst = wst2_r.next()
                        S.dma("sp", "ld_" + tst.name, lambda e, stg=stg, k=k: e.dma_start(out=stg[:, 0:W], in_=srcv[:, k, col0:col0 + W]), writes=[tst])
                        if k % 2 == 0:
                            S.op("act", lambda e, stg=stg, k=k: e.copy(out=dst[:, k, dcol0:dcol0 + W], in_=stg[:, 0:W]), reads=[tst], writes=[T_dst])
                        else:
                            S.op("dve", lambda e, stg=stg, k=k: e.tensor_copy(out=dst[:, k, dcol0:dcol0 + W], in_=stg[:, 0:W]), reads=[tst], writes=[T_dst])
                for hf in range(2):
                    load_w2(w_in, 3584 + hf * 512, 512, wg, T_wg, 0)
                    load_w2(w_in, 4608 + hf * 512, 512, wg, T_wg, 512)
                    load_w2(w_out, hf * 512, 512, wo, T_wo, 0)
                    for i in range(NT):
                        tc0, tc1 = i * 128, (i + 1) * 128
                        pzz, tzz = pz[i % 2], T_pz[i % 2]
                        for k in range(8):
                            S.op("pe", lambda e, k=k, p=pzz[0]: e.matmul(p[:], lhsT=hT[:, k, tc0:tc1], rhs=wg[:, k, 0:512], start=(k == 0), stop=(k == 7)),
                                 reads=[T_hT[i // 4], T_wg], writes=[tzz[0]], signal=(k == 7))
                        for k in range(8):
                            S.op("pe", lambda e, k=k, p=pzz[1]: e.matmul(p[:], lhsT=hT[:, k, tc0:tc1], rhs=wg[:, k, 512:1024], start=(k == 0), stop=(k == 7)),
                                 reads=[T_hT[i // 4], T_wg], writes=[tzz[1]], signal=(k == 7))
                        for k in range(4):
                            S.op("pe", lambda e, k=k, p=pzz[2]: e.matmul(p[:], lhsT=mixT[:, k, tc0:tc1], rhs=wo[:, k, :], start=(k == 0), stop=(k == 3)),
                                 reads=[T_mixA[i], T_wo], writes=[tzz[2]], signal=(k == 3))
                        for k in range(4, 8):
                            S.op("pe", lambda e, k=k, p=pzz[3]: e.matmul(p[:], lhsT=mixT[:, k, tc0:tc1], rhs=wo[:, k, :], start=(k == 4), stop=(k == 7)),
                                 reads=[T_mixD[i], T_wo], writes=[tzz[3]], signal=(k == 7))
                        sa, tsa = sa_r.next(); sdg, tsd = sd_r.next(); t1, tt1 = t1_r.next(); t2, tt2 = t2_r.next(); xh, txh = xh_r.next()
                        S.dma("sp", "ld_" + txh.name, lambda e, xh=xh, i=i, hf=hf: e.dma_start(out=xh[:], in_=x[i * 128:(i + 1) * 128, hf * 512:(hf + 1) * 512]), writes=[txh])
                        S.op("act", lambda e, sa=sa, p=pzz[0]: e.activation(out=sa[:], in_=p[:], func=AF.Sigmoid), reads=[tzz[0]], writes=[tsa])
                        S.op("act", lambda e, sdg=sdg, p=pzz[1]: e.activation(out=sdg[:], in_=p[:], func=AF.Sigmoid), reads=[tzz[1]], writes=[tsd])
                        S.op("dve", lambda e, t1=t1, sa=sa, p=pzz[2]: e.tensor_tensor(out=t1[:], in0=sa[:], in1=p[:], op=ALU.mult), reads=[tsa, tzz[2]], writes=[tt1])
                        S.op("dve", lambda e, t2=t2, sdg=sdg, p=pzz[3]: e.tensor_tensor(out=t2[:], in0=sdg[:], in1=p[:], op=ALU.mult), reads=[tsd, tzz[3]], writes=[tt2])
                        S.op("pool", lambda e, t1=t1, t2=t2: e.tensor_tensor(out=t1[:], in0=t1[:], in1=t2[:], op=ALU.add), reads=[tt1, tt2], writes=[tt1])
                        S.op("dve", lambda e, t1=t1, hf=hf: e.tensor_tensor(out=t1[:], in0=t1[:], in1=g1bc[:, hf * 512:(hf + 1) * 512], op=ALU.mult), reads=[tt1, T_g1], writes=[tt1])
                        S.op("dve", lambda e, t1=t1, xh=xh, i=i, hf=hf: e.tensor_tensor(out=acc[:, i, hf * 512:(hf + 1) * 512], in0=t1[:], in1=xh[:], op=ALU.add),
                             reads=[tt1, txh], writes=[T_acc[i][hf]])
            S.barrier()
            if "x1" in dbg:
                for i in range(NT):
                    tt = Tok("x1d%d" % i)
                    tt.w = T_acc[i][1].w
                    dump("x1_%d" % i, acc[:, i, :], tt, [128, D], F32)
            if stop_after == "C3":
                S.final_wait("sp", out_toks)
                return nc, dbg_outs

            with ExitStack() as sf:
                NSLOT = 10
                ring2 = sbuf(sf, "ring2", [128, NSLOT - 8, 2048], BF16)

                def slot_ap(sl):
                    base = bufB[:, sl, :] if sl < 8 else ring2[:, sl - 8, :]
                    return base.rearrange("p (k c) -> p k c", k=8)
                T_slot = [Tok("slot%d" % i) for i in range(NSLOT)]
                w2b = sbuf(sf, "w2b", [128, 8, D], BF16); T_w2b = Tok("w2b")
                stg_r = Ring([sbuf(sf, "stg%d" % i, [128, 1024], F32) for i in range(4)], "stg")
                wrb = sbuf(sf, "wrb", [128, 8, NE], BF16); T_wrb = Tok("wrb")
                wrs = sbuf(sf, "wrs", [128, 8, NE], F32)
                b2g = sbuf(sf, "b2g", [NE, D], F32); T_b2g = Tok("b2g")
                gates = sbuf(sf, "gates", [128, 8, NE], F32); T_gates = [Tok("gates%d" % i) for i in range(8)]
                h2T = bufA
                T_h2T = [Tok("h2T0"), Tok("h2T1")]
                T_actT = [[Tok("actT%d_%d" % (j, tg)) for tg in range(2)] for j in range(8)]
                S.dma("sp", "ld_wrs", lambda e: e.dma_start(out=wrs[:], in_=w_r.rearrange("(k p) n -> p k n", p=128)), writes=[T_wrb])
                S.op("dve", lambda e: e.tensor_copy(out=wrb[:], in_=wrs[:]), reads=[T_wrb], writes=[T_wrb])
                S.dma("sp", "ld_b2g", lambda e: e.dma_start(out=b2g[:], in_=b2[:, :]), writes=[T_b2g])
                S.op("dve", lambda e: e.tensor_tensor(out=b2g[:], in0=b2g[:], in1=g2bc[0:NE, :], op=ALU.mult), reads=[T_b2g, T_g2], writes=[T_b2g])
                slot_ctr = [0]
                for th in range(2):
                    tiles = list(range(th * 8, th * 8 + 8))
                    with ExitStack() as sn:
                        srcs = [(acc[:, i, :], T_acc[i]) for i in tiles]
                        for _ in norm_transpose(sn, "D%d" % th, srcs, h2T, a2, 3, T_a2, T_h2T, False, nxn=4):
                            pass
                    S.barrier()
                    with ExitStack() as se:
                        pgb = [psum(se, "pgb%d_%d" % (th, a), [128, 512], F32) for a in range(2)]; T_pgb = [Tok("pgb0"), Tok("pgb1")]
                        plb = [psum(se, "plb%d_%d" % (th, a), [128, 512], F32) for a in range(2)]; T_plb = [Tok("plb0"), Tok("plb1")]
                        pyb = [psum(se, "pyb%d_%d" % (th, a), [128, 512], F32) for a in range(4)]; T_pyb = [Tok("pyb%d" % a) for a in range(4)]
                        gm_r = Ring([sbuf(se, "gm%d_%d" % (th, a), [128, 512], F32) for a in range(2)], "gm")
                        sg_r = Ring([sbuf(se, "sg%d_%d" % (th, a), [128, 512], F32) for a in range(2)], "sg")
                        lm_r = Ring([sbuf(se, "lm%d_%d" % (th, a), [128, 512], F32) for a in range(1)], "lm")
                        tt_r = Ring([sbuf(se, "tt%d_%d" % (th, a), [128, 512], F32) for a in range(1)], "tt")
                        n1 = 0
                        n2 = 0
                        if th == 0:
                            print("sbuf bytes remaining in FFN expert scope:", nc.sbuf_bytes_remaining)
                        ne_run = NE if stop_after == "all" else int(stop_after[1:]) if stop_after.startswith("E") else NE
                        pend = []

                        def flush_casts():
                            while pend:
                                pend.pop(0)()

                        def load_piece(qidx):
                            e_, j_ = divmod(qidx, 8)
                            if e_ >= ne_run:
                                return
                            w1v = w1p[e_].rearrange("(k p) n -> p k n", p=128)
                            sl = qidx % NSLOT
                            sap = slot_ap(sl)
                            for part in range(2):
                                stg, tst = stg_r.next()
                                cc0 = part * D + j_ * 128
                                S.dma("sp", "ld_" + tst.name, lambda e, stg=stg, cc0=cc0: e.dma_start(
                                    out=stg[:].rearrange("p (k c) -> p k c", k=8), in_=w1v[:, :, cc0:cc0 + 128]), writes=[tst])
                                pend.append(lambda stg=stg, tst=tst, sap=sap, part=part, sl=sl: S.op("act", lambda e: e.copy(
                                    out=sap[:, :, part * 128:(part + 1) * 128], in_=stg[:].rearrange("p (k c) -> p k c", k=8)),
                                    reads=[tst], writes=[T_slot[sl]]))

                        def load_w2chunk(e_, k):
                            if e_ >= ne_run:
                                return
                            stg, tst = stg_r.next()
                            S.dma("sp", "ld_" + tst.name, lambda e, stg=stg, k=k: e.dma_start(out=stg[:], in_=w2[e_, k * 128:(k + 1) * 128, :]), writes=[tst])
                            pend.append(lambda stg=stg, tst=tst, k=k: S.op("dve" if k % 4 != 3 else "pool", lambda e: e.tensor_tensor(
                                out=w2b[:, k, :], in0=stg[:], in1=g2bc[:], op=ALU.mult), reads=[tst, T_g2], writes=[T_w2b]))
                        for qidx in range(NSLOT):
                            load_piece(qidx)
                            flush_casts()
                        rtmp = []
                        for ch in range(2):
                            rtmp.append(dict(
                                lg=sbuf(se, "lg%d_%d" % (th, ch), [128, NE], F32), T_lg=Tok("lg"),
                                top8=sbuf(se, "top8_%d_%d" % (th, ch), [128, 8], F32), T_top8=Tok("top8"),
                                msk=sbuf(se, "msk%d_%d" % (th, ch), [128, NE], F32), T_msk=Tok("msk"),
                                ex=sbuf(se, "ex%d_%d" % (th, ch), [128, NE], F32), T_ex=Tok("ex"),
                                sm=sbuf(se, "sm%d_%d" % (th, ch), [128, 4], F32), T_sm=Tok("sm"),
                                gT=sbuf(se, "gT%d_%d" % (th, ch), [NE, 128], F32), T_gT=Tok("gT")))

                        def router_tile(il, ch):
                            i = tiles[il]
                            c0, c1 = il * 128, (il + 1) * 128
                            R = rtmp[ch]
                            lg, top8, msk, ex, sm, gT = R["lg"], R["top8"], R["msk"], R["ex"], R["sm"], R["gT"]
                            T_lg, T_top8, T_msk, T_ex, T_sm, T_gT = R["T_lg"], R["T_top8"], R["T_msk"], R["T_ex"], R["T_sm"], R["T_gT"]
                            plog, T_plog = pgb[ch], T_pgb[ch]
                            pgT, T_pgT = plb[ch], T_plb[ch]
                            pbk, T_pbk = pyb[ch], T_pyb[ch]
                            for k in range(8):
                                S.op("pe", lambda e, k=k: e.matmul(plog[:, 0:NE], lhsT=h2T[:, k, c0:c1], rhs=wrb[:, k, :], start=(k == 0), stop=(k == 7)),
                                     reads=[T_h2T[il // 4], T_wrb], writes=[T_plog], signal=(k == 7))
                            yield
                            S.op("dve", lambda e: e.tensor_tensor(out=lg[:], in0=plog[:, 0:NE], in1=brbc[:], op=ALU.add), reads=[T_plog, T_in0], writes=[T_lg]); yield
                            S.op("dve", lambda e: e.max(out=top8[:], in_=lg[:]), reads=[T_lg], writes=[T_top8]); yield
                            S.op("dve", lambda e: e.tensor_scalar(out=msk[:], in0=lg[:], scalar1=top8[:, 3:4], scalar2=None, op0=ALU.is_ge), reads=[T_lg, T_top8], writes=[T_msk]); yield
                            S.op("dve", lambda e: e.tensor_scalar(out=sm[:, 0:1], in0=top8[:, 0:1], scalar1=-1.0, scalar2=None, op0=ALU.mult), reads=[T_top8], writes=[T_sm]); yield
                            S.op("act", lambda e: e.activation(out=ex[:], in_=lg[:], func=AF.Exp, bias=sm[:, 0:1]), reads=[T_lg, T_sm], writes=[T_ex]); yield
                            S.op("dve", lambda e: e.tensor_tensor(out=ex[:], in0=ex[:], in1=msk[:], op=ALU.mult), reads=[T_ex, T_msk], writes=[T_ex]); yield
                            S.op("dve", lambda e: e.reduce_sum(out=sm[:, 1:2], in_=ex[:], axis=AX.X), reads=[T_ex], writes=[T_sm]); yield
                            S.op("dve", lambda e: e.reciprocal(out=sm[:, 2:3], in_=sm[:, 1:2]), reads=[T_sm], writes=[T_sm]); yield
                            S.op("dve", lambda e: e.tensor_scalar(out=gates[:, il, :], in0=ex[:], scalar1=sm[:, 2:3], scalar2=None, op0=ALU.mult),
                                 reads=[T_ex, T_sm], writes=[T_gates[il]]); yield
                            S.op("pe", lambda e: e.matmul(pgT[0:NE, 0:128], lhsT=gates[:, il, :], rhs=identf[:], start=True, stop=True),
                                 reads=[T_gates[il], T_c], writes=[T_pgT]); yield
                            S.op("act", lambda e: e.copy(out=gT[:], in_=pgT[0:NE, 0:128]), reads=[T_pgT], writes=[T_gT]); yield
                            for nh in range(2):
                                S.op("pe", lambda e: e.matmul(pbk[:], lhsT=gT[:], rhs=b2g[:, nh * 512:(nh + 1) * 512], start=True, stop=True),
                                     reads=[T_gT, T_b2g], writes=[T_pbk]); yield
                                S.op("dve", lambda e: e.tensor_tensor(out=acc[:, i, nh * 512:(nh + 1) * 512], in0=acc[:, i, nh * 512:(nh + 1) * 512], in1=pbk[:], op=ALU.add),
                                     reads=[T_pbk, T_acc[i][nh]], writes=[T_acc[i][nh]]); yield
                        for il2 in range(4):
                            interleave(router_tile(2 * il2, 0), router_tile(2 * il2 + 1, 1))
                        if "gates" in dbg and th == 0:
                            tt_ = Tok("gd"); tt_.w = T_gates[7].w
                            dump("gates", gates[:].rearrange("p a b -> p (a b)"), tt_, [128, 8 * NE], F32)
                        for ex_i in range(ne_run):
                            slots = [(ex_i * 8 + j) % NSLOT for j in range(8)]
                            for j in range(8):
                                sl = slots[j]
                                sap = slot_ap(sl)
                                for tg in range(2):
                                    a = n1 % 2
                                    n1 += 1
                                    for k in range(8):
                                        S.op("pe", lambda e, k=k, a=a, sap=sap, tg=tg: e.matmul(pgb[a][:], lhsT=sap[:, k, 0:128], rhs=h2T[:, k, tg * 512:(tg + 1) * 512],
                                                                                          start=(k == 0), stop=(k == 7)),
                                             reads=[T_slot[sl], T_h2T[tg]], writes=[T_pgb[a]] + ([T_plb[a]] if k == 0 else []), signal=(k == 7))
                                    for k in range(8):
                                        S.op("pe", lambda e, k=k, a=a, sap=sap, tg=tg: e.matmul(plb[a][:], lhsT=sap[:, k, 128:256], rhs=h2T[:, k, tg * 512:(tg + 1) * 512],
                                                                                          start=(k == 0), stop=(k == 7)),
                                             reads=[T_slot[sl], T_h2T[tg]], writes=[T_plb[a]], signal=(k == 7))
                                    gm, tgm = gm_r.next(); sg, tsg = sg_r.next(); lm, tlm = lm_r.next(); tt, ttt = tt_r.next()
                                    bg = b1s[:, ex_i * 16 + j:ex_i * 16 + j + 1]
                                    bl = b1s[:, ex_i * 16 + 8 + j:ex_i * 16 + 8 + j + 1]
                                    S.op("dve", lambda e, gm=gm, a=a, bg=bg: e.tensor_scalar(out=gm[:], in0=pgb[a][:], scalar1=bg, scalar2=7.0, op0=ALU.add, op1=ALU.min),
                                         reads=[T_pgb[a], T_misc], writes=[tgm])
                                    S.op("act", lambda e, gm=gm, sg=sg: e.activation(out=sg[:], in_=gm[:], func=AF.Sigmoid, scale=1.702), reads=[tgm], writes=[tsg])
                                    S.op("dve", lambda e, lm=lm, a=a, bl=bl: e.tensor_scalar(out=lm[:], in0=plb[a][:], scalar1=bl, scalar2=8.0, op0=ALU.add, op1=ALU.min),
                                         reads=[T_plb[a], T_misc], writes=[tlm])
                                    S.op("pool", lambda e, gm=gm, sg=sg, tt=tt: e.tensor_tensor(out=tt[:], in0=gm[:], in1=sg[:], op=ALU.mult), reads=[tgm, tsg], writes=[ttt])
                                    S.op("dve", lambda e, lm=lm, tt=tt, j=j, tg=tg: e.scalar_tensor_tensor(
                                        out=bufA[:, j, 1024 + tg * 512:1024 + (tg + 1) * 512], in0=lm[:], scalar=-6.0, in1=tt[:], op0=ALU.max, op1=ALU.mult),
                                        reads=[tlm, ttt], writes=[T_actT[j][tg]])
                                flush_casts()
                                if j < 4:
                                    load_w2chunk(ex_i, 2 * j)
                                    load_w2chunk(ex_i, 2 * j + 1)
                                load_piece(ex_i * 8 + j + NSLOT)
                            flush_casts()
                            for il, i in enumerate(tiles):
                                for nh in range(2):
                                    a = n2 % 4
                                    hoist2 = [T_pyb[(a + 1) % 4]] if n2 % 2 == 0 else []
                                    n2 += 1
                                    for k in range(8):
                                        S.op("pe", lambda e, k=k, a=a, il=il, nh=nh: e.matmul(
                                            pyb[a][:], lhsT=bufA[:, k, 1024 + il * 128:1024 + (il + 1) * 128], rhs=w2b[:, k, nh * 512:(nh + 1) * 512],
                                            start=(k == 0), stop=(k == 7)),
                                            reads=[T_actT[k][il // 4], T_w2b], writes=[T_pyb[a]] + (hoist2 if k == 0 else []), signal=(k == 7))
                                    S.op("dve", lambda e, a=a, il=il, i=i, nh=nh: e.scalar_tensor_tensor(
                                        out=acc[:, i, nh * 512:(nh + 1) * 512], in0=pyb[a][:], scalar=gates[:, il, ex_i:ex_i + 1], in1=acc[:, i, nh * 512:(nh + 1) * 512],
                                        op0=ALU.mult, op1=ALU.add),
                                        reads=[T_pyb[a], T_gates[il], T_acc[i][nh]], writes=[T_acc[i][nh]])
                        for i in tiles:
                            to = Tok("out%d" % i)
                            S.dma("sp", "st_out", lambda e, i=i: e.dma_start(out=out[i * 128:(i + 1) * 128, :], in_=acc[:, i, :]), reads=[T_acc[i][0], T_acc[i][1]], writes=[to])
                            out_toks.append(to)
                    S.barrier()

        S.final_wait("sp", out_toks)
    return nc, dbg_outs


def _consts():
    s = np.arange(128)
    mid = 63
    amat = np.zeros((128, 130), np.float32)
    amat[:, :128] = (s[:, None] <= s[None, :]).astype(np.float32) - (s[:, None] <= mid).astype(np.float32)
    amat[:, 128] = (s <= mid).astype(np.float32)
    amat[:, 129] = 1.0
    tri = (s[None, :] >= s[:, None]).astype(np.float32)
    bones = np.zeros((128, 128), np.float32)
    bones[:64, :64] = 1.0
    bones[64:, 64:] = 1.0
    return {
        "identb": np.eye(128, dtype=np.float32).astype(NPBF),
        "identf": np.eye(128, dtype=np.float32),
        "amat": amat,
        "tri": tri,
        "bones": bones.astype(NPBF),
    }


def _prep_shared(inp):
    f = lambda a: np.ascontiguousarray(np.asarray(a, dtype=np.float32))
    w1 = f(inp["w1"])[0]
    w1p = np.ascontiguousarray(np.concatenate([w1[:, :, 0::2], w1[:, :, 1::2]], axis=2))
    b1 = f(inp["b1"])[0]
    b1p = np.concatenate([b1[:, 0::2], b1[:, 1::2]], axis=1)
    b1F = np.ascontiguousarray(b1p.reshape(NE, 16, 128).transpose(2, 0, 1).reshape(128, NE * 16))
    sh = {
        "w_ada": f(inp["w_ada"])[0],
        "badaF": np.ascontiguousarray(f(inp["b_ada"])[0].reshape(48, 128).T),
        "bada_row": f(inp["b_ada"])[0].reshape(1, -1),
        "gmixF": np.ascontiguousarray(f(inp["mix_norm_g"])[0].reshape(8, 128).T),
        "gffnF": np.ascontiguousarray(f(inp["ffn_norm_g"])[0].reshape(8, 128).T),
        "w_in": f(inp["w_in"])[0],
        "lbl": f(inp["hg_lower_bound_logits"]).reshape(1, 1024),
        "hgn": f(inp["hg_out_norm_g"]).reshape(1, 128),
        "gq": np.tile(f(inp["da_q_norm_g"]).reshape(64), 2).reshape(128, 1),
        "gk": np.tile(f(inp["da_k_norm_g"]).reshape(64), 2).reshape(128, 1),
        "lam4": np.concatenate([f(inp["da_lambda_q1"]).reshape(64), f(inp["da_lambda_k1"]).reshape(64),
                                f(inp["da_lambda_q2"]).reshape(64), f(inp["da_lambda_k2"]).reshape(64)]).reshape(1, 256),
        "subg": f(inp["da_subln_g"]).reshape(1, 128),
        "w_out": f(inp["w_out"])[0],
        "w_r": f(inp["w_router"])[0],
        "b_r": f(inp["b_router"]).reshape(1, NE),
        "w1p": w1p,
        "b1F": b1F,
        "w2": f(inp["w2"])[0],
        "b2": f(inp["b2"])[0],
    }
    sh.update(_consts())
    return sh


def _prep_core(inp, sh, b):
    m = dict(sh)
    m["x"] = np.ascontiguousarray(np.asarray(inp["x"], dtype=np.float32)[b])
    m["cT"] = np.ascontiguousarray(np.asarray(inp["c"], dtype=np.float32)[b].reshape(8, 128).T)
    return m


def kernel(**inputs):
    nc, _ = build_nc()
    sh = _prep_shared(inputs)
    in_maps = [_prep_core(inputs, sh, b) for b in range(8)]
    res = run_bass_kernel_spmd(nc, in_maps, core_ids=list(range(8)))
    return np.stack([np.asarray(r["out"], dtype=np.float32) for r in res.results], axis=0)
```
